# Optimizing a Trainium2 kernel written in Bass

```python
import jax, jax.numpy as jnp
from jax import lax
import numpy as np

D_MODEL = 1024
BATCH = 16
SEQ = 2048
DEPTH = 4

GRID_W = 64
CTX_LEN = 256
N_MIXERS = 2
N_FNET_LAYERS = (DEPTH + 1) // 2
N_RWKV_LAYERS = DEPTH // 2
N_VRES_LAYERS = N_RWKV_LAYERS - 1
FNET_GROUPS = 4
FNET_GROUP_DIM = D_MODEL // FNET_GROUPS
HEAD_SIZE = 64
N_HEADS = D_MODEL // HEAD_SIZE
D_DECAY_LORA = 64
D_AAA_LORA = 64
D_MV_LORA = 32
D_GATE_LORA = 128
N_DIRS = 2
N_EXPERTS = 16
D_EXPERT = 1024
CAPACITY_FACTOR = 2
NORM_EPS = 1e-6
LNX_EPS = 64e-5

kernel_name = 'hybrid_fnet_rwkv7_ecmoe_dit'


def rmsnorm(h, g):
    hf = h.astype(jnp.float32)
    hf = hf * lax.rsqrt(jnp.mean(hf * hf, axis=-1, keepdims=True) + NORM_EPS)
    return (hf * g.astype(jnp.float32)).astype(h.dtype)


def adaln_params(cvec, w, b):
    return jnp.split(jax.nn.silu(cvec) @ w + b, 6, axis=-1)


def fourier_mix(h, w_o, b_o):
    bsz, t, _ = h.shape
    hg = h.astype(jnp.float32).reshape(bsz, t, FNET_GROUPS, FNET_GROUP_DIM)
    f = jnp.fft.fft2(hg, axes=(1, 3), norm='ortho').real
    return f.reshape(bsz, t, D_MODEL).astype(h.dtype) @ w_o + b_o


def qshift_grid(h):
    bsz, t, d = h.shape
    rows = t // GRID_W
    g = h.reshape(bsz, rows, GRID_W, d)
    q = d // 4
    left = jnp.pad(g[:, :, :-1, :q], ((0, 0), (0, 0), (1, 0), (0, 0)))
    right = jnp.pad(g[:, :, 1:, q:2 * q], ((0, 0), (0, 0), (0, 1), (0, 0)))
    up = jnp.pad(g[:, :-1, :, 2 * q:3 * q], ((0, 0), (1, 0), (0, 0), (0, 0)))
    down = jnp.pad(g[:, 1:, :, 3 * q:], ((0, 0), (0, 1), (0, 0), (0, 0)))
    return jnp.concatenate([left, right, up, down], axis=-1).reshape(bsz, t, d)


def shift_seq(h):
    half = h.shape[-1] // 2
    prev = jnp.pad(h[:, :-1, :half], ((0, 0), (1, 0), (0, 0)))
    nxt = jnp.pad(h[:, 1:, half:], ((0, 0), (0, 1), (0, 0)))
    return jnp.concatenate([prev, nxt], axis=-1)


def to_heads(t):
    return t.reshape(t.shape[0], t.shape[1], N_HEADS, HEAD_SIZE)


def rwkv_project(h, h_shift, mix, wr, wk, wv, w0, w1, w2, a0, a1, a2, g1, g2, k_k, k_a, v_first, vres):
    xx = h_shift - h
    xr, xw, xk, xv, xa, xg = [h + xx * mix[j] for j in range(6)]
    r = xr @ wr
    k = xk @ wk
    v = xv @ wv
    if vres is not None:
        v0, v1, v2 = vres
        v = v + (v_first - v) * jax.nn.sigmoid(v0 + (xv @ v1) @ v2)
    g = jax.nn.sigmoid(xg @ g1) @ g2
    kk = to_heads((k * k_k).astype(jnp.float32))
    kk = kk * lax.rsqrt(jnp.sum(kk * kk, axis=-1, keepdims=True) + 1e-12)
    kf = k.astype(jnp.float32)
    dirs = []
    for d in range(N_DIRS):
        wlog = (w0[d] + jnp.tanh(xw @ w1[d]) @ w2[d]).astype(jnp.float32)
        wlog = -jax.nn.softplus(-wlog) - 0.5
        decay = jnp.exp(-jnp.exp(wlog))
        a = jax.nn.sigmoid((a0[d] + (xa @ a1[d]) @ a2[d]).astype(jnp.float32))
        k_d = kf * (1.0 + (a - 1.0) * k_a.astype(jnp.float32))
        dirs.append((to_heads(decay), to_heads(k_d), to_heads(a)))
    return to_heads(r.astype(jnp.float32)), v, g, kk, dirs


def wkv_scan(r, decay, k, v, a_vec, b_vec, s0, reverse):
    def step(s, inp):
        r_t, w_t, k_t, v_t, a_t, b_t = inp
        sa = jnp.einsum('bhvk,bhk->bhv', s, a_t)
        s = s * w_t[:, :, None, :] + sa[..., None] * b_t[:, :, None, :] + v_t[..., None] * k_t[:, :, None, :]
        return s, jnp.einsum('bhvk,bhk->bhv', s, r_t)
    xs = tuple(jnp.swapaxes(t, 0, 1) for t in (r, decay, k, v, a_vec, b_vec))
    s_final, ys = lax.scan(step, s0, xs, reverse=reverse)
    return jnp.swapaxes(ys, 0, 1), s_final


def rwkv_output(y, r, v, g, k_dirs, r_k, lnx_w, lnx_b, wo, dtype):
    mu = jnp.mean(y, axis=-1, keepdims=True)
    var = jnp.mean(jnp.square(y - mu), axis=-1, keepdims=True)
    yn = (y - mu) * lax.rsqrt(var + LNX_EPS)
    bsz, t = y.shape[:2]
    yn = yn.reshape(bsz, t, D_MODEL) * lnx_w.astype(jnp.float32) + lnx_b.astype(jnp.float32)
    rk = r_k.astype(jnp.float32)
    bonus = sum(jnp.sum(r * k_d * rk, axis=-1, keepdims=True) for k_d in k_dirs) * v
    out = (yn + bonus.reshape(bsz, t, D_MODEL)).astype(dtype) * g
    return out @ wo


def ec_moe(h, router, wg, wu, wd):
    bsz, t, d = h.shape
    cap = CAPACITY_FACTOR * t // N_EXPERTS
    aff = jax.nn.softmax((h @ router).astype(jnp.float32), axis=-1)
    gates, idx = lax.top_k(jnp.swapaxes(aff, 1, 2), cap)
    xs = jax.vmap(lambda hb, ib: hb[ib])(h, idx)
    hid = jax.nn.silu(jnp.einsum('becd,edf->becf', xs, wg)) * jnp.einsum('becd,edf->becf', xs, wu)
    ys = jnp.einsum('becf,efd->becd', hid, wd) * gates[..., None].astype(h.dtype)
    return jax.vmap(lambda ib, yb: jnp.zeros((t, d), h.dtype).at[ib.reshape(-1)].add(yb.reshape(-1, d)))(idx, ys)


def setup_inputs(seed: int = 0) -> dict:
    key = jax.random.key(seed)
    ks = iter(jax.random.split(key, 40))

    def nrm(shape, scale):
        return jax.random.normal(next(ks), shape, jnp.float32) * scale

    D = D_MODEL
    inv = D ** -0.5
    NR = N_RWKV_LAYERS
    NV = N_VRES_LAYERS
    return {
        'x': nrm((BATCH, SEQ, D), 1.0),
        'c': nrm((BATCH, D), 1.0),
        'ctx': nrm((BATCH, CTX_LEN, D), 1.0),
        'c_ctx': nrm((D,), 1.0),
        'ada_w': nrm((DEPTH, D, 6 * D), 0.5 * inv),
        'ada_b': nrm((DEPTH, 6 * D), 0.01),
        'norm_g': 1.0 + nrm((DEPTH, 2, D), 0.1),
        'fnet_wo': nrm((N_FNET_LAYERS, D, D), inv),
        'fnet_bo': nrm((N_FNET_LAYERS, D), 0.01),
        'rw_mix': jax.random.uniform(next(ks), (NR, 6, D), jnp.float32),
        'rw_wr': nrm((NR, D, D), inv),
        'rw_wk': nrm((NR, D, D), inv),
        'rw_wv': nrm((NR, D, D), inv),
        'rw_wo': nrm((NR, D, D), inv),
        'rw_w0': jax.random.uniform(next(ks), (NR, N_DIRS, D), jnp.float32, -6.0, 1.0),
        'rw_w1': nrm((NR, N_DIRS, D, D_DECAY_LORA), inv),
        'rw_w2': nrm((NR, N_DIRS, D_DECAY_LORA, D), 0.1),
        'rw_a0': nrm((NR, N_DIRS, D), 0.5),
        'rw_a1': nrm((NR, N_DIRS, D, D_AAA_LORA), inv),
        'rw_a2': nrm((NR, N_DIRS, D_AAA_LORA, D), 0.1),
        'rw_v0': nrm((NV, D), 0.5),
        'rw_v1': nrm((NV, D, D_MV_LORA), inv),
        'rw_v2': nrm((NV, D_MV_LORA, D), 0.1),
        'rw_g1': nrm((NR, D, D_GATE_LORA), inv),
        'rw_g2': nrm((NR, D_GATE_LORA, D), D_GATE_LORA ** -0.5),
        'rw_kk': 0.85 + nrm((NR, D), 0.1),
        'rw_ka': 1.0 + nrm((NR, D), 0.1),
        'rw_rk': nrm((NR, N_HEADS, HEAD_SIZE), 0.1),
        'rw_lnx_w': 1.0 + nrm((NR, D), 0.1),
        'rw_lnx_b': nrm((NR, D), 0.01),
        'moe_router': nrm((DEPTH, D, N_EXPERTS), inv),
        'moe_wg': nrm((DEPTH, N_EXPERTS, D, D_EXPERT), inv),
        'moe_wu': nrm((DEPTH, N_EXPERTS, D, D_EXPERT), inv),
        'moe_wd': nrm((DEPTH, N_EXPERTS, D_EXPERT, D), D_EXPERT ** -0.5),
        'final_g': 1.0 + nrm((D,), 0.1),
    }


def reference(x, c, ctx, c_ctx, ada_w, ada_b, norm_g, fnet_wo, fnet_bo, rw_mix, rw_wr, rw_wk, rw_wv, rw_wo,
              rw_w0, rw_w1, rw_w2, rw_a0, rw_a1, rw_a2, rw_v0, rw_v1, rw_v2, rw_g1, rw_g2, rw_kk, rw_ka, rw_rk,
              rw_lnx_w, rw_lnx_b, moe_router, moe_wg, moe_wu, moe_wd, final_g):
    dtype = x.dtype
    v_first_x = None
    v_first_c = None
    for i in range(DEPTH):
        need_ctx = i < DEPTH - 1
        sh1x, sc1x, g1x, sh2x, sc2x, g2x = [m[:, None, :] for m in adaln_params(c, ada_w[i], ada_b[i])]
        sh1c, sc1c, g1c, sh2c, sc2c, g2c = adaln_params(c_ctx, ada_w[i], ada_b[i])
        hx = rmsnorm(x, norm_g[i, 0]) * (1.0 + sc1x) + sh1x
        if i % N_MIXERS == 0:
            fi = i // N_MIXERS
            x = x + g1x * fourier_mix(hx, fnet_wo[fi], fnet_bo[fi])
            if need_ctx:
                hc = rmsnorm(ctx, norm_g[i, 0]) * (1.0 + sc1c) + sh1c
                ctx = ctx + g1c * fourier_mix(hc, fnet_wo[fi], fnet_bo[fi])
        else:
            ri = i // N_MIXERS
            hc = rmsnorm(ctx, norm_g[i, 0]) * (1.0 + sc1c) + sh1c
            vres = None if ri == 0 else (rw_v0[ri - 1], rw_v1[ri - 1], rw_v2[ri - 1])
            prm = (rw_mix[ri], rw_wr[ri], rw_wk[ri], rw_wv[ri], rw_w0[ri], rw_w1[ri], rw_w2[ri],
                   rw_a0[ri], rw_a1[ri], rw_a2[ri], rw_g1[ri], rw_g2[ri], rw_kk[ri], rw_ka[ri])
            r_x, v_x, gt_x, kk_x, dirs_x = rwkv_project(hx, qshift_grid(hx), *prm, v_first_x, vres)
            r_c, v_c, gt_c, kk_c, dirs_c = rwkv_project(hc, shift_seq(hc), *prm, v_first_c, vres)
            if ri == 0:
                v_first_x, v_first_c = v_x, v_c
            vh_x = to_heads(v_x.astype(jnp.float32))
            vh_c = to_heads(v_c.astype(jnp.float32))
            s0 = jnp.zeros((x.shape[0], N_HEADS, HEAD_SIZE, HEAD_SIZE), jnp.float32)
            ys_x = []
            ys_c = []
            for d in range(N_DIRS):
                reverse = d == 1
                dec_c, k_c, a_c = dirs_c[d]
                yc_d, s_ctx = wkv_scan(r_c, dec_c, k_c, vh_c, -kk_c, kk_c * a_c, s0, reverse)
                dec_x, k_x, a_x = dirs_x[d]
                yx_d, _ = wkv_scan(r_x, dec_x, k_x, vh_x, -kk_x, kk_x * a_x, s_ctx, reverse)
                ys_x.append(yx_d)
                ys_c.append(yc_d)
            x = x + g1x * rwkv_output(ys_x[0] + ys_x[1], r_x, vh_x, gt_x, [dd[1] for dd in dirs_x],
                                      rw_rk[ri], rw_lnx_w[ri], rw_lnx_b[ri], rw_wo[ri], dtype)
            if need_ctx:
                ctx = ctx + g1c * rwkv_output(ys_c[0] + ys_c[1], r_c, vh_c, gt_c, [dd[1] for dd in dirs_c],
                                              rw_rk[ri], rw_lnx_w[ri], rw_lnx_b[ri], rw_wo[ri], dtype)
        hx = rmsnorm(x, norm_g[i, 1]) * (1.0 + sc2x) + sh2x
        x = x + g2x * ec_moe(hx, moe_router[i], moe_wg[i], moe_wu[i], moe_wd[i])
        if need_ctx:
            hc = rmsnorm(ctx, norm_g[i, 1]) * (1.0 + sc2c) + sh2c
            ctx = ctx + g2c * ec_moe(hc, moe_router[i], moe_wg[i], moe_wu[i], moe_wd[i])
    return rmsnorm(x, final_g)
```

```python
import contextlib
import numpy as np
import ml_dtypes
import concourse.bass as bass
import concourse.mybir as mybir
from concourse.bass_utils import run_bass_kernel_spmd

F32 = mybir.dt.float32
BF16 = mybir.dt.bfloat16
I32 = mybir.dt.int32
U32 = mybir.dt.uint32
AF = mybir.ActivationFunctionType
ALU = mybir.AluOpType
AX = mybir.AxisListType

PE, DVE, ACT, POOL, SP = 0, 1, 2, 3, 4
EPOCH = 30000
N_DSEM = 40


class Res:
    __slots__ = ("w", "r", "name", "excl")

    def __init__(self, name="", excl=False):
        self.w = None
        self.r = {}
        self.name = name
        self.excl = excl


class T:
    __slots__ = ("t", "res")

    def __init__(self, t, name=""):
        self.t = t
        self.res = Res(name)

    def __getitem__(self, k):
        return self.t[k]


class Prog:
    def __init__(self, nc, es):
        self.nc = nc
        self.es = es
        self.eng = [nc.tensor, nc.vector, nc.scalar, nc.gpsimd, nc.sync]
        self.cnt = [0] * 5
        self.epoch = [0] * 5
        self.esem = [[] for _ in range(5)]
        for e in range(4):
            self.esem[e].append(es.enter_context(nc.semaphore(f"e{e}_0")))
        self.known = [dict() for _ in range(5)]
        self.knownd = [dict() for _ in range(5)]
        self.dsem = [es.enter_context(nc.semaphore(f"d{i}")) for i in range(N_DSEM)]
        self.dval = [0] * N_DSEM
        self.dnext = 0
        self.n_inst = 0
        self.n_wait = 0
        self.psum_tiles = []
        self.psum_i = 0

    def wait_tok(self, me, tok):
        if tok is None:
            return
        if tok[0] == "e":
            _, e, ep, c = tok
            if e == me and me == PE:
                return
            k = self.known[me].get(e, (-1, 0))
            if k >= (ep, c):
                return
            self.eng[me].wait_ge(self.esem[e][ep], c)
            self.n_wait += 1
            self.known[me][e] = (ep, c)
        else:
            _, s, v = tok
            if self.knownd[me].get(s, 0) >= v:
                return
            self.eng[me].wait_ge(self.dsem[s], v)
            self.n_wait += 1
            self.knownd[me][s] = v

    def _deps(self, me, ins, outs):
        for r in ins:
            r = getattr(r, "res", r)
            self.wait_tok(me, r.w)
            if r.excl:
                for t in r.r.values():
                    self.wait_tok(me, t)
        for r in outs:
            r = getattr(r, "res", r)
            self.wait_tok(me, r.w)
            for t in r.r.values():
                self.wait_tok(me, t)

    def _mark(self, tok, key, ins, outs):
        for r in ins:
            r = getattr(r, "res", r)
            if r.excl:
                r.w = tok
                r.r = {}
            else:
                r.r[key] = tok
        for r in outs:
            r = getattr(r, "res", r)
            r.w = tok
            r.r = {}

    def op(self, me, fn, ins=(), outs=()):
        if getattr(self, "muted", False):
            return None
        self._deps(me, ins, outs)
        if self.cnt[me] >= EPOCH:
            self.epoch[me] += 1
            self.cnt[me] = 0
            self.esem[me].append(self.es.enter_context(self.nc.semaphore(f"e{me}_{self.epoch[me]}")))
        inst = fn()
        self.cnt[me] += 1
        inst.then_inc(self.esem[me][self.epoch[me]], 1)
        self.n_inst += 1
        tok = ("e", me, self.epoch[me], self.cnt[me])
        self._mark(tok, me, ins, outs)
        return tok

    def dma(self, me, fn, ins=(), outs=()):
        if getattr(self, "muted", False):
            return None
        self._deps(me, ins, outs)
        s = self.dnext
        self.dnext = (self.dnext + 1) % N_DSEM
        if self.dval[s] > 0:
            self.wait_tok(me, ("d", s, self.dval[s]))
        inst = fn()
        self.dval[s] += 16
        inst.then_inc(self.dsem[s], 16)
        self.n_inst += 1
        tok = ("d", s, self.dval[s])
        self._mark(tok, ("d", s), ins, outs)
        return tok

    def last_tok(self, e):
        if e == SP or (self.cnt[e] == 0 and self.epoch[e] == 0):
            return None
        return ("e", e, self.epoch[e], self.cnt[e])

    def barrier(self, engines=(PE, DVE, ACT, POOL, SP)):
        toks = [self.last_tok(e) for e in range(4)]
        for me in engines:
            for e in range(4):
                if e != me:
                    self.wait_tok(me, toks[e])
            for s in range(N_DSEM):
                if self.dval[s] > 0:
                    self.wait_tok(me, ("d", s, self.dval[s]))

    def sb(self, st, name, shape, dt):
        self.uid = getattr(self, "uid", 0) + 1
        return T(st.enter_context(self.nc.sbuf_tensor(f"s{self.uid}_{name}", list(shape), dt)), name)

    def init_psum(self, st, n=8):
        self.psum_tiles = [T(st.enter_context(self.nc.psum_tensor(f"psb{i}", [128, 512], F32)), f"psb{i}")
                           for i in range(n)]
        self.psum_i = 0

    def ps(self):
        t = self.psum_tiles[self.psum_i % len(self.psum_tiles)]
        self.psum_i += 1
        return t


D = 1024
NBC = 2
TX = 2048
TC = 256
TL = TC + TX
NE = 16
KAPPA = float(np.exp(-0.5))
DEPTH = 4

W_NAMES = ["ada_w", "ada_b", "norm_g", "fnet_wo", "fnet_bo", "rw_mix", "rw_wr", "rw_wk", "rw_wv", "rw_wo",
           "rw_w0", "rw_w1", "rw_w2", "rw_a0", "rw_a1", "rw_a2", "rw_v0", "rw_v1", "rw_v2", "rw_g1", "rw_g2",
           "rw_kk", "rw_ka", "rw_rk", "rw_lnx_w", "rw_lnx_b", "moe_router", "moe_wg", "moe_wu", "moe_wd", "final_g"]
W_SHAPES = {
    "ada_w": [4, 1024, 6144], "ada_b": [4, 6144], "norm_g": [4, 2, 1024], "fnet_wo": [2, 1024, 1024],
    "fnet_bo": [2, 1024], "rw_mix": [2, 6, 1024], "rw_wr": [2, 1024, 1024], "rw_wk": [2, 1024, 1024],
    "rw_wv": [2, 1024, 1024], "rw_wo": [2, 1024, 1024], "rw_w0": [2, 2, 1024], "rw_w1": [2, 2, 1024, 64],
    "rw_w2": [2, 2, 64, 1024], "rw_a0": [2, 2, 1024], "rw_a1": [2, 2, 1024, 64], "rw_a2": [2, 2, 64, 1024],
    "rw_v0": [1, 1024], "rw_v1": [1, 1024, 32], "rw_v2": [1, 32, 1024], "rw_g1": [2, 1024, 128],
    "rw_g2": [2, 128, 1024], "rw_kk": [2, 1024], "rw_ka": [2, 1024], "rw_rk": [2, 1024],
    "rw_lnx_w": [2, 1024], "rw_lnx_b": [2, 1024], "moe_router": [4, 1024, 16],
    "moe_wg": [4, 16, 1024, 1024], "moe_wu": [4, 16, 1024, 1024], "moe_wd": [4, 16, 1024, 1024], "final_g": [1, 1024],
}


def make_consts():
    bf = ml_dtypes.bfloat16
    c = {}
    c["ident_f"] = np.eye(128, dtype=np.float32)
    c["ident_b"] = np.eye(128, dtype=np.float32).astype(bf)
    cc = np.arange(256)
    ang = 2 * np.pi * np.outer(cc, cc) / 256.0
    cs = np.concatenate([np.cos(ang), np.sin(ang)], axis=1) / 16.0
    c["cs_tab"] = cs.reshape(2, 128, 512).transpose(1, 0, 2).astype(bf).copy()
    for T in (TX, TC):
        t = np.arange(T)
        a = 2 * np.pi * ((np.outer(t, t)) % T) / T
        tab = np.stack([np.cos(a), -np.sin(a)], axis=1) / np.sqrt(T)
        c[f"tok_tab{T}"] = tab.astype(bf)
    p = np.arange(128)
    blk = (p[:, None] // 64 == p[None, :] // 64)
    c["blockones_f"] = blk.astype(np.float32)
    c["blkmask"] = blk.astype(np.float32).astype(bf)
    row = p[:, None] % 64
    col = p[None, :] % 64
    mT = np.zeros((2, 128, 256), np.float32)
    mA = np.zeros((2, 128, 128), np.float32)
    mT[0, :, :128] = blk & (col > row)
    mT[0, :, 128:] = blk & (col >= row)
    mT[1, :, :128] = blk & (col < row)
    mT[1, :, 128:] = blk & (col <= row)
    mA[0] = blk & (col < row)
    mA[1] = blk & (col > row)
    c["mT"] = mT.transpose(1, 0, 2).copy()
    c["mA"] = mA.transpose(1, 0, 2).copy()
    seg = np.ones((128, 256), np.float32)
    seg[:, ::64] = 0
    c["segmask"] = seg
    sel = np.zeros((128, 64), np.float32)
    sel[p, p % 64] = 1.0
    c["sel"] = sel
    return c


PER_LAYER = ("ada_w", "moe_wg", "moe_wu", "moe_wd")
NP2DT = {np.dtype(np.float32): F32, np.dtype(ml_dtypes.bfloat16): BF16, np.dtype(np.int32): I32}


class PT:
    def __init__(self, hf, hb, off, n, res):
        self.hf, self.hb, self.off, self.n = hf, hb, off, n
        self.res = res

    def f(self, a=0, b=None, rows=128):
        b = self.n if b is None else b
        return self.hf[0:rows, self.off + a:self.off + b]

    def b(self, a=0, b=None, rows=128):
        b = 2 * self.n if b is None else b
        return self.hb[0:rows, 2 * self.off + a:2 * self.off + b]


class Multi:
    def __init__(self, aps):
        self.aps = aps

    def __getitem__(self, k):
        if isinstance(k, tuple):
            return self.aps[k[0]][k[1:]]
        return self.aps[k]


class Ctx:
    def __init__(self, nc, es, consts, debug):
        self.nc = nc
        self.es = es
        self.P = Prog(nc, es)
        self.debug = debug
        self.dram = {}
        self.wdecl = []
        self.consts_np = consts
        self.banks = []
        for i in range(8):
            h = es.enter_context(nc.psum_tensor(f"bank{i}", [128, 512], F32))
            self.banks.append((h, h.bitcast(BF16), Res(f"bank{i}", excl=True)))
        self.set_psum("full")
        self.uid = 0

    def set_psum(self, mode):
        self.ps_tiles = []
        for i, (hf, hb, res) in enumerate(self.banks):
            self.ps_tiles.append(PT(hf, hb, 0, 512, res))
        self.ps_i = 0

    def ps(self):
        t = self.ps_tiles[self.ps_i % len(self.ps_tiles)]
        self.ps_i += 1
        return t

    def din(self, name, shape, dt):
        self.dram[name] = self.nc.dram_tensor(name, list(shape), dt, kind="ExternalInput").ap()
        return self.dram[name]

    def w(self, name, layer=None):
        if layer is not None and name in PER_LAYER:
            key = f"{name}_{layer}"
            if key not in self.dram:
                self.din(key, W_SHAPES[name][1:], F32)
                self.wdecl.append((key, name, layer))
            return self.dram[key]
        if name not in self.dram:
            self.din(name, W_SHAPES[name], F32)
            self.wdecl.append((name, name, None))
        ap = self.dram[name]
        return ap if layer is None else ap[layer]

    def dout(self, name, shape, dt):
        self.dram[name] = self.nc.dram_tensor(name, list(shape), dt, kind="ExternalOutput").ap()
        return self.dram[name]

    def dscr(self, name, shape, dt):
        self.dram[name] = self.nc.dram_tensor(name, list(shape), dt).ap()
        return self.dram[name]

    def sb(self, st, name, shape, dt):
        return self.P.sb(st, name, shape, dt)

    def X(self, eng, name, ins, outs, **kw):
        e = self.P.eng[eng]
        return self.P.op(eng, lambda: getattr(e, name)(**kw), ins=ins, outs=outs)

    def mm(self, ps, out, lhsT, rhs, ins, start=True, stop=True):
        return self.P.op(PE, lambda: self.nc.tensor.matmul(out, lhsT=lhsT, rhs=rhs, start=start, stop=stop),
                         ins=ins, outs=[ps])

    def tr(self, ps, out, in_, ident, ins):
        return self.P.op(PE, lambda: self.nc.tensor.transpose(out, in_, ident), ins=ins, outs=[ps])

    def Dm(self, q, ins, outs, **kw):
        e = self.P.eng[q]
        return self.P.dma(q, lambda: e.dma_start(**kw), ins=ins, outs=outs)

    def DI(self, ins, outs, **kw):
        return self.P.dma(POOL, lambda: self.nc.gpsimd.indirect_dma_start(**kw), ins=ins, outs=outs)

    def act(self, ins, outs, **kw):
        return self.X(ACT, "activation", ins, outs, **kw)

    def barrier(self):
        self.P.barrier()


def load_consts(K, st):
    nc = K.nc
    C = {}
    for name in ["ident_f", "ident_b", "blockones_f", "blkmask", "mT", "mA", "segmask", "sel", "cs_tab"]:
        arr = K.consts_np[name]
        d = K.din("c_" + name, arr.shape, NP2DT[arr.dtype])
        t = K.sb(st, name, arr.shape, NP2DT[arr.dtype])
        K.Dm(SP, [], [t], out=t[:], in_=d)
        C[name] = t
    for T in (TX, TC):
        arr = K.consts_np[f"tok_tab{T}"]
        K.din(f"c_tok_tab{T}", arr.shape, BF16)
    eps = K.sb(st, "eps", [128, 4], F32)
    K.X(DVE, "memset", [], [eps], ap=eps[:, 0:1], constant=1e-6)
    K.X(DVE, "memset", [], [eps], ap=eps[:, 1:2], constant=1e-12)
    K.X(DVE, "memset", [], [eps], ap=eps[:, 2:3], constant=64e-5)
    K.X(DVE, "memset", [], [eps], ap=eps[:, 3:4], constant=0.0)
    C["eps"] = eps
    K.C = C


def phase_adaln(K, st_global, n_layers):
    nc, C = K.nc, K.C
    modA = K.sb(st_global, "modA", [128, DEPTH, 48, 3], F32)
    K.modA = modA
    modrow = K.dscr("modrow", [DEPTH, 3, 6144], F32)
    ada_b = K.w("ada_b")
    with contextlib.ExitStack() as st:
        crow = K.sb(st, "crow", [3, 1024], F32)
        K.Dm(SP, [], [crow], out=crow[0:2, :], in_=K.dram["c"])
        K.Dm(SP, [crow], [crow], out=crow[2:3, :], in_=K.dram["c_ctx"])
        srow = K.sb(st, "srow", [3, 1024], F32)
        K.act([crow], [srow], out=srow[:], in_=crow[:], func=AF.Silu)
        sT = K.sb(st, "sT", [128, 8, 3], BF16)
        for ch in range(8):
            ps = K.ps()
            K.tr(ps, ps.f(0, 3), srow[0:3, ch * 128:(ch + 1) * 128], C["ident_f"][0:3, 0:3], [srow, C["ident_f"]])
            K.X(DVE, "tensor_copy", [ps], [sT], out=sT[:, ch, :], in_=ps.f(0, 3))
        wt = [K.sb(st, f"adaw{i}", [128, 8, 512], BF16) for i in range(2)]
        brow = [K.sb(st, f"adab{i}", [3, 512], F32) for i in range(2)]
        rows = [K.sb(st, f"adar{i}", [3, 512], F32) for i in range(2)]
        it = 0
        for i in range(n_layers):
            for cb in range(12):
                w = wt[it % 2]
                bb = brow[it % 2]
                rr = rows[it % 2]
                it += 1
                K.Dm(POOL, [], [w], out=w[:], in_=K.w("ada_w", i)[:, cb * 512:(cb + 1) * 512].rearrange("(kc p) n -> p kc n", p=128))
                K.Dm(SP, [], [bb], out=bb[:], in_=ada_b[i:i + 1, cb * 512:(cb + 1) * 512].broadcast_to([3, 512]))
                ps = K.ps()
                for kc in range(8):
                    K.mm(ps, ps.f(0, 512, rows=3), sT[:, kc, :], w[:, kc, :], [sT, w], start=(kc == 0), stop=(kc == 7))
                K.X(DVE, "tensor_tensor", [ps, bb], [rr], out=rr[:], in0=ps.f(0, 512, rows=3), in1=bb[:], op=ALU.add)
                K.Dm(SP, [rr], [], out=modrow[i, :, cb * 512:(cb + 1) * 512], in_=rr[:])
                ps2 = K.ps()
                for oc in range(4):
                    K.tr(ps2, ps2.f(oc * 4, oc * 4 + 3), rr[0:3, oc * 128:(oc + 1) * 128], C["ident_f"][0:3, 0:3], [rr, C["ident_f"]])
                K.X(ACT, "copy", [ps2], [modA], out=modA[:, i, cb * 4:(cb + 1) * 4, :],
                    in_=ps2.f(0, 16).rearrange("p (a b) -> p a b", b=4)[:, :, 0:3])
    K.barrier()


def load_cols(K, st, name, row_aps):
    C = K.C
    n = len(row_aps)
    out = K.sb(st, name, [128, 8, n], F32)
    with contextlib.ExitStack() as s2:
        rows = K.sb(s2, name + "_rows", [n, 1024], F32)
        for j, ap in enumerate(row_aps):
            K.Dm(SP, [rows] if j else [], [rows], out=rows[j:j + 1, :], in_=ap)
        for ch in range(8):
            ps = K.ps()
            K.tr(ps, ps.f(0, n), rows[0:n, ch * 128:(ch + 1) * 128], C["ident_f"][0:n, 0:n], [rows, C["ident_f"]])
            K.X(DVE, "tensor_copy", [ps], [out], out=out[:, ch, :], in_=ps.f(0, n))
        K.barrier()
    return out


def mod_scalars(K, st, i, which_sc, which_sh, ng_cols, name, ng_idx=0):
    modA = K.modA
    scl = K.sb(st, name + "_scl", [128, 8, 3], F32)
    sh = K.sb(st, name + "_sh", [128, 8, 3], F32)
    K.X(DVE, "tensor_scalar", [modA], [scl], out=scl[:], in0=modA[:, i, which_sc * 8:(which_sc + 1) * 8, :],
        scalar1=1.0, scalar2=None, op0=ALU.add)
    K.X(DVE, "tensor_tensor", [scl, ng_cols], [scl], out=scl[:], in0=scl[:], in1=ng_cols[:, :, ng_idx:ng_idx + 1].broadcast_to([128, 8, 3]),
        op=ALU.mult)
    K.X(DVE, "tensor_copy", [modA], [sh], out=sh[:], in_=modA[:, i, which_sh * 8:(which_sh + 1) * 8, :])
    return scl, sh


def norm_hT(K, st, src, T, scl, sh, j, hT, tmp):
    nc, C = K.nc, K.C
    xin, xnb, stat = tmp
    nt = T // 128
    K.Dm(SP, [], [xin[0]], out=xin[0][:], in_=src[0:128, :])
    for tt in range(nt):
        xt = xin[tt % 2]
        if tt + 1 < nt:
            K.Dm(SP, [], [xin[(tt + 1) % 2]], out=xin[(tt + 1) % 2][:], in_=src[(tt + 1) * 128:(tt + 2) * 128, :])
        xb = xnb[tt % 2]
        s = stat[tt % 2]
        K.act([xt], [xb, s], out=xb[:], in_=xt[:], func=AF.Square, accum_out=s[:, 0:1])
        K.act([s, C["eps"]], [s], out=s[:, 1:2], in_=s[:, 0:1], func=AF.Sqrt, bias=C["eps"][:, 0:1], scale=1.0 / D)
        K.X(DVE, "reciprocal", [s], [s], out=s[:, 2:3], in_=s[:, 1:2])
        K.act([xt, s], [xb], out=xb[:], in_=xt[:], func=AF.Copy, scale=s[:, 2:3])
        for hf in range(2):
            ps = K.ps()
            for c4 in range(4):
                ch = hf * 4 + c4
                K.tr(ps, ps.b(c4 * 128, (c4 + 1) * 128), xb[:, ch * 128:(ch + 1) * 128], C["ident_b"][:], [xb, C["ident_b"]])
            for c4 in range(4):
                ch = hf * 4 + c4
                if hf == 0:
                    K.X(DVE, "tensor_scalar", [ps, scl, sh], [hT], out=hT[:, ch, tt * 128:(tt + 1) * 128],
                        in0=ps.b(c4 * 128, (c4 + 1) * 128), scalar1=scl[:, ch, j:j + 1], scalar2=sh[:, ch, j:j + 1],
                        op0=ALU.mult, op1=ALU.add)
                else:
                    K.act([ps, scl, sh], [hT], out=hT[:, ch, tt * 128:(tt + 1) * 128], in_=ps.b(c4 * 128, (c4 + 1) * 128),
                          func=AF.Identity, scale=scl[:, ch, j:j + 1], bias=sh[:, ch, j:j + 1])


def norm_tmp(K, st):
    xin = [K.sb(st, f"nx{i}", [128, 1024], F32) for i in range(2)]
    xnb = [K.sb(st, f"nb{i}", [128, 1024], BF16) for i in range(2)]
    stat = [K.sb(st, f"ns{i}", [128, 4], F32) for i in range(2)]
    return xin, xnb, stat


def seq_list(K, need_ctx):
    l = [("x", b, TX, K.dram["xs"][b], b) for b in range(NBC)]
    if need_ctx:
        l += [("c", b, TC, K.dram["cs"][b], 2) for b in range(NBC)]
    return l


def bcast_row(K, t, row_ap):
    n = row_ap.shape[-1]
    K.Dm(SP, [], [t], out=t[:], in_=row_ap.broadcast_to([128, n]))


def proj_residual(K, st, fT, T, wo, res_dram, g1bc, gb, tmp):
    xh, t1, t2 = tmp
    it = 0
    for tt in range(T // 128):
        for dh in range(2):
            k = it % 2
            it += 1
            sl = slice(dh * 512, (dh + 1) * 512)
            K.Dm(SP, [], [xh[k]], out=xh[k][:], in_=res_dram[tt * 128:(tt + 1) * 128, sl])
            ps = K.ps()
            for fc in range(8):
                K.mm(ps, ps.f(), fT[:, fc, tt * 128:(tt + 1) * 128], wo[:, fc, sl], [fT, wo], start=(fc == 0), stop=(fc == 7))
            K.X(DVE, "tensor_tensor", [ps, g1bc], [t1[k]], out=t1[k][:], in0=ps.f(), in1=g1bc[:, sl], op=ALU.mult)
            if gb is not None:
                K.X(POOL, "tensor_tensor", [xh[k], gb], [xh[k]], out=xh[k][:], in0=xh[k][:], in1=gb[:, sl], op=ALU.add)
            K.X(POOL, "tensor_tensor", [xh[k], t1[k]], [t2[k]], out=t2[k][:], in0=xh[k][:], in1=t1[k][:], op=ALU.add)
            K.Dm(ACT, [t2[k]], [], out=res_dram[tt * 128:(tt + 1) * 128, sl], in_=t2[k][:])


def proj_tmp(K, st):
    return ([K.sb(st, f"pxh{i}", [128, 512], F32) for i in range(2)],
            [K.sb(st, f"pt1{i}", [128, 512], F32) for i in range(2)],
            [K.sb(st, f"pt2{i}", [128, 512], F32) for i in range(2)])


def phase_fnet(K, i, fi, need_ctx):
    nc, C = K.nc, K.C
    with contextlib.ExitStack() as st:
        ng = load_cols(K, st, "ng", [K.w("norm_g")[i, 0:1, :]])
        scl, sh = mod_scalars(K, st, i, 1, 0, ng, "f")
        wo = K.sb(st, "fwo", [128, 8, 1024], BF16)
        K.Dm(POOL, [], [wo], out=wo[:], in_=K.w("fnet_wo")[fi].rearrange("(kc p) n -> p kc n", p=128))
        bobc = K.sb(st, "bobc", [128, 1024], F32)
        bcast_row(K, bobc, K.w("fnet_bo")[fi:fi + 1, :])
        hT = K.sb(st, "hT", [128, 8, TX], BF16)
        XCS = K.sb(st, "XCS", [128, 16, 2, 512], BF16)
        tabs = [K.sb(st, f"tab{k}", [128, 16, 2, 256], BF16) for k in range(2)]
        g1bc = K.sb(st, "g1bc", [128, 1024], F32)
        gb = K.sb(st, "gb", [128, 1024], F32)
        ntmp = norm_tmp(K, st)
        ptmp = proj_tmp(K, st)
        tab_it = 0
        for (kind, b, T, res, j) in seq_list(K, need_ctx):
            nt = T // 128
            tokt = K.dram[f"c_tok_tab{T}"]
            bcast_row(K, g1bc, K.dram["modrow"][i, j:j + 1, 2 * 1024:3 * 1024])
            K.X(POOL, "tensor_tensor", [g1bc, bobc], [gb], out=gb[:], in0=g1bc[:], in1=bobc[:], op=ALU.mult)
            norm_hT(K, st, res, T, scl, sh, j, hT, ntmp)
            fT = hT
            for half in range(2):
                for tt in range(nt):
                    for gg in range(2):
                        g = 2 * half + gg
                        ps = K.ps()
                        for ci in range(2):
                            K.mm(ps, ps.f(), hT[:, 2 * g + ci, tt * 128:(tt + 1) * 128], C["cs_tab"][:, ci, :],
                                 [hT, C["cs_tab"]], start=(ci == 0), stop=(ci == 1))
                        eng = DVE if (tt + gg) % 2 == 0 else ACT
                        if eng == DVE:
                            K.X(DVE, "tensor_copy", [ps], [XCS], out=XCS[:, tt, :, gg * 256:(gg + 1) * 256],
                                in_=ps.f().rearrange("p (a c) -> p a c", a=2))
                        else:
                            K.X(ACT, "copy", [ps], [XCS], out=XCS[:, tt, :, gg * 256:(gg + 1) * 256],
                                in_=ps.f().rearrange("p (a c) -> p a c", a=2))
                NBK = 256
                for tb in range(T // NBK):
                    tab = tabs[tab_it % 2]
                    tab_it += 1
                    for a in range(2):
                        K.Dm(SP, [tab] if a else [], [tab], out=tab[:, 0:nt, a, :],
                             in_=tokt[:, a, tb * NBK:(tb + 1) * NBK].rearrange("(tt p) n -> p tt n", p=128))
                    for fq in range(4):
                        fc = 4 * half + fq
                        ps = K.ps()
                        for tt in range(nt):
                            for a in range(2):
                                K.mm(ps, ps.f(0, NBK), XCS[:, tt, a, fq * 128:(fq + 1) * 128], tab[:, tt, a, :],
                                     [XCS, tab], start=(tt == 0 and a == 0), stop=(tt == nt - 1 and a == 1))
                        if fq % 2 == 0:
                            K.X(DVE, "tensor_copy", [ps], [fT], out=fT[:, fc, tb * NBK:(tb + 1) * NBK], in_=ps.f(0, NBK))
                        else:
                            K.X(ACT, "copy", [ps], [fT], out=fT[:, fc, tb * NBK:(tb + 1) * NBK], in_=ps.f(0, NBK))
            proj_residual(K, st, fT, T, wo, res, g1bc, gb, ptmp)
        K.barrier()


def phase_final(K):
    nc, C = K.nc, K.C
    with contextlib.ExitStack() as st:
        fg = K.sb(st, "fgbc", [128, 1024], F32)
        bcast_row(K, fg, K.w("final_g")[0:1, :])
        xin = [K.sb(st, f"fx{i}", [128, 1024], F32) for i in range(2)]
        xo = [K.sb(st, f"fo{i}", [128, 1024], F32) for i in range(2)]
        stat = [K.sb(st, f"fs{i}", [128, 4], F32) for i in range(2)]
        it = 0
        for b in range(NBC):
            for tt in range(TX // 128):
                k = it % 2
                it += 1
                K.Dm(SP, [], [xin[k]], out=xin[k][:], in_=K.dram["xs"][b, tt * 128:(tt + 1) * 128, :])
                s = stat[k]
                K.act([xin[k]], [xo[k], s], out=xo[k][:], in_=xin[k][:], func=AF.Square, accum_out=s[:, 0:1])
                K.act([s, C["eps"]], [s], out=s[:, 1:2], in_=s[:, 0:1], func=AF.Sqrt, bias=C["eps"][:, 0:1], scale=1.0 / D)
                K.X(DVE, "reciprocal", [s], [s], out=s[:, 2:3], in_=s[:, 1:2])
                K.X(DVE, "scalar_tensor_tensor", [xin[k], s, fg], [xo[k]], out=xo[k][:], in0=xin[k][:], scalar=s[:, 2:3],
                    in1=fg[:], op0=ALU.mult, op1=ALU.mult)
                K.Dm(ACT, [xo[k]], [], out=K.dram["out"][b, tt * 128:(tt + 1) * 128, :], in_=xo[k][:])
        K.barrier()


def build(n_layers=DEPTH, debug=False, phases=("fnet", "rwkv", "moe"), moe_route_only=False, moe_no_ctx=False, rw_stop=99, prep_stop=-1, prep_cnt=1):
    nc = bass.Bass("TRN2", target_bir_lowering=False)
    consts = make_consts()
    es = contextlib.ExitStack()
    dbg_names = []
    with es:
        K = Ctx(nc, es, consts, debug)
        K.moe_route_only = moe_route_only
        K.moe_no_ctx = moe_no_ctx
        K.rw_stop = rw_stop
        K.prep_stop = prep_stop
        K.prep_cnt = prep_cnt
        K.din("x", [NBC, TX, D], F32)
        K.din("c", [NBC, D], F32)
        K.din("ctx", [NBC, TC, D], F32)
        K.din("c_ctx", [1, D], F32)
        K.dout("out", [NBC, TX, D], F32)
        K.dram["xs"] = Multi([K.dscr(f"xs{b}", [TX, D], F32) for b in range(NBC)])
        K.dram["cs"] = Multi([K.dscr(f"cs{b}", [TC, D], F32) for b in range(NBC)])

        def dbg(name):
            if not debug:
                return
            ox = K.dout("dbg_x_" + name, [NBC, TX, D], F32)
            oc = K.dout("dbg_c_" + name, [NBC, TC, D], F32)
            for b in range(NBC):
                K.Dm(SP, [], [], out=ox[b], in_=K.dram["xs"][b])
                K.Dm(SP, [], [], out=oc[b], in_=K.dram["cs"][b])
            dbg_names.append(name)
            K.barrier()

        load_consts(K, es)
        phase_adaln(K, es, n_layers)
        if debug:
            om = K.dout("dbg_modrow", [DEPTH, 3, 6144], F32)
            K.Dm(SP, [], [], out=om, in_=K.dram["modrow"])
        for b in range(NBC):
            K.Dm(SP, [], [], out=K.dram["xs"][b], in_=K.dram["x"][b])
            K.Dm(SP, [], [], out=K.dram["cs"][b], in_=K.dram["ctx"][b])
        K.barrier()
        try:
            for i in range(n_layers):
                need_ctx = i < DEPTH - 1
                if i % 2 == 0:
                    if "fnet" in phases:
                        phase_fnet(K, i, i // 2, need_ctx)
                else:
                    if "rwkv" in phases:
                        phase_rwkv(K, i, i // 2, need_ctx)
                dbg(f"mix{i}")
                if "moe" in phases:
                    phase_moe(K, i, need_ctx)
                dbg(f"moe{i}")
        except _Stop:
            pass
        K.P.muted = False
        K.barrier()
        phase_final(K)
        print(f"[build] instructions={K.P.n_inst} waits={K.P.n_wait}", flush=True)
    return nc, consts, dbg_names, K.wdecl


def core_inputs(inputs, consts, core, wdecl):
    b0 = core * NBC
    m = {
        "x": np.ascontiguousarray(inputs["x"][b0:b0 + NBC], dtype=np.float32),
        "c": np.ascontiguousarray(inputs["c"][b0:b0 + NBC], dtype=np.float32),
        "ctx": np.ascontiguousarray(inputs["ctx"][b0:b0 + NBC], dtype=np.float32),
        "c_ctx": np.ascontiguousarray(np.asarray(inputs["c_ctx"], dtype=np.float32).reshape(1, D)),
    }
    for key, n, layer in wdecl:
        a = np.asarray(inputs[n], dtype=np.float32).reshape(W_SHAPES[n])
        m[key] = np.ascontiguousarray(a if layer is None else a[layer])
    for k, v in consts.items():
        m["c_" + k] = v
    return m


def kernel(**inputs):
    nc, consts, _, wdecl = build()
    n_cores = 8
    in_maps = [core_inputs(inputs, consts, c, wdecl) for c in range(n_cores)]
    res = run_bass_kernel_spmd(nc, in_maps, core_ids=list(range(n_cores)))
    return np.concatenate([np.asarray(r["out"]) for r in res.results], axis=0).astype(np.float32)


def IOA(ap):
    return bass.IndirectOffsetOnAxis(ap=ap, axis=0)


def topk_rows(K, aff, work, vals, idxs, rounds):
    cur = aff
    for r in range(rounds):
        sl = slice(r * 8, (r + 1) * 8)
        K.X(DVE, "max", [cur], [vals], out=vals[:, sl], in_=cur[:])
        K.X(DVE, "max_index", [vals, cur], [idxs], out=idxs[:, sl], in_max=vals[:, sl], in_values=cur[:])
        if r + 1 < rounds:
            K.X(DVE, "match_replace", [vals, cur], [work], out=work[:], in_to_replace=vals[:, sl], in_values=cur[:],
                imm_value=-1.0)
            cur = work


def phase_moe(K, i, need_ctx):
    nc, C = K.nc, K.C
    if getattr(K, "moe_no_ctx", False):
        need_ctx = False
    if "hsrc_x" not in K.dram:
        K.dram["hsrc_x"] = Multi([K.dscr(f"hsrc_x{b}", [TX, D], BF16) for b in range(NBC)])
        K.dram["hsrc_c"] = Multi([K.dscr(f"hsrc_c{b}", [TC, D], BF16) for b in range(NBC)])
    hsrc = {"x": K.dram["hsrc_x"], "c": K.dram["hsrc_c"]}
    resid = {"x": K.dram["xs"], "c": K.dram["cs"]}
    kinds = [("x", TX, 256)] + ([("c", TC, 32)] if need_ctx else [])
    modrow = K.dram["modrow"]
    with contextlib.ExitStack() as st0:
        idxT = K.sb(st0, "idxT", [128, 2, 48], I32)
        gateT = K.sb(st0, "gateT", [128, 2, 48], F32)
        idxTc = K.sb(st0, "idxTc", [32, 48], I32)
        gcT = K.sb(st0, "gcT", [32, 48], F32)
        g2bc = [K.sb(st0, f"g2bc{j}", [128, 1024], F32) for j in range(3)]
        for j in range(3):
            bcast_row(K, g2bc[j], modrow[i, j:j + 1, 5 * 1024:6 * 1024])
        with contextlib.ExitStack() as st:
            ngbc = K.sb(st, "ngbc", [128, 1024], F32)
            bcast_row(K, ngbc, K.w("norm_g")[i, 1:2, :])
            sclbc, shbc = [], []
            for j in range(3):
                a = K.sb(st, f"scl2bc{j}", [128, 1024], F32)
                b_ = K.sb(st, f"sh2bc{j}", [128, 1024], F32)
                bcast_row(K, a, modrow[i, j:j + 1, 4 * 1024:5 * 1024])
                bcast_row(K, b_, modrow[i, j:j + 1, 3 * 1024:4 * 1024])
                K.X(DVE, "scalar_tensor_tensor", [a, ngbc], [a], out=a[:], in0=a[:], scalar=1.0, in1=ngbc[:],
                    op0=ALU.add, op1=ALU.mult)
                sclbc.append(a)
                shbc.append(b_)
            router = K.sb(st, "router", [128, 8, 16], F32)
            K.Dm(SP, [], [router], out=router[:], in_=K.w("moe_router")[i].rearrange("(kc p) n -> p kc n", p=128))
            xin = [K.sb(st, f"mx{k}", [128, 1024], F32) for k in range(2)]
            hf = [K.sb(st, f"mh{k}", [128, 1024], F32) for k in range(2)]
            hb = [K.sb(st, f"mhb{k}", [128, 1024], BF16) for k in range(2)]
            hTt = [K.sb(st, f"mhT{k}", [128, 8, 128], F32) for k in range(2)]
            stat = [K.sb(st, f"ms{k}", [128, 8], F32) for k in range(2)]
            ex = [K.sb(st, f"mex{k}", [128, 16], F32) for k in range(2)]
            affp = [K.sb(st, f"affp{k}", [128, 48], F32) for k in range(2)]
            for k in range(2):
                K.X(DVE, "memset", [], [affp[k]], ap=affp[k][:], constant=0.0)
            it = 0
            for (kind, T, cap) in kinds:
                nt = T // 128
                affT = K.sb(st, f"affT{kind}", [48, T], F32)
                work = K.sb(st, f"work{kind}", [48, T], F32)
                vals = K.sb(st, f"vals{kind}", [48, max(cap, 64)], F32)
                idxs = K.sb(st, f"idxs{kind}", [48, max(cap, 64)], U32)
                idxf = K.sb(st, f"idxf{kind}", [48, max(cap, 64)], F32)
                for tt in range(nt):
                    ap_ = affp[tt % 2]
                    for si in range(NBC):
                        j = si if kind == "x" else 2
                        k = it % 2
                        it += 1
                        xt, h, s = xin[k], hf[k], stat[k]
                        K.Dm(SP, [], [xt], out=xt[:], in_=resid[kind][si, tt * 128:(tt + 1) * 128, :])
                        K.act([xt], [hb[k], s], out=hb[k][:], in_=xt[:], func=AF.Square, accum_out=s[:, 0:1])
                        K.act([s, C["eps"]], [s], out=s[:, 1:2], in_=s[:, 0:1], func=AF.Sqrt, bias=C["eps"][:, 0:1], scale=1.0 / D)
                        K.X(DVE, "reciprocal", [s], [s], out=s[:, 2:3], in_=s[:, 1:2])
                        K.X(DVE, "scalar_tensor_tensor", [xt, s, sclbc[j]], [h], out=h[:], in0=xt[:], scalar=s[:, 2:3],
                            in1=sclbc[j][:], op0=ALU.mult, op1=ALU.mult)
                        K.X(POOL, "tensor_tensor", [h, shbc[j]], [h], out=h[:], in0=h[:], in1=shbc[j][:], op=ALU.add)
                        K.X(ACT, "copy", [h], [hb[k]], out=hb[k][:], in_=h[:])
                        K.Dm(ACT, [hb[k]], [], out=hsrc[kind][si, tt * 128:(tt + 1) * 128, :], in_=hb[k][:])
                        for hh in range(2):
                            ps = K.ps()
                            for c4 in range(4):
                                ch = hh * 4 + c4
                                K.tr(ps, ps.f(c4 * 128, (c4 + 1) * 128), h[:, ch * 128:(ch + 1) * 128], C["ident_f"][:],
                                     [h, C["ident_f"]])
                            if hh == 0:
                                K.X(DVE, "tensor_copy", [ps], [hTt[k]], out=hTt[k][:, 0:4, :],
                                    in_=ps.f().rearrange("p (a c) -> p a c", a=4))
                            else:
                                K.X(ACT, "copy", [ps], [hTt[k]], out=hTt[k][:, 4:8, :],
                                    in_=ps.f().rearrange("p (a c) -> p a c", a=4))
                        ps = K.ps()
                        for kc in range(8):
                            K.mm(ps, ps.f(0, 16), hTt[k][:, kc, :], router[:, kc, :], [hTt[k], router],
                                 start=(kc == 0), stop=(kc == 7))
                        K.X(DVE, "tensor_reduce", [ps], [s], out=s[:, 3:4], in_=ps.f(0, 16), axis=AX.X, op=ALU.max)
                        K.X(DVE, "tensor_scalar", [s], [s], out=s[:, 4:5], in0=s[:, 3:4], scalar1=-1.0, scalar2=None, op0=ALU.mult)
                        K.act([ps, s], [ex[k], s], out=ex[k][:], in_=ps.f(0, 16), func=AF.Exp, bias=s[:, 4:5], scale=1.0,
                              accum_out=s[:, 5:6])
                        K.X(DVE, "reciprocal", [s], [s], out=s[:, 6:7], in_=s[:, 5:6])
                        K.X(DVE, "tensor_scalar", [ex[k], s], [ap_], out=ap_[:, si * 32:si * 32 + 16], in0=ex[k][:],
                            scalar1=s[:, 6:7], scalar2=None, op0=ALU.mult)
                    ps = K.ps()
                    K.tr(ps, ps.f(0, 128, rows=48), ap_[:, 0:48], C["ident_f"][:], [ap_, C["ident_f"]])
                    K.X(ACT, "copy", [ps], [affT], out=affT[:, tt * 128:(tt + 1) * 128], in_=ps.f(0, 128, rows=48))
                topk_rows(K, affT, work, vals, idxs, cap // 8)
                if kind == "x":
                    K.X(DVE, "tensor_copy", [idxs], [idxf], out=idxf[:, 0:256], in_=idxs[:, 0:256])
                    for ch in range(2):
                        ps = K.ps()
                        K.tr(ps, ps.f(0, 48), idxf[0:48, ch * 128:(ch + 1) * 128], C["ident_f"][0:48, 0:48], [idxf, C["ident_f"]])
                        K.tr(ps, ps.f(64, 112), vals[0:48, ch * 128:(ch + 1) * 128], C["ident_f"][0:48, 0:48], [vals, C["ident_f"]])
                        K.X(DVE, "tensor_copy", [ps], [idxT], out=idxT[:, ch, :], in_=ps.f(0, 48))
                        K.X(DVE, "tensor_copy", [ps], [gateT], out=gateT[:, ch, :], in_=ps.f(64, 112))
                else:
                    K.X(DVE, "tensor_copy", [idxs], [idxf], out=idxf[:, 0:32], in_=idxs[:, 0:32])
                    ps = K.ps()
                    K.tr(ps, ps.f(0, 48, rows=32), idxf[0:48, 0:32], C["ident_f"][0:48, 0:48], [idxf, C["ident_f"]])
                    K.tr(ps, ps.f(64, 112, rows=32), vals[0:48, 0:32], C["ident_f"][0:48, 0:48], [vals, C["ident_f"]])
                    K.X(DVE, "tensor_copy", [ps], [idxTc], out=idxTc[:], in_=ps.f(0, 48, rows=32))
                    K.X(DVE, "tensor_copy", [ps], [gcT], out=gcT[:], in_=ps.f(64, 112, rows=32))
            K.barrier()
            if K.debug and i == 0:
                for nm, t, shp, dt in [("idxT", idxT, [128, 2, 48], I32), ("gateT", gateT, [128, 2, 48], F32),
                                       ("idxTc", idxTc, [32, 48], I32), ("gcT", gcT, [32, 48], F32)]:
                    o = K.dout("dbg_" + nm, shp, dt)
                    K.Dm(SP, [t], [], out=o, in_=t[:])
                o = K.dout("dbg_hsrc_x", [NBC, TX, D], BF16)
                for b in range(NBC):
                    K.Dm(SP, [], [], out=o[b], in_=hsrc["x"][b])
                K.barrier()
        if getattr(K, "moe_route_only", False):
            return
        with contextlib.ExitStack() as st:
            wbuf = [[K.sb(st, f"mw{k}{m}", [128, 8, 1024], BF16) for m in range(3)] for k in range(2)]
            xg = [[K.sb(st, f"xg{k}{m}", [128, 1024], BF16) for m in range(4)] for k in range(2)]
            xgc = [[K.sb(st, f"xgc{k}{b}", [32, 1024], BF16) for b in range(NBC)] for k in range(2)]
            xsT = [K.sb(st, f"xsT{k}", [128, 8, 576], BF16) for k in range(2)]
            hidT = K.sb(st, "hidT", [128, 8, 576], BF16)
            sg = [K.sb(st, f"sg{k}", [128, 576], F32) for k in range(2)]
            yo = [K.sb(st, f"yo{k}", [128, 1024], F32) for k in range(3)]
            wnames = ["moe_wg", "moe_wu", "moe_wd"]
            res_x = [Res(f"xs{b}") for b in range(NBC)]
            res_c = [Res(f"cs{b}") for b in range(NBC)]
            NCX = 576 if need_ctx else 512
            yo_it = 0
            sg_it = 0

            def load_w(e):
                k = e % 2
                for m in range(3):
                    K.Dm(POOL, [], [wbuf[k][m]], out=wbuf[k][m][:],
                         in_=K.w(wnames[m], i)[e].rearrange("(kc p) n -> p kc n", p=128))

            def gather(e):
                k = e % 2
                for si in range(NBC):
                    for ch in range(2):
                        col = si * 32 + e
                        K.DI([idxT], [xg[k][si * 2 + ch]], out=xg[k][si * 2 + ch][:], out_offset=None,
                             in_=hsrc["x"][si], in_offset=IOA(idxT[:, ch, col:col + 1]))
                if need_ctx:
                    for b in range(NBC):
                        K.DI([idxTc], [xgc[k][b]], out=xgc[k][b][:], out_offset=None, in_=hsrc["c"][b],
                             in_offset=IOA(idxTc[:, b * 32 + e:b * 32 + e + 1]))

            load_w(0)
            gather(0)
            for e in range(NE):
                k = e % 2
                if e + 1 < NE:
                    load_w(e + 1)
                    gather(e + 1)
                wg, wu, wd = wbuf[k]
                for f in range(8):
                    ps = K.ps()
                    for t4 in range(4):
                        K.tr(ps, ps.b(t4 * 128, (t4 + 1) * 128), xg[k][t4][:, f * 128:(f + 1) * 128], C["ident_b"][:],
                             [xg[k][t4], C["ident_b"]])
                    if need_ctx:
                        for b in range(NBC):
                            K.tr(ps, ps.b(512 + b * 32, 544 + b * 32), xgc[k][b][:, f * 128:(f + 1) * 128], C["ident_b"][0:32, 0:32],
                                 [xgc[k][b], C["ident_b"]])
                    if f % 2 == 0:
                        K.X(DVE, "tensor_copy", [ps], [xsT[k]], out=xsT[k][:, f, 0:NCX], in_=ps.b(0, NCX))
                    else:
                        K.X(ACT, "copy", [ps], [xsT[k]], out=xsT[k][:, f, 0:NCX], in_=ps.b(0, NCX))
                for fc in range(8):
                    fs = slice(fc * 128, (fc + 1) * 128)
                    psG = K.ps()
                    for f in range(8):
                        K.mm(psG, psG.f(), wg[:, f, fs], xsT[k][:, f, 0:512], [wg, xsT[k]], start=(f == 0), stop=(f == 7))
                    psU = K.ps()
                    for f in range(8):
                        K.mm(psU, psU.f(), wu[:, f, fs], xsT[k][:, f, 0:512], [wu, xsT[k]], start=(f == 0), stop=(f == 7))
                    s_ = sg[sg_it % 2]
                    sg_it += 1
                    K.act([psG], [s_], out=s_[:, 0:512], in_=psG.f(), func=AF.Silu)
                    K.X(DVE, "tensor_tensor", [psU, s_], [hidT], out=hidT[:, fc, 0:512], in0=psU.f(), in1=s_[:, 0:512], op=ALU.mult)
                    if need_ctx:
                        psC = K.ps()
                        for f in range(8):
                            K.mm(psC, psC.f(0, 64), wg[:, f, fs], xsT[k][:, f, 512:576], [wg, xsT[k]], start=(f == 0), stop=(f == 7))
                        for f in range(8):
                            K.mm(psC, psC.f(64, 128), wu[:, f, fs], xsT[k][:, f, 512:576], [wu, xsT[k]], start=(f == 0), stop=(f == 7))
                        K.act([psC], [s_], out=s_[:, 512:576], in_=psC.f(0, 64), func=AF.Silu)
                        K.X(DVE, "tensor_tensor", [psC, s_], [hidT], out=hidT[:, fc, 512:576], in0=psC.f(64, 128), in1=s_[:, 512:576], op=ALU.mult)
                for t4 in range(4):
                    si, ch = t4 // 2, t4 % 2
                    col = si * 32 + e
                    y = yo[yo_it % 3]
                    yo_it += 1
                    for dh in range(2):
                        sl = slice(dh * 512, (dh + 1) * 512)
                        ps = K.ps()
                        for fc in range(8):
                            K.mm(ps, ps.f(), hidT[:, fc, t4 * 128:(t4 + 1) * 128], wd[:, fc, sl], [hidT, wd],
                                 start=(fc == 0), stop=(fc == 7))
                        K.X(DVE, "scalar_tensor_tensor", [ps, gateT, g2bc[si]], [y], out=y[:, sl], in0=ps.f(),
                            scalar=gateT[:, ch, col:col + 1], in1=g2bc[si][:, sl], op0=ALU.mult, op1=ALU.mult)
                    K.DI([y, idxT], [res_x[si]], out=resid["x"][si], out_offset=IOA(idxT[:, ch, col:col + 1]),
                         in_=y[:], in_offset=None, compute_op=ALU.add)
                if need_ctx:
                    for b in range(NBC):
                        y = yo[yo_it % 3]
                        yo_it += 1
                        col = b * 32 + e
                        for dh in range(2):
                            sl = slice(dh * 512, (dh + 1) * 512)
                            ps = K.ps()
                            for fc in range(8):
                                K.mm(ps, ps.f(0, 512, rows=32), hidT[:, fc, 512 + b * 32:544 + b * 32], wd[:, fc, sl], [hidT, wd],
                                     start=(fc == 0), stop=(fc == 7))
                            K.X(DVE, "scalar_tensor_tensor", [ps, gcT, g2bc[2]], [y], out=y[0:32, sl], in0=ps.f(0, 512, rows=32),
                                scalar=gcT[:, col:col + 1], in1=g2bc[2][0:32, sl], op0=ALU.mult, op1=ALU.mult)
                        K.DI([y, idxTc], [res_c[b]], out=resid["c"][b], out_offset=IOA(idxTc[:, col:col + 1]),
                             in_=y[0:32, :], in_offset=None, compute_op=ALU.add)
            K.barrier()


NCH = TL // 64
CMIX, CW0, CA0, CKK, CKA, CRK, CV0, CNG, CLW, CLB = 0, 6, 8, 10, 11, 12, 13, 14, 15, 16


def rw_scratch(K):
    if hasattr(K, "rw"):
        return K.rw
    S = {}
    for d in range(2):
        for n in ("at", "bt", "kt", "rt"):
            S[f"{n}{d}"] = K.dscr(f"rw_{n}{d}", [NBC, D, TL], BF16)
        S[f"GL{d}"] = K.dscr(f"rw_GL{d}", [NBC, D, NCH], F32)
        S[f"y{d}"] = K.dscr(f"rw_y{d}", [NBC, D, TL], F32)
    S["v"] = K.dscr("rw_v", [NBC, D, TL], BF16)
    S["bv"] = K.dscr("rw_bv", [NBC, D, TL], F32)
    S["g"] = K.dscr("rw_g", [NBC, D, TL], F32)
    S["vfirst"] = K.dscr("rw_vfirst", [NBC, D, TL], F32)
    K.rw = S
    return S


def rwkv_cols(K, st, i, ri):
    w = K.w
    rows = [w("rw_mix")[ri, jj:jj + 1, :] for jj in range(6)]
    rows += [w("rw_w0")[ri, 0:1, :], w("rw_w0")[ri, 1:2, :], w("rw_a0")[ri, 0:1, :], w("rw_a0")[ri, 1:2, :]]
    rows += [w("rw_kk")[ri:ri + 1, :], w("rw_ka")[ri:ri + 1, :], w("rw_rk")[ri:ri + 1, :]]
    rows += [w("rw_v0")[0:1, :]]
    rows += [w("norm_g")[i, 0:1, :], w("rw_lnx_w")[ri:ri + 1, :], w("rw_lnx_b")[ri:ri + 1, :]]
    return load_cols(K, st, "rwc", rows)


class _Stop(Exception):
    pass


def rwkv_prep(K, i, ri):
    nc, C = K.nc, K.C

    def chk(k):
        if getattr(K, "prep_stop", -1) == k:
            K.prep_seen = getattr(K, "prep_seen", 0) + 1
            if K.prep_seen >= K.prep_cnt:
                K.P.muted = True
    S = rw_scratch(K)
    with contextlib.ExitStack() as st:
        cols = rwkv_cols(K, st, i, ri)
        omka = K.sb(st, "omka", [128, 8, 1], F32)
        K.X(DVE, "tensor_scalar", [cols], [omka], out=omka[:], in0=cols[:, :, CKA:CKA + 1], scalar1=-1.0, scalar2=1.0,
            op0=ALU.mult, op1=ALU.add)
        scl, sh = mod_scalars(K, st, i, 1, 0, cols, "r", ng_idx=CNG)
        wr, wk, wv = [K.sb(st, n, [128, 8, 1024], BF16) for n in ("wr", "wk", "wv")]
        for t, n in ((wr, "rw_wr"), (wk, "rw_wk"), (wv, "rw_wv")):
            K.Dm(POOL, [], [t], out=t[:], in_=K.w(n)[ri].rearrange("(kc p) n -> p kc n", p=128))
        w1c = K.sb(st, "w1c", [128, 8, 128], BF16)
        a1c = K.sb(st, "a1c", [128, 8, 128], BF16)
        g1w = K.sb(st, "g1w", [128, 8, 128], BF16)
        for d in range(2):
            K.Dm(POOL, [w1c] if d else [], [w1c], out=w1c[:, :, d * 64:(d + 1) * 64],
                 in_=K.w("rw_w1")[ri, d].rearrange("(kc p) n -> p kc n", p=128))
            K.Dm(POOL, [a1c] if d else [], [a1c], out=a1c[:, :, d * 64:(d + 1) * 64],
                 in_=K.w("rw_a1")[ri, d].rearrange("(kc p) n -> p kc n", p=128))
        K.Dm(POOL, [], [g1w], out=g1w[:], in_=K.w("rw_g1")[ri].rearrange("(kc p) n -> p kc n", p=128))
        w2a = K.sb(st, "w2a", [128, 1024], BF16)
        a2a = K.sb(st, "a2a", [128, 1024], BF16)
        g2w = K.sb(st, "g2w", [128, 1024], BF16)
        for d in range(2):
            K.Dm(POOL, [w2a] if d else [], [w2a], out=w2a[d * 64:(d + 1) * 64, :], in_=K.w("rw_w2")[ri, d])
            K.Dm(POOL, [a2a] if d else [], [a2a], out=a2a[d * 64:(d + 1) * 64, :], in_=K.w("rw_a2")[ri, d])
        K.Dm(POOL, [], [g2w], out=g2w[:], in_=K.w("rw_g2")[ri])
        if ri == 1:
            v1w = K.sb(st, "v1w", [128, 8, 32], BF16)
            v2w = K.sb(st, "v2w", [32, 1024], BF16)
            K.Dm(POOL, [], [v1w], out=v1w[:], in_=K.w("rw_v1")[0].rearrange("(kc p) n -> p kc n", p=128))
            K.Dm(POOL, [], [v2w], out=v2w[:], in_=K.w("rw_v2")[0])
        hT = K.sb(st, "rhT", [128, 8, TX], BF16)
        xx = K.sb(st, "rxx", [128, 8, 256], BF16)
        xtmp = K.sb(st, "rxtmp", [128, 8, 256], BF16)
        xj = [K.sb(st, f"rxj{jj}", [128, 8, 256], BF16) for jj in range(6)]
        tw = K.sb(st, "rtw", [128, 256], BF16)
        ta = K.sb(st, "rta", [128, 256], BF16)
        tg = K.sb(st, "rtg", [128, 256], BF16)
        tgf = K.sb(st, "rtgf", [128, 256], F32)
        hcols = K.sb(st, "hcols", [128, 8, 17], F32)
        K.X(DVE, "tensor_scalar", [cols], [hcols], out=hcols[:], in0=cols[:], scalar1=0.5, scalar2=None, op0=ALU.mult)
        tv = K.sb(st, "rtv", [32, 256], BF16)
        ntmp = norm_tmp(K, st)

        def f32t(n, w_=256):
            return K.sb(st, n, [128, w_], F32)
        RK, VG = f32t("RK", 512), f32t("VG", 512)
        SGW, ASIG = K.sb(st, "SGW", [128, 2, 256], F32), K.sb(st, "ASIG", [128, 2, 256], F32)
        SV, KR, SQ_, RN, KKN, NKK, VF, DV = [f32t(n) for n in ("SV", "KR", "SQ", "RN", "KKN", "NKK", "VF", "DV")]
        TMP, BVEC, CUM, CUMR, T3, E1, E2, E3 = [f32t(n) for n in ("TMP", "BVEC", "CUM", "CUMR", "T3", "E1", "E2", "E3")]
        KD = [f32t("KD0"), f32t("KD1")]
        RKR, KS, PR, BVV = [f32t(n) for n in ("RKR", "KS", "PR", "BVV")]
        VB16 = K.sb(st, "VB16", [128, 256], BF16)
        stg = {(n, d): K.sb(st, f"stg_{n}{d}", [128, 256], BF16) for n in ("at", "bt", "kt", "rt") for d in range(2)}
        GLt = [K.sb(st, f"GLt{d}", [128, 4], F32) for d in range(2)]

        def pool_tt(out_t, out, a_t, a, b_t, b, op):
            K.X(POOL, "tensor_tensor", [a_t, b_t], [out_t], out=out, in0=a, in1=b, op=op)

        def shift_block(kind, T, t0):
            def sub(c0, c1, oa, ob, ia0, ia1, ib0, ib1):
                pool_tt(xx, xx[:, c0:c1, oa:ob], hT, hT[:, c0:c1, ia0:ia1], hT, hT[:, c0:c1, ib0:ib1], ALU.subtract)

            def neg(c0, c1, oa, ob, ia, ib):
                K.X(POOL, "tensor_scalar", [hT], [xx], out=xx[:, c0:c1, oa:ob], in0=hT[:, c0:c1, ia:ib], scalar1=-1.0,
                    scalar2=None, op0=ALU.mult)
            if kind == "c":
                sub(0, 4, 1, 256, 0, 255, 1, 256)
                neg(0, 4, 0, 1, 0, 1)
                sub(4, 8, 0, 255, 1, 256, 0, 255)
                neg(4, 8, 255, 256, 255, 256)
                return
            hv = lambda c0, c1: hT[:, c0:c1, t0:t0 + 256].rearrange("p c (r w) -> p c r w", w=64)
            xv = lambda c0, c1: xx[:, c0:c1, :].rearrange("p c (r w) -> p c r w", w=64)
            K.X(POOL, "tensor_tensor", [hT], [xx], out=xv(0, 2)[:, :, :, 1:64], in0=hv(0, 2)[:, :, :, 0:63],
                in1=hv(0, 2)[:, :, :, 1:64], op=ALU.subtract)
            K.X(POOL, "tensor_scalar", [hT], [xx], out=xv(0, 2)[:, :, :, 0:1], in0=hv(0, 2)[:, :, :, 0:1], scalar1=-1.0,
                scalar2=None, op0=ALU.mult)
            K.X(POOL, "tensor_tensor", [hT], [xx], out=xv(2, 4)[:, :, :, 0:63], in0=hv(2, 4)[:, :, :, 1:64],
                in1=hv(2, 4)[:, :, :, 0:63], op=ALU.subtract)
            K.X(POOL, "tensor_scalar", [hT], [xx], out=xv(2, 4)[:, :, :, 63:64], in0=hv(2, 4)[:, :, :, 63:64], scalar1=-1.0,
                scalar2=None, op0=ALU.mult)
            if t0 == 0:
                neg(4, 6, 0, 64, 0, 64)
                sub(4, 6, 64, 256, 0, 192, 64, 256)
            else:
                sub(4, 6, 0, 256, t0 - 64, t0 + 192, t0, t0 + 256)
            if t0 + 256 == T:
                sub(6, 8, 0, 192, t0 + 64, t0 + 256, t0, t0 + 192)
                neg(6, 8, 192, 256, t0 + 192, t0 + 256)
            else:
                sub(6, 8, 0, 256, t0 + 64, t0 + 320, t0, t0 + 256)

        for b in range(NBC):
            for (kind, T, src, j, tl0) in (("c", TC, K.dram["cs"][b], 2, 0), ("x", TX, K.dram["xs"][b], b, TC)):
                chk(0)
                norm_hT(K, st, src, T, scl, sh, j, hT, ntmp)
                chk(1)
                for tb in range(T // 256):
                    t0 = tb * 256
                    tl = tl0 + t0
                    shift_block(kind, T, t0)
                    chk(2)
                    for jj in range(6):
                        eng = DVE if jj % 2 == 0 else POOL
                        K.X(eng, "tensor_tensor", [xx, cols], [xtmp], out=xtmp[:], in0=xx[:],
                            in1=cols[:, :, CMIX + jj:CMIX + jj + 1].broadcast_to([128, 8, 256]), op=ALU.mult)
                        K.X(eng, "tensor_tensor", [xtmp, hT], [xj[jj]], out=xj[jj][:], in0=xtmp[:], in1=hT[:, :, t0:t0 + 256],
                            op=ALU.add)
                    xr, xw, xk, xv_, xa, xg_ = xj
                    chk(3)
                    ps = K.ps()
                    for f in range(8):
                        K.mm(ps, ps.f(0, 256), w1c[:, f, :], xw[:, f, :], [w1c, xw], start=(f == 0), stop=(f == 7))
                    chk(31)
                    for f in range(8):
                        K.mm(ps, ps.f(256, 512), a1c[:, f, :], xa[:, f, :], [a1c, xa], start=(f == 0), stop=(f == 7))
                    chk(32)
                    K.act([ps], [tw], out=tw[:], in_=ps.f(0, 256), func=AF.Tanh)
                    chk(33)
                    K.X(ACT, "copy", [ps], [ta], out=ta[:], in_=ps.f(256, 512))
                    chk(34)
                    ps = K.ps()
                    for f in range(8):
                        K.mm(ps, ps.f(0, 256), g1w[:, f, :], xg_[:, f, :], [g1w, xg_], start=(f == 0), stop=(f == 7))
                    if ri == 1:
                        for f in range(8):
                            K.mm(ps, ps.f(256, 512, rows=32), v1w[:, f, :], xv_[:, f, :], [v1w, xv_], start=(f == 0), stop=(f == 7))
                    chk(35)
                    K.act([ps], [tgf], out=tgf[:], in_=ps.f(0, 256), func=AF.Tanh, scale=0.5)
                    K.X(DVE, "tensor_scalar", [tgf], [tg], out=tg[:], in0=tgf[:], scalar1=0.5, scalar2=0.5, op0=ALU.mult, op1=ALU.add)
                    if ri == 1:
                        K.X(ACT, "copy", [ps], [tv], out=tv[:], in_=ps.f(256, 512, rows=32))
                    chk(4)
                    for dc in range(8):
                        fs = slice(dc * 128, (dc + 1) * 128)
                        drow = slice(dc * 128, (dc + 1) * 128)
                        dsl = (b, drow, slice(tl, tl + 256))
                        ps1 = K.ps()
                        for f in range(8):
                            K.mm(ps1, ps1.f(0, 256), wr[:, f, fs], xr[:, f, :], [wr, xr], start=(f == 0), stop=(f == 7))
                        for f in range(8):
                            K.mm(ps1, ps1.f(256, 512), wk[:, f, fs], xk[:, f, :], [wk, xk], start=(f == 0), stop=(f == 7))
                        K.X(ACT, "copy", [ps1], [RK], out=RK[:], in_=ps1.f())
                        chk(41)
                        ps2 = K.ps()
                        for f in range(8):
                            K.mm(ps2, ps2.f(0, 256), wv[:, f, fs], xv_[:, f, :], [wv, xv_], start=(f == 0), stop=(f == 7))
                        K.mm(ps2, ps2.f(256, 512), g2w[:, fs], tg[:], [g2w, tg])
                        K.X(DVE, "tensor_copy", [ps2], [VG], out=VG[:], in_=ps2.f())
                        chk(42)
                        for (wt2, tin, dst, cidx) in ((w2a, tw, SGW, CW0), (a2a, ta, ASIG, CA0)):
                            pss = [K.ps(), K.ps()]
                            for d in range(2):
                                K.mm(pss[d], pss[d].f(0, 256), wt2[d * 64:(d + 1) * 64, fs], tin[d * 64:(d + 1) * 64, :], [wt2, tin])
                            for d in range(2):
                                K.act([pss[d], hcols], [dst], out=dst[:, d, :], in_=pss[d].f(0, 256), func=AF.Tanh,
                                      bias=hcols[:, dc, cidx + d:cidx + d + 1], scale=0.5)
                            K.X(POOL, "tensor_scalar", [dst], [dst], out=dst[:], in0=dst[:], scalar1=0.5, scalar2=0.5, op0=ALU.mult, op1=ALU.add)
                        chk(44)
                        R_, Kk, V_, G_ = RK[:, 0:256], RK[:, 256:512], VG[:, 0:256], VG[:, 256:512]
                        chk(5)
                        K.Dm(SP, [VG], [], out=S["g"][dsl], in_=G_)
                        if ri == 1:
                            ps5 = K.ps()
                            K.mm(ps5, ps5.f(0, 256), v2w[0:32, fs], tv[0:32, :], [v2w, tv])
                            K.act([ps5, hcols], [SV], out=SV[:], in_=ps5.f(0, 256), func=AF.Tanh, bias=hcols[:, dc, CV0:CV0 + 1], scale=0.5)
                            K.X(POOL, "tensor_scalar", [SV], [SV], out=SV[:], in0=SV[:], scalar1=0.5, scalar2=0.5, op0=ALU.mult, op1=ALU.add)
                            K.Dm(SP, [], [VF], out=VF[:], in_=S["vfirst"][dsl])
                            pool_tt(DV, DV[:], VF, VF[:], VG, V_, ALU.subtract)
                            pool_tt(DV, DV[:], DV, DV[:], SV, SV[:], ALU.mult)
                            pool_tt(VG, V_, VG, V_, DV, DV[:], ALU.add)
                        else:
                            K.Dm(SP, [VG], [], out=S["vfirst"][dsl], in_=V_)
                        K.X(ACT, "copy", [VG], [VB16], out=VB16[:], in_=V_)
                        K.Dm(SP, [VB16], [], out=S["v"][dsl], in_=VB16[:])
                        chk(6)
                        K.X(POOL, "tensor_scalar", [RK, cols], [KR], out=KR[:], in0=Kk, scalar1=cols[:, dc, CKK:CKK + 1], scalar2=None, op0=ALU.mult)
                        pool_tt(SQ_, SQ_[:], KR, KR[:], KR, KR[:], ALU.mult)
                        ps6 = K.ps()
                        K.mm(ps6, ps6.f(0, 256), C["blockones_f"][:], SQ_[:], [C["blockones_f"], SQ_])
                        K.act([ps6, C["eps"]], [RN], out=RN[:], in_=ps6.f(0, 256), func=AF.Sqrt, bias=C["eps"][:, 1:2], scale=1.0)
                        K.X(DVE, "reciprocal", [RN], [RN], out=RN[:], in_=RN[:])
                        pool_tt(KKN, KKN[:], KR, KR[:], RN, RN[:], ALU.mult)
                        K.X(POOL, "tensor_scalar", [KKN], [NKK], out=NKK[:], in0=KKN[:], scalar1=-1.0, scalar2=None, op0=ALU.mult)
                        chk(7)
                        for d in range(2):
                            K.X(POOL, "tensor_scalar", [ASIG, cols, omka], [TMP], out=TMP[:], in0=ASIG[:, d, :],
                                scalar1=cols[:, dc, CKA:CKA + 1], scalar2=omka[:, dc, 0:1], op0=ALU.mult, op1=ALU.add)
                            pool_tt(KD[d], KD[d][:], RK, Kk, TMP, TMP[:], ALU.mult)
                            pool_tt(BVEC, BVEC[:], KKN, KKN[:], ASIG, ASIG[:, d, :], ALU.mult)
                            K.X(DVE, "tensor_tensor_scan", [SGW, C["segmask"]], [CUM], out=CUM[:], data0=C["segmask"][:],
                                data1=SGW[:, d, :], initial=0.0, op0=ALU.mult, op1=ALU.add)
                            K.act([CUM], [GLt[d]], out=GLt[d][:], in_=CUM[:].rearrange("p (c j) -> p c j", j=64)[:, :, 63],
                                  func=AF.Exp, scale=-KAPPA)
                            K.Dm(SP, [GLt[d]], [], out=S[f"GL{d}"][b, drow, tl // 64:tl // 64 + 4], in_=GLt[d][:])
                            cum = CUM
                            if d == 1:
                                pool_tt(T3, T3[:], CUM, CUM[:], SGW, SGW[:, d, :], ALU.subtract)
                                K.X(POOL, "tensor_tensor", [CUM, T3], [CUMR], out=CUMR[:].rearrange("p (c j) -> p c j", j=64),
                                    in0=CUM[:].rearrange("p (c j) -> p c j", j=64)[:, :, 63:64].broadcast_to([128, 4, 64]),
                                    in1=T3[:].rearrange("p (c j) -> p c j", j=64), op=ALU.subtract)
                                cum = CUMR
                            K.act([cum], [E1], out=E1[:], in_=cum[:], func=AF.Exp, scale=-KAPPA)
                            pool_tt(stg[("rt", d)], stg[("rt", d)][:], RK, R_, E1, E1[:], ALU.mult)
                            pool_tt(T3, T3[:], cum, cum[:], SGW, SGW[:, d, :], ALU.subtract)
                            K.act([T3], [E2], out=E2[:], in_=T3[:], func=AF.Exp, scale=-KAPPA)
                            pool_tt(stg[("at", d)], stg[("at", d)][:], NKK, NKK[:], E2, E2[:], ALU.mult)
                            K.act([cum], [E3], out=E3[:], in_=cum[:], func=AF.Exp, scale=KAPPA)
                            K.X(DVE, "tensor_tensor", [BVEC, E3], [stg[("bt", d)]], out=stg[("bt", d)][:], in0=BVEC[:], in1=E3[:], op=ALU.mult)
                            K.X(DVE, "tensor_tensor", [KD[d], E3], [stg[("kt", d)]], out=stg[("kt", d)][:], in0=KD[d][:], in1=E3[:], op=ALU.mult)
                            for n in ("at", "bt", "kt", "rt"):
                                K.Dm(SP, [stg[(n, d)]], [], out=S[f"{n}{d}"][dsl], in_=stg[(n, d)][:])
                        chk(8)
                        K.X(POOL, "tensor_scalar", [RK, cols], [RKR], out=RKR[:], in0=R_, scalar1=cols[:, dc, CRK:CRK + 1], scalar2=None, op0=ALU.mult)
                        pool_tt(KS, KS[:], KD[0], KD[0][:], KD[1], KD[1][:], ALU.add)
                        pool_tt(PR, PR[:], RKR, RKR[:], KS, KS[:], ALU.mult)
                        ps7 = K.ps()
                        K.mm(ps7, ps7.f(0, 256), C["blockones_f"][:], PR[:], [C["blockones_f"], PR])
                        K.X(DVE, "tensor_tensor", [ps7, VG], [BVV], out=BVV[:], in0=ps7.f(0, 256), in1=V_, op=ALU.mult)
                        K.Dm(SP, [BVV], [], out=S["bv"][dsl], in_=BVV[:])
                        chk(9)
        K.barrier()


def rwkv_scan(K):
    nc, C = K.nc, K.C
    S = K.rw
    order = {0: list(range(NCH)), 1: [3, 2, 1, 0] + list(range(NCH - 1, 3, -1))}
    identb = C["ident_b"]

    def ecopy(eng, ins, out_t, out, in_):
        if eng == ACT:
            K.X(ACT, "copy", ins, [out_t], out=out, in_=in_)
        else:
            K.X(DVE, "tensor_copy", ins, [out_t], out=out, in_=in_)

    for hpg in range(4):
        with contextlib.ExitStack() as st:
            chains = []
            for b in range(NBC):
                for hq in range(2):
                    for d in range(2):
                        c = dict(b=b, hp=2 * hpg + hq, d=d)
                        nm = f"{b}{hq}{d}"
                        c["cin"] = [{n: K.sb(st, f"ci{nm}{k}{n}", [128, 256], BF16) for n in ("at", "rt", "bt", "kt", "v")}
                                    for k in range(2)]
                        c["ARB"] = K.sb(st, "ARB" + nm, [128, 4, 256], BF16)
                        c["BTB"] = K.sb(st, "BTB" + nm, [128, 4, 128], BF16)
                        c["KTB"] = K.sb(st, "KTB" + nm, [128, 4, 128], BF16)
                        c["VB"] = K.sb(st, "VB" + nm, [128, 4, 128], BF16)
                        c["M1"] = K.sb(st, "M1" + nm, [128, 256], BF16)
                        c["M2"] = K.sb(st, "M2" + nm, [128, 256], BF16)
                        c["A0"] = K.sb(st, "A0" + nm, [128, 128], BF16)
                        c["TF"] = K.sb(st, "TF" + nm, [128, 384], BF16)
                        c["X"] = [K.sb(st, f"X{k}" + nm, [128, 256], BF16) for k in range(2)]
                        c["IP"] = [K.sb(st, f"IP{k}" + nm, [128, 128], BF16) for k in range(2)]
                        c["SQ"] = [K.sb(st, f"SQ{k}" + nm, [128, 256], BF16) for k in range(2)]
                        c["WT"] = K.sb(st, "WT" + nm, [128, 128], BF16)
                        c["U"] = K.sb(st, "U" + nm, [128, 128], BF16)
                        c["Ybd"] = K.sb(st, "Ybd" + nm, [128, 128], F32)
                        c["Hf"] = K.sb(st, "Hf" + nm, [128, 128], F32)
                        c["hg"] = K.sb(st, "hg" + nm, [128, 128], F32)
                        c["Hbf"] = K.sb(st, "Hbf" + nm, [128, 128], BF16)
                        c["yst"] = [K.sb(st, f"yst{k}" + nm, [128, 256], F32) for k in range(2)]
                        c["GL"] = K.sb(st, "GL" + nm, [128, NCH], F32)
                        rows = slice(c["hp"] * 128, (c["hp"] + 1) * 128)
                        c["rows"] = rows
                        K.Dm(SP, [], [c["GL"]], out=c["GL"][:], in_=S[f"GL{d}"][b, rows, :])
                        K.X(DVE, "memset", [], [c["Hf"]], ap=c["Hf"][:], constant=0.0)
                        K.X(DVE, "tensor_copy", [c["Hf"]], [c["Hbf"]], out=c["Hbf"][:], in_=c["Hf"][:])
                        chains.append(c)

            def load_group(c, grp, k):
                ci = c["cin"][k]
                b, d, rows = c["b"], c["d"], c["rows"]
                tsl = slice(grp * 256, (grp + 1) * 256)
                for n in ("at", "rt", "bt", "kt"):
                    K.Dm(SP, [], [ci[n]], out=ci[n][:], in_=S[f"{n}{d}"][b, rows, tsl])
                K.Dm(SP, [], [ci["v"]], out=ci["v"][:], in_=S["v"][b, rows, tsl])

            def expand_group(c, k):
                ci = c["cin"][k]
                msk = C["blkmask"][:].rearrange("p (h i) -> p h i", h=2).unsqueeze(1).broadcast_to([128, 4, 2, 64])

                def ex(src, dst_t, dst_ap):
                    K.X(POOL, "tensor_tensor", [src, C["blkmask"]], [dst_t],
                        out=dst_ap.rearrange("p c (h i) -> p c h i", h=2),
                        in0=src[:].rearrange("p (c i) -> p c i", i=64).unsqueeze(2).broadcast_to([128, 4, 2, 64]),
                        in1=msk, op=ALU.mult)
                ex(ci["at"], c["ARB"], c["ARB"][:, :, 0:128])
                ex(ci["rt"], c["ARB"], c["ARB"][:, :, 128:256])
                ex(ci["bt"], c["BTB"], c["BTB"][:, :, :])
                ex(ci["kt"], c["KTB"], c["KTB"][:, :, :])
                ex(ci["v"], c["VB"], c["VB"][:, :, :])

            def stage(per_bank, pe_fn, ev_fn, engs=(DVE, ACT)):
                for gi in range(0, len(chains), per_bank):
                    ps = K.ps()
                    grp = chains[gi:gi + per_bank]
                    for si, c in enumerate(grp):
                        pe_fn(c, ps, si)
                    eng = engs[(gi // per_bank) % len(engs)]
                    for si, c in enumerate(grp):
                        ev_fn(c, ps, si, eng)

            for c in chains:
                load_group(c, order[c["d"]][0] // 4, 0)
            for r in range(NCH):
                for c in chains:
                    n = order[c["d"]][r]
                    c["n"], c["wi"], c["grp"] = n, n % 4, n // 4
                    c["gk"] = (r // 4) % 2
                    if r % 4 == 0:
                        expand_group(c, c["gk"])
                        if r + 4 < NCH:
                            load_group(c, order[c["d"]][r + 4] // 4, 1 - c["gk"])
                stage(2, lambda c, ps, s: K.mm(ps, ps.f(s * 256, s * 256 + 256), c["BTB"][:, c["wi"], :], c["ARB"][:, c["wi"], :],
                                               [c["BTB"], c["ARB"]]),
                      lambda c, ps, s, e: K.X(DVE, "tensor_tensor", [ps, C["mT"]], [c["M1"]], out=c["M1"][:],
                                              in0=ps.f(s * 256, s * 256 + 256), in1=C["mT"][:, c["d"], :], op=ALU.mult), engs=(DVE,))
                stage(2, lambda c, ps, s: K.mm(ps, ps.f(s * 256, s * 256 + 256), c["KTB"][:, c["wi"], :], c["ARB"][:, c["wi"], :],
                                               [c["KTB"], c["ARB"]]),
                      lambda c, ps, s, e: K.X(DVE, "tensor_tensor", [ps, C["mT"]], [c["M2"]], out=c["M2"][:],
                                              in0=ps.f(s * 256, s * 256 + 256), in1=C["mT"][:, c["d"], :], op=ALU.mult), engs=(DVE,))
                stage(4, lambda c, ps, s: K.mm(ps, ps.f(s * 128, s * 128 + 128), c["ARB"][:, c["wi"], 0:128], c["BTB"][:, c["wi"], :],
                                               [c["ARB"], c["BTB"]]),
                      lambda c, ps, s, e: K.X(DVE, "tensor_tensor", [ps, C["mA"]], [c["A0"]], out=c["A0"][:],
                                              in0=ps.f(s * 128, s * 128 + 128), in1=C["mA"][:, c["d"], :], op=ALU.mult), engs=(DVE,))
                for c in chains:
                    K.X(POOL, "tensor_tensor", [c["M1"], identb], [c["IP"][0]], out=c["IP"][0][:], in0=c["M1"][:, 0:128],
                        in1=identb[:], op=ALU.add)

                def tr_pe(c, ps, s):
                    srcs = [(c["ARB"], c["ARB"][:, c["wi"], 0:128]), (c["BTB"], c["BTB"][:, c["wi"], :]),
                            (c["KTB"], c["KTB"][:, c["wi"], :]), (c["VB"], c["VB"][:, c["wi"], :])]
                    for q, (t, ap) in enumerate(srcs):
                        K.tr(ps, ps.b(s * 512 + q * 128, s * 512 + (q + 1) * 128), ap, identb[:], [t, identb])

                def tr_ev(c, ps, s, e):
                    ecopy(e, [ps], c["TF"], c["TF"][:], ps.b(s * 512 + 128, s * 512 + 512))
                    ecopy(e, [ps], c["X"][0], c["X"][0][:, 0:128], ps.b(s * 512, s * 512 + 128))
                stage(2, tr_pe, tr_ev)
                stage(4, lambda c, ps, s: K.mm(ps, ps.f(s * 128, s * 128 + 128), c["M2"][:, 0:128], c["TF"][:, 256:384], [c["M2"], c["TF"]]),
                      lambda c, ps, s, e: ecopy(e, [ps], c["X"][0], c["X"][0][:, 128:256], ps.f(s * 128, s * 128 + 128)))

                def sq_pe(m):
                    def f(c, ps, s):
                        if m == 0:
                            A_t, A_ap, AT_t, AT_ap = c["A0"], c["A0"][:], c["M1"], c["M1"][:, 0:128]
                        else:
                            q = c["SQ"][(m - 1) % 2]
                            A_t, A_ap, AT_t, AT_ap = q, q[:, 0:128], q, q[:, 128:256]
                        K.mm(ps, ps.f(s * 256, s * 256 + 128), AT_ap, A_ap, [A_t, AT_t])
                        K.mm(ps, ps.f(s * 256 + 128, s * 256 + 256), A_ap, AT_ap, [A_t, AT_t])
                    return f

                def sq_ev(m):
                    def f(c, ps, s, e):
                        ecopy(e, [ps], c["SQ"][m % 2], c["SQ"][m % 2][:], ps.f(s * 256, s * 256 + 256))
                    return f

                def neu_pe(m):
                    def f(c, ps, s):
                        K.mm(ps, ps.f(s * 256, s * 256 + 256), c["IP"][m % 2][:], c["X"][m % 2][:], [c["IP"][m % 2], c["X"][m % 2]])
                    return f

                def neu_ev(m):
                    def f(c, ps, s, e):
                        ecopy(e, [ps], c["X"][(m + 1) % 2], c["X"][(m + 1) % 2][:], ps.f(s * 256, s * 256 + 256))
                    return f
                for m in range(5):
                    stage(2, sq_pe(m), sq_ev(m))
                    stage(2, neu_pe(m), neu_ev(m), engs=(ACT, DVE))
                    for c in chains:
                        K.X(POOL, "tensor_tensor", [c["SQ"][m % 2], identb], [c["IP"][(m + 1) % 2]], out=c["IP"][(m + 1) % 2][:],
                            in0=c["SQ"][m % 2][:, 128:256], in1=identb[:], op=ALU.add)
                stage(2, neu_pe(5), neu_ev(5))
                stage(4, lambda c, ps, s: K.tr(ps, ps.b(s * 256, s * 256 + 128), c["X"][0][:, 0:128], identb[:], [c["X"][0], identb]),
                      lambda c, ps, s, e: ecopy(e, [ps], c["WT"], c["WT"][:], ps.b(s * 256, s * 256 + 128)))
                stage(4, lambda c, ps, s: K.mm(ps, ps.f(s * 128, s * 128 + 128), c["WT"][:], c["Hbf"][:], [c["WT"], c["Hbf"]]),
                      lambda c, ps, s, e: K.X(DVE, "tensor_tensor", [ps, c["X"][0]], [c["U"]], out=c["U"][:],
                                              in0=ps.f(s * 128, s * 128 + 128), in1=c["X"][0][:, 128:256], op=ALU.add), engs=(DVE,))

                def y_pe(c, ps, s):
                    o = ps.f(s * 128, s * 128 + 128)
                    K.mm(ps, o, c["ARB"][:, c["wi"], 128:256], c["Hbf"][:], [c["ARB"], c["Hbf"]], start=True, stop=False)
                    K.mm(ps, o, c["M1"][:, 128:256], c["U"][:], [c["M1"], c["U"]], start=False, stop=False)
                    K.mm(ps, o, c["M2"][:, 128:256], c["TF"][:, 256:384], [c["M2"], c["TF"]], start=False, stop=True)
                stage(4, y_pe, lambda c, ps, s, e: ecopy(e, [ps], c["Ybd"], c["Ybd"][:], ps.f(s * 128, s * 128 + 128)), engs=(ACT,))
                for c in chains:
                    K.X(POOL, "tensor_scalar", [c["Hf"], c["GL"]], [c["hg"]], out=c["hg"][:], in0=c["Hf"][:],
                        scalar1=c["GL"][:, c["n"]:c["n"] + 1], scalar2=None, op0=ALU.mult)

                def h_pe(c, ps, s):
                    o = ps.f(s * 128, s * 128 + 128)
                    K.mm(ps, o, c["TF"][:, 0:128], c["U"][:], [c["TF"], c["U"]], start=True, stop=False)
                    K.mm(ps, o, c["TF"][:, 128:256], c["TF"][:, 256:384], [c["TF"]], start=False, stop=True)

                def h_ev(c, ps, s, e):
                    K.X(DVE, "scalar_tensor_tensor", [ps, c["GL"], c["hg"]], [c["Hf"]], out=c["Hf"][:], in0=ps.f(s * 128, s * 128 + 128),
                        scalar=c["GL"][:, c["n"]:c["n"] + 1], in1=c["hg"][:], op0=ALU.mult, op1=ALU.add)
                stage(4, h_pe, h_ev, engs=(DVE,))
                for c in chains:
                    K.X(ACT, "copy", [c["Hf"]], [c["Hbf"]], out=c["Hbf"][:], in_=c["Hf"][:])
                stage(4, lambda c, ps, s: K.mm(ps, ps.f(s * 128, s * 128 + 64), c["Ybd"][:], C["sel"][:], [c["Ybd"], C["sel"]]),
                      lambda c, ps, s, e: ecopy(e, [ps], c["yst"][c["gk"]], c["yst"][c["gk"]][:, c["wi"] * 64:(c["wi"] + 1) * 64],
                                                ps.f(s * 128, s * 128 + 64)), engs=(ACT, DVE))
                if r % 4 == 3:
                    for c in chains:
                        ys = c["yst"][c["gk"]]
                        K.Dm(SP, [ys], [], out=S[f"y{c['d']}"][c["b"], c["rows"], c["grp"] * 256:(c["grp"] + 1) * 256], in_=ys[:])
            K.barrier()


def rwkv_out(K, i, ri, need_ctx):
    nc, C = K.nc, K.C
    S = K.rw
    with contextlib.ExitStack() as st:
        cols = rwkv_cols(K, st, i, ri)
        wo = K.sb(st, "rwo", [128, 8, 1024], BF16)
        K.Dm(POOL, [], [wo], out=wo[:], in_=K.w("rw_wo")[ri].rearrange("(kc p) n -> p kc n", p=128))
        g1bc = K.sb(st, "rg1bc", [128, 1024], F32)
        oT = K.sb(st, "roT", [128, 8, 256], BF16)
        ptmp = proj_tmp(K, st)

        def f32t(n):
            return [K.sb(st, f"{n}{k}", [128, 256], F32) for k in range(2)]
        Y0, Y1, YQ, MN, MQ, VAR, Z, BVt, Gt = [f32t(n) for n in ("oY0", "oY1", "oYQ", "oMN", "oMQ", "oVAR", "oZ", "oBV", "oG")]
        it = 0
        for b in range(NBC):
            seqs = [("x", TX, K.dram["xs"][b], b, TC)]
            if need_ctx:
                seqs = [("c", TC, K.dram["cs"][b], 2, 0)] + seqs
            for (kind, T, res, j, tl0) in seqs:
                bcast_row(K, g1bc, K.dram["modrow"][i, j:j + 1, 2 * 1024:3 * 1024])
                for tb in range(T // 256):
                    tl = tl0 + tb * 256
                    for dc in range(8):
                        k = it % 2
                        it += 1
                        dsl = (b, slice(dc * 128, (dc + 1) * 128), slice(tl, tl + 256))
                        y0, y1, yq, mn, mq, var, z, bv, g = Y0[k], Y1[k], YQ[k], MN[k], MQ[k], VAR[k], Z[k], BVt[k], Gt[k]
                        K.Dm(SP, [], [y0], out=y0[:], in_=S["y0"][dsl])
                        K.Dm(SP, [], [y1], out=y1[:], in_=S["y1"][dsl])
                        K.Dm(SP, [], [bv], out=bv[:], in_=S["bv"][dsl])
                        K.Dm(SP, [], [g], out=g[:], in_=S["g"][dsl])
                        K.X(POOL, "tensor_tensor", [y0, y1], [y0], out=y0[:], in0=y0[:], in1=y1[:], op=ALU.add)
                        K.X(POOL, "tensor_tensor", [y0], [yq], out=yq[:], in0=y0[:], in1=y0[:], op=ALU.mult)
                        ps = K.ps()
                        K.mm(ps, ps.f(0, 256), C["blockones_f"][:], y0[:], [C["blockones_f"], y0])
                        K.mm(ps, ps.f(256, 512), C["blockones_f"][:], yq[:], [C["blockones_f"], yq])
                        K.act([ps], [mn], out=mn[:], in_=ps.f(0, 256), func=AF.Copy, scale=1.0 / 64)
                        K.act([ps], [var], out=var[:], in_=ps.f(256, 512), func=AF.Copy, scale=1.0 / 64)
                        K.X(POOL, "tensor_tensor", [mn], [mq], out=mq[:], in0=mn[:], in1=mn[:], op=ALU.mult)
                        K.X(POOL, "tensor_tensor", [var, mq], [var], out=var[:], in0=var[:], in1=mq[:], op=ALU.subtract)
                        K.act([var, C["eps"]], [var], out=var[:], in_=var[:], func=AF.Sqrt, bias=C["eps"][:, 2:3], scale=1.0)
                        K.X(DVE, "reciprocal", [var], [var], out=var[:], in_=var[:])
                        K.X(POOL, "tensor_tensor", [y0, mn], [z], out=z[:], in0=y0[:], in1=mn[:], op=ALU.subtract)
                        K.X(POOL, "tensor_tensor", [z, var], [z], out=z[:], in0=z[:], in1=var[:], op=ALU.mult)
                        K.X(DVE, "tensor_scalar", [z, cols], [z], out=z[:], in0=z[:], scalar1=cols[:, dc, CLW:CLW + 1],
                            scalar2=cols[:, dc, CLB:CLB + 1], op0=ALU.mult, op1=ALU.add)
                        K.X(POOL, "tensor_tensor", [z, bv], [z], out=z[:], in0=z[:], in1=bv[:], op=ALU.add)
                        K.X(DVE, "tensor_tensor", [z, g], [oT], out=oT[:, dc, :], in0=z[:], in1=g[:], op=ALU.mult)
                    proj_residual(K, st, oT, 256, wo, res[tb * 256:(tb + 1) * 256, :], g1bc, None, ptmp)
        K.barrier()


def phase_rwkv(K, i, ri, need_ctx):
    K.set_psum("full")
    rwkv_prep(K, i, ri)
    if K.rw_stop >= 1:
        rwkv_scan(K)
    if K.debug and i == 1:
        for nm, dt in (("y0", F32), ("y1", F32), ("v", BF16), ("GL0", F32), ("GL1", F32), ("rt0", BF16), ("kt0", BF16), ("bv", F32), ("at1", BF16), ("bt1", BF16), ("at0", BF16), ("bt0", BF16), ("kt1", BF16), ("rt1", BF16), ("g", F32)):
            o = K.dout("dbg_rw_" + nm, list(K.rw[nm].shape), dt)
            for b in range(NBC):
                for dc in range(8):
                    K.Dm(SP, [], [], out=o[b, dc * 128:(dc + 1) * 128, :], in_=K.rw[nm][b, dc * 128:(dc + 1) * 128, :])
        K.barrier()
    if K.rw_stop >= 2:
        rwkv_out(K, i, ri, need_ctx)
```

```python
import contextlib
import numpy as np
import ml_dtypes
import concourse.bass as bass
import concourse.mybir as mybir
from concourse.bass_utils import run_bass_kernel_spmd

F32 = mybir.dt.float32
BF16 = mybir.dt.bfloat16
I32 = mybir.dt.int32
U32 = mybir.dt.uint32
AF = mybir.ActivationFunctionType
ALU = mybir.AluOpType
AX = mybir.AxisListType

PE, DVE, ACT, POOL, SP = 0, 1, 2, 3, 4
EPOCH = 30000
N_DSEM = 40


class Res:
    __slots__ = ("w", "r", "name", "excl")

    def __init__(self, name="", excl=False):
        self.w = None
        self.r = {}
        self.name = name
        self.excl = excl


class T:
    __slots__ = ("t", "res")

    def __init__(self, t, name=""):
        self.t = t
        self.res = Res(name)

    def __getitem__(self, k):
        return self.t[k]


class Prog:
    def __init__(self, nc, es):
        self.nc = nc
        self.es = es
        self.eng = [nc.tensor, nc.vector, nc.scalar, nc.gpsimd, nc.sync]
        self.cnt = [0] * 5
        self.epoch = [0] * 5
        self.esem = [[] for _ in range(5)]
        for e in range(4):
            self.esem[e].append(es.enter_context(nc.semaphore(f"e{e}_0")))
        self.known = [dict() for _ in range(5)]
        self.knownd = [dict() for _ in range(5)]
        self.dsem = [es.enter_context(nc.semaphore(f"d{i}")) for i in range(N_DSEM)]
        self.dval = [0] * N_DSEM
        self.dnext = 0
        self.n_inst = 0
        self.n_wait = 0
        self.psum_tiles = []
        self.psum_i = 0
        self.snap = {}

    def _learn(self, me, tok):
        sn = self.snap.get(tok)
        if sn is None:
            return
        km, kd = self.known[me], self.knownd[me]
        for e, v in sn[0].items():
            if km.get(e, (-1, 0)) < v:
                km[e] = v
        for s_, v in sn[1].items():
            if kd.get(s_, 0) < v:
                kd[s_] = v

    def wait_tok(self, me, tok):
        if tok is None:
            return
        if tok[0] == "e":
            _, e, ep, c = tok
            if e == me and me == PE:
                return
            k = self.known[me].get(e, (-1, 0))
            if k >= (ep, c):
                return
            self.eng[me].wait_ge(self.esem[e][ep], c)
            self.n_wait += 1
            self.known[me][e] = (ep, c)
            self._learn(me, tok)
        else:
            _, s, v = tok
            if self.knownd[me].get(s, 0) >= v:
                return
            self.eng[me].wait_ge(self.dsem[s], v)
            self.n_wait += 1
            self.knownd[me][s] = v
            self._learn(me, tok)

    def _deps(self, me, ins, outs):
        for r in ins:
            r = getattr(r, "res", r)
            self.wait_tok(me, r.w)
            if r.excl:
                for t in r.r.values():
                    self.wait_tok(me, t)
        for r in outs:
            r = getattr(r, "res", r)
            self.wait_tok(me, r.w)
            for t in r.r.values():
                self.wait_tok(me, t)

    def _mark(self, tok, key, ins, outs):
        for r in ins:
            r = getattr(r, "res", r)
            if r.excl:
                r.w = tok
                r.r = {}
            else:
                r.r[key] = tok
        for r in outs:
            r = getattr(r, "res", r)
            r.w = tok
            r.r = {}

    def op(self, me, fn, ins=(), outs=()):
        if getattr(self, "muted", False):
            return None
        self._deps(me, ins, outs)
        if self.cnt[me] >= EPOCH:
            self.epoch[me] += 1
            self.cnt[me] = 0
            self.esem[me].append(self.es.enter_context(self.nc.semaphore(f"e{me}_{self.epoch[me]}")))
        inst = fn()
        self.cnt[me] += 1
        inst.then_inc(self.esem[me][self.epoch[me]], 1)
        self.n_inst += 1
        tok = ("e", me, self.epoch[me], self.cnt[me])
        self.snap[tok] = (dict(self.known[me]), dict(self.knownd[me]))
        self._mark(tok, me, ins, outs)
        return tok

    def dma(self, me, fn, ins=(), outs=()):
        if getattr(self, "muted", False):
            return None
        self._deps(me, ins, outs)
        s = self.dnext
        self.dnext = (self.dnext + 1) % N_DSEM
        if self.dval[s] > 0:
            self.wait_tok(me, ("d", s, self.dval[s]))
        inst = fn()
        self.dval[s] += 16
        inst.then_inc(self.dsem[s], 16)
        self.n_inst += 1
        tok = ("d", s, self.dval[s])
        self.snap[tok] = (dict(self.known[me]), dict(self.knownd[me]))
        self._mark(tok, ("d", s), ins, outs)
        return tok

    def last_tok(self, e):
        if e == SP or (self.cnt[e] == 0 and self.epoch[e] == 0):
            return None
        return ("e", e, self.epoch[e], self.cnt[e])

    def barrier(self, engines=(PE, DVE, ACT, POOL, SP)):
        toks = [self.last_tok(e) for e in range(4)]
        for me in engines:
            for e in range(4):
                if e != me:
                    self.wait_tok(me, toks[e])
            for s in range(N_DSEM):
                if self.dval[s] > 0:
                    self.wait_tok(me, ("d", s, self.dval[s]))

    def sb(self, st, name, shape, dt):
        self.uid = getattr(self, "uid", 0) + 1
        return T(st.enter_context(self.nc.sbuf_tensor(f"s{self.uid}_{name}", list(shape), dt)), name)

    def init_psum(self, st, n=8):
        self.psum_tiles = [T(st.enter_context(self.nc.psum_tensor(f"psb{i}", [128, 512], F32)), f"psb{i}")
                           for i in range(n)]
        self.psum_i = 0

    def ps(self):
        t = self.psum_tiles[self.psum_i % len(self.psum_tiles)]
        self.psum_i += 1
        return t


D = 1024
NBC = 2
TX = 2048
TC = 256
TL = TC + TX
NE = 16
KAPPA = float(np.exp(-0.5))
DEPTH = 4

W_NAMES = ["ada_w", "ada_b", "norm_g", "fnet_wo", "fnet_bo", "rw_mix", "rw_wr", "rw_wk", "rw_wv", "rw_wo",
           "rw_w0", "rw_w1", "rw_w2", "rw_a0", "rw_a1", "rw_a2", "rw_v0", "rw_v1", "rw_v2", "rw_g1", "rw_g2",
           "rw_kk", "rw_ka", "rw_rk", "rw_lnx_w", "rw_lnx_b", "moe_router", "moe_wg", "moe_wu", "moe_wd", "final_g"]
W_SHAPES = {
    "ada_w": [4, 1024, 6144], "ada_b": [4, 6144], "norm_g": [4, 2, 1024], "fnet_wo": [2, 1024, 1024],
    "fnet_bo": [2, 1024], "rw_mix": [2, 6, 1024], "rw_wr": [2, 1024, 1024], "rw_wk": [2, 1024, 1024],
    "rw_wv": [2, 1024, 1024], "rw_wo": [2, 1024, 1024], "rw_w0": [2, 2, 1024], "rw_w1": [2, 2, 1024, 64],
    "rw_w2": [2, 2, 64, 1024], "rw_a0": [2, 2, 1024], "rw_a1": [2, 2, 1024, 64], "rw_a2": [2, 2, 64, 1024],
    "rw_v0": [1, 1024], "rw_v1": [1, 1024, 32], "rw_v2": [1, 32, 1024], "rw_g1": [2, 1024, 128],
    "rw_g2": [2, 128, 1024], "rw_kk": [2, 1024], "rw_ka": [2, 1024], "rw_rk": [2, 1024],
    "rw_lnx_w": [2, 1024], "rw_lnx_b": [2, 1024], "moe_router": [4, 1024, 16],
    "moe_wg": [4, 16, 1024, 1024], "moe_wu": [4, 16, 1024, 1024], "moe_wd": [4, 16, 1024, 1024], "final_g": [1, 1024],
}


def make_consts():
    bf = ml_dtypes.bfloat16
    c = {}
    c["ident_f"] = np.eye(128, dtype=np.float32)
    c["ident_b"] = np.eye(128, dtype=np.float32).astype(bf)
    cc = np.arange(256)
    ang = 2 * np.pi * np.outer(cc, cc) / 256.0
    cs = np.concatenate([np.cos(ang), np.sin(ang)], axis=1) / 16.0
    c["cs_tab"] = cs.reshape(2, 128, 512).transpose(1, 0, 2).astype(bf).copy()
    for T in (TX, TC):
        t = np.arange(T)
        a = 2 * np.pi * ((np.outer(t, t)) % T) / T
        tab = np.stack([np.cos(a), -np.sin(a)], axis=1) / np.sqrt(T)
        c[f"tok_tab{T}"] = tab.astype(bf)
    p = np.arange(128)
    blk = (p[:, None] // 64 == p[None, :] // 64)
    c["blockones_f"] = blk.astype(np.float32)
    c["blkmask"] = blk.astype(np.float32).astype(bf)
    row = p[:, None] % 64
    col = p[None, :] % 64
    mT = np.zeros((2, 128, 256), np.float32)
    mA = np.zeros((2, 128, 128), np.float32)
    mT[0, :, :128] = blk & (col > row)
    mT[0, :, 128:] = blk & (col >= row)
    mT[1, :, :128] = blk & (col < row)
    mT[1, :, 128:] = blk & (col <= row)
    mA[0] = blk & (col < row)
    mA[1] = blk & (col > row)
    c["mT"] = mT.transpose(1, 0, 2).copy()
    c["mA"] = mA.transpose(1, 0, 2).copy()
    seg = np.ones((128, 256), np.float32)
    seg[:, ::64] = 0
    c["segmask"] = seg
    sel = np.zeros((128, 64), np.float32)
    sel[p, p % 64] = 1.0
    c["sel"] = sel
    return c


PER_LAYER = ("ada_w", "moe_wg", "moe_wu", "moe_wd")
NP2DT = {np.dtype(np.float32): F32, np.dtype(ml_dtypes.bfloat16): BF16, np.dtype(np.int32): I32}


class PT:
    def __init__(self, hf, hb, off, n, res):
        self.hf, self.hb, self.off, self.n = hf, hb, off, n
        self.res = res

    def f(self, a=0, b=None, rows=128):
        b = self.n if b is None else b
        return self.hf[0:rows, self.off + a:self.off + b]

    def b(self, a=0, b=None, rows=128):
        b = 2 * self.n if b is None else b
        return self.hb[0:rows, 2 * self.off + a:2 * self.off + b]


class Multi:
    def __init__(self, aps):
        self.aps = aps

    def __getitem__(self, k):
        if isinstance(k, tuple):
            return self.aps[k[0]][k[1:]]
        return self.aps[k]


class Ctx:
    def __init__(self, nc, es, consts, debug):
        self.nc = nc
        self.es = es
        self.P = Prog(nc, es)
        self.debug = debug
        self.dram = {}
        self.wdecl = []
        self.consts_np = consts
        self.banks = []
        for i in range(8):
            h = es.enter_context(nc.psum_tensor(f"bank{i}", [128, 512], F32))
            self.banks.append((h, h.bitcast(BF16), Res(f"bank{i}", excl=True)))
        self.set_psum("full")
        self.uid = 0

    def set_psum(self, mode):
        self.ps_tiles = []
        for i, (hf, hb, res) in enumerate(self.banks):
            self.ps_tiles.append(PT(hf, hb, 0, 512, res))
        self.ps_i = 0

    def ps(self):
        t = self.ps_tiles[self.ps_i % len(self.ps_tiles)]
        self.ps_i += 1
        return t

    def din(self, name, shape, dt):
        self.dram[name] = self.nc.dram_tensor(name, list(shape), dt, kind="ExternalInput").ap()
        return self.dram[name]

    def w(self, name, layer=None):
        if layer is not None and name in PER_LAYER:
            key = f"{name}_{layer}"
            if key not in self.dram:
                self.din(key, W_SHAPES[name][1:], F32)
                self.wdecl.append((key, name, layer))
            return self.dram[key]
        if name not in self.dram:
            self.din(name, W_SHAPES[name], F32)
            self.wdecl.append((name, name, None))
        ap = self.dram[name]
        return ap if layer is None else ap[layer]

    def dout(self, name, shape, dt):
        self.dram[name] = self.nc.dram_tensor(name, list(shape), dt, kind="ExternalOutput").ap()
        return self.dram[name]

    def dscr(self, name, shape, dt):
        self.dram[name] = self.nc.dram_tensor(name, list(shape), dt).ap()
        return self.dram[name]

    def sb(self, st, name, shape, dt):
        return self.P.sb(st, name, shape, dt)

    def X(self, eng, name, ins, outs, **kw):
        e = self.P.eng[eng]
        return self.P.op(eng, lambda: getattr(e, name)(**kw), ins=ins, outs=outs)

    def mm(self, ps, out, lhsT, rhs, ins, start=True, stop=True):
        return self.P.op(PE, lambda: self.nc.tensor.matmul(out, lhsT=lhsT, rhs=rhs, start=start, stop=stop),
                         ins=ins, outs=[ps])

    def tr(self, ps, out, in_, ident, ins):
        return self.P.op(PE, lambda: self.nc.tensor.transpose(out, in_, ident), ins=ins, outs=[ps])

    def Dm(self, q, ins, outs, **kw):
        e = self.P.eng[q]
        return self.P.dma(q, lambda: e.dma_start(**kw), ins=ins, outs=outs)

    def DI(self, ins, outs, **kw):
        return self.P.dma(POOL, lambda: self.nc.gpsimd.indirect_dma_start(**kw), ins=ins, outs=outs)

    def act(self, ins, outs, **kw):
        return self.X(ACT, "activation", ins, outs, **kw)

    def barrier(self):
        self.P.barrier()


def load_consts(K, st):
    nc = K.nc
    C = {}
    for name in ["ident_f", "ident_b", "blockones_f", "blkmask", "mT", "mA", "segmask", "sel", "cs_tab"]:
        arr = K.consts_np[name]
        d = K.din("c_" + name, arr.shape, NP2DT[arr.dtype])
        t = K.sb(st, name, arr.shape, NP2DT[arr.dtype])
        K.Dm(SP, [], [t], out=t[:], in_=d)
        C[name] = t
    for T in (TX, TC):
        arr = K.consts_np[f"tok_tab{T}"]
        K.din(f"c_tok_tab{T}", arr.shape, BF16)
    eps = K.sb(st, "eps", [128, 4], F32)
    K.X(DVE, "memset", [], [eps], ap=eps[:, 0:1], constant=1e-6)
    K.X(DVE, "memset", [], [eps], ap=eps[:, 1:2], constant=1e-12)
    K.X(DVE, "memset", [], [eps], ap=eps[:, 2:3], constant=64e-5)
    K.X(DVE, "memset", [], [eps], ap=eps[:, 3:4], constant=0.0)
    C["eps"] = eps
    K.C = C


def phase_adaln(K, st_global, n_layers):
    nc, C = K.nc, K.C
    modA = K.sb(st_global, "modA", [128, DEPTH, 48, 3], F32)
    K.modA = modA
    modrow = K.dscr("modrow", [DEPTH, 3, 6144], F32)
    ada_b = K.w("ada_b")
    with contextlib.ExitStack() as st:
        crow = K.sb(st, "crow", [3, 1024], F32)
        K.Dm(SP, [], [crow], out=crow[0:2, :], in_=K.dram["c"])
        K.Dm(SP, [crow], [crow], out=crow[2:3, :], in_=K.dram["c_ctx"])
        srow = K.sb(st, "srow", [3, 1024], F32)
        K.act([crow], [srow], out=srow[:], in_=crow[:], func=AF.Silu)
        sT = K.sb(st, "sT", [128, 8, 3], BF16)
        for ch in range(8):
            ps = K.ps()
            K.tr(ps, ps.f(0, 3), srow[0:3, ch * 128:(ch + 1) * 128], C["ident_f"][0:3, 0:3], [srow, C["ident_f"]])
            K.X(DVE, "tensor_copy", [ps], [sT], out=sT[:, ch, :], in_=ps.f(0, 3))
        wt = [K.sb(st, f"adaw{i}", [128, 8, 512], BF16) for i in range(2)]
        brow = [K.sb(st, f"adab{i}", [3, 512], F32) for i in range(2)]
        rows = [K.sb(st, f"adar{i}", [3, 512], F32) for i in range(2)]
        it = 0
        for i in range(n_layers):
            for cb in range(12):
                w = wt[it % 2]
                bb = brow[it % 2]
                rr = rows[it % 2]
                it += 1
                K.Dm(POOL, [], [w], out=w[:], in_=K.w("ada_w", i)[:, cb * 512:(cb + 1) * 512].rearrange("(kc p) n -> p kc n", p=128))
                K.Dm(SP, [], [bb], out=bb[:], in_=ada_b[i:i + 1, cb * 512:(cb + 1) * 512].broadcast_to([3, 512]))
                ps = K.ps()
                for kc in range(8):
                    K.mm(ps, ps.f(0, 512, rows=3), sT[:, kc, :], w[:, kc, :], [sT, w], start=(kc == 0), stop=(kc == 7))
                K.X(DVE, "tensor_tensor", [ps, bb], [rr], out=rr[:], in0=ps.f(0, 512, rows=3), in1=bb[:], op=ALU.add)
                K.Dm(SP, [rr], [], out=modrow[i, :, cb * 512:(cb + 1) * 512], in_=rr[:])
                ps2 = K.ps()
                for oc in range(4):
                    K.tr(ps2, ps2.f(oc * 4, oc * 4 + 3), rr[0:3, oc * 128:(oc + 1) * 128], C["ident_f"][0:3, 0:3], [rr, C["ident_f"]])
                K.X(ACT, "copy", [ps2], [modA], out=modA[:, i, cb * 4:(cb + 1) * 4, :],
                    in_=ps2.f(0, 16).rearrange("p (a b) -> p a b", b=4)[:, :, 0:3])
    K.barrier()


def load_cols(K, st, name, row_aps):
    C = K.C
    n = len(row_aps)
    out = K.sb(st, name, [128, 8, n], F32)
    with contextlib.ExitStack() as s2:
        rows = K.sb(s2, name + "_rows", [n, 1024], F32)
        for j, ap in enumerate(row_aps):
            K.Dm(SP, [rows] if j else [], [rows], out=rows[j:j + 1, :], in_=ap)
        for ch in range(8):
            ps = K.ps()
            K.tr(ps, ps.f(0, n), rows[0:n, ch * 128:(ch + 1) * 128], C["ident_f"][0:n, 0:n], [rows, C["ident_f"]])
            K.X(DVE, "tensor_copy", [ps], [out], out=out[:, ch, :], in_=ps.f(0, n))
        K.barrier()
    return out


def mod_scalars(K, st, i, which_sc, which_sh, ng_cols, name, ng_idx=0):
    modA = K.modA
    scl = K.sb(st, name + "_scl", [128, 8, 3], F32)
    sh = K.sb(st, name + "_sh", [128, 8, 3], F32)
    K.X(DVE, "tensor_scalar", [modA], [scl], out=scl[:], in0=modA[:, i, which_sc * 8:(which_sc + 1) * 8, :],
        scalar1=1.0, scalar2=None, op0=ALU.add)
    K.X(DVE, "tensor_tensor", [scl, ng_cols], [scl], out=scl[:], in0=scl[:], in1=ng_cols[:, :, ng_idx:ng_idx + 1].broadcast_to([128, 8, 3]),
        op=ALU.mult)
    K.X(DVE, "tensor_copy", [modA], [sh], out=sh[:], in_=modA[:, i, which_sh * 8:(which_sh + 1) * 8, :])
    return scl, sh


def norm_hT(K, st, src, T, scl, sh, j, hT, tmp):
    nc, C = K.nc, K.C
    xin, xnb, stat = tmp
    nt = T // 128
    K.Dm(SP, [], [xin[0]], out=xin[0][:], in_=src[0:128, :])
    for tt in range(nt):
        xt = xin[tt % 2]
        if tt + 1 < nt:
            K.Dm(SP, [], [xin[(tt + 1) % 2]], out=xin[(tt + 1) % 2][:], in_=src[(tt + 1) * 128:(tt + 2) * 128, :])
        xb = xnb[tt % 2]
        s = stat[tt % 2]
        K.act([xt], [xb, s], out=xb[:], in_=xt[:], func=AF.Square, accum_out=s[:, 0:1])
        K.act([s, C["eps"]], [s], out=s[:, 1:2], in_=s[:, 0:1], func=AF.Sqrt, bias=C["eps"][:, 0:1], scale=1.0 / D)
        K.X(DVE, "reciprocal", [s], [s], out=s[:, 2:3], in_=s[:, 1:2])
        K.act([xt, s], [xb], out=xb[:], in_=xt[:], func=AF.Copy, scale=s[:, 2:3])
        for hf in range(2):
            ps = K.ps()
            for c4 in range(4):
                ch = hf * 4 + c4
                K.tr(ps, ps.b(c4 * 128, (c4 + 1) * 128), xb[:, ch * 128:(ch + 1) * 128], C["ident_b"][:], [xb, C["ident_b"]])
            for c4 in range(4):
                ch = hf * 4 + c4
                if hf == 0:
                    K.X(DVE, "tensor_scalar", [ps, scl, sh], [hT], out=hT[:, ch, tt * 128:(tt + 1) * 128],
                        in0=ps.b(c4 * 128, (c4 + 1) * 128), scalar1=scl[:, ch, j:j + 1], scalar2=sh[:, ch, j:j + 1],
                        op0=ALU.mult, op1=ALU.add)
                else:
                    K.act([ps, scl, sh], [hT], out=hT[:, ch, tt * 128:(tt + 1) * 128], in_=ps.b(c4 * 128, (c4 + 1) * 128),
                          func=AF.Identity, scale=scl[:, ch, j:j + 1], bias=sh[:, ch, j:j + 1])


def norm_tmp(K, st):
    xin = [K.sb(st, f"nx{i}", [128, 1024], F32) for i in range(2)]
    xnb = [K.sb(st, f"nb{i}", [128, 1024], BF16) for i in range(2)]
    stat = [K.sb(st, f"ns{i}", [128, 4], F32) for i in range(2)]
    return xin, xnb, stat


def seq_list(K, need_ctx):
    l = [("x", b, TX, K.dram["xs"][b], b) for b in range(NBC)]
    if need_ctx:
        l += [("c", b, TC, K.dram["cs"][b], 2) for b in range(NBC)]
    return l


def bcast_row(K, t, row_ap):
    n = row_ap.shape[-1]
    K.Dm(SP, [], [t], out=t[:], in_=row_ap.broadcast_to([128, n]))


def proj_residual(K, st, fT, T, wo, res_dram, g1bc, gb, tmp):
    xh, t1, t2 = tmp
    it = 0
    for tt in range(T // 128):
        for dh in range(2):
            k = it % 2
            it += 1
            sl = slice(dh * 512, (dh + 1) * 512)
            K.Dm(SP, [], [xh[k]], out=xh[k][:], in_=res_dram[tt * 128:(tt + 1) * 128, sl])
            ps = K.ps()
            for fc in range(8):
                K.mm(ps, ps.f(), fT[:, fc, tt * 128:(tt + 1) * 128], wo[:, fc, sl], [fT, wo], start=(fc == 0), stop=(fc == 7))
            K.X(DVE, "tensor_tensor", [ps, g1bc], [t1[k]], out=t1[k][:], in0=ps.f(), in1=g1bc[:, sl], op=ALU.mult)
            if gb is not None:
                K.X(POOL, "tensor_tensor", [xh[k], gb], [xh[k]], out=xh[k][:], in0=xh[k][:], in1=gb[:, sl], op=ALU.add)
            K.X(POOL, "tensor_tensor", [xh[k], t1[k]], [t2[k]], out=t2[k][:], in0=xh[k][:], in1=t1[k][:], op=ALU.add)
            K.Dm(ACT, [t2[k]], [], out=res_dram[tt * 128:(tt + 1) * 128, sl], in_=t2[k][:])


def proj_tmp(K, st):
    return ([K.sb(st, f"pxh{i}", [128, 512], F32) for i in range(2)],
            [K.sb(st, f"pt1{i}", [128, 512], F32) for i in range(2)],
            [K.sb(st, f"pt2{i}", [128, 512], F32) for i in range(2)])


def phase_fnet(K, i, fi, need_ctx):
    nc, C = K.nc, K.C
    with contextlib.ExitStack() as st:
        ng = load_cols(K, st, "ng", [K.w("norm_g")[i, 0:1, :]])
        scl, sh = mod_scalars(K, st, i, 1, 0, ng, "f")
        wo = K.sb(st, "fwo", [128, 8, 1024], BF16)
        K.Dm(POOL, [], [wo], out=wo[:], in_=K.w("fnet_wo")[fi].rearrange("(kc p) n -> p kc n", p=128))
        bobc = K.sb(st, "bobc", [128, 1024], F32)
        bcast_row(K, bobc, K.w("fnet_bo")[fi:fi + 1, :])
        hT = K.sb(st, "hT", [128, 8, TX], BF16)
        XCS = K.sb(st, "XCS", [128, 16, 2, 512], BF16)
        tabs = [K.sb(st, f"tab{k}", [128, 16, 2, 256], BF16) for k in range(2)]
        g1bc = K.sb(st, "g1bc", [128, 1024], F32)
        gb = K.sb(st, "gb", [128, 1024], F32)
        ntmp = norm_tmp(K, st)
        ptmp = proj_tmp(K, st)
        tab_it = 0
        for (kind, b, T, res, j) in seq_list(K, need_ctx):
            nt = T // 128
            tokt = K.dram[f"c_tok_tab{T}"]
            bcast_row(K, g1bc, K.dram["modrow"][i, j:j + 1, 2 * 1024:3 * 1024])
            K.X(POOL, "tensor_tensor", [g1bc, bobc], [gb], out=gb[:], in0=g1bc[:], in1=bobc[:], op=ALU.mult)
            norm_hT(K, st, res, T, scl, sh, j, hT, ntmp)
            fT = hT
            for half in range(2):
                for tt in range(nt):
                    for gg in range(2):
                        g = 2 * half + gg
                        ps = K.ps()
                        for ci in range(2):
                            K.mm(ps, ps.f(), hT[:, 2 * g + ci, tt * 128:(tt + 1) * 128], C["cs_tab"][:, ci, :],
                                 [hT, C["cs_tab"]], start=(ci == 0), stop=(ci == 1))
                        eng = DVE if (tt + gg) % 2 == 0 else ACT
                        if eng == DVE:
                            K.X(DVE, "tensor_copy", [ps], [XCS], out=XCS[:, tt, :, gg * 256:(gg + 1) * 256],
                                in_=ps.f().rearrange("p (a c) -> p a c", a=2))
                        else:
                            K.X(ACT, "copy", [ps], [XCS], out=XCS[:, tt, :, gg * 256:(gg + 1) * 256],
                                in_=ps.f().rearrange("p (a c) -> p a c", a=2))
                NBK = 256
                for tb in range(T // NBK):
                    tab = tabs[tab_it % 2]
                    tab_it += 1
                    for a in range(2):
                        K.Dm(SP, [tab] if a else [], [tab], out=tab[:, 0:nt, a, :],
                             in_=tokt[:, a, tb * NBK:(tb + 1) * NBK].rearrange("(tt p) n -> p tt n", p=128))
                    for fq in range(4):
                        fc = 4 * half + fq
                        ps = K.ps()
                        for tt in range(nt):
                            for a in range(2):
                                K.mm(ps, ps.f(0, NBK), XCS[:, tt, a, fq * 128:(fq + 1) * 128], tab[:, tt, a, :],
                                     [XCS, tab], start=(tt == 0 and a == 0), stop=(tt == nt - 1 and a == 1))
                        if fq % 2 == 0:
                            K.X(DVE, "tensor_copy", [ps], [fT], out=fT[:, fc, tb * NBK:(tb + 1) * NBK], in_=ps.f(0, NBK))
                        else:
                            K.X(ACT, "copy", [ps], [fT], out=fT[:, fc, tb * NBK:(tb + 1) * NBK], in_=ps.f(0, NBK))
            proj_residual(K, st, fT, T, wo, res, g1bc, gb, ptmp)
        K.barrier()


def phase_final(K):
    nc, C = K.nc, K.C
    with contextlib.ExitStack() as st:
        fg = K.sb(st, "fgbc", [128, 1024], F32)
        bcast_row(K, fg, K.w("final_g")[0:1, :])
        xin = [K.sb(st, f"fx{i}", [128, 1024], F32) for i in range(2)]
        xo = [K.sb(st, f"fo{i}", [128, 1024], F32) for i in range(2)]
        stat = [K.sb(st, f"fs{i}", [128, 4], F32) for i in range(2)]
        it = 0
        for b in range(NBC):
            for tt in range(TX // 128):
                k = it % 2
                it += 1
                K.Dm(SP, [], [xin[k]], out=xin[k][:], in_=K.dram["xs"][b, tt * 128:(tt + 1) * 128, :])
                s = stat[k]
                K.act([xin[k]], [xo[k], s], out=xo[k][:], in_=xin[k][:], func=AF.Square, accum_out=s[:, 0:1])
                K.act([s, C["eps"]], [s], out=s[:, 1:2], in_=s[:, 0:1], func=AF.Sqrt, bias=C["eps"][:, 0:1], scale=1.0 / D)
                K.X(DVE, "reciprocal", [s], [s], out=s[:, 2:3], in_=s[:, 1:2])
                K.X(DVE, "scalar_tensor_tensor", [xin[k], s, fg], [xo[k]], out=xo[k][:], in0=xin[k][:], scalar=s[:, 2:3],
                    in1=fg[:], op0=ALU.mult, op1=ALU.mult)
                K.Dm(ACT, [xo[k]], [], out=K.dram["out"][b, tt * 128:(tt + 1) * 128, :], in_=xo[k][:])
        K.barrier()


def build(n_layers=DEPTH, debug=False, phases=("fnet", "rwkv", "moe"), moe_route_only=False, moe_no_ctx=False, rw_stop=99, prep_stop=-1, prep_cnt=1):
    nc = bass.Bass("TRN2", target_bir_lowering=False)
    consts = make_consts()
    es = contextlib.ExitStack()
    dbg_names = []
    with es:
        K = Ctx(nc, es, consts, debug)
        K.moe_route_only = moe_route_only
        K.moe_no_ctx = moe_no_ctx
        K.rw_stop = rw_stop
        K.prep_stop = prep_stop
        K.prep_cnt = prep_cnt
        K.din("x", [NBC, TX, D], F32)
        K.din("c", [NBC, D], F32)
        K.din("ctx", [NBC, TC, D], F32)
        K.din("c_ctx", [1, D], F32)
        K.dout("out", [NBC, TX, D], F32)
        K.dram["xs"] = Multi([K.dscr(f"xs{b}", [TX, D], F32) for b in range(NBC)])
        K.dram["cs"] = Multi([K.dscr(f"cs{b}", [TC, D], F32) for b in range(NBC)])

        def dbg(name):
            if not debug:
                return
            ox = K.dout("dbg_x_" + name, [NBC, TX, D], F32)
            oc = K.dout("dbg_c_" + name, [NBC, TC, D], F32)
            for b in range(NBC):
                K.Dm(SP, [], [], out=ox[b], in_=K.dram["xs"][b])
                K.Dm(SP, [], [], out=oc[b], in_=K.dram["cs"][b])
            dbg_names.append(name)
            K.barrier()

        load_consts(K, es)
        phase_adaln(K, es, n_layers)
        if debug:
            om = K.dout("dbg_modrow", [DEPTH, 3, 6144], F32)
            K.Dm(SP, [], [], out=om, in_=K.dram["modrow"])
        for b in range(NBC):
            K.Dm(SP, [], [], out=K.dram["xs"][b], in_=K.dram["x"][b])
            K.Dm(SP, [], [], out=K.dram["cs"][b], in_=K.dram["ctx"][b])
        K.barrier()
        try:
            for i in range(n_layers):
                need_ctx = i < DEPTH - 1
                if i % 2 == 0:
                    if "fnet" in phases:
                        phase_fnet(K, i, i // 2, need_ctx)
                else:
                    if "rwkv" in phases:
                        phase_rwkv(K, i, i // 2, need_ctx)
                dbg(f"mix{i}")
                if "moe" in phases:
                    phase_moe(K, i, need_ctx)
                dbg(f"moe{i}")
        except _Stop:
            pass
        K.P.muted = False
        K.barrier()
        phase_final(K)
        print(f"[build] instructions={K.P.n_inst} waits={K.P.n_wait}", flush=True)
    return nc, consts, dbg_names, K.wdecl


def core_inputs(inputs, consts, core, wdecl):
    b0 = core * NBC
    m = {
        "x": np.ascontiguousarray(inputs["x"][b0:b0 + NBC], dtype=np.float32),
        "c": np.ascontiguousarray(inputs["c"][b0:b0 + NBC], dtype=np.float32),
        "ctx": np.ascontiguousarray(inputs["ctx"][b0:b0 + NBC], dtype=np.float32),
        "c_ctx": np.ascontiguousarray(np.asarray(inputs["c_ctx"], dtype=np.float32).reshape(1, D)),
    }
    for key, n, layer in wdecl:
        a = np.asarray(inputs[n], dtype=np.float32).reshape(W_SHAPES[n])
        m[key] = np.ascontiguousarray(a if layer is None else a[layer])
    for k, v in consts.items():
        m["c_" + k] = v
    return m


def kernel(**inputs):
    nc, consts, _, wdecl = build()
    n_cores = 8
    in_maps = [core_inputs(inputs, consts, c, wdecl) for c in range(n_cores)]
    res = run_bass_kernel_spmd(nc, in_maps, core_ids=list(range(n_cores)))
    return np.concatenate([np.asarray(r["out"]) for r in res.results], axis=0).astype(np.float32)


def IOA(ap):
    return bass.IndirectOffsetOnAxis(ap=ap, axis=0)


def topk_rows(K, aff, work, vals, idxs, rounds):
    cur = aff
    for r in range(rounds):
        sl = slice(r * 8, (r + 1) * 8)
        K.X(DVE, "max", [cur], [vals], out=vals[:, sl], in_=cur[:])
        K.X(DVE, "max_index", [vals, cur], [idxs], out=idxs[:, sl], in_max=vals[:, sl], in_values=cur[:])
        if r + 1 < rounds:
            K.X(DVE, "match_replace", [vals, cur], [work], out=work[:], in_to_replace=vals[:, sl], in_values=cur[:],
                imm_value=-1.0)
            cur = work


def phase_moe(K, i, need_ctx):
    nc, C = K.nc, K.C
    if getattr(K, "moe_no_ctx", False):
        need_ctx = False
    if "hsrc_x" not in K.dram:
        K.dram["hsrc_x"] = Multi([K.dscr(f"hsrc_x{b}", [TX, D], BF16) for b in range(NBC)])
        K.dram["hsrc_c"] = Multi([K.dscr(f"hsrc_c{b}", [TC, D], BF16) for b in range(NBC)])
    hsrc = {"x": K.dram["hsrc_x"], "c": K.dram["hsrc_c"]}
    resid = {"x": K.dram["xs"], "c": K.dram["cs"]}
    kinds = [("x", TX, 256)] + ([("c", TC, 32)] if need_ctx else [])
    modrow = K.dram["modrow"]
    with contextlib.ExitStack() as st0:
        idxT = K.sb(st0, "idxT", [128, 2, 48], I32)
        gateT = K.sb(st0, "gateT", [128, 2, 48], F32)
        idxTc = K.sb(st0, "idxTc", [32, 48], I32)
        gcT = K.sb(st0, "gcT", [32, 48], F32)
        g2bc = [K.sb(st0, f"g2bc{j}", [128, 1024], F32) for j in range(3)]
        for j in range(3):
            bcast_row(K, g2bc[j], modrow[i, j:j + 1, 5 * 1024:6 * 1024])
        with contextlib.ExitStack() as st:
            ngbc = K.sb(st, "ngbc", [128, 1024], F32)
            bcast_row(K, ngbc, K.w("norm_g")[i, 1:2, :])
            sclbc, shbc = [], []
            for j in range(3):
                a = K.sb(st, f"scl2bc{j}", [128, 1024], F32)
                b_ = K.sb(st, f"sh2bc{j}", [128, 1024], F32)
                bcast_row(K, a, modrow[i, j:j + 1, 4 * 1024:5 * 1024])
                bcast_row(K, b_, modrow[i, j:j + 1, 3 * 1024:4 * 1024])
                K.X(DVE, "scalar_tensor_tensor", [a, ngbc], [a], out=a[:], in0=a[:], scalar=1.0, in1=ngbc[:],
                    op0=ALU.add, op1=ALU.mult)
                sclbc.append(a)
                shbc.append(b_)
            router = K.sb(st, "router", [128, 8, 16], F32)
            K.Dm(SP, [], [router], out=router[:], in_=K.w("moe_router")[i].rearrange("(kc p) n -> p kc n", p=128))
            xin = [K.sb(st, f"mx{k}", [128, 1024], F32) for k in range(2)]
            hf = [K.sb(st, f"mh{k}", [128, 1024], F32) for k in range(2)]
            hb = [K.sb(st, f"mhb{k}", [128, 1024], BF16) for k in range(2)]
            hTt = [K.sb(st, f"mhT{k}", [128, 8, 128], F32) for k in range(2)]
            stat = [K.sb(st, f"ms{k}", [128, 8], F32) for k in range(2)]
            ex = [K.sb(st, f"mex{k}", [128, 16], F32) for k in range(2)]
            affp = [K.sb(st, f"affp{k}", [128, 48], F32) for k in range(2)]
            for k in range(2):
                K.X(DVE, "memset", [], [affp[k]], ap=affp[k][:], constant=0.0)
            it = 0
            for (kind, T, cap) in kinds:
                nt = T // 128
                affT = K.sb(st, f"affT{kind}", [48, T], F32)
                work = K.sb(st, f"work{kind}", [48, T], F32)
                vals = K.sb(st, f"vals{kind}", [48, max(cap, 64)], F32)
                idxs = K.sb(st, f"idxs{kind}", [48, max(cap, 64)], U32)
                idxf = K.sb(st, f"idxf{kind}", [48, max(cap, 64)], F32)
                for tt in range(nt):
                    ap_ = affp[tt % 2]
                    for si in range(NBC):
                        j = si if kind == "x" else 2
                        k = it % 2
                        it += 1
                        xt, h, s = xin[k], hf[k], stat[k]
                        K.Dm(SP, [], [xt], out=xt[:], in_=resid[kind][si, tt * 128:(tt + 1) * 128, :])
                        K.act([xt], [hb[k], s], out=hb[k][:], in_=xt[:], func=AF.Square, accum_out=s[:, 0:1])
                        K.act([s, C["eps"]], [s], out=s[:, 1:2], in_=s[:, 0:1], func=AF.Sqrt, bias=C["eps"][:, 0:1], scale=1.0 / D)
                        K.X(DVE, "reciprocal", [s], [s], out=s[:, 2:3], in_=s[:, 1:2])
                        K.X(DVE, "scalar_tensor_tensor", [xt, s, sclbc[j]], [h], out=h[:], in0=xt[:], scalar=s[:, 2:3],
                            in1=sclbc[j][:], op0=ALU.mult, op1=ALU.mult)
                        K.X(POOL, "tensor_tensor", [h, shbc[j]], [h], out=h[:], in0=h[:], in1=shbc[j][:], op=ALU.add)
                        K.X(ACT, "copy", [h], [hb[k]], out=hb[k][:], in_=h[:])
                        K.Dm(ACT, [hb[k]], [], out=hsrc[kind][si, tt * 128:(tt + 1) * 128, :], in_=hb[k][:])
                        for hh in range(2):
                            ps = K.ps()
                            for c4 in range(4):
                                ch = hh * 4 + c4
                                K.tr(ps, ps.f(c4 * 128, (c4 + 1) * 128), h[:, ch * 128:(ch + 1) * 128], C["ident_f"][:],
                                     [h, C["ident_f"]])
                            if hh == 0:
                                K.X(DVE, "tensor_copy", [ps], [hTt[k]], out=hTt[k][:, 0:4, :],
                                    in_=ps.f().rearrange("p (a c) -> p a c", a=4))
                            else:
                                K.X(ACT, "copy", [ps], [hTt[k]], out=hTt[k][:, 4:8, :],
                                    in_=ps.f().rearrange("p (a c) -> p a c", a=4))
                        ps = K.ps()
                        for kc in range(8):
                            K.mm(ps, ps.f(0, 16), hTt[k][:, kc, :], router[:, kc, :], [hTt[k], router],
                                 start=(kc == 0), stop=(kc == 7))
                        K.X(DVE, "tensor_reduce", [ps], [s], out=s[:, 3:4], in_=ps.f(0, 16), axis=AX.X, op=ALU.max)
                        K.X(DVE, "tensor_scalar", [s], [s], out=s[:, 4:5], in0=s[:, 3:4], scalar1=-1.0, scalar2=None, op0=ALU.mult)
                        K.act([ps, s], [ex[k], s], out=ex[k][:], in_=ps.f(0, 16), func=AF.Exp, bias=s[:, 4:5], scale=1.0,
                              accum_out=s[:, 5:6])
                        K.X(DVE, "reciprocal", [s], [s], out=s[:, 6:7], in_=s[:, 5:6])
                        K.X(DVE, "tensor_scalar", [ex[k], s], [ap_], out=ap_[:, si * 32:si * 32 + 16], in0=ex[k][:],
                            scalar1=s[:, 6:7], scalar2=None, op0=ALU.mult)
                    ps = K.ps()
                    K.tr(ps, ps.f(0, 128, rows=48), ap_[:, 0:48], C["ident_f"][:], [ap_, C["ident_f"]])
                    K.X(ACT, "copy", [ps], [affT], out=affT[:, tt * 128:(tt + 1) * 128], in_=ps.f(0, 128, rows=48))
                topk_rows(K, affT, work, vals, idxs, cap // 8)
                if kind == "x":
                    K.X(DVE, "tensor_copy", [idxs], [idxf], out=idxf[:, 0:256], in_=idxs[:, 0:256])
                    for ch in range(2):
                        ps = K.ps()
                        K.tr(ps, ps.f(0, 48), idxf[0:48, ch * 128:(ch + 1) * 128], C["ident_f"][0:48, 0:48], [idxf, C["ident_f"]])
                        K.tr(ps, ps.f(64, 112), vals[0:48, ch * 128:(ch + 1) * 128], C["ident_f"][0:48, 0:48], [vals, C["ident_f"]])
                        K.X(DVE, "tensor_copy", [ps], [idxT], out=idxT[:, ch, :], in_=ps.f(0, 48))
                        K.X(DVE, "tensor_copy", [ps], [gateT], out=gateT[:, ch, :], in_=ps.f(64, 112))
                else:
                    K.X(DVE, "tensor_copy", [idxs], [idxf], out=idxf[:, 0:32], in_=idxs[:, 0:32])
                    ps = K.ps()
                    K.tr(ps, ps.f(0, 48, rows=32), idxf[0:48, 0:32], C["ident_f"][0:48, 0:48], [idxf, C["ident_f"]])
                    K.tr(ps, ps.f(64, 112, rows=32), vals[0:48, 0:32], C["ident_f"][0:48, 0:48], [vals, C["ident_f"]])
                    K.X(DVE, "tensor_copy", [ps], [idxTc], out=idxTc[:], in_=ps.f(0, 48, rows=32))
                    K.X(DVE, "tensor_copy", [ps], [gcT], out=gcT[:], in_=ps.f(64, 112, rows=32))
            K.barrier()
            if K.debug and i == 0:
                for nm, t, shp, dt in [("idxT", idxT, [128, 2, 48], I32), ("gateT", gateT, [128, 2, 48], F32),
                                       ("idxTc", idxTc, [32, 48], I32), ("gcT", gcT, [32, 48], F32)]:
                    o = K.dout("dbg_" + nm, shp, dt)
                    K.Dm(SP, [t], [], out=o, in_=t[:])
                o = K.dout("dbg_hsrc_x", [NBC, TX, D], BF16)
                for b in range(NBC):
                    K.Dm(SP, [], [], out=o[b], in_=hsrc["x"][b])
                K.barrier()
        if getattr(K, "moe_route_only", False):
            return
        with contextlib.ExitStack() as st:
            wbuf = [[K.sb(st, f"mw{k}{m}", [128, 8, 1024], BF16) for m in range(3)] for k in range(2)]
            xg = [[K.sb(st, f"xg{k}{m}", [128, 1024], BF16) for m in range(4)] for k in range(2)]
            xgc = [[K.sb(st, f"xgc{k}{b}", [32, 1024], BF16) for b in range(NBC)] for k in range(2)]
            xsT = [K.sb(st, f"xsT{k}", [128, 8, 576], BF16) for k in range(2)]
            hidT = K.sb(st, "hidT", [128, 8, 576], BF16)
            sg = [K.sb(st, f"sg{k}", [128, 576], F32) for k in range(2)]
            yo = [K.sb(st, f"yo{k}", [128, 1024], F32) for k in range(3)]
            wnames = ["moe_wg", "moe_wu", "moe_wd"]
            res_x = [Res(f"xs{b}") for b in range(NBC)]
            res_c = [Res(f"cs{b}") for b in range(NBC)]
            NCX = 576 if need_ctx else 512
            yo_it = 0
            sg_it = 0

            def load_w(e):
                k = e % 2
                for m in range(3):
                    K.Dm(POOL, [], [wbuf[k][m]], out=wbuf[k][m][:],
                         in_=K.w(wnames[m], i)[e].rearrange("(kc p) n -> p kc n", p=128))

            def gather(e):
                k = e % 2
                for si in range(NBC):
                    for ch in range(2):
                        col = si * 32 + e
                        K.DI([idxT], [xg[k][si * 2 + ch]], out=xg[k][si * 2 + ch][:], out_offset=None,
                             in_=hsrc["x"][si], in_offset=IOA(idxT[:, ch, col:col + 1]))
                if need_ctx:
                    for b in range(NBC):
                        K.DI([idxTc], [xgc[k][b]], out=xgc[k][b][:], out_offset=None, in_=hsrc["c"][b],
                             in_offset=IOA(idxTc[:, b * 32 + e:b * 32 + e + 1]))

            load_w(0)
            gather(0)
            for e in range(NE):
                k = e % 2
                if e + 1 < NE:
                    load_w(e + 1)
                    gather(e + 1)
                wg, wu, wd = wbuf[k]
                for f in range(8):
                    ps = K.ps()
                    for t4 in range(4):
                        K.tr(ps, ps.b(t4 * 128, (t4 + 1) * 128), xg[k][t4][:, f * 128:(f + 1) * 128], C["ident_b"][:],
                             [xg[k][t4], C["ident_b"]])
                    if need_ctx:
                        for b in range(NBC):
                            K.tr(ps, ps.b(512 + b * 32, 544 + b * 32), xgc[k][b][:, f * 128:(f + 1) * 128], C["ident_b"][0:32, 0:32],
                                 [xgc[k][b], C["ident_b"]])
                    if f % 2 == 0:
                        K.X(DVE, "tensor_copy", [ps], [xsT[k]], out=xsT[k][:, f, 0:NCX], in_=ps.b(0, NCX))
                    else:
                        K.X(ACT, "copy", [ps], [xsT[k]], out=xsT[k][:, f, 0:NCX], in_=ps.b(0, NCX))
                for fc in range(8):
                    fs = slice(fc * 128, (fc + 1) * 128)
                    psG = K.ps()
                    for f in range(8):
                        K.mm(psG, psG.f(), wg[:, f, fs], xsT[k][:, f, 0:512], [wg, xsT[k]], start=(f == 0), stop=(f == 7))
                    psU = K.ps()
                    for f in range(8):
                        K.mm(psU, psU.f(), wu[:, f, fs], xsT[k][:, f, 0:512], [wu, xsT[k]], start=(f == 0), stop=(f == 7))
                    s_ = sg[sg_it % 2]
                    sg_it += 1
                    K.act([psG], [s_], out=s_[:, 0:512], in_=psG.f(), func=AF.Silu)
                    K.X(DVE, "tensor_tensor", [psU, s_], [hidT], out=hidT[:, fc, 0:512], in0=psU.f(), in1=s_[:, 0:512], op=ALU.mult)
                    if need_ctx:
                        psC = K.ps()
                        for f in range(8):
                            K.mm(psC, psC.f(0, 64), wg[:, f, fs], xsT[k][:, f, 512:576], [wg, xsT[k]], start=(f == 0), stop=(f == 7))
                        for f in range(8):
                            K.mm(psC, psC.f(64, 128), wu[:, f, fs], xsT[k][:, f, 512:576], [wu, xsT[k]], start=(f == 0), stop=(f == 7))
                        K.act([psC], [s_], out=s_[:, 512:576], in_=psC.f(0, 64), func=AF.Silu)
                        K.X(DVE, "tensor_tensor", [psC, s_], [hidT], out=hidT[:, fc, 512:576], in0=psC.f(64, 128), in1=s_[:, 512:576], op=ALU.mult)
                for t4 in range(4):
                    si, ch = t4 // 2, t4 % 2
                    col = si * 32 + e
                    y = yo[yo_it % 3]
                    yo_it += 1
                    for dh in range(2):
                        sl = slice(dh * 512, (dh + 1) * 512)
                        ps = K.ps()
                        for fc in range(8):
                            K.mm(ps, ps.f(), hidT[:, fc, t4 * 128:(t4 + 1) * 128], wd[:, fc, sl], [hidT, wd],
                                 start=(fc == 0), stop=(fc == 7))
                        K.X(DVE, "scalar_tensor_tensor", [ps, gateT, g2bc[si]], [y], out=y[:, sl], in0=ps.f(),
                            scalar=gateT[:, ch, col:col + 1], in1=g2bc[si][:, sl], op0=ALU.mult, op1=ALU.mult)
                    K.DI([y, idxT], [res_x[si]], out=resid["x"][si], out_offset=IOA(idxT[:, ch, col:col + 1]),
                         in_=y[:], in_offset=None, compute_op=ALU.add)
                if need_ctx:
                    for b in range(NBC):
                        y = yo[yo_it % 3]
                        yo_it += 1
                        col = b * 32 + e
                        for dh in range(2):
                            sl = slice(dh * 512, (dh + 1) * 512)
                            ps = K.ps()
                            for fc in range(8):
                                K.mm(ps, ps.f(0, 512, rows=32), hidT[:, fc, 512 + b * 32:544 + b * 32], wd[:, fc, sl], [hidT, wd],
                                     start=(fc == 0), stop=(fc == 7))
                            K.X(DVE, "scalar_tensor_tensor", [ps, gcT, g2bc[2]], [y], out=y[0:32, sl], in0=ps.f(0, 512, rows=32),
                                scalar=gcT[:, col:col + 1], in1=g2bc[2][0:32, sl], op0=ALU.mult, op1=ALU.mult)
                        K.DI([y, idxTc], [res_c[b]], out=resid["c"][b], out_offset=IOA(idxTc[:, col:col + 1]),
                             in_=y[0:32, :], in_offset=None, compute_op=ALU.add)
            K.barrier()


NCH = TL // 64
CMIX, CW0, CA0, CKK, CKA, CRK, CV0, CNG, CLW, CLB = 0, 6, 8, 10, 11, 12, 13, 14, 15, 16


def rw_scratch(K):
    if hasattr(K, "rw"):
        return K.rw
    S = {}
    for d in range(2):
        for n in ("at", "bt", "kt", "rt"):
            S[f"{n}{d}"] = K.dscr(f"rw_{n}{d}", [NBC, D, TL], BF16)
        S[f"GL{d}"] = K.dscr(f"rw_GL{d}", [NBC, D, NCH], F32)
        S[f"y{d}"] = K.dscr(f"rw_y{d}", [NBC, D, TL], F32)
    S["v"] = K.dscr("rw_v", [NBC, D, TL], BF16)
    S["bv"] = K.dscr("rw_bv", [NBC, D, TL], F32)
    S["g"] = K.dscr("rw_g", [NBC, D, TL], F32)
    S["vfirst"] = K.dscr("rw_vfirst", [NBC, D, TL], F32)
    K.rw = S
    return S


def rwkv_cols(K, st, i, ri):
    w = K.w
    rows = [w("rw_mix")[ri, jj:jj + 1, :] for jj in range(6)]
    rows += [w("rw_w0")[ri, 0:1, :], w("rw_w0")[ri, 1:2, :], w("rw_a0")[ri, 0:1, :], w("rw_a0")[ri, 1:2, :]]
    rows += [w("rw_kk")[ri:ri + 1, :], w("rw_ka")[ri:ri + 1, :], w("rw_rk")[ri:ri + 1, :]]
    rows += [w("rw_v0")[0:1, :]]
    rows += [w("norm_g")[i, 0:1, :], w("rw_lnx_w")[ri:ri + 1, :], w("rw_lnx_b")[ri:ri + 1, :]]
    return load_cols(K, st, "rwc", rows)


class _Stop(Exception):
    pass


def rwkv_prep(K, i, ri):
    nc, C = K.nc, K.C

    def chk(k):
        if getattr(K, "prep_stop", -1) == k:
            K.prep_seen = getattr(K, "prep_seen", 0) + 1
            if K.prep_seen >= K.prep_cnt:
                K.P.muted = True
    S = rw_scratch(K)
    with contextlib.ExitStack() as st:
        cols = rwkv_cols(K, st, i, ri)
        omka = K.sb(st, "omka", [128, 8, 1], F32)
        K.X(DVE, "tensor_scalar", [cols], [omka], out=omka[:], in0=cols[:, :, CKA:CKA + 1], scalar1=-1.0, scalar2=1.0,
            op0=ALU.mult, op1=ALU.add)
        scl, sh = mod_scalars(K, st, i, 1, 0, cols, "r", ng_idx=CNG)
        wr, wk, wv = [K.sb(st, n, [128, 8, 1024], BF16) for n in ("wr", "wk", "wv")]
        for t, n in ((wr, "rw_wr"), (wk, "rw_wk"), (wv, "rw_wv")):
            K.Dm(POOL, [], [t], out=t[:], in_=K.w(n)[ri].rearrange("(kc p) n -> p kc n", p=128))
        w1c = K.sb(st, "w1c", [128, 8, 128], BF16)
        a1c = K.sb(st, "a1c", [128, 8, 128], BF16)
        g1w = K.sb(st, "g1w", [128, 8, 128], BF16)
        for d in range(2):
            K.Dm(POOL, [w1c] if d else [], [w1c], out=w1c[:, :, d * 64:(d + 1) * 64],
                 in_=K.w("rw_w1")[ri, d].rearrange("(kc p) n -> p kc n", p=128))
            K.Dm(POOL, [a1c] if d else [], [a1c], out=a1c[:, :, d * 64:(d + 1) * 64],
                 in_=K.w("rw_a1")[ri, d].rearrange("(kc p) n -> p kc n", p=128))
        K.Dm(POOL, [], [g1w], out=g1w[:], in_=K.w("rw_g1")[ri].rearrange("(kc p) n -> p kc n", p=128))
        w2a = K.sb(st, "w2a", [128, 1024], BF16)
        a2a = K.sb(st, "a2a", [128, 1024], BF16)
        g2w = K.sb(st, "g2w", [128, 1024], BF16)
        for d in range(2):
            K.Dm(POOL, [w2a] if d else [], [w2a], out=w2a[d * 64:(d + 1) * 64, :], in_=K.w("rw_w2")[ri, d])
            K.Dm(POOL, [a2a] if d else [], [a2a], out=a2a[d * 64:(d + 1) * 64, :], in_=K.w("rw_a2")[ri, d])
        K.Dm(POOL, [], [g2w], out=g2w[:], in_=K.w("rw_g2")[ri])
        if ri == 1:
            v1w = K.sb(st, "v1w", [128, 8, 32], BF16)
            v2w = K.sb(st, "v2w", [32, 1024], BF16)
            K.Dm(POOL, [], [v1w], out=v1w[:], in_=K.w("rw_v1")[0].rearrange("(kc p) n -> p kc n", p=128))
            K.Dm(POOL, [], [v2w], out=v2w[:], in_=K.w("rw_v2")[0])
        hT = K.sb(st, "rhT", [128, 8, TX], BF16)
        xx = K.sb(st, "rxx", [128, 8, 256], BF16)
        xtmp = K.sb(st, "rxtmp", [128, 8, 256], BF16)
        xj = [K.sb(st, f"rxj{jj}", [128, 8, 256], BF16) for jj in range(6)]
        tw = K.sb(st, "rtw", [128, 256], BF16)
        ta = K.sb(st, "rta", [128, 256], BF16)
        tg = K.sb(st, "rtg", [128, 256], BF16)
        tgf = K.sb(st, "rtgf", [128, 256], F32)
        hcols = K.sb(st, "hcols", [128, 8, 17], F32)
        K.X(DVE, "tensor_scalar", [cols], [hcols], out=hcols[:], in0=cols[:], scalar1=0.5, scalar2=None, op0=ALU.mult)
        tv = K.sb(st, "rtv", [32, 256], BF16)
        ntmp = norm_tmp(K, st)

        def f32t(n, w_=256):
            return K.sb(st, n, [128, w_], F32)
        RK, VG = f32t("RK", 512), f32t("VG", 512)
        SGW, ASIG = K.sb(st, "SGW", [128, 2, 256], F32), K.sb(st, "ASIG", [128, 2, 256], F32)
        SV, KR, SQ_, RN, KKN, NKK, VF, DV = [f32t(n) for n in ("SV", "KR", "SQ", "RN", "KKN", "NKK", "VF", "DV")]
        TMP, BVEC, CUM, CUMR, T3, E1, E2, E3 = [f32t(n) for n in ("TMP", "BVEC", "CUM", "CUMR", "T3", "E1", "E2", "E3")]
        KD = [f32t("KD0"), f32t("KD1")]
        RKR, KS, PR, BVV = [f32t(n) for n in ("RKR", "KS", "PR", "BVV")]
        VB16 = K.sb(st, "VB16", [128, 256], BF16)
        stg = {(n, d): K.sb(st, f"stg_{n}{d}", [128, 256], BF16) for n in ("at", "bt", "kt", "rt") for d in range(2)}
        GLt = [K.sb(st, f"GLt{d}", [128, 4], F32) for d in range(2)]

        def pool_tt(out_t, out, a_t, a, b_t, b, op):
            K.X(POOL, "tensor_tensor", [a_t, b_t], [out_t], out=out, in0=a, in1=b, op=op)

        def shift_block(kind, T, t0):
            def sub(c0, c1, oa, ob, ia0, ia1, ib0, ib1):
                pool_tt(xx, xx[:, c0:c1, oa:ob], hT, hT[:, c0:c1, ia0:ia1], hT, hT[:, c0:c1, ib0:ib1], ALU.subtract)

            def neg(c0, c1, oa, ob, ia, ib):
                K.X(POOL, "tensor_scalar", [hT], [xx], out=xx[:, c0:c1, oa:ob], in0=hT[:, c0:c1, ia:ib], scalar1=-1.0,
                    scalar2=None, op0=ALU.mult)
            if kind == "c":
                sub(0, 4, 1, 256, 0, 255, 1, 256)
                neg(0, 4, 0, 1, 0, 1)
                sub(4, 8, 0, 255, 1, 256, 0, 255)
                neg(4, 8, 255, 256, 255, 256)
                return
            hv = lambda c0, c1: hT[:, c0:c1, t0:t0 + 256].rearrange("p c (r w) -> p c r w", w=64)
            xv = lambda c0, c1: xx[:, c0:c1, :].rearrange("p c (r w) -> p c r w", w=64)
            K.X(POOL, "tensor_tensor", [hT], [xx], out=xv(0, 2)[:, :, :, 1:64], in0=hv(0, 2)[:, :, :, 0:63],
                in1=hv(0, 2)[:, :, :, 1:64], op=ALU.subtract)
            K.X(POOL, "tensor_scalar", [hT], [xx], out=xv(0, 2)[:, :, :, 0:1], in0=hv(0, 2)[:, :, :, 0:1], scalar1=-1.0,
                scalar2=None, op0=ALU.mult)
            K.X(POOL, "tensor_tensor", [hT], [xx], out=xv(2, 4)[:, :, :, 0:63], in0=hv(2, 4)[:, :, :, 1:64],
                in1=hv(2, 4)[:, :, :, 0:63], op=ALU.subtract)
            K.X(POOL, "tensor_scalar", [hT], [xx], out=xv(2, 4)[:, :, :, 63:64], in0=hv(2, 4)[:, :, :, 63:64], scalar1=-1.0,
                scalar2=None, op0=ALU.mult)
            if t0 == 0:
                neg(4, 6, 0, 64, 0, 64)
                sub(4, 6, 64, 256, 0, 192, 64, 256)
            else:
                sub(4, 6, 0, 256, t0 - 64, t0 + 192, t0, t0 + 256)
            if t0 + 256 == T:
                sub(6, 8, 0, 192, t0 + 64, t0 + 256, t0, t0 + 192)
                neg(6, 8, 192, 256, t0 + 192, t0 + 256)
            else:
                sub(6, 8, 0, 256, t0 + 64, t0 + 320, t0, t0 + 256)

        for b in range(NBC):
            for (kind, T, src, j, tl0) in (("c", TC, K.dram["cs"][b], 2, 0), ("x", TX, K.dram["xs"][b], b, TC)):
                chk(0)
                norm_hT(K, st, src, T, scl, sh, j, hT, ntmp)
                chk(1)
                for tb in range(T // 256):
                    t0 = tb * 256
                    tl = tl0 + t0
                    shift_block(kind, T, t0)
                    chk(2)
                    for jj in range(6):
                        eng = DVE if jj % 2 == 0 else POOL
                        K.X(eng, "tensor_tensor", [xx, cols], [xtmp], out=xtmp[:], in0=xx[:],
                            in1=cols[:, :, CMIX + jj:CMIX + jj + 1].broadcast_to([128, 8, 256]), op=ALU.mult)
                        K.X(eng, "tensor_tensor", [xtmp, hT], [xj[jj]], out=xj[jj][:], in0=xtmp[:], in1=hT[:, :, t0:t0 + 256],
                            op=ALU.add)
                    xr, xw, xk, xv_, xa, xg_ = xj
                    chk(3)
                    ps = K.ps()
                    for f in range(8):
                        K.mm(ps, ps.f(0, 256), w1c[:, f, :], xw[:, f, :], [w1c, xw], start=(f == 0), stop=(f == 7))
                    chk(31)
                    for f in range(8):
                        K.mm(ps, ps.f(256, 512), a1c[:, f, :], xa[:, f, :], [a1c, xa], start=(f == 0), stop=(f == 7))
                    chk(32)
                    K.act([ps], [tw], out=tw[:], in_=ps.f(0, 256), func=AF.Tanh)
                    chk(33)
                    K.X(ACT, "copy", [ps], [ta], out=ta[:], in_=ps.f(256, 512))
                    chk(34)
                    ps = K.ps()
                    for f in range(8):
                        K.mm(ps, ps.f(0, 256), g1w[:, f, :], xg_[:, f, :], [g1w, xg_], start=(f == 0), stop=(f == 7))
                    if ri == 1:
                        for f in range(8):
                            K.mm(ps, ps.f(256, 512, rows=32), v1w[:, f, :], xv_[:, f, :], [v1w, xv_], start=(f == 0), stop=(f == 7))
                    chk(35)
                    K.act([ps], [tgf], out=tgf[:], in_=ps.f(0, 256), func=AF.Tanh, scale=0.5)
                    K.X(DVE, "tensor_scalar", [tgf], [tg], out=tg[:], in0=tgf[:], scalar1=0.5, scalar2=0.5, op0=ALU.mult, op1=ALU.add)
                    if ri == 1:
                        K.X(ACT, "copy", [ps], [tv], out=tv[:], in_=ps.f(256, 512, rows=32))
                    chk(4)
                    for dc in range(8):
                        fs = slice(dc * 128, (dc + 1) * 128)
                        drow = slice(dc * 128, (dc + 1) * 128)
                        dsl = (b, drow, slice(tl, tl + 256))
                        ps1 = K.ps()
                        for f in range(8):
                            K.mm(ps1, ps1.f(0, 256), wr[:, f, fs], xr[:, f, :], [wr, xr], start=(f == 0), stop=(f == 7))
                        for f in range(8):
                            K.mm(ps1, ps1.f(256, 512), wk[:, f, fs], xk[:, f, :], [wk, xk], start=(f == 0), stop=(f == 7))
                        K.X(ACT, "copy", [ps1], [RK], out=RK[:], in_=ps1.f())
                        chk(41)
                        ps2 = K.ps()
                        for f in range(8):
                            K.mm(ps2, ps2.f(0, 256), wv[:, f, fs], xv_[:, f, :], [wv, xv_], start=(f == 0), stop=(f == 7))
                        K.mm(ps2, ps2.f(256, 512), g2w[:, fs], tg[:], [g2w, tg])
                        K.X(DVE, "tensor_copy", [ps2], [VG], out=VG[:], in_=ps2.f())
                        chk(42)
                        for (wt2, tin, dst, cidx) in ((w2a, tw, SGW, CW0), (a2a, ta, ASIG, CA0)):
                            pss = [K.ps(), K.ps()]
                            for d in range(2):
                                K.mm(pss[d], pss[d].f(0, 256), wt2[d * 64:(d + 1) * 64, fs], tin[d * 64:(d + 1) * 64, :], [wt2, tin])
                            for d in range(2):
                                K.act([pss[d], hcols], [dst], out=dst[:, d, :], in_=pss[d].f(0, 256), func=AF.Tanh,
                                      bias=hcols[:, dc, cidx + d:cidx + d + 1], scale=0.5)
                            K.X(POOL, "tensor_scalar", [dst], [dst], out=dst[:], in0=dst[:], scalar1=0.5, scalar2=0.5, op0=ALU.mult, op1=ALU.add)
                        chk(44)
                        R_, Kk, V_, G_ = RK[:, 0:256], RK[:, 256:512], VG[:, 0:256], VG[:, 256:512]
                        chk(5)
                        K.Dm(SP, [VG], [], out=S["g"][dsl], in_=G_)
                        if ri == 1:
                            ps5 = K.ps()
                            K.mm(ps5, ps5.f(0, 256), v2w[0:32, fs], tv[0:32, :], [v2w, tv])
                            K.act([ps5, hcols], [SV], out=SV[:], in_=ps5.f(0, 256), func=AF.Tanh, bias=hcols[:, dc, CV0:CV0 + 1], scale=0.5)
                            K.X(POOL, "tensor_scalar", [SV], [SV], out=SV[:], in0=SV[:], scalar1=0.5, scalar2=0.5, op0=ALU.mult, op1=ALU.add)
                            K.Dm(SP, [], [VF], out=VF[:], in_=S["vfirst"][dsl])
                            pool_tt(DV, DV[:], VF, VF[:], VG, V_, ALU.subtract)
                            pool_tt(DV, DV[:], DV, DV[:], SV, SV[:], ALU.mult)
                            pool_tt(VG, V_, VG, V_, DV, DV[:], ALU.add)
                        else:
                            K.Dm(SP, [VG], [], out=S["vfirst"][dsl], in_=V_)
                        K.X(ACT, "copy", [VG], [VB16], out=VB16[:], in_=V_)
                        K.Dm(SP, [VB16], [], out=S["v"][dsl], in_=VB16[:])
                        chk(6)
                        K.X(POOL, "tensor_scalar", [RK, cols], [KR], out=KR[:], in0=Kk, scalar1=cols[:, dc, CKK:CKK + 1], scalar2=None, op0=ALU.mult)
                        pool_tt(SQ_, SQ_[:], KR, KR[:], KR, KR[:], ALU.mult)
                        ps6 = K.ps()
                        K.mm(ps6, ps6.f(0, 256), C["blockones_f"][:], SQ_[:], [C["blockones_f"], SQ_])
                        K.act([ps6, C["eps"]], [RN], out=RN[:], in_=ps6.f(0, 256), func=AF.Sqrt, bias=C["eps"][:, 1:2], scale=1.0)
                        K.X(DVE, "reciprocal", [RN], [RN], out=RN[:], in_=RN[:])
                        pool_tt(KKN, KKN[:], KR, KR[:], RN, RN[:], ALU.mult)
                        K.X(POOL, "tensor_scalar", [KKN], [NKK], out=NKK[:], in0=KKN[:], scalar1=-1.0, scalar2=None, op0=ALU.mult)
                        chk(7)
                        for d in range(2):
                            K.X(POOL, "tensor_scalar", [ASIG, cols, omka], [TMP], out=TMP[:], in0=ASIG[:, d, :],
                                scalar1=cols[:, dc, CKA:CKA + 1], scalar2=omka[:, dc, 0:1], op0=ALU.mult, op1=ALU.add)
                            pool_tt(KD[d], KD[d][:], RK, Kk, TMP, TMP[:], ALU.mult)
                            pool_tt(BVEC, BVEC[:], KKN, KKN[:], ASIG, ASIG[:, d, :], ALU.mult)
                            K.X(DVE, "tensor_tensor_scan", [SGW, C["segmask"]], [CUM], out=CUM[:], data0=C["segmask"][:],
                                data1=SGW[:, d, :], initial=0.0, op0=ALU.mult, op1=ALU.add)
                            K.act([CUM], [GLt[d]], out=GLt[d][:], in_=CUM[:].rearrange("p (c j) -> p c j", j=64)[:, :, 63],
                                  func=AF.Exp, scale=-KAPPA)
                            K.Dm(SP, [GLt[d]], [], out=S[f"GL{d}"][b, drow, tl // 64:tl // 64 + 4], in_=GLt[d][:])
                            cum = CUM
                            if d == 1:
                                pool_tt(T3, T3[:], CUM, CUM[:], SGW, SGW[:, d, :], ALU.subtract)
                                K.X(POOL, "tensor_tensor", [CUM, T3], [CUMR], out=CUMR[:].rearrange("p (c j) -> p c j", j=64),
                                    in0=CUM[:].rearrange("p (c j) -> p c j", j=64)[:, :, 63:64].broadcast_to([128, 4, 64]),
                                    in1=T3[:].rearrange("p (c j) -> p c j", j=64), op=ALU.subtract)
                                cum = CUMR
                            K.act([cum], [E1], out=E1[:], in_=cum[:], func=AF.Exp, scale=-KAPPA)
                            pool_tt(stg[("rt", d)], stg[("rt", d)][:], RK, R_, E1, E1[:], ALU.mult)
                            pool_tt(T3, T3[:], cum, cum[:], SGW, SGW[:, d, :], ALU.subtract)
                            K.act([T3], [E2], out=E2[:], in_=T3[:], func=AF.Exp, scale=-KAPPA)
                            pool_tt(stg[("at", d)], stg[("at", d)][:], NKK, NKK[:], E2, E2[:], ALU.mult)
                            K.act([cum], [E3], out=E3[:], in_=cum[:], func=AF.Exp, scale=KAPPA)
                            K.X(DVE, "tensor_tensor", [BVEC, E3], [stg[("bt", d)]], out=stg[("bt", d)][:], in0=BVEC[:], in1=E3[:], op=ALU.mult)
                            K.X(DVE, "tensor_tensor", [KD[d], E3], [stg[("kt", d)]], out=stg[("kt", d)][:], in0=KD[d][:], in1=E3[:], op=ALU.mult)
                            for n in ("at", "bt", "kt", "rt"):
                                K.Dm(SP, [stg[(n, d)]], [], out=S[f"{n}{d}"][dsl], in_=stg[(n, d)][:])
                        chk(8)
                        K.X(POOL, "tensor_scalar", [RK, cols], [RKR], out=RKR[:], in0=R_, scalar1=cols[:, dc, CRK:CRK + 1], scalar2=None, op0=ALU.mult)
                        pool_tt(KS, KS[:], KD[0], KD[0][:], KD[1], KD[1][:], ALU.add)
                        pool_tt(PR, PR[:], RKR, RKR[:], KS, KS[:], ALU.mult)
                        ps7 = K.ps()
                        K.mm(ps7, ps7.f(0, 256), C["blockones_f"][:], PR[:], [C["blockones_f"], PR])
                        K.X(DVE, "tensor_tensor", [ps7, VG], [BVV], out=BVV[:], in0=ps7.f(0, 256), in1=V_, op=ALU.mult)
                        K.Dm(SP, [BVV], [], out=S["bv"][dsl], in_=BVV[:])
                        chk(9)
        K.barrier()


def rwkv_scan(K):
    nc, C = K.nc, K.C
    S = K.rw
    order = {0: list(range(NCH)), 1: [3, 2, 1, 0] + list(range(NCH - 1, 3, -1))}
    identb = C["ident_b"]

    def ecopy(eng, ins, out_t, out, in_):
        if eng == ACT:
            K.X(ACT, "copy", ins, [out_t], out=out, in_=in_)
        else:
            K.X(DVE, "tensor_copy", ins, [out_t], out=out, in_=in_)

    for hpg in range(4):
        with contextlib.ExitStack() as st:
            chains = []
            for b in range(NBC):
                for hq in range(2):
                    for d in range(2):
                        c = dict(b=b, hp=2 * hpg + hq, d=d)
                        nm = f"{b}{hq}{d}"
                        c["cin"] = [{n: K.sb(st, f"ci{nm}{k}{n}", [128, 256], BF16) for n in ("at", "rt", "bt", "kt", "v")}
                                    for k in range(2)]
                        c["ARB"] = K.sb(st, "ARB" + nm, [128, 4, 256], BF16)
                        c["BTB"] = K.sb(st, "BTB" + nm, [128, 4, 128], BF16)
                        c["KTB"] = K.sb(st, "KTB" + nm, [128, 4, 128], BF16)
                        c["VB"] = K.sb(st, "VB" + nm, [128, 4, 128], BF16)
                        c["M1"] = K.sb(st, "M1" + nm, [128, 256], BF16)
                        c["M2"] = K.sb(st, "M2" + nm, [128, 256], BF16)
                        c["A0"] = K.sb(st, "A0" + nm, [128, 128], BF16)
                        c["TF"] = K.sb(st, "TF" + nm, [128, 384], BF16)
                        c["X"] = [K.sb(st, f"X{k}" + nm, [128, 256], BF16) for k in range(2)]
                        c["IP"] = [K.sb(st, f"IP{k}" + nm, [128, 128], BF16) for k in range(2)]
                        c["SQ"] = [K.sb(st, f"SQ{k}" + nm, [128, 256], BF16) for k in range(2)]
                        c["WT"] = K.sb(st, "WT" + nm, [128, 128], BF16)
                        c["U"] = K.sb(st, "U" + nm, [128, 128], BF16)
                        c["Ybd"] = K.sb(st, "Ybd" + nm, [128, 128], F32)
                        c["Hf"] = K.sb(st, "Hf" + nm, [128, 128], F32)
                        c["hg"] = K.sb(st, "hg" + nm, [128, 128], F32)
                        c["Hbf"] = K.sb(st, "Hbf" + nm, [128, 128], BF16)
                        c["yst"] = [K.sb(st, f"yst{k}" + nm, [128, 256], F32) for k in range(2)]
                        c["GL"] = K.sb(st, "GL" + nm, [128, NCH], F32)
                        rows = slice(c["hp"] * 128, (c["hp"] + 1) * 128)
                        c["rows"] = rows
                        K.Dm(SP, [], [c["GL"]], out=c["GL"][:], in_=S[f"GL{d}"][b, rows, :])
                        K.X(DVE, "memset", [], [c["Hf"]], ap=c["Hf"][:], constant=0.0)
                        K.X(DVE, "tensor_copy", [c["Hf"]], [c["Hbf"]], out=c["Hbf"][:], in_=c["Hf"][:])
                        chains.append(c)

            def load_group(c, grp, k):
                ci = c["cin"][k]
                b, d, rows = c["b"], c["d"], c["rows"]
                tsl = slice(grp * 256, (grp + 1) * 256)
                for n in ("at", "rt", "bt", "kt"):
                    K.Dm(SP, [], [ci[n]], out=ci[n][:], in_=S[f"{n}{d}"][b, rows, tsl])
                K.Dm(SP, [], [ci["v"]], out=ci["v"][:], in_=S["v"][b, rows, tsl])

            def expand_group(c, k):
                ci = c["cin"][k]
                msk = C["blkmask"][:].rearrange("p (h i) -> p h i", h=2).unsqueeze(1).broadcast_to([128, 4, 2, 64])

                def ex(src, dst_t, dst_ap):
                    K.X(POOL, "tensor_tensor", [src, C["blkmask"]], [dst_t],
                        out=dst_ap.rearrange("p c (h i) -> p c h i", h=2),
                        in0=src[:].rearrange("p (c i) -> p c i", i=64).unsqueeze(2).broadcast_to([128, 4, 2, 64]),
                        in1=msk, op=ALU.mult)
                ex(ci["at"], c["ARB"], c["ARB"][:, :, 0:128])
                ex(ci["rt"], c["ARB"], c["ARB"][:, :, 128:256])
                ex(ci["bt"], c["BTB"], c["BTB"][:, :, :])
                ex(ci["kt"], c["KTB"], c["KTB"][:, :, :])
                ex(ci["v"], c["VB"], c["VB"][:, :, :])

            def stage(per_bank, pe_fn, ev_fn, engs=(DVE, ACT)):
                for gi in range(0, len(chains), per_bank):
                    ps = K.ps()
                    grp = chains[gi:gi + per_bank]
                    for si, c in enumerate(grp):
                        pe_fn(c, ps, si)
                    eng = engs[(gi // per_bank) % len(engs)]
                    for si, c in enumerate(grp):
                        ev_fn(c, ps, si, eng)

            for c in chains:
                load_group(c, order[c["d"]][0] // 4, 0)
            for r in range(NCH):
                for c in chains:
                    n = order[c["d"]][r]
                    c["n"], c["wi"], c["grp"] = n, n % 4, n // 4
                    c["gk"] = (r // 4) % 2
                    if r % 4 == 0:
                        expand_group(c, c["gk"])
                        if r + 4 < NCH:
                            load_group(c, order[c["d"]][r + 4] // 4, 1 - c["gk"])
                stage(2, lambda c, ps, s: K.mm(ps, ps.f(s * 256, s * 256 + 256), c["BTB"][:, c["wi"], :], c["ARB"][:, c["wi"], :],
                                               [c["BTB"], c["ARB"]]),
                      lambda c, ps, s, e: K.X(DVE, "tensor_tensor", [ps, C["mT"]], [c["M1"]], out=c["M1"][:],
                                              in0=ps.f(s * 256, s * 256 + 256), in1=C["mT"][:, c["d"], :], op=ALU.mult), engs=(DVE,))
                stage(2, lambda c, ps, s: K.mm(ps, ps.f(s * 256, s * 256 + 256), c["KTB"][:, c["wi"], :], c["ARB"][:, c["wi"], :],
                                               [c["KTB"], c["ARB"]]),
                      lambda c, ps, s, e: K.X(DVE, "tensor_tensor", [ps, C["mT"]], [c["M2"]], out=c["M2"][:],
                                              in0=ps.f(s * 256, s * 256 + 256), in1=C["mT"][:, c["d"], :], op=ALU.mult), engs=(DVE,))
                stage(4, lambda c, ps, s: K.mm(ps, ps.f(s * 128, s * 128 + 128), c["ARB"][:, c["wi"], 0:128], c["BTB"][:, c["wi"], :],
                                               [c["ARB"], c["BTB"]]),
                      lambda c, ps, s, e: K.X(DVE, "tensor_tensor", [ps, C["mA"]], [c["A0"]], out=c["A0"][:],
                                              in0=ps.f(s * 128, s * 128 + 128), in1=C["mA"][:, c["d"], :], op=ALU.mult), engs=(DVE,))
                for c in chains:
                    K.X(POOL, "tensor_tensor", [c["M1"], identb], [c["IP"][0]], out=c["IP"][0][:], in0=c["M1"][:, 0:128],
                        in1=identb[:], op=ALU.add)

                def tr_pe(c, ps, s):
                    srcs = [(c["ARB"], c["ARB"][:, c["wi"], 0:128]), (c["BTB"], c["BTB"][:, c["wi"], :]),
                            (c["KTB"], c["KTB"][:, c["wi"], :]), (c["VB"], c["VB"][:, c["wi"], :])]
                    for q, (t, ap) in enumerate(srcs):
                        K.tr(ps, ps.b(s * 512 + q * 128, s * 512 + (q + 1) * 128), ap, identb[:], [t, identb])

                def tr_ev(c, ps, s, e):
                    ecopy(e, [ps], c["TF"], c["TF"][:], ps.b(s * 512 + 128, s * 512 + 512))
                    ecopy(e, [ps], c["X"][0], c["X"][0][:, 0:128], ps.b(s * 512, s * 512 + 128))
                stage(2, tr_pe, tr_ev)
                stage(4, lambda c, ps, s: K.mm(ps, ps.f(s * 128, s * 128 + 128), c["M2"][:, 0:128], c["TF"][:, 256:384], [c["M2"], c["TF"]]),
                      lambda c, ps, s, e: ecopy(e, [ps], c["X"][0], c["X"][0][:, 128:256], ps.f(s * 128, s * 128 + 128)))

                def sq_pe(m):
                    def f(c, ps, s):
                        if m == 0:
                            A_t, A_ap, AT_t, AT_ap = c["A0"], c["A0"][:], c["M1"], c["M1"][:, 0:128]
                        else:
                            q = c["SQ"][(m - 1) % 2]
                            A_t, A_ap, AT_t, AT_ap = q, q[:, 0:128], q, q[:, 128:256]
                        K.mm(ps, ps.f(s * 256, s * 256 + 128), AT_ap, A_ap, [A_t, AT_t])
                        K.mm(ps, ps.f(s * 256 + 128, s * 256 + 256), A_ap, AT_ap, [A_t, AT_t])
                    return f

                def sq_ev(m):
                    def f(c, ps, s, e):
                        ecopy(e, [ps], c["SQ"][m % 2], c["SQ"][m % 2][:], ps.f(s * 256, s * 256 + 256))
                    return f

                def neu_pe(m):
                    def f(c, ps, s):
                        K.mm(ps, ps.f(s * 256, s * 256 + 256), c["IP"][m % 2][:], c["X"][m % 2][:], [c["IP"][m % 2], c["X"][m % 2]])
                    return f

                def neu_ev(m):
                    def f(c, ps, s, e):
                        ecopy(e, [ps], c["X"][(m + 1) % 2], c["X"][(m + 1) % 2][:], ps.f(s * 256, s * 256 + 256))
                    return f
                for m in range(5):
                    stage(2, sq_pe(m), sq_ev(m))
                    stage(2, neu_pe(m), neu_ev(m), engs=(ACT, DVE))
                    for c in chains:
                        K.X(POOL, "tensor_tensor", [c["SQ"][m % 2], identb], [c["IP"][(m + 1) % 2]], out=c["IP"][(m + 1) % 2][:],
                            in0=c["SQ"][m % 2][:, 128:256], in1=identb[:], op=ALU.add)
                stage(2, neu_pe(5), neu_ev(5))
                stage(4, lambda c, ps, s: K.tr(ps, ps.b(s * 256, s * 256 + 128), c["X"][0][:, 0:128], identb[:], [c["X"][0], identb]),
                      lambda c, ps, s, e: ecopy(e, [ps], c["WT"], c["WT"][:], ps.b(s * 256, s * 256 + 128)))
                stage(4, lambda c, ps, s: K.mm(ps, ps.f(s * 128, s * 128 + 128), c["WT"][:], c["Hbf"][:], [c["WT"], c["Hbf"]]),
                      lambda c, ps, s, e: K.X(DVE, "tensor_tensor", [ps, c["X"][0]], [c["U"]], out=c["U"][:],
                                              in0=ps.f(s * 128, s * 128 + 128), in1=c["X"][0][:, 128:256], op=ALU.add), engs=(DVE,))

                def y_pe(c, ps, s):
                    o = ps.f(s * 128, s * 128 + 128)
                    K.mm(ps, o, c["ARB"][:, c["wi"], 128:256], c["Hbf"][:], [c["ARB"], c["Hbf"]], start=True, stop=False)
                    K.mm(ps, o, c["M1"][:, 128:256], c["U"][:], [c["M1"], c["U"]], start=False, stop=False)
                    K.mm(ps, o, c["M2"][:, 128:256], c["TF"][:, 256:384], [c["M2"], c["TF"]], start=False, stop=True)
                stage(4, y_pe, lambda c, ps, s, e: ecopy(e, [ps], c["Ybd"], c["Ybd"][:], ps.f(s * 128, s * 128 + 128)), engs=(ACT,))
                for c in chains:
                    K.X(POOL, "tensor_scalar", [c["Hf"], c["GL"]], [c["hg"]], out=c["hg"][:], in0=c["Hf"][:],
                        scalar1=c["GL"][:, c["n"]:c["n"] + 1], scalar2=None, op0=ALU.mult)

                def h_pe(c, ps, s):
                    o = ps.f(s * 128, s * 128 + 128)
                    K.mm(ps, o, c["TF"][:, 0:128], c["U"][:], [c["TF"], c["U"]], start=True, stop=False)
                    K.mm(ps, o, c["TF"][:, 128:256], c["TF"][:, 256:384], [c["TF"]], start=False, stop=True)

                def h_ev(c, ps, s, e):
                    K.X(DVE, "scalar_tensor_tensor", [ps, c["GL"], c["hg"]], [c["Hf"]], out=c["Hf"][:], in0=ps.f(s * 128, s * 128 + 128),
                        scalar=c["GL"][:, c["n"]:c["n"] + 1], in1=c["hg"][:], op0=ALU.mult, op1=ALU.add)
                stage(4, h_pe, h_ev, engs=(DVE,))
                for c in chains:
                    K.X(ACT, "copy", [c["Hf"]], [c["Hbf"]], out=c["Hbf"][:], in_=c["Hf"][:])
                stage(4, lambda c, ps, s: K.mm(ps, ps.f(s * 128, s * 128 + 64), c["Ybd"][:], C["sel"][:], [c["Ybd"], C["sel"]]),
                      lambda c, ps, s, e: ecopy(e, [ps], c["yst"][c["gk"]], c["yst"][c["gk"]][:, c["wi"] * 64:(c["wi"] + 1) * 64],
                                                ps.f(s * 128, s * 128 + 64)), engs=(ACT, DVE))
                if r % 4 == 3:
                    for c in chains:
                        ys = c["yst"][c["gk"]]
                        K.Dm(SP, [ys], [], out=S[f"y{c['d']}"][c["b"], c["rows"], c["grp"] * 256:(c["grp"] + 1) * 256], in_=ys[:])
            K.barrier()


def rwkv_out(K, i, ri, need_ctx):
    nc, C = K.nc, K.C
    S = K.rw
    with contextlib.ExitStack() as st:
        cols = rwkv_cols(K, st, i, ri)
        wo = K.sb(st, "rwo", [128, 8, 1024], BF16)
        K.Dm(POOL, [], [wo], out=wo[:], in_=K.w("rw_wo")[ri].rearrange("(kc p) n -> p kc n", p=128))
        g1bc = K.sb(st, "rg1bc", [128, 1024], F32)
        oT = K.sb(st, "roT", [128, 8, 256], BF16)
        ptmp = proj_tmp(K, st)

        def f32t(n):
            return [K.sb(st, f"{n}{k}", [128, 256], F32) for k in range(2)]
        Y0, Y1, YQ, MN, MQ, VAR, Z, BVt, Gt = [f32t(n) for n in ("oY0", "oY1", "oYQ", "oMN", "oMQ", "oVAR", "oZ", "oBV", "oG")]
        it = 0
        for b in range(NBC):
            seqs = [("x", TX, K.dram["xs"][b], b, TC)]
            if need_ctx:
                seqs = [("c", TC, K.dram["cs"][b], 2, 0)] + seqs
            for (kind, T, res, j, tl0) in seqs:
                bcast_row(K, g1bc, K.dram["modrow"][i, j:j + 1, 2 * 1024:3 * 1024])
                for tb in range(T // 256):
                    tl = tl0 + tb * 256
                    for dc in range(8):
                        k = it % 2
                        it += 1
                        dsl = (b, slice(dc * 128, (dc + 1) * 128), slice(tl, tl + 256))
                        y0, y1, yq, mn, mq, var, z, bv, g = Y0[k], Y1[k], YQ[k], MN[k], MQ[k], VAR[k], Z[k], BVt[k], Gt[k]
                        K.Dm(SP, [], [y0], out=y0[:], in_=S["y0"][dsl])
                        K.Dm(SP, [], [y1], out=y1[:], in_=S["y1"][dsl])
                        K.Dm(SP, [], [bv], out=bv[:], in_=S["bv"][dsl])
                        K.Dm(SP, [], [g], out=g[:], in_=S["g"][dsl])
                        K.X(POOL, "tensor_tensor", [y0, y1], [y0], out=y0[:], in0=y0[:], in1=y1[:], op=ALU.add)
                        K.X(POOL, "tensor_tensor", [y0], [yq], out=yq[:], in0=y0[:], in1=y0[:], op=ALU.mult)
                        ps = K.ps()
                        K.mm(ps, ps.f(0, 256), C["blockones_f"][:], y0[:], [C["blockones_f"], y0])
                        K.mm(ps, ps.f(256, 512), C["blockones_f"][:], yq[:], [C["blockones_f"], yq])
                        K.act([ps], [mn], out=mn[:], in_=ps.f(0, 256), func=AF.Copy, scale=1.0 / 64)
                        K.act([ps], [var], out=var[:], in_=ps.f(256, 512), func=AF.Copy, scale=1.0 / 64)
                        K.X(POOL, "tensor_tensor", [mn], [mq], out=mq[:], in0=mn[:], in1=mn[:], op=ALU.mult)
                        K.X(POOL, "tensor_tensor", [var, mq], [var], out=var[:], in0=var[:], in1=mq[:], op=ALU.subtract)
                        K.act([var, C["eps"]], [var], out=var[:], in_=var[:], func=AF.Sqrt, bias=C["eps"][:, 2:3], scale=1.0)
                        K.X(DVE, "reciprocal", [var], [var], out=var[:], in_=var[:])
                        K.X(POOL, "tensor_tensor", [y0, mn], [z], out=z[:], in0=y0[:], in1=mn[:], op=ALU.subtract)
                        K.X(POOL, "tensor_tensor", [z, var], [z], out=z[:], in0=z[:], in1=var[:], op=ALU.mult)
                        K.X(DVE, "tensor_scalar", [z, cols], [z], out=z[:], in0=z[:], scalar1=cols[:, dc, CLW:CLW + 1],
                            scalar2=cols[:, dc, CLB:CLB + 1], op0=ALU.mult, op1=ALU.add)
                        K.X(POOL, "tensor_tensor", [z, bv], [z], out=z[:], in0=z[:], in1=bv[:], op=ALU.add)
                        K.X(DVE, "tensor_tensor", [z, g], [oT], out=oT[:, dc, :], in0=z[:], in1=g[:], op=ALU.mult)
                    proj_residual(K, st, oT, 256, wo, res[tb * 256:(tb + 1) * 256, :], g1bc, None, ptmp)
        K.barrier()


def phase_rwkv(K, i, ri, need_ctx):
    K.set_psum("full")
    rwkv_prep(K, i, ri)
    if K.rw_stop >= 1:
        rwkv_scan(K)
    if K.debug and i == 1:
        for nm, dt in (("y0", F32), ("y1", F32), ("v", BF16), ("GL0", F32), ("GL1", F32), ("rt0", BF16), ("kt0", BF16), ("bv", F32), ("at1", BF16), ("bt1", BF16), ("at0", BF16), ("bt0", BF16), ("kt1", BF16), ("rt1", BF16), ("g", F32)):
            o = K.dout("dbg_rw_" + nm, list(K.rw[nm].shape), dt)
            for b in range(NBC):
                for dc in range(8):
                    K.Dm(SP, [], [], out=o[b, dc * 128:(dc + 1) * 128, :], in_=K.rw[nm][b, dc * 128:(dc + 1) * 128, :])
        K.barrier()
    if K.rw_stop >= 2:
        rwkv_out(K, i, ri, need_ctx)
```

```python
import contextlib
import numpy as np
import ml_dtypes
import concourse.bass as bass
import concourse.mybir as mybir
from concourse.bass_utils import run_bass_kernel_spmd

F32 = mybir.dt.float32
BF16 = mybir.dt.bfloat16
I32 = mybir.dt.int32
U32 = mybir.dt.uint32
AF = mybir.ActivationFunctionType
ALU = mybir.AluOpType
AX = mybir.AxisListType

PE, DVE, ACT, POOL, SP = 0, 1, 2, 3, 4
EPOCH = 30000
N_DSEM = 40


class Res:
    __slots__ = ("w", "r", "name", "excl")

    def __init__(self, name="", excl=False):
        self.w = None
        self.r = {}
        self.name = name
        self.excl = excl


class T:
    __slots__ = ("t", "res")

    def __init__(self, t, name=""):
        self.t = t
        self.res = Res(name)

    def __getitem__(self, k):
        return self.t[k]


class Prog:
    def __init__(self, nc, es):
        self.nc = nc
        self.es = es
        self.eng = [nc.tensor, nc.vector, nc.scalar, nc.gpsimd, nc.sync]
        self.cnt = [0] * 5
        self.epoch = [0] * 5
        self.esem = [[] for _ in range(5)]
        for e in range(4):
            self.esem[e].append(es.enter_context(nc.semaphore(f"e{e}_0")))
        self.known = [dict() for _ in range(5)]
        self.knownd = [dict() for _ in range(5)]
        self.dsem = [es.enter_context(nc.semaphore(f"d{i}")) for i in range(N_DSEM)]
        self.dval = [0] * N_DSEM
        self.dnext = 0
        self.n_inst = 0
        self.n_wait = 0
        self.psum_tiles = []
        self.psum_i = 0
        self.snap = {}

    def _learn(self, me, tok):
        sn = self.snap.get(tok)
        if sn is None:
            return
        km, kd = self.known[me], self.knownd[me]
        for e, v in sn[0].items():
            if km.get(e, (-1, 0)) < v:
                km[e] = v
        for s_, v in sn[1].items():
            if kd.get(s_, 0) < v:
                kd[s_] = v

    def wait_tok(self, me, tok):
        if tok is None:
            return
        if tok[0] == "e":
            _, e, ep, c = tok
            if e == me and me == PE:
                return
            k = self.known[me].get(e, (-1, 0))
            if k >= (ep, c):
                return
            self.eng[me].wait_ge(self.esem[e][ep], c)
            self.n_wait += 1
            self.known[me][e] = (ep, c)
            self._learn(me, tok)
        else:
            _, s, v = tok
            if self.knownd[me].get(s, 0) >= v:
                return
            self.eng[me].wait_ge(self.dsem[s], v)
            self.n_wait += 1
            self.knownd[me][s] = v
            self._learn(me, tok)

    def _deps(self, me, ins, outs):
        for r in ins:
            r = getattr(r, "res", r)
            self.wait_tok(me, r.w)
            if r.excl:
                for t in r.r.values():
                    self.wait_tok(me, t)
        for r in outs:
            r = getattr(r, "res", r)
            self.wait_tok(me, r.w)
            for t in r.r.values():
                self.wait_tok(me, t)

    def _mark(self, tok, key, ins, outs):
        for r in ins:
            r = getattr(r, "res", r)
            if r.excl:
                r.w = tok
                r.r = {}
            else:
                r.r[key] = tok
        for r in outs:
            r = getattr(r, "res", r)
            r.w = tok
            r.r = {}

    def op(self, me, fn, ins=(), outs=()):
        if getattr(self, "muted", False):
            return None
        self._deps(me, ins, outs)
        if self.cnt[me] >= EPOCH:
            self.epoch[me] += 1
            self.cnt[me] = 0
            self.esem[me].append(self.es.enter_context(self.nc.semaphore(f"e{me}_{self.epoch[me]}")))
        inst = fn()
        self.cnt[me] += 1
        inst.then_inc(self.esem[me][self.epoch[me]], 1)
        self.n_inst += 1
        tok = ("e", me, self.epoch[me], self.cnt[me])
        self.snap[tok] = (dict(self.known[me]), dict(self.knownd[me]))
        self._mark(tok, me, ins, outs)
        return tok

    def dma(self, me, fn, ins=(), outs=()):
        if getattr(self, "muted", False):
            return None
        self._deps(me, ins, outs)
        s = self.dnext
        self.dnext = (self.dnext + 1) % N_DSEM
        if self.dval[s] > 0:
            self.wait_tok(me, ("d", s, self.dval[s]))
        inst = fn()
        self.dval[s] += 16
        inst.then_inc(self.dsem[s], 16)
        self.n_inst += 1
        tok = ("d", s, self.dval[s])
        self.snap[tok] = (dict(self.known[me]), dict(self.knownd[me]))
        self._mark(tok, ("d", s), ins, outs)
        return tok

    def last_tok(self, e):
        if e == SP or (self.cnt[e] == 0 and self.epoch[e] == 0):
            return None
        return ("e", e, self.epoch[e], self.cnt[e])

    def barrier(self, engines=(PE, DVE, ACT, POOL, SP)):
        toks = [self.last_tok(e) for e in range(4)]
        for me in engines:
            for e in range(4):
                if e != me:
                    self.wait_tok(me, toks[e])
            for s in range(N_DSEM):
                if self.dval[s] > 0:
                    self.wait_tok(me, ("d", s, self.dval[s]))

    def sb(self, st, name, shape, dt):
        self.uid = getattr(self, "uid", 0) + 1
        return T(st.enter_context(self.nc.sbuf_tensor(f"s{self.uid}_{name}", list(shape), dt)), name)

    def init_psum(self, st, n=8):
        self.psum_tiles = [T(st.enter_context(self.nc.psum_tensor(f"psb{i}", [128, 512], F32)), f"psb{i}")
                           for i in range(n)]
        self.psum_i = 0

    def ps(self):
        t = self.psum_tiles[self.psum_i % len(self.psum_tiles)]
        self.psum_i += 1
        return t


D = 1024
NBC = 2
TX = 2048
TC = 256
TL = TC + TX
NE = 16
KAPPA = float(np.exp(-0.5))
DEPTH = 4

W_NAMES = ["ada_w", "ada_b", "norm_g", "fnet_wo", "fnet_bo", "rw_mix", "rw_wr", "rw_wk", "rw_wv", "rw_wo",
           "rw_w0", "rw_w1", "rw_w2", "rw_a0", "rw_a1", "rw_a2", "rw_v0", "rw_v1", "rw_v2", "rw_g1", "rw_g2",
           "rw_kk", "rw_ka", "rw_rk", "rw_lnx_w", "rw_lnx_b", "moe_router", "moe_wg", "moe_wu", "moe_wd", "final_g"]
W_SHAPES = {
    "ada_w": [4, 1024, 6144], "ada_b": [4, 6144], "norm_g": [4, 2, 1024], "fnet_wo": [2, 1024, 1024],
    "fnet_bo": [2, 1024], "rw_mix": [2, 6, 1024], "rw_wr": [2, 1024, 1024], "rw_wk": [2, 1024, 1024],
    "rw_wv": [2, 1024, 1024], "rw_wo": [2, 1024, 1024], "rw_w0": [2, 2, 1024], "rw_w1": [2, 2, 1024, 64],
    "rw_w2": [2, 2, 64, 1024], "rw_a0": [2, 2, 1024], "rw_a1": [2, 2, 1024, 64], "rw_a2": [2, 2, 64, 1024],
    "rw_v0": [1, 1024], "rw_v1": [1, 1024, 32], "rw_v2": [1, 32, 1024], "rw_g1": [2, 1024, 128],
    "rw_g2": [2, 128, 1024], "rw_kk": [2, 1024], "rw_ka": [2, 1024], "rw_rk": [2, 1024],
    "rw_lnx_w": [2, 1024], "rw_lnx_b": [2, 1024], "moe_router": [4, 1024, 16],
    "moe_wg": [4, 16, 1024, 1024], "moe_wu": [4, 16, 1024, 1024], "moe_wd": [4, 16, 1024, 1024], "final_g": [1, 1024],
}


def make_consts():
    bf = ml_dtypes.bfloat16
    c = {}
    c["ident_f"] = np.eye(128, dtype=np.float32)
    c["ident_b"] = np.eye(128, dtype=np.float32).astype(bf)
    cc = np.arange(256)
    ang = 2 * np.pi * np.outer(cc, cc) / 256.0
    cs = np.concatenate([np.cos(ang), np.sin(ang)], axis=1) / 16.0
    c["cs_tab"] = cs.reshape(2, 128, 512).transpose(1, 0, 2).astype(bf).copy()
    for T in (TX, TC):
        t = np.arange(T)
        a = 2 * np.pi * ((np.outer(t, t)) % T) / T
        tab = np.stack([np.cos(a), -np.sin(a)], axis=1) / np.sqrt(T)
        c[f"tok_tab{T}"] = tab.astype(bf)
    p = np.arange(128)
    blk = (p[:, None] // 64 == p[None, :] // 64)
    c["blockones_f"] = blk.astype(np.float32)
    c["blkmask"] = blk.astype(np.float32).astype(bf)
    row = p[:, None] % 64
    col = p[None, :] % 64
    mT = np.zeros((2, 128, 256), np.float32)
    mA = np.zeros((2, 128, 128), np.float32)
    mT[0, :, :128] = blk & (col > row)
    mT[0, :, 128:] = blk & (col >= row)
    mT[1, :, :128] = blk & (col < row)
    mT[1, :, 128:] = blk & (col <= row)
    mA[0] = blk & (col < row)
    mA[1] = blk & (col > row)
    c["mT"] = mT.transpose(1, 0, 2).copy()
    c["mA"] = mA.transpose(1, 0, 2).copy()
    seg = np.ones((128, 256), np.float32)
    seg[:, ::64] = 0
    c["segmask"] = seg
    sel = np.zeros((128, 64), np.float32)
    sel[p, p % 64] = 1.0
    c["sel"] = sel
    return c


PER_LAYER = ("ada_w", "moe_wg", "moe_wu", "moe_wd")
NP2DT = {np.dtype(np.float32): F32, np.dtype(ml_dtypes.bfloat16): BF16, np.dtype(np.int32): I32}


class PT:
    def __init__(self, hf, hb, off, n, res):
        self.hf, self.hb, self.off, self.n = hf, hb, off, n
        self.res = res

    def f(self, a=0, b=None, rows=128):
        b = self.n if b is None else b
        return self.hf[0:rows, self.off + a:self.off + b]

    def b(self, a=0, b=None, rows=128):
        b = 2 * self.n if b is None else b
        return self.hb[0:rows, 2 * self.off + a:2 * self.off + b]


class Multi:
    def __init__(self, aps):
        self.aps = aps

    def __getitem__(self, k):
        if isinstance(k, tuple):
            return self.aps[k[0]][k[1:]]
        return self.aps[k]


class Ctx:
    def __init__(self, nc, es, consts, debug):
        self.nc = nc
        self.es = es
        self.P = Prog(nc, es)
        self.debug = debug
        self.dram = {}
        self.wdecl = []
        self.consts_np = consts
        self.banks = []
        for i in range(8):
            h = es.enter_context(nc.psum_tensor(f"bank{i}", [128, 512], F32))
            self.banks.append((h, h.bitcast(BF16), Res(f"bank{i}", excl=True)))
        self.set_psum("full")
        self.uid = 0

    def set_psum(self, mode):
        self.ps_tiles = []
        for i, (hf, hb, res) in enumerate(self.banks):
            self.ps_tiles.append(PT(hf, hb, 0, 512, res))
        self.ps_i = 0

    def ps(self):
        t = self.ps_tiles[self.ps_i % len(self.ps_tiles)]
        self.ps_i += 1
        return t

    def din(self, name, shape, dt):
        self.dram[name] = self.nc.dram_tensor(name, list(shape), dt, kind="ExternalInput").ap()
        return self.dram[name]

    def w(self, name, layer=None):
        if layer is not None and name in PER_LAYER:
            key = f"{name}_{layer}"
            if key not in self.dram:
                self.din(key, W_SHAPES[name][1:], F32)
                self.wdecl.append((key, name, layer))
            return self.dram[key]
        if name not in self.dram:
            self.din(name, W_SHAPES[name], F32)
            self.wdecl.append((name, name, None))
        ap = self.dram[name]
        return ap if layer is None else ap[layer]

    def dout(self, name, shape, dt):
        self.dram[name] = self.nc.dram_tensor(name, list(shape), dt, kind="ExternalOutput").ap()
        return self.dram[name]

    def dscr(self, name, shape, dt):
        self.dram[name] = self.nc.dram_tensor(name, list(shape), dt).ap()
        return self.dram[name]

    def sb(self, st, name, shape, dt):
        return self.P.sb(st, name, shape, dt)

    def X(self, eng, name, ins, outs, **kw):
        e = self.P.eng[eng]
        return self.P.op(eng, lambda: getattr(e, name)(**kw), ins=ins, outs=outs)

    def mm(self, ps, out, lhsT, rhs, ins, start=True, stop=True):
        return self.P.op(PE, lambda: self.nc.tensor.matmul(out, lhsT=lhsT, rhs=rhs, start=start, stop=stop),
                         ins=ins, outs=[ps])

    def tr(self, ps, out, in_, ident, ins):
        return self.P.op(PE, lambda: self.nc.tensor.transpose(out, in_, ident), ins=ins, outs=[ps])

    def Dm(self, q, ins, outs, **kw):
        e = self.P.eng[q]
        return self.P.dma(q, lambda: e.dma_start(**kw), ins=ins, outs=outs)

    def DI(self, ins, outs, **kw):
        return self.P.dma(POOL, lambda: self.nc.gpsimd.indirect_dma_start(**kw), ins=ins, outs=outs)

    def act(self, ins, outs, **kw):
        return self.X(ACT, "activation", ins, outs, **kw)

    def barrier(self):
        self.P.barrier()


def load_consts(K, st):
    nc = K.nc
    C = {}
    for name in ["ident_f", "ident_b", "blockones_f", "blkmask", "mT", "mA", "segmask", "sel", "cs_tab"]:
        arr = K.consts_np[name]
        d = K.din("c_" + name, arr.shape, NP2DT[arr.dtype])
        t = K.sb(st, name, arr.shape, NP2DT[arr.dtype])
        K.Dm(SP, [], [t], out=t[:], in_=d)
        C[name] = t
    for T in (TX, TC):
        arr = K.consts_np[f"tok_tab{T}"]
        K.din(f"c_tok_tab{T}", arr.shape, BF16)
    eps = K.sb(st, "eps", [128, 4], F32)
    K.X(DVE, "memset", [], [eps], ap=eps[:, 0:1], constant=1e-6)
    K.X(DVE, "memset", [], [eps], ap=eps[:, 1:2], constant=1e-12)
    K.X(DVE, "memset", [], [eps], ap=eps[:, 2:3], constant=64e-5)
    K.X(DVE, "memset", [], [eps], ap=eps[:, 3:4], constant=0.0)
    C["eps"] = eps
    K.C = C


def phase_adaln(K, st_global, n_layers):
    nc, C = K.nc, K.C
    modA = K.sb(st_global, "modA", [128, DEPTH, 48, 3], F32)
    K.modA = modA
    modrow = K.dscr("modrow", [DEPTH, 3, 6144], F32)
    ada_b = K.w("ada_b")
    with contextlib.ExitStack() as st:
        crow = K.sb(st, "crow", [3, 1024], F32)
        K.Dm(SP, [], [crow], out=crow[0:2, :], in_=K.dram["c"])
        K.Dm(SP, [crow], [crow], out=crow[2:3, :], in_=K.dram["c_ctx"])
        srow = K.sb(st, "srow", [3, 1024], F32)
        K.act([crow], [srow], out=srow[:], in_=crow[:], func=AF.Silu)
        sT = K.sb(st, "sT", [128, 8, 3], BF16)
        for ch in range(8):
            ps = K.ps()
            K.tr(ps, ps.f(0, 3), srow[0:3, ch * 128:(ch + 1) * 128], C["ident_f"][0:3, 0:3], [srow, C["ident_f"]])
            K.X(DVE, "tensor_copy", [ps], [sT], out=sT[:, ch, :], in_=ps.f(0, 3))
        wt = [K.sb(st, f"adaw{i}", [128, 8, 512], BF16) for i in range(2)]
        brow = [K.sb(st, f"adab{i}", [3, 512], F32) for i in range(2)]
        rows = [K.sb(st, f"adar{i}", [3, 512], F32) for i in range(2)]
        it = 0
        for i in range(n_layers):
            for cb in range(12):
                w = wt[it % 2]
                bb = brow[it % 2]
                rr = rows[it % 2]
                it += 1
                K.Dm(POOL, [], [w], out=w[:], in_=K.w("ada_w", i)[:, cb * 512:(cb + 1) * 512].rearrange("(kc p) n -> p kc n", p=128))
                K.Dm(SP, [], [bb], out=bb[:], in_=ada_b[i:i + 1, cb * 512:(cb + 1) * 512].broadcast_to([3, 512]))
                ps = K.ps()
                for kc in range(8):
                    K.mm(ps, ps.f(0, 512, rows=3), sT[:, kc, :], w[:, kc, :], [sT, w], start=(kc == 0), stop=(kc == 7))
                K.X(DVE, "tensor_tensor", [ps, bb], [rr], out=rr[:], in0=ps.f(0, 512, rows=3), in1=bb[:], op=ALU.add)
                K.Dm(SP, [rr], [], out=modrow[i, :, cb * 512:(cb + 1) * 512], in_=rr[:])
                ps2 = K.ps()
                for oc in range(4):
                    K.tr(ps2, ps2.f(oc * 4, oc * 4 + 3), rr[0:3, oc * 128:(oc + 1) * 128], C["ident_f"][0:3, 0:3], [rr, C["ident_f"]])
                K.X(ACT, "copy", [ps2], [modA], out=modA[:, i, cb * 4:(cb + 1) * 4, :],
                    in_=ps2.f(0, 16).rearrange("p (a b) -> p a b", b=4)[:, :, 0:3])
    K.barrier()


def load_cols(K, st, name, row_aps):
    C = K.C
    n = len(row_aps)
    out = K.sb(st, name, [128, 8, n], F32)
    with contextlib.ExitStack() as s2:
        rows = K.sb(s2, name + "_rows", [n, 1024], F32)
        for j, ap in enumerate(row_aps):
            K.Dm(SP, [rows] if j else [], [rows], out=rows[j:j + 1, :], in_=ap)
        for ch in range(8):
            ps = K.ps()
            K.tr(ps, ps.f(0, n), rows[0:n, ch * 128:(ch + 1) * 128], C["ident_f"][0:n, 0:n], [rows, C["ident_f"]])
            K.X(DVE, "tensor_copy", [ps], [out], out=out[:, ch, :], in_=ps.f(0, n))
        K.barrier()
    return out


def mod_scalars(K, st, i, which_sc, which_sh, ng_cols, name, ng_idx=0):
    modA = K.modA
    scl = K.sb(st, name + "_scl", [128, 8, 3], F32)
    sh = K.sb(st, name + "_sh", [128, 8, 3], F32)
    K.X(DVE, "tensor_scalar", [modA], [scl], out=scl[:], in0=modA[:, i, which_sc * 8:(which_sc + 1) * 8, :],
        scalar1=1.0, scalar2=None, op0=ALU.add)
    K.X(DVE, "tensor_tensor", [scl, ng_cols], [scl], out=scl[:], in0=scl[:], in1=ng_cols[:, :, ng_idx:ng_idx + 1].broadcast_to([128, 8, 3]),
        op=ALU.mult)
    K.X(DVE, "tensor_copy", [modA], [sh], out=sh[:], in_=modA[:, i, which_sh * 8:(which_sh + 1) * 8, :])
    return scl, sh


def norm_hT(K, st, src, T, scl, sh, j, hT, tmp):
    nc, C = K.nc, K.C
    xin, xnb, stat = tmp
    nt = T // 128
    K.Dm(SP, [], [xin[0]], out=xin[0][:], in_=src[0:128, :])
    for tt in range(nt):
        xt = xin[tt % 2]
        if tt + 1 < nt:
            K.Dm(SP, [], [xin[(tt + 1) % 2]], out=xin[(tt + 1) % 2][:], in_=src[(tt + 1) * 128:(tt + 2) * 128, :])
        xb = xnb[tt % 2]
        s = stat[tt % 2]
        K.act([xt], [xb, s], out=xb[:], in_=xt[:], func=AF.Square, accum_out=s[:, 0:1])
        K.act([s, C["eps"]], [s], out=s[:, 1:2], in_=s[:, 0:1], func=AF.Sqrt, bias=C["eps"][:, 0:1], scale=1.0 / D)
        K.X(DVE, "reciprocal", [s], [s], out=s[:, 2:3], in_=s[:, 1:2])
        K.act([xt, s], [xb], out=xb[:], in_=xt[:], func=AF.Copy, scale=s[:, 2:3])
        for hf in range(2):
            ps = K.ps()
            for c4 in range(4):
                ch = hf * 4 + c4
                K.tr(ps, ps.b(c4 * 128, (c4 + 1) * 128), xb[:, ch * 128:(ch + 1) * 128], C["ident_b"][:], [xb, C["ident_b"]])
            for c4 in range(4):
                ch = hf * 4 + c4
                if hf == 0:
                    K.X(DVE, "tensor_scalar", [ps, scl, sh], [hT], out=hT[:, ch, tt * 128:(tt + 1) * 128],
                        in0=ps.b(c4 * 128, (c4 + 1) * 128), scalar1=scl[:, ch, j:j + 1], scalar2=sh[:, ch, j:j + 1],
                        op0=ALU.mult, op1=ALU.add)
                else:
                    K.act([ps, scl, sh], [hT], out=hT[:, ch, tt * 128:(tt + 1) * 128], in_=ps.b(c4 * 128, (c4 + 1) * 128),
                          func=AF.Identity, scale=scl[:, ch, j:j + 1], bias=sh[:, ch, j:j + 1])


def norm_tmp(K, st):
    xin = [K.sb(st, f"nx{i}", [128, 1024], F32) for i in range(2)]
    xnb = [K.sb(st, f"nb{i}", [128, 1024], BF16) for i in range(2)]
    stat = [K.sb(st, f"ns{i}", [128, 4], F32) for i in range(2)]
    return xin, xnb, stat


def seq_list(K, need_ctx):
    l = [("x", b, TX, K.dram["xs"][b], b) for b in range(NBC)]
    if need_ctx:
        l += [("c", b, TC, K.dram["cs"][b], 2) for b in range(NBC)]
    return l


def bcast_row(K, t, row_ap):
    n = row_ap.shape[-1]
    K.Dm(SP, [], [t], out=t[:], in_=row_ap.broadcast_to([128, n]))


def proj_residual(K, st, fT, T, wo, res_dram, g1bc, gb, tmp):
    xh, t1, t2 = tmp
    it = 0
    for tt in range(T // 128):
        for dh in range(2):
            k = it % 2
            it += 1
            sl = slice(dh * 512, (dh + 1) * 512)
            K.Dm(SP, [], [xh[k]], out=xh[k][:], in_=res_dram[tt * 128:(tt + 1) * 128, sl])
            ps = K.ps()
            for fc in range(8):
                K.mm(ps, ps.f(), fT[:, fc, tt * 128:(tt + 1) * 128], wo[:, fc, sl], [fT, wo], start=(fc == 0), stop=(fc == 7))
            K.X(DVE, "tensor_tensor", [ps, g1bc], [t1[k]], out=t1[k][:], in0=ps.f(), in1=g1bc[:, sl], op=ALU.mult)
            if gb is not None:
                K.X(POOL, "tensor_tensor", [xh[k], gb], [xh[k]], out=xh[k][:], in0=xh[k][:], in1=gb[:, sl], op=ALU.add)
            K.X(POOL, "tensor_tensor", [xh[k], t1[k]], [t2[k]], out=t2[k][:], in0=xh[k][:], in1=t1[k][:], op=ALU.add)
            K.Dm(ACT, [t2[k]], [], out=res_dram[tt * 128:(tt + 1) * 128, sl], in_=t2[k][:])


def proj_tmp(K, st):
    return ([K.sb(st, f"pxh{i}", [128, 512], F32) for i in range(2)],
            [K.sb(st, f"pt1{i}", [128, 512], F32) for i in range(2)],
            [K.sb(st, f"pt2{i}", [128, 512], F32) for i in range(2)])


def phase_fnet(K, i, fi, need_ctx):
    nc, C = K.nc, K.C
    with contextlib.ExitStack() as st:
        ng = load_cols(K, st, "ng", [K.w("norm_g")[i, 0:1, :]])
        scl, sh = mod_scalars(K, st, i, 1, 0, ng, "f")
        wo = K.sb(st, "fwo", [128, 8, 1024], BF16)
        K.Dm(POOL, [], [wo], out=wo[:], in_=K.w("fnet_wo")[fi].rearrange("(kc p) n -> p kc n", p=128))
        bobc = K.sb(st, "bobc", [128, 1024], F32)
        bcast_row(K, bobc, K.w("fnet_bo")[fi:fi + 1, :])
        hT = K.sb(st, "hT", [128, 8, TX], BF16)
        XCS = K.sb(st, "XCS", [128, 16, 2, 512], BF16)
        tabs = [K.sb(st, f"tab{k}", [128, 16, 2, 256], BF16) for k in range(2)]
        g1bc = K.sb(st, "g1bc", [128, 1024], F32)
        gb = K.sb(st, "gb", [128, 1024], F32)
        ntmp = norm_tmp(K, st)
        ptmp = proj_tmp(K, st)
        tab_it = 0
        for (kind, b, T, res, j) in seq_list(K, need_ctx):
            nt = T // 128
            tokt = K.dram[f"c_tok_tab{T}"]
            bcast_row(K, g1bc, K.dram["modrow"][i, j:j + 1, 2 * 1024:3 * 1024])
            K.X(POOL, "tensor_tensor", [g1bc, bobc], [gb], out=gb[:], in0=g1bc[:], in1=bobc[:], op=ALU.mult)
            norm_hT(K, st, res, T, scl, sh, j, hT, ntmp)
            fT = hT
            for half in range(2):
                for tt in range(nt):
                    for gg in range(2):
                        g = 2 * half + gg
                        ps = K.ps()
                        for ci in range(2):
                            K.mm(ps, ps.f(), hT[:, 2 * g + ci, tt * 128:(tt + 1) * 128], C["cs_tab"][:, ci, :],
                                 [hT, C["cs_tab"]], start=(ci == 0), stop=(ci == 1))
                        eng = DVE if (tt + gg) % 2 == 0 else ACT
                        if eng == DVE:
                            K.X(DVE, "tensor_copy", [ps], [XCS], out=XCS[:, tt, :, gg * 256:(gg + 1) * 256],
                                in_=ps.f().rearrange("p (a c) -> p a c", a=2))
                        else:
                            K.X(ACT, "copy", [ps], [XCS], out=XCS[:, tt, :, gg * 256:(gg + 1) * 256],
                                in_=ps.f().rearrange("p (a c) -> p a c", a=2))
                NBK = 256
                for tb in range(T // NBK):
                    tab = tabs[tab_it % 2]
                    tab_it += 1
                    for a in range(2):
                        K.Dm(SP, [tab] if a else [], [tab], out=tab[:, 0:nt, a, :],
                             in_=tokt[:, a, tb * NBK:(tb + 1) * NBK].rearrange("(tt p) n -> p tt n", p=128))
                    for fq in range(4):
                        fc = 4 * half + fq
                        ps = K.ps()
                        for tt in range(nt):
                            for a in range(2):
                                K.mm(ps, ps.f(0, NBK), XCS[:, tt, a, fq * 128:(fq + 1) * 128], tab[:, tt, a, :],
                                     [XCS, tab], start=(tt == 0 and a == 0), stop=(tt == nt - 1 and a == 1))
                        if fq % 2 == 0:
                            K.X(DVE, "tensor_copy", [ps], [fT], out=fT[:, fc, tb * NBK:(tb + 1) * NBK], in_=ps.f(0, NBK))
                        else:
                            K.X(ACT, "copy", [ps], [fT], out=fT[:, fc, tb * NBK:(tb + 1) * NBK], in_=ps.f(0, NBK))
            proj_residual(K, st, fT, T, wo, res, g1bc, gb, ptmp)
        K.barrier()


def phase_final(K):
    nc, C = K.nc, K.C
    with contextlib.ExitStack() as st:
        fg = K.sb(st, "fgbc", [128, 1024], F32)
        bcast_row(K, fg, K.w("final_g")[0:1, :])
        xin = [K.sb(st, f"fx{i}", [128, 1024], F32) for i in range(2)]
        xo = [K.sb(st, f"fo{i}", [128, 1024], F32) for i in range(2)]
        stat = [K.sb(st, f"fs{i}", [128, 4], F32) for i in range(2)]
        it = 0
        for b in range(NBC):
            for tt in range(TX // 128):
                k = it % 2
                it += 1
                K.Dm(SP, [], [xin[k]], out=xin[k][:], in_=K.dram["xs"][b, tt * 128:(tt + 1) * 128, :])
                s = stat[k]
                K.act([xin[k]], [xo[k], s], out=xo[k][:], in_=xin[k][:], func=AF.Square, accum_out=s[:, 0:1])
                K.act([s, C["eps"]], [s], out=s[:, 1:2], in_=s[:, 0:1], func=AF.Sqrt, bias=C["eps"][:, 0:1], scale=1.0 / D)
                K.X(DVE, "reciprocal", [s], [s], out=s[:, 2:3], in_=s[:, 1:2])
                K.X(DVE, "scalar_tensor_tensor", [xin[k], s, fg], [xo[k]], out=xo[k][:], in0=xin[k][:], scalar=s[:, 2:3],
                    in1=fg[:], op0=ALU.mult, op1=ALU.mult)
                K.Dm(ACT, [xo[k]], [], out=K.dram["out"][b, tt * 128:(tt + 1) * 128, :], in_=xo[k][:])
        K.barrier()


def build(n_layers=DEPTH, debug=False, phases=("fnet", "rwkv", "moe"), moe_route_only=False, moe_no_ctx=False, rw_stop=99, prep_stop=-1, prep_cnt=1):
    nc = bass.Bass("TRN2", target_bir_lowering=False)
    consts = make_consts()
    es = contextlib.ExitStack()
    dbg_names = []
    with es:
        K = Ctx(nc, es, consts, debug)
        K.moe_route_only = moe_route_only
        K.moe_no_ctx = moe_no_ctx
        K.rw_stop = rw_stop
        K.prep_stop = prep_stop
        K.prep_cnt = prep_cnt
        K.din("x", [NBC, TX, D], F32)
        K.din("c", [NBC, D], F32)
        K.din("ctx", [NBC, TC, D], F32)
        K.din("c_ctx", [1, D], F32)
        K.dout("out", [NBC, TX, D], F32)
        K.dram["xs"] = Multi([K.dscr(f"xs{b}", [TX, D], F32) for b in range(NBC)])
        K.dram["cs"] = Multi([K.dscr(f"cs{b}", [TC, D], F32) for b in range(NBC)])

        def dbg(name):
            if not debug:
                return
            ox = K.dout("dbg_x_" + name, [NBC, TX, D], F32)
            oc = K.dout("dbg_c_" + name, [NBC, TC, D], F32)
            for b in range(NBC):
                K.Dm(SP, [], [], out=ox[b], in_=K.dram["xs"][b])
                K.Dm(SP, [], [], out=oc[b], in_=K.dram["cs"][b])
            dbg_names.append(name)
            K.barrier()

        load_consts(K, es)
        phase_adaln(K, es, n_layers)
        if debug:
            om = K.dout("dbg_modrow", [DEPTH, 3, 6144], F32)
            K.Dm(SP, [], [], out=om, in_=K.dram["modrow"])
        for b in range(NBC):
            K.Dm(SP, [], [], out=K.dram["xs"][b], in_=K.dram["x"][b])
            K.Dm(SP, [], [], out=K.dram["cs"][b], in_=K.dram["ctx"][b])
        K.barrier()
        try:
            for i in range(n_layers):
                need_ctx = i < DEPTH - 1
                if i % 2 == 0:
                    if "fnet" in phases:
                        phase_fnet(K, i, i // 2, need_ctx)
                else:
                    if "rwkv" in phases:
                        phase_rwkv(K, i, i // 2, need_ctx)
                dbg(f"mix{i}")
                if "moe" in phases:
                    phase_moe(K, i, need_ctx)
                dbg(f"moe{i}")
        except _Stop:
            pass
        K.P.muted = False
        K.barrier()
        phase_final(K)
        print(f"[build] instructions={K.P.n_inst} waits={K.P.n_wait}", flush=True)
    return nc, consts, dbg_names, K.wdecl


def core_inputs(inputs, consts, core, wdecl):
    b0 = core * NBC
    m = {
        "x": np.ascontiguousarray(inputs["x"][b0:b0 + NBC], dtype=np.float32),
        "c": np.ascontiguousarray(inputs["c"][b0:b0 + NBC], dtype=np.float32),
        "ctx": np.ascontiguousarray(inputs["ctx"][b0:b0 + NBC], dtype=np.float32),
        "c_ctx": np.ascontiguousarray(np.asarray(inputs["c_ctx"], dtype=np.float32).reshape(1, D)),
    }
    for key, n, layer in wdecl:
        a = np.asarray(inputs[n], dtype=np.float32).reshape(W_SHAPES[n])
        m[key] = np.ascontiguousarray(a if layer is None else a[layer])
    for k, v in consts.items():
        m["c_" + k] = v
    return m


def kernel(**inputs):
    nc, consts, _, wdecl = build()
    n_cores = 8
    in_maps = [core_inputs(inputs, consts, c, wdecl) for c in range(n_cores)]
    res = run_bass_kernel_spmd(nc, in_maps, core_ids=list(range(n_cores)))
    return np.concatenate([np.asarray(r["out"]) for r in res.results], axis=0).astype(np.float32)


def IOA(ap):
    return bass.IndirectOffsetOnAxis(ap=ap, axis=0)


def topk_rows(K, aff, work, vals, idxs, rounds):
    cur = aff
    for r in range(rounds):
        sl = slice(r * 8, (r + 1) * 8)
        K.X(DVE, "max", [cur], [vals], out=vals[:, sl], in_=cur[:])
        K.X(DVE, "max_index", [vals, cur], [idxs], out=idxs[:, sl], in_max=vals[:, sl], in_values=cur[:])
        if r + 1 < rounds:
            K.X(DVE, "match_replace", [vals, cur], [work], out=work[:], in_to_replace=vals[:, sl], in_values=cur[:],
                imm_value=-1.0)
            cur = work


def phase_moe(K, i, need_ctx):
    nc, C = K.nc, K.C
    if getattr(K, "moe_no_ctx", False):
        need_ctx = False
    if "hsrc_x" not in K.dram:
        K.dram["hsrc_x"] = Multi([K.dscr(f"hsrc_x{b}", [TX, D], BF16) for b in range(NBC)])
        K.dram["hsrc_c"] = Multi([K.dscr(f"hsrc_c{b}", [TC, D], BF16) for b in range(NBC)])
    hsrc = {"x": K.dram["hsrc_x"], "c": K.dram["hsrc_c"]}
    resid = {"x": K.dram["xs"], "c": K.dram["cs"]}
    kinds = [("x", TX, 256)] + ([("c", TC, 32)] if need_ctx else [])
    modrow = K.dram["modrow"]
    with contextlib.ExitStack() as st0:
        idxT = K.sb(st0, "idxT", [128, 2, 48], I32)
        gateT = K.sb(st0, "gateT", [128, 2, 48], F32)
        idxTc = K.sb(st0, "idxTc", [32, 48], I32)
        gcT = K.sb(st0, "gcT", [32, 48], F32)
        g2bc = [K.sb(st0, f"g2bc{j}", [128, 1024], F32) for j in range(3)]
        for j in range(3):
            bcast_row(K, g2bc[j], modrow[i, j:j + 1, 5 * 1024:6 * 1024])
        with contextlib.ExitStack() as st:
            ngbc = K.sb(st, "ngbc", [128, 1024], F32)
            bcast_row(K, ngbc, K.w("norm_g")[i, 1:2, :])
            sclbc, shbc = [], []
            for j in range(3):
                a = K.sb(st, f"scl2bc{j}", [128, 1024], F32)
                b_ = K.sb(st, f"sh2bc{j}", [128, 1024], F32)
                bcast_row(K, a, modrow[i, j:j + 1, 4 * 1024:5 * 1024])
                bcast_row(K, b_, modrow[i, j:j + 1, 3 * 1024:4 * 1024])
                K.X(DVE, "scalar_tensor_tensor", [a, ngbc], [a], out=a[:], in0=a[:], scalar=1.0, in1=ngbc[:],
                    op0=ALU.add, op1=ALU.mult)
                sclbc.append(a)
                shbc.append(b_)
            router = K.sb(st, "router", [128, 8, 16], F32)
            K.Dm(SP, [], [router], out=router[:], in_=K.w("moe_router")[i].rearrange("(kc p) n -> p kc n", p=128))
            xin = [K.sb(st, f"mx{k}", [128, 1024], F32) for k in range(2)]
            hf = [K.sb(st, f"mh{k}", [128, 1024], F32) for k in range(2)]
            hb = [K.sb(st, f"mhb{k}", [128, 1024], BF16) for k in range(2)]
            hTt = [K.sb(st, f"mhT{k}", [128, 8, 128], F32) for k in range(2)]
            stat = [K.sb(st, f"ms{k}", [128, 8], F32) for k in range(2)]
            ex = [K.sb(st, f"mex{k}", [128, 16], F32) for k in range(2)]
            affp = [K.sb(st, f"affp{k}", [128, 48], F32) for k in range(2)]
            for k in range(2):
                K.X(DVE, "memset", [], [affp[k]], ap=affp[k][:], constant=0.0)
            it = 0
            for (kind, T, cap) in kinds:
                nt = T // 128
                affT = K.sb(st, f"affT{kind}", [48, T], F32)
                work = K.sb(st, f"work{kind}", [48, T], F32)
                vals = K.sb(st, f"vals{kind}", [48, max(cap, 64)], F32)
                idxs = K.sb(st, f"idxs{kind}", [48, max(cap, 64)], U32)
                idxf = K.sb(st, f"idxf{kind}", [48, max(cap, 64)], F32)
                for tt in range(nt):
                    ap_ = affp[tt % 2]
                    for si in range(NBC):
                        j = si if kind == "x" else 2
                        k = it % 2
                        it += 1
                        xt, h, s = xin[k], hf[k], stat[k]
                        K.Dm(SP, [], [xt], out=xt[:], in_=resid[kind][si, tt * 128:(tt + 1) * 128, :])
                        K.act([xt], [hb[k], s], out=hb[k][:], in_=xt[:], func=AF.Square, accum_out=s[:, 0:1])
                        K.act([s, C["eps"]], [s], out=s[:, 1:2], in_=s[:, 0:1], func=AF.Sqrt, bias=C["eps"][:, 0:1], scale=1.0 / D)
                        K.X(DVE, "reciprocal", [s], [s], out=s[:, 2:3], in_=s[:, 1:2])
                        K.X(DVE, "scalar_tensor_tensor", [xt, s, sclbc[j]], [h], out=h[:], in0=xt[:], scalar=s[:, 2:3],
                            in1=sclbc[j][:], op0=ALU.mult, op1=ALU.mult)
                        K.X(POOL, "tensor_tensor", [h, shbc[j]], [h], out=h[:], in0=h[:], in1=shbc[j][:], op=ALU.add)
                        K.X(ACT, "copy", [h], [hb[k]], out=hb[k][:], in_=h[:])
                        K.Dm(ACT, [hb[k]], [], out=hsrc[kind][si, tt * 128:(tt + 1) * 128, :], in_=hb[k][:])
                        for hh in range(2):
                            ps = K.ps()
                            for c4 in range(4):
                                ch = hh * 4 + c4
                                K.tr(ps, ps.f(c4 * 128, (c4 + 1) * 128), h[:, ch * 128:(ch + 1) * 128], C["ident_f"][:],
                                     [h, C["ident_f"]])
                            if hh == 0:
                                K.X(DVE, "tensor_copy", [ps], [hTt[k]], out=hTt[k][:, 0:4, :],
                                    in_=ps.f().rearrange("p (a c) -> p a c", a=4))
                            else:
                                K.X(ACT, "copy", [ps], [hTt[k]], out=hTt[k][:, 4:8, :],
                                    in_=ps.f().rearrange("p (a c) -> p a c", a=4))
                        ps = K.ps()
                        for kc in range(8):
                            K.mm(ps, ps.f(0, 16), hTt[k][:, kc, :], router[:, kc, :], [hTt[k], router],
                                 start=(kc == 0), stop=(kc == 7))
                        K.X(DVE, "tensor_reduce", [ps], [s], out=s[:, 3:4], in_=ps.f(0, 16), axis=AX.X, op=ALU.max)
                        K.X(DVE, "tensor_scalar", [s], [s], out=s[:, 4:5], in0=s[:, 3:4], scalar1=-1.0, scalar2=None, op0=ALU.mult)
                        K.act([ps, s], [ex[k], s], out=ex[k][:], in_=ps.f(0, 16), func=AF.Exp, bias=s[:, 4:5], scale=1.0,
                              accum_out=s[:, 5:6])
                        K.X(DVE, "reciprocal", [s], [s], out=s[:, 6:7], in_=s[:, 5:6])
                        K.X(DVE, "tensor_scalar", [ex[k], s], [ap_], out=ap_[:, si * 32:si * 32 + 16], in0=ex[k][:],
                            scalar1=s[:, 6:7], scalar2=None, op0=ALU.mult)
                    ps = K.ps()
                    K.tr(ps, ps.f(0, 128, rows=48), ap_[:, 0:48], C["ident_f"][:], [ap_, C["ident_f"]])
                    K.X(ACT, "copy", [ps], [affT], out=affT[:, tt * 128:(tt + 1) * 128], in_=ps.f(0, 128, rows=48))
                topk_rows(K, affT, work, vals, idxs, cap // 8)
                if kind == "x":
                    K.X(DVE, "tensor_copy", [idxs], [idxf], out=idxf[:, 0:256], in_=idxs[:, 0:256])
                    for ch in range(2):
                        ps = K.ps()
                        K.tr(ps, ps.f(0, 48), idxf[0:48, ch * 128:(ch + 1) * 128], C["ident_f"][0:48, 0:48], [idxf, C["ident_f"]])
                        K.tr(ps, ps.f(64, 112), vals[0:48, ch * 128:(ch + 1) * 128], C["ident_f"][0:48, 0:48], [vals, C["ident_f"]])
                        K.X(DVE, "tensor_copy", [ps], [idxT], out=idxT[:, ch, :], in_=ps.f(0, 48))
                        K.X(DVE, "tensor_copy", [ps], [gateT], out=gateT[:, ch, :], in_=ps.f(64, 112))
                else:
                    K.X(DVE, "tensor_copy", [idxs], [idxf], out=idxf[:, 0:32], in_=idxs[:, 0:32])
                    ps = K.ps()
                    K.tr(ps, ps.f(0, 48, rows=32), idxf[0:48, 0:32], C["ident_f"][0:48, 0:48], [idxf, C["ident_f"]])
                    K.tr(ps, ps.f(64, 112, rows=32), vals[0:48, 0:32], C["ident_f"][0:48, 0:48], [vals, C["ident_f"]])
                    K.X(DVE, "tensor_copy", [ps], [idxTc], out=idxTc[:], in_=ps.f(0, 48, rows=32))
                    K.X(DVE, "tensor_copy", [ps], [gcT], out=gcT[:], in_=ps.f(64, 112, rows=32))
            K.barrier()
            if K.debug and i == 0:
                for nm, t, shp, dt in [("idxT", idxT, [128, 2, 48], I32), ("gateT", gateT, [128, 2, 48], F32),
                                       ("idxTc", idxTc, [32, 48], I32), ("gcT", gcT, [32, 48], F32)]:
                    o = K.dout("dbg_" + nm, shp, dt)
                    K.Dm(SP, [t], [], out=o, in_=t[:])
                o = K.dout("dbg_hsrc_x", [NBC, TX, D], BF16)
                for b in range(NBC):
                    K.Dm(SP, [], [], out=o[b], in_=hsrc["x"][b])
                K.barrier()
        if getattr(K, "moe_route_only", False):
            return
        with contextlib.ExitStack() as st:
            wbuf = [[K.sb(st, f"mw{k}{m}", [128, 8, 1024], BF16) for m in range(3)] for k in range(2)]
            xg = [[K.sb(st, f"xg{k}{m}", [128, 1024], BF16) for m in range(4)] for k in range(2)]
            xgc = [[K.sb(st, f"xgc{k}{b}", [32, 1024], BF16) for b in range(NBC)] for k in range(2)]
            xsT = [K.sb(st, f"xsT{k}", [128, 8, 576], BF16) for k in range(2)]
            hidT = K.sb(st, "hidT", [128, 8, 576], BF16)
            sg = [K.sb(st, f"sg{k}", [128, 576], F32) for k in range(2)]
            yo = [K.sb(st, f"yo{k}", [128, 1024], F32) for k in range(3)]
            wnames = ["moe_wg", "moe_wu", "moe_wd"]
            res_x = [Res(f"xs{b}") for b in range(NBC)]
            res_c = [Res(f"cs{b}") for b in range(NBC)]
            NCX = 576 if need_ctx else 512
            yo_it = 0
            sg_it = 0

            def load_w(e):
                k = e % 2
                for m in range(3):
                    K.Dm(POOL, [], [wbuf[k][m]], out=wbuf[k][m][:],
                         in_=K.w(wnames[m], i)[e].rearrange("(kc p) n -> p kc n", p=128))

            def gather(e):
                k = e % 2
                for si in range(NBC):
                    for ch in range(2):
                        col = si * 32 + e
                        K.DI([idxT], [xg[k][si * 2 + ch]], out=xg[k][si * 2 + ch][:], out_offset=None,
                             in_=hsrc["x"][si], in_offset=IOA(idxT[:, ch, col:col + 1]))
                if need_ctx:
                    for b in range(NBC):
                        K.DI([idxTc], [xgc[k][b]], out=xgc[k][b][:], out_offset=None, in_=hsrc["c"][b],
                             in_offset=IOA(idxTc[:, b * 32 + e:b * 32 + e + 1]))

            load_w(0)
            gather(0)
            for e in range(NE):
                k = e % 2
                if e + 1 < NE:
                    load_w(e + 1)
                    gather(e + 1)
                wg, wu, wd = wbuf[k]
                for f in range(8):
                    ps = K.ps()
                    for t4 in range(4):
                        K.tr(ps, ps.b(t4 * 128, (t4 + 1) * 128), xg[k][t4][:, f * 128:(f + 1) * 128], C["ident_b"][:],
                             [xg[k][t4], C["ident_b"]])
                    if need_ctx:
                        for b in range(NBC):
                            K.tr(ps, ps.b(512 + b * 32, 544 + b * 32), xgc[k][b][:, f * 128:(f + 1) * 128], C["ident_b"][0:32, 0:32],
                                 [xgc[k][b], C["ident_b"]])
                    if f % 2 == 0:
                        K.X(DVE, "tensor_copy", [ps], [xsT[k]], out=xsT[k][:, f, 0:NCX], in_=ps.b(0, NCX))
                    else:
                        K.X(ACT, "copy", [ps], [xsT[k]], out=xsT[k][:, f, 0:NCX], in_=ps.b(0, NCX))
                for fc in range(8):
                    fs = slice(fc * 128, (fc + 1) * 128)
                    psG = K.ps()
                    for f in range(8):
                        K.mm(psG, psG.f(), wg[:, f, fs], xsT[k][:, f, 0:512], [wg, xsT[k]], start=(f == 0), stop=(f == 7))
                    psU = K.ps()
                    for f in range(8):
                        K.mm(psU, psU.f(), wu[:, f, fs], xsT[k][:, f, 0:512], [wu, xsT[k]], start=(f == 0), stop=(f == 7))
                    s_ = sg[sg_it % 2]
                    sg_it += 1
                    K.act([psG], [s_], out=s_[:, 0:512], in_=psG.f(), func=AF.Silu)
                    K.X(DVE, "tensor_tensor", [psU, s_], [hidT], out=hidT[:, fc, 0:512], in0=psU.f(), in1=s_[:, 0:512], op=ALU.mult)
                    if need_ctx:
                        psC = K.ps()
                        for f in range(8):
                            K.mm(psC, psC.f(0, 64), wg[:, f, fs], xsT[k][:, f, 512:576], [wg, xsT[k]], start=(f == 0), stop=(f == 7))
                        for f in range(8):
                            K.mm(psC, psC.f(64, 128), wu[:, f, fs], xsT[k][:, f, 512:576], [wu, xsT[k]], start=(f == 0), stop=(f == 7))
                        K.act([psC], [s_], out=s_[:, 512:576], in_=psC.f(0, 64), func=AF.Silu)
                        K.X(DVE, "tensor_tensor", [psC, s_], [hidT], out=hidT[:, fc, 512:576], in0=psC.f(64, 128), in1=s_[:, 512:576], op=ALU.mult)
                for t4 in range(4):
                    si, ch = t4 // 2, t4 % 2
                    col = si * 32 + e
                    y = yo[yo_it % 3]
                    yo_it += 1
                    for dh in range(2):
                        sl = slice(dh * 512, (dh + 1) * 512)
                        ps = K.ps()
                        for fc in range(8):
                            K.mm(ps, ps.f(), hidT[:, fc, t4 * 128:(t4 + 1) * 128], wd[:, fc, sl], [hidT, wd],
                                 start=(fc == 0), stop=(fc == 7))
                        K.X(DVE, "scalar_tensor_tensor", [ps, gateT, g2bc[si]], [y], out=y[:, sl], in0=ps.f(),
                            scalar=gateT[:, ch, col:col + 1], in1=g2bc[si][:, sl], op0=ALU.mult, op1=ALU.mult)
                    K.DI([y, idxT], [res_x[si]], out=resid["x"][si], out_offset=IOA(idxT[:, ch, col:col + 1]),
                         in_=y[:], in_offset=None, compute_op=ALU.add)
                if need_ctx:
                    for b in range(NBC):
                        y = yo[yo_it % 3]
                        yo_it += 1
                        col = b * 32 + e
                        for dh in range(2):
                            sl = slice(dh * 512, (dh + 1) * 512)
                            ps = K.ps()
                            for fc in range(8):
                                K.mm(ps, ps.f(0, 512, rows=32), hidT[:, fc, 512 + b * 32:544 + b * 32], wd[:, fc, sl], [hidT, wd],
                                     start=(fc == 0), stop=(fc == 7))
                            K.X(DVE, "scalar_tensor_tensor", [ps, gcT, g2bc[2]], [y], out=y[0:32, sl], in0=ps.f(0, 512, rows=32),
                                scalar=gcT[:, col:col + 1], in1=g2bc[2][0:32, sl], op0=ALU.mult, op1=ALU.mult)
                        K.DI([y, idxTc], [res_c[b]], out=resid["c"][b], out_offset=IOA(idxTc[:, col:col + 1]),
                             in_=y[0:32, :], in_offset=None, compute_op=ALU.add)
            K.barrier()


NCH = TL // 64
CMIX, CW0, CA0, CKK, CKA, CRK, CV0, CNG, CLW, CLB = 0, 6, 8, 10, 11, 12, 13, 14, 15, 16


def rw_scratch(K):
    if hasattr(K, "rw"):
        return K.rw
    S = {}
    for d in range(2):
        for n in ("at", "bt", "kt", "rt"):
            S[f"{n}{d}"] = K.dscr(f"rw_{n}{d}", [NBC, D, TL], BF16)
        S[f"GL{d}"] = K.dscr(f"rw_GL{d}", [NBC, D, NCH], F32)
        S[f"y{d}"] = K.dscr(f"rw_y{d}", [NBC, D, TL], F32)
    S["v"] = K.dscr("rw_v", [NBC, D, TL], BF16)
    S["bv"] = K.dscr("rw_bv", [NBC, D, TL], F32)
    S["g"] = K.dscr("rw_g", [NBC, D, TL], F32)
    S["vfirst"] = K.dscr("rw_vfirst", [NBC, D, TL], F32)
    K.rw = S
    return S


def rwkv_cols(K, st, i, ri):
    w = K.w
    rows = [w("rw_mix")[ri, jj:jj + 1, :] for jj in range(6)]
    rows += [w("rw_w0")[ri, 0:1, :], w("rw_w0")[ri, 1:2, :], w("rw_a0")[ri, 0:1, :], w("rw_a0")[ri, 1:2, :]]
    rows += [w("rw_kk")[ri:ri + 1, :], w("rw_ka")[ri:ri + 1, :], w("rw_rk")[ri:ri + 1, :]]
    rows += [w("rw_v0")[0:1, :]]
    rows += [w("norm_g")[i, 0:1, :], w("rw_lnx_w")[ri:ri + 1, :], w("rw_lnx_b")[ri:ri + 1, :]]
    return load_cols(K, st, "rwc", rows)


class _Stop(Exception):
    pass


def rwkv_prep(K, i, ri):
    nc, C = K.nc, K.C

    def chk(k):
        if getattr(K, "prep_stop", -1) == k:
            K.prep_seen = getattr(K, "prep_seen", 0) + 1
            if K.prep_seen >= K.prep_cnt:
                K.P.muted = True
    S = rw_scratch(K)
    with contextlib.ExitStack() as st:
        cols = rwkv_cols(K, st, i, ri)
        omka = K.sb(st, "omka", [128, 8, 1], F32)
        K.X(DVE, "tensor_scalar", [cols], [omka], out=omka[:], in0=cols[:, :, CKA:CKA + 1], scalar1=-1.0, scalar2=1.0,
            op0=ALU.mult, op1=ALU.add)
        scl, sh = mod_scalars(K, st, i, 1, 0, cols, "r", ng_idx=CNG)
        wr, wk, wv = [K.sb(st, n, [128, 8, 1024], BF16) for n in ("wr", "wk", "wv")]
        for t, n in ((wr, "rw_wr"), (wk, "rw_wk"), (wv, "rw_wv")):
            K.Dm(POOL, [], [t], out=t[:], in_=K.w(n)[ri].rearrange("(kc p) n -> p kc n", p=128))
        w1c = K.sb(st, "w1c", [128, 8, 128], BF16)
        a1c = K.sb(st, "a1c", [128, 8, 128], BF16)
        g1w = K.sb(st, "g1w", [128, 8, 128], BF16)
        for d in range(2):
            K.Dm(POOL, [w1c] if d else [], [w1c], out=w1c[:, :, d * 64:(d + 1) * 64],
                 in_=K.w("rw_w1")[ri, d].rearrange("(kc p) n -> p kc n", p=128))
            K.Dm(POOL, [a1c] if d else [], [a1c], out=a1c[:, :, d * 64:(d + 1) * 64],
                 in_=K.w("rw_a1")[ri, d].rearrange("(kc p) n -> p kc n", p=128))
        K.Dm(POOL, [], [g1w], out=g1w[:], in_=K.w("rw_g1")[ri].rearrange("(kc p) n -> p kc n", p=128))
        w2a = K.sb(st, "w2a", [128, 1024], BF16)
        a2a = K.sb(st, "a2a", [128, 1024], BF16)
        g2w = K.sb(st, "g2w", [128, 1024], BF16)
        for d in range(2):
            K.Dm(POOL, [w2a] if d else [], [w2a], out=w2a[d * 64:(d + 1) * 64, :], in_=K.w("rw_w2")[ri, d])
            K.Dm(POOL, [a2a] if d else [], [a2a], out=a2a[d * 64:(d + 1) * 64, :], in_=K.w("rw_a2")[ri, d])
        K.Dm(POOL, [], [g2w], out=g2w[:], in_=K.w("rw_g2")[ri])
        if ri == 1:
            v1w = K.sb(st, "v1w", [128, 8, 32], BF16)
            v2w = K.sb(st, "v2w", [32, 1024], BF16)
            K.Dm(POOL, [], [v1w], out=v1w[:], in_=K.w("rw_v1")[0].rearrange("(kc p) n -> p kc n", p=128))
            K.Dm(POOL, [], [v2w], out=v2w[:], in_=K.w("rw_v2")[0])
        hT = K.sb(st, "rhT", [128, 8, TX], BF16)
        xx = K.sb(st, "rxx", [128, 8, 256], BF16)
        xtmp = K.sb(st, "rxtmp", [128, 8, 256], BF16)
        xj = [K.sb(st, f"rxj{jj}", [128, 8, 256], BF16) for jj in range(6)]
        tw = K.sb(st, "rtw", [128, 256], BF16)
        ta = K.sb(st, "rta", [128, 256], BF16)
        tg = K.sb(st, "rtg", [128, 256], BF16)
        tgf = K.sb(st, "rtgf", [128, 256], F32)
        hcols = K.sb(st, "hcols", [128, 8, 17], F32)
        K.X(DVE, "tensor_scalar", [cols], [hcols], out=hcols[:], in0=cols[:], scalar1=0.5, scalar2=None, op0=ALU.mult)
        tv = K.sb(st, "rtv", [32, 256], BF16)
        ntmp = norm_tmp(K, st)

        def f32t(n, w_=256):
            return K.sb(st, n, [128, w_], F32)
        RK, VG = f32t("RK", 512), f32t("VG", 512)
        SGW, ASIG = K.sb(st, "SGW", [128, 2, 256], F32), K.sb(st, "ASIG", [128, 2, 256], F32)
        SV, KR, SQ_, RN, KKN, NKK, VF, DV = [f32t(n) for n in ("SV", "KR", "SQ", "RN", "KKN", "NKK", "VF", "DV")]
        TMP, BVEC, CUM, CUMR, T3, E1, E2, E3 = [f32t(n) for n in ("TMP", "BVEC", "CUM", "CUMR", "T3", "E1", "E2", "E3")]
        KD = [f32t("KD0"), f32t("KD1")]
        RKR, KS, PR, BVV = [f32t(n) for n in ("RKR", "KS", "PR", "BVV")]
        VB16 = K.sb(st, "VB16", [128, 256], BF16)
        stg = {(n, d): K.sb(st, f"stg_{n}{d}", [128, 256], BF16) for n in ("at", "bt", "kt", "rt") for d in range(2)}
        GLt = [K.sb(st, f"GLt{d}", [128, 4], F32) for d in range(2)]

        def pool_tt(out_t, out, a_t, a, b_t, b, op):
            K.X(POOL, "tensor_tensor", [a_t, b_t], [out_t], out=out, in0=a, in1=b, op=op)

        def shift_block(kind, T, t0):
            def sub(c0, c1, oa, ob, ia0, ia1, ib0, ib1):
                pool_tt(xx, xx[:, c0:c1, oa:ob], hT, hT[:, c0:c1, ia0:ia1], hT, hT[:, c0:c1, ib0:ib1], ALU.subtract)

            def neg(c0, c1, oa, ob, ia, ib):
                K.X(POOL, "tensor_scalar", [hT], [xx], out=xx[:, c0:c1, oa:ob], in0=hT[:, c0:c1, ia:ib], scalar1=-1.0,
                    scalar2=None, op0=ALU.mult)
            if kind == "c":
                sub(0, 4, 1, 256, 0, 255, 1, 256)
                neg(0, 4, 0, 1, 0, 1)
                sub(4, 8, 0, 255, 1, 256, 0, 255)
                neg(4, 8, 255, 256, 255, 256)
                return
            hv = lambda c0, c1: hT[:, c0:c1, t0:t0 + 256].rearrange("p c (r w) -> p c r w", w=64)
            xv = lambda c0, c1: xx[:, c0:c1, :].rearrange("p c (r w) -> p c r w", w=64)
            K.X(POOL, "tensor_tensor", [hT], [xx], out=xv(0, 2)[:, :, :, 1:64], in0=hv(0, 2)[:, :, :, 0:63],
                in1=hv(0, 2)[:, :, :, 1:64], op=ALU.subtract)
            K.X(POOL, "tensor_scalar", [hT], [xx], out=xv(0, 2)[:, :, :, 0:1], in0=hv(0, 2)[:, :, :, 0:1], scalar1=-1.0,
                scalar2=None, op0=ALU.mult)
            K.X(POOL, "tensor_tensor", [hT], [xx], out=xv(2, 4)[:, :, :, 0:63], in0=hv(2, 4)[:, :, :, 1:64],
                in1=hv(2, 4)[:, :, :, 0:63], op=ALU.subtract)
            K.X(POOL, "tensor_scalar", [hT], [xx], out=xv(2, 4)[:, :, :, 63:64], in0=hv(2, 4)[:, :, :, 63:64], scalar1=-1.0,
                scalar2=None, op0=ALU.mult)
            if t0 == 0:
                neg(4, 6, 0, 64, 0, 64)
                sub(4, 6, 64, 256, 0, 192, 64, 256)
            else:
                sub(4, 6, 0, 256, t0 - 64, t0 + 192, t0, t0 + 256)
            if t0 + 256 == T:
                sub(6, 8, 0, 192, t0 + 64, t0 + 256, t0, t0 + 192)
                neg(6, 8, 192, 256, t0 + 192, t0 + 256)
            else:
                sub(6, 8, 0, 256, t0 + 64, t0 + 320, t0, t0 + 256)

        for b in range(NBC):
            for (kind, T, src, j, tl0) in (("c", TC, K.dram["cs"][b], 2, 0), ("x", TX, K.dram["xs"][b], b, TC)):
                chk(0)
                norm_hT(K, st, src, T, scl, sh, j, hT, ntmp)
                chk(1)
                for tb in range(T // 256):
                    t0 = tb * 256
                    tl = tl0 + t0
                    shift_block(kind, T, t0)
                    chk(2)
                    for jj in range(6):
                        eng = DVE if jj % 2 == 0 else POOL
                        K.X(eng, "tensor_tensor", [xx, cols], [xtmp], out=xtmp[:], in0=xx[:],
                            in1=cols[:, :, CMIX + jj:CMIX + jj + 1].broadcast_to([128, 8, 256]), op=ALU.mult)
                        K.X(eng, "tensor_tensor", [xtmp, hT], [xj[jj]], out=xj[jj][:], in0=xtmp[:], in1=hT[:, :, t0:t0 + 256],
                            op=ALU.add)
                    xr, xw, xk, xv_, xa, xg_ = xj
                    chk(3)
                    ps = K.ps()
                    for f in range(8):
                        K.mm(ps, ps.f(0, 256), w1c[:, f, :], xw[:, f, :], [w1c, xw], start=(f == 0), stop=(f == 7))
                    chk(31)
                    for f in range(8):
                        K.mm(ps, ps.f(256, 512), a1c[:, f, :], xa[:, f, :], [a1c, xa], start=(f == 0), stop=(f == 7))
                    chk(32)
                    K.act([ps], [tw], out=tw[:], in_=ps.f(0, 256), func=AF.Tanh)
                    chk(33)
                    K.X(ACT, "copy", [ps], [ta], out=ta[:], in_=ps.f(256, 512))
                    chk(34)
                    ps = K.ps()
                    for f in range(8):
                        K.mm(ps, ps.f(0, 256), g1w[:, f, :], xg_[:, f, :], [g1w, xg_], start=(f == 0), stop=(f == 7))
                    if ri == 1:
                        for f in range(8):
                            K.mm(ps, ps.f(256, 512, rows=32), v1w[:, f, :], xv_[:, f, :], [v1w, xv_], start=(f == 0), stop=(f == 7))
                    chk(35)
                    K.act([ps], [tgf], out=tgf[:], in_=ps.f(0, 256), func=AF.Tanh, scale=0.5)
                    K.X(DVE, "tensor_scalar", [tgf], [tg], out=tg[:], in0=tgf[:], scalar1=0.5, scalar2=0.5, op0=ALU.mult, op1=ALU.add)
                    if ri == 1:
                        K.X(ACT, "copy", [ps], [tv], out=tv[:], in_=ps.f(256, 512, rows=32))
                    chk(4)
                    for dc in range(8):
                        fs = slice(dc * 128, (dc + 1) * 128)
                        drow = slice(dc * 128, (dc + 1) * 128)
                        dsl = (b, drow, slice(tl, tl + 256))
                        ps1 = K.ps()
                        for f in range(8):
                            K.mm(ps1, ps1.f(0, 256), wr[:, f, fs], xr[:, f, :], [wr, xr], start=(f == 0), stop=(f == 7))
                        for f in range(8):
                            K.mm(ps1, ps1.f(256, 512), wk[:, f, fs], xk[:, f, :], [wk, xk], start=(f == 0), stop=(f == 7))
                        K.X(ACT, "copy", [ps1], [RK], out=RK[:], in_=ps1.f())
                        chk(41)
                        ps2 = K.ps()
                        for f in range(8):
                            K.mm(ps2, ps2.f(0, 256), wv[:, f, fs], xv_[:, f, :], [wv, xv_], start=(f == 0), stop=(f == 7))
                        K.mm(ps2, ps2.f(256, 512), g2w[:, fs], tg[:], [g2w, tg])
                        K.X(DVE, "tensor_copy", [ps2], [VG], out=VG[:], in_=ps2.f())
                        chk(42)
                        for (wt2, tin, dst, cidx) in ((w2a, tw, SGW, CW0), (a2a, ta, ASIG, CA0)):
                            pss = [K.ps(), K.ps()]
                            for d in range(2):
                                K.mm(pss[d], pss[d].f(0, 256), wt2[d * 64:(d + 1) * 64, fs], tin[d * 64:(d + 1) * 64, :], [wt2, tin])
                            for d in range(2):
                                K.act([pss[d], hcols], [dst], out=dst[:, d, :], in_=pss[d].f(0, 256), func=AF.Tanh,
                                      bias=hcols[:, dc, cidx + d:cidx + d + 1], scale=0.5)
                            K.X(POOL, "tensor_scalar", [dst], [dst], out=dst[:], in0=dst[:], scalar1=0.5, scalar2=0.5, op0=ALU.mult, op1=ALU.add)
                        chk(44)
                        R_, Kk, V_, G_ = RK[:, 0:256], RK[:, 256:512], VG[:, 0:256], VG[:, 256:512]
                        chk(5)
                        K.Dm(SP, [VG], [], out=S["g"][dsl], in_=G_)
                        if ri == 1:
                            ps5 = K.ps()
                            K.mm(ps5, ps5.f(0, 256), v2w[0:32, fs], tv[0:32, :], [v2w, tv])
                            K.act([ps5, hcols], [SV], out=SV[:], in_=ps5.f(0, 256), func=AF.Tanh, bias=hcols[:, dc, CV0:CV0 + 1], scale=0.5)
                            K.X(POOL, "tensor_scalar", [SV], [SV], out=SV[:], in0=SV[:], scalar1=0.5, scalar2=0.5, op0=ALU.mult, op1=ALU.add)
                            K.Dm(SP, [], [VF], out=VF[:], in_=S["vfirst"][dsl])
                            pool_tt(DV, DV[:], VF, VF[:], VG, V_, ALU.subtract)
                            pool_tt(DV, DV[:], DV, DV[:], SV, SV[:], ALU.mult)
                            pool_tt(VG, V_, VG, V_, DV, DV[:], ALU.add)
                        else:
                            K.Dm(SP, [VG], [], out=S["vfirst"][dsl], in_=V_)
                        K.X(ACT, "copy", [VG], [VB16], out=VB16[:], in_=V_)
                        K.Dm(SP, [VB16], [], out=S["v"][dsl], in_=VB16[:])
                        chk(6)
                        K.X(POOL, "tensor_scalar", [RK, cols], [KR], out=KR[:], in0=Kk, scalar1=cols[:, dc, CKK:CKK + 1], scalar2=None, op0=ALU.mult)
                        pool_tt(SQ_, SQ_[:], KR, KR[:], KR, KR[:], ALU.mult)
                        ps6 = K.ps()
                        K.mm(ps6, ps6.f(0, 256), C["blockones_f"][:], SQ_[:], [C["blockones_f"], SQ_])
                        K.act([ps6, C["eps"]], [RN], out=RN[:], in_=ps6.f(0, 256), func=AF.Sqrt, bias=C["eps"][:, 1:2], scale=1.0)
                        K.X(DVE, "reciprocal", [RN], [RN], out=RN[:], in_=RN[:])
                        pool_tt(KKN, KKN[:], KR, KR[:], RN, RN[:], ALU.mult)
                        K.X(POOL, "tensor_scalar", [KKN], [NKK], out=NKK[:], in0=KKN[:], scalar1=-1.0, scalar2=None, op0=ALU.mult)
                        chk(7)
                        for d in range(2):
                            K.X(POOL, "tensor_scalar", [ASIG, cols, omka], [TMP], out=TMP[:], in0=ASIG[:, d, :],
                                scalar1=cols[:, dc, CKA:CKA + 1], scalar2=omka[:, dc, 0:1], op0=ALU.mult, op1=ALU.add)
                            pool_tt(KD[d], KD[d][:], RK, Kk, TMP, TMP[:], ALU.mult)
                            pool_tt(BVEC, BVEC[:], KKN, KKN[:], ASIG, ASIG[:, d, :], ALU.mult)
                            K.X(DVE, "tensor_tensor_scan", [SGW, C["segmask"]], [CUM], out=CUM[:], data0=C["segmask"][:],
                                data1=SGW[:, d, :], initial=0.0, op0=ALU.mult, op1=ALU.add)
                            K.act([CUM], [GLt[d]], out=GLt[d][:], in_=CUM[:].rearrange("p (c j) -> p c j", j=64)[:, :, 63],
                                  func=AF.Exp, scale=-KAPPA)
                            K.Dm(SP, [GLt[d]], [], out=S[f"GL{d}"][b, drow, tl // 64:tl // 64 + 4], in_=GLt[d][:])
                            cum = CUM
                            if d == 1:
                                pool_tt(T3, T3[:], CUM, CUM[:], SGW, SGW[:, d, :], ALU.subtract)
                                K.X(POOL, "tensor_tensor", [CUM, T3], [CUMR], out=CUMR[:].rearrange("p (c j) -> p c j", j=64),
                                    in0=CUM[:].rearrange("p (c j) -> p c j", j=64)[:, :, 63:64].broadcast_to([128, 4, 64]),
                                    in1=T3[:].rearrange("p (c j) -> p c j", j=64), op=ALU.subtract)
                                cum = CUMR
                            K.act([cum], [E1], out=E1[:], in_=cum[:], func=AF.Exp, scale=-KAPPA)
                            pool_tt(stg[("rt", d)], stg[("rt", d)][:], RK, R_, E1, E1[:], ALU.mult)
                            pool_tt(T3, T3[:], cum, cum[:], SGW, SGW[:, d, :], ALU.subtract)
                            K.act([T3], [E2], out=E2[:], in_=T3[:], func=AF.Exp, scale=-KAPPA)
                            pool_tt(stg[("at", d)], stg[("at", d)][:], NKK, NKK[:], E2, E2[:], ALU.mult)
                            K.act([cum], [E3], out=E3[:], in_=cum[:], func=AF.Exp, scale=KAPPA)
                            K.X(DVE, "tensor_tensor", [BVEC, E3], [stg[("bt", d)]], out=stg[("bt", d)][:], in0=BVEC[:], in1=E3[:], op=ALU.mult)
                            K.X(DVE, "tensor_tensor", [KD[d], E3], [stg[("kt", d)]], out=stg[("kt", d)][:], in0=KD[d][:], in1=E3[:], op=ALU.mult)
                            for n in ("at", "bt", "kt", "rt"):
                                K.Dm(SP, [stg[(n, d)]], [], out=S[f"{n}{d}"][dsl], in_=stg[(n, d)][:])
                        chk(8)
                        K.X(POOL, "tensor_scalar", [RK, cols], [RKR], out=RKR[:], in0=R_, scalar1=cols[:, dc, CRK:CRK + 1], scalar2=None, op0=ALU.mult)
                        pool_tt(KS, KS[:], KD[0], KD[0][:], KD[1], KD[1][:], ALU.add)
                        pool_tt(PR, PR[:], RKR, RKR[:], KS, KS[:], ALU.mult)
                        ps7 = K.ps()
                        K.mm(ps7, ps7.f(0, 256), C["blockones_f"][:], PR[:], [C["blockones_f"], PR])
                        K.X(DVE, "tensor_tensor", [ps7, VG], [BVV], out=BVV[:], in0=ps7.f(0, 256), in1=V_, op=ALU.mult)
                        K.Dm(SP, [BVV], [], out=S["bv"][dsl], in_=BVV[:])
                        chk(9)
        K.barrier()


def rwkv_scan_v1(K):
    nc, C = K.nc, K.C
    S = K.rw
    order = {0: list(range(NCH)), 1: [3, 2, 1, 0] + list(range(NCH - 1, 3, -1))}
    identb = C["ident_b"]

    def ecopy(eng, ins, out_t, out, in_):
        if eng == ACT:
            K.X(ACT, "copy", ins, [out_t], out=out, in_=in_)
        else:
            K.X(DVE, "tensor_copy", ins, [out_t], out=out, in_=in_)

    for hpg in range(4):
        with contextlib.ExitStack() as st:
            chains = []
            for b in range(NBC):
                for hq in range(2):
                    for d in range(2):
                        c = dict(b=b, hp=2 * hpg + hq, d=d)
                        nm = f"{b}{hq}{d}"
                        c["cin"] = [{n: K.sb(st, f"ci{nm}{k}{n}", [128, 256], BF16) for n in ("at", "rt", "bt", "kt", "v")}
                                    for k in range(2)]
                        c["ARB"] = K.sb(st, "ARB" + nm, [128, 4, 256], BF16)
                        c["BTB"] = K.sb(st, "BTB" + nm, [128, 4, 128], BF16)
                        c["KTB"] = K.sb(st, "KTB" + nm, [128, 4, 128], BF16)
                        c["VB"] = K.sb(st, "VB" + nm, [128, 4, 128], BF16)
                        c["M1"] = K.sb(st, "M1" + nm, [128, 256], BF16)
                        c["M2"] = K.sb(st, "M2" + nm, [128, 256], BF16)
                        c["A0"] = K.sb(st, "A0" + nm, [128, 128], BF16)
                        c["TF"] = K.sb(st, "TF" + nm, [128, 384], BF16)
                        c["X"] = [K.sb(st, f"X{k}" + nm, [128, 256], BF16) for k in range(2)]
                        c["IP"] = [K.sb(st, f"IP{k}" + nm, [128, 128], BF16) for k in range(2)]
                        c["SQ"] = [K.sb(st, f"SQ{k}" + nm, [128, 256], BF16) for k in range(2)]
                        c["WT"] = K.sb(st, "WT" + nm, [128, 128], BF16)
                        c["U"] = K.sb(st, "U" + nm, [128, 128], BF16)
                        c["Ybd"] = K.sb(st, "Ybd" + nm, [128, 128], F32)
                        c["Hf"] = K.sb(st, "Hf" + nm, [128, 128], F32)
                        c["hg"] = K.sb(st, "hg" + nm, [128, 128], F32)
                        c["Hbf"] = K.sb(st, "Hbf" + nm, [128, 128], BF16)
                        c["yst"] = [K.sb(st, f"yst{k}" + nm, [128, 256], F32) for k in range(2)]
                        c["GL"] = K.sb(st, "GL" + nm, [128, NCH], F32)
                        rows = slice(c["hp"] * 128, (c["hp"] + 1) * 128)
                        c["rows"] = rows
                        K.Dm(SP, [], [c["GL"]], out=c["GL"][:], in_=S[f"GL{d}"][b, rows, :])
                        K.X(DVE, "memset", [], [c["Hf"]], ap=c["Hf"][:], constant=0.0)
                        K.X(DVE, "tensor_copy", [c["Hf"]], [c["Hbf"]], out=c["Hbf"][:], in_=c["Hf"][:])
                        chains.append(c)

            def load_group(c, grp, k):
                ci = c["cin"][k]
                b, d, rows = c["b"], c["d"], c["rows"]
                tsl = slice(grp * 256, (grp + 1) * 256)
                for n in ("at", "rt", "bt", "kt"):
                    K.Dm(SP, [], [ci[n]], out=ci[n][:], in_=S[f"{n}{d}"][b, rows, tsl])
                K.Dm(SP, [], [ci["v"]], out=ci["v"][:], in_=S["v"][b, rows, tsl])

            def expand_group(c, k):
                ci = c["cin"][k]
                msk = C["blkmask"][:].rearrange("p (h i) -> p h i", h=2).unsqueeze(1).broadcast_to([128, 4, 2, 64])

                def ex(src, dst_t, dst_ap):
                    K.X(POOL, "tensor_tensor", [src, C["blkmask"]], [dst_t],
                        out=dst_ap.rearrange("p c (h i) -> p c h i", h=2),
                        in0=src[:].rearrange("p (c i) -> p c i", i=64).unsqueeze(2).broadcast_to([128, 4, 2, 64]),
                        in1=msk, op=ALU.mult)
                ex(ci["at"], c["ARB"], c["ARB"][:, :, 0:128])
                ex(ci["rt"], c["ARB"], c["ARB"][:, :, 128:256])
                ex(ci["bt"], c["BTB"], c["BTB"][:, :, :])
                ex(ci["kt"], c["KTB"], c["KTB"][:, :, :])
                ex(ci["v"], c["VB"], c["VB"][:, :, :])

            def stage(per_bank, pe_fn, ev_fn, engs=(DVE, ACT)):
                for gi in range(0, len(chains), per_bank):
                    ps = K.ps()
                    grp = chains[gi:gi + per_bank]
                    for si, c in enumerate(grp):
                        pe_fn(c, ps, si)
                    eng = engs[(gi // per_bank) % len(engs)]
                    for si, c in enumerate(grp):
                        ev_fn(c, ps, si, eng)

            for c in chains:
                load_group(c, order[c["d"]][0] // 4, 0)
            for r in range(NCH):
                for c in chains:
                    n = order[c["d"]][r]
                    c["n"], c["wi"], c["grp"] = n, n % 4, n // 4
                    c["gk"] = (r // 4) % 2
                    if r % 4 == 0:
                        expand_group(c, c["gk"])
                        if r + 4 < NCH:
                            load_group(c, order[c["d"]][r + 4] // 4, 1 - c["gk"])
                stage(2, lambda c, ps, s: K.mm(ps, ps.f(s * 256, s * 256 + 256), c["BTB"][:, c["wi"], :], c["ARB"][:, c["wi"], :],
                                               [c["BTB"], c["ARB"]]),
                      lambda c, ps, s, e: K.X(DVE, "tensor_tensor", [ps, C["mT"]], [c["M1"]], out=c["M1"][:],
                                              in0=ps.f(s * 256, s * 256 + 256), in1=C["mT"][:, c["d"], :], op=ALU.mult), engs=(DVE,))
                stage(2, lambda c, ps, s: K.mm(ps, ps.f(s * 256, s * 256 + 256), c["KTB"][:, c["wi"], :], c["ARB"][:, c["wi"], :],
                                               [c["KTB"], c["ARB"]]),
                      lambda c, ps, s, e: K.X(DVE, "tensor_tensor", [ps, C["mT"]], [c["M2"]], out=c["M2"][:],
                                              in0=ps.f(s * 256, s * 256 + 256), in1=C["mT"][:, c["d"], :], op=ALU.mult), engs=(DVE,))
                stage(4, lambda c, ps, s: K.mm(ps, ps.f(s * 128, s * 128 + 128), c["ARB"][:, c["wi"], 0:128], c["BTB"][:, c["wi"], :],
                                               [c["ARB"], c["BTB"]]),
                      lambda c, ps, s, e: K.X(DVE, "tensor_tensor", [ps, C["mA"]], [c["A0"]], out=c["A0"][:],
                                              in0=ps.f(s * 128, s * 128 + 128), in1=C["mA"][:, c["d"], :], op=ALU.mult), engs=(DVE,))
                for c in chains:
                    K.X(POOL, "tensor_tensor", [c["M1"], identb], [c["IP"][0]], out=c["IP"][0][:], in0=c["M1"][:, 0:128],
                        in1=identb[:], op=ALU.add)

                def tr_pe(c, ps, s):
                    srcs = [(c["ARB"], c["ARB"][:, c["wi"], 0:128]), (c["BTB"], c["BTB"][:, c["wi"], :]),
                            (c["KTB"], c["KTB"][:, c["wi"], :]), (c["VB"], c["VB"][:, c["wi"], :])]
                    for q, (t, ap) in enumerate(srcs):
                        K.tr(ps, ps.b(s * 512 + q * 128, s * 512 + (q + 1) * 128), ap, identb[:], [t, identb])

                def tr_ev(c, ps, s, e):
                    ecopy(e, [ps], c["TF"], c["TF"][:], ps.b(s * 512 + 128, s * 512 + 512))
                    ecopy(e, [ps], c["X"][0], c["X"][0][:, 0:128], ps.b(s * 512, s * 512 + 128))
                stage(2, tr_pe, tr_ev)
                stage(4, lambda c, ps, s: K.mm(ps, ps.f(s * 128, s * 128 + 128), c["M2"][:, 0:128], c["TF"][:, 256:384], [c["M2"], c["TF"]]),
                      lambda c, ps, s, e: ecopy(e, [ps], c["X"][0], c["X"][0][:, 128:256], ps.f(s * 128, s * 128 + 128)))

                def sq_pe(m):
                    def f(c, ps, s):
                        if m == 0:
                            A_t, A_ap, AT_t, AT_ap = c["A0"], c["A0"][:], c["M1"], c["M1"][:, 0:128]
                        else:
                            q = c["SQ"][(m - 1) % 2]
                            A_t, A_ap, AT_t, AT_ap = q, q[:, 0:128], q, q[:, 128:256]
                        K.mm(ps, ps.f(s * 256, s * 256 + 128), AT_ap, A_ap, [A_t, AT_t])
                        K.mm(ps, ps.f(s * 256 + 128, s * 256 + 256), A_ap, AT_ap, [A_t, AT_t])
                    return f

                def sq_ev(m):
                    def f(c, ps, s, e):
                        ecopy(e, [ps], c["SQ"][m % 2], c["SQ"][m % 2][:], ps.f(s * 256, s * 256 + 256))
                    return f

                def neu_pe(m):
                    def f(c, ps, s):
                        K.mm(ps, ps.f(s * 256, s * 256 + 256), c["IP"][m % 2][:], c["X"][m % 2][:], [c["IP"][m % 2], c["X"][m % 2]])
                    return f

                def neu_ev(m):
                    def f(c, ps, s, e):
                        ecopy(e, [ps], c["X"][(m + 1) % 2], c["X"][(m + 1) % 2][:], ps.f(s * 256, s * 256 + 256))
                    return f
                for m in range(5):
                    stage(2, sq_pe(m), sq_ev(m))
                    stage(2, neu_pe(m), neu_ev(m), engs=(ACT, DVE))
                    for c in chains:
                        K.X(POOL, "tensor_tensor", [c["SQ"][m % 2], identb], [c["IP"][(m + 1) % 2]], out=c["IP"][(m + 1) % 2][:],
                            in0=c["SQ"][m % 2][:, 128:256], in1=identb[:], op=ALU.add)
                stage(2, neu_pe(5), neu_ev(5))
                stage(4, lambda c, ps, s: K.tr(ps, ps.b(s * 256, s * 256 + 128), c["X"][0][:, 0:128], identb[:], [c["X"][0], identb]),
                      lambda c, ps, s, e: ecopy(e, [ps], c["WT"], c["WT"][:], ps.b(s * 256, s * 256 + 128)))
                stage(4, lambda c, ps, s: K.mm(ps, ps.f(s * 128, s * 128 + 128), c["WT"][:], c["Hbf"][:], [c["WT"], c["Hbf"]]),
                      lambda c, ps, s, e: K.X(DVE, "tensor_tensor", [ps, c["X"][0]], [c["U"]], out=c["U"][:],
                                              in0=ps.f(s * 128, s * 128 + 128), in1=c["X"][0][:, 128:256], op=ALU.add), engs=(DVE,))

                def y_pe(c, ps, s):
                    o = ps.f(s * 128, s * 128 + 128)
                    K.mm(ps, o, c["ARB"][:, c["wi"], 128:256], c["Hbf"][:], [c["ARB"], c["Hbf"]], start=True, stop=False)
                    K.mm(ps, o, c["M1"][:, 128:256], c["U"][:], [c["M1"], c["U"]], start=False, stop=False)
                    K.mm(ps, o, c["M2"][:, 128:256], c["TF"][:, 256:384], [c["M2"], c["TF"]], start=False, stop=True)
                stage(4, y_pe, lambda c, ps, s, e: ecopy(e, [ps], c["Ybd"], c["Ybd"][:], ps.f(s * 128, s * 128 + 128)), engs=(ACT,))
                for c in chains:
                    K.X(POOL, "tensor_scalar", [c["Hf"], c["GL"]], [c["hg"]], out=c["hg"][:], in0=c["Hf"][:],
                        scalar1=c["GL"][:, c["n"]:c["n"] + 1], scalar2=None, op0=ALU.mult)

                def h_pe(c, ps, s):
                    o = ps.f(s * 128, s * 128 + 128)
                    K.mm(ps, o, c["TF"][:, 0:128], c["U"][:], [c["TF"], c["U"]], start=True, stop=False)
                    K.mm(ps, o, c["TF"][:, 128:256], c["TF"][:, 256:384], [c["TF"]], start=False, stop=True)

                def h_ev(c, ps, s, e):
                    K.X(DVE, "scalar_tensor_tensor", [ps, c["GL"], c["hg"]], [c["Hf"]], out=c["Hf"][:], in0=ps.f(s * 128, s * 128 + 128),
                        scalar=c["GL"][:, c["n"]:c["n"] + 1], in1=c["hg"][:], op0=ALU.mult, op1=ALU.add)
                stage(4, h_pe, h_ev, engs=(DVE,))
                for c in chains:
                    K.X(ACT, "copy", [c["Hf"]], [c["Hbf"]], out=c["Hbf"][:], in_=c["Hf"][:])
                stage(4, lambda c, ps, s: K.mm(ps, ps.f(s * 128, s * 128 + 64), c["Ybd"][:], C["sel"][:], [c["Ybd"], C["sel"]]),
                      lambda c, ps, s, e: ecopy(e, [ps], c["yst"][c["gk"]], c["yst"][c["gk"]][:, c["wi"] * 64:(c["wi"] + 1) * 64],
                                                ps.f(s * 128, s * 128 + 64)), engs=(ACT, DVE))
                if r % 4 == 3:
                    for c in chains:
                        ys = c["yst"][c["gk"]]
                        K.Dm(SP, [ys], [], out=S[f"y{c['d']}"][c["b"], c["rows"], c["grp"] * 256:(c["grp"] + 1) * 256], in_=ys[:])
            K.barrier()


def rwkv_scan(K):
    nc, C = K.nc, K.C
    S = K.rw
    order = {0: list(range(NCH)), 1: [3, 2, 1, 0] + list(range(NCH - 1, 3, -1))}
    identb = C["ident_b"]
    id2 = identb[:].unsqueeze(1).broadcast_to([128, 2, 128])

    def ecopy(eng, ins, out_t, out, in_):
        if eng == ACT:
            K.X(ACT, "copy", ins, [out_t], out=out, in_=in_)
        else:
            K.X(DVE, "tensor_copy", ins, [out_t], out=out, in_=in_)

    def v3(ap, a=2):
        return ap.rearrange("p (a b) -> p a b", a=a)

    for hpg in range(4):
        with contextlib.ExitStack() as st:
            pairs, chains = [], []
            for b in range(NBC):
                for hq in range(2):
                    nm = f"{b}{hq}"
                    p = dict(b=b, hp=2 * hpg + hq, idx=len(pairs))
                    p["M1"] = K.sb(st, "M1" + nm, [128, 2, 256], BF16)
                    p["M2"] = K.sb(st, "M2" + nm, [128, 2, 256], BF16)
                    p["A0"] = K.sb(st, "A0" + nm, [128, 2, 128], BF16)
                    p["TF"] = K.sb(st, "TF" + nm, [128, 2, 384], BF16)
                    p["X"] = [K.sb(st, f"X{k}" + nm, [128, 2, 256], BF16) for k in range(2)]
                    p["IP"] = [K.sb(st, f"IP{k}" + nm, [128, 2, 128], BF16) for k in range(2)]
                    p["SQ"] = [K.sb(st, f"SQ{k}" + nm, [128, 2, 256], BF16) for k in range(2)]
                    p["WT"] = K.sb(st, "WT" + nm, [128, 2, 128], BF16)
                    p["U"] = K.sb(st, "U" + nm, [128, 2, 128], BF16)
                    p["Ybd"] = K.sb(st, "Ybd" + nm, [128, 2, 128], F32)
                    p["Hf"] = K.sb(st, "Hf" + nm, [128, 2, 128], F32)
                    p["hg"] = K.sb(st, "hg" + nm, [128, 2, 128], F32)
                    p["Hbf"] = K.sb(st, "Hbf" + nm, [128, 2, 128], BF16)
                    K.X(DVE, "memset", [], [p["Hf"]], ap=p["Hf"][:], constant=0.0)
                    K.X(DVE, "tensor_copy", [p["Hf"]], [p["Hbf"]], out=p["Hbf"][:], in_=p["Hf"][:])
                    rows = slice(p["hp"] * 128, (p["hp"] + 1) * 128)
                    p["ch"] = []
                    for d in range(2):
                        cn = nm + str(d)
                        c = dict(b=b, hp=p["hp"], d=d, pair=p, rows=rows)
                        c["cin"] = [{n: K.sb(st, f"ci{cn}{k}{n}", [128, 256], BF16) for n in ("at", "rt", "bt", "kt", "v")}
                                    for k in range(2)]
                        c["ARB"] = K.sb(st, "ARB" + cn, [128, 4, 256], BF16)
                        c["BTB"] = K.sb(st, "BTB" + cn, [128, 4, 128], BF16)
                        c["KTB"] = K.sb(st, "KTB" + cn, [128, 4, 128], BF16)
                        c["VB"] = K.sb(st, "VB" + cn, [128, 4, 128], BF16)
                        c["yst"] = [K.sb(st, f"yst{k}" + cn, [128, 256], F32) for k in range(2)]
                        c["GL"] = K.sb(st, "GL" + cn, [128, NCH], F32)
                        K.Dm(SP, [], [c["GL"]], out=c["GL"][:], in_=S[f"GL{d}"][b, rows, :])
                        p["ch"].append(c)
                        chains.append(c)
                    pairs.append(p)

            def load_group(c, grp, k):
                ci = c["cin"][k]
                b, d, rows = c["b"], c["d"], c["rows"]
                tsl = slice(grp * 256, (grp + 1) * 256)
                for n in ("at", "rt", "bt", "kt"):
                    K.Dm(SP, [], [ci[n]], out=ci[n][:], in_=S[f"{n}{d}"][b, rows, tsl])
                K.Dm(SP, [], [ci["v"]], out=ci["v"][:], in_=S["v"][b, rows, tsl])

            def expand_group(c, k):
                ci = c["cin"][k]
                msk = C["blkmask"][:].rearrange("p (h i) -> p h i", h=2).unsqueeze(1).broadcast_to([128, 4, 2, 64])

                def ex(src, dst_t, dst_ap):
                    K.X(POOL, "tensor_tensor", [src, C["blkmask"]], [dst_t],
                        out=dst_ap.rearrange("p c (h i) -> p c h i", h=2),
                        in0=src[:].rearrange("p (c i) -> p c i", i=64).unsqueeze(2).broadcast_to([128, 4, 2, 64]),
                        in1=msk, op=ALU.mult)
                ex(ci["at"], c["ARB"], c["ARB"][:, :, 0:128])
                ex(ci["rt"], c["ARB"], c["ARB"][:, :, 128:256])
                ex(ci["bt"], c["BTB"], c["BTB"][:, :, :])
                ex(ci["kt"], c["KTB"], c["KTB"][:, :, :])
                ex(ci["v"], c["VB"], c["VB"][:, :, :])

            def stage(pe_fn, ev_fn, engs=(DVE, ACT)):
                for p in pairs:
                    ps = K.ps()
                    for c in p["ch"]:
                        pe_fn(c, p, ps, c["d"])
                    ev_fn(p, ps, engs[p["idx"] % len(engs)])

            for c in chains:
                load_group(c, order[c["d"]][0] // 4, 0)
            for r in range(NCH):
                for c in chains:
                    n = order[c["d"]][r]
                    c["n"], c["wi"], c["grp"] = n, n % 4, n // 4
                    c["gk"] = (r // 4) % 2
                    if r % 4 == 0:
                        expand_group(c, c["gk"])
                        if r + 4 < NCH:
                            load_group(c, order[c["d"]][r + 4] // 4, 1 - c["gk"])
                stage(lambda c, p, ps, d: K.mm(ps, ps.f(d * 256, d * 256 + 256), c["BTB"][:, c["wi"], :], c["ARB"][:, c["wi"], :],
                                               [c["BTB"], c["ARB"]]),
                      lambda p, ps, e: K.X(DVE, "tensor_tensor", [ps, C["mT"]], [p["M1"]], out=p["M1"][:], in0=v3(ps.f(0, 512)),
                                           in1=C["mT"][:], op=ALU.mult), engs=(DVE,))
                stage(lambda c, p, ps, d: K.mm(ps, ps.f(d * 256, d * 256 + 256), c["KTB"][:, c["wi"], :], c["ARB"][:, c["wi"], :],
                                               [c["KTB"], c["ARB"]]),
                      lambda p, ps, e: K.X(DVE, "tensor_tensor", [ps, C["mT"]], [p["M2"]], out=p["M2"][:], in0=v3(ps.f(0, 512)),
                                           in1=C["mT"][:], op=ALU.mult), engs=(DVE,))
                stage(lambda c, p, ps, d: K.mm(ps, ps.f(d * 128, d * 128 + 128), c["ARB"][:, c["wi"], 0:128], c["BTB"][:, c["wi"], :],
                                               [c["ARB"], c["BTB"]]),
                      lambda p, ps, e: K.X(DVE, "tensor_tensor", [ps, C["mA"]], [p["A0"]], out=p["A0"][:], in0=v3(ps.f(0, 256)),
                                           in1=C["mA"][:], op=ALU.mult), engs=(DVE,))

                def tr_pe(c, p, ps, d):
                    srcs = [(c["ARB"], c["ARB"][:, c["wi"], 0:128]), (c["BTB"], c["BTB"][:, c["wi"], :]),
                            (c["KTB"], c["KTB"][:, c["wi"], :]), (c["VB"], c["VB"][:, c["wi"], :])]
                    for q, (t, ap) in enumerate(srcs):
                        K.tr(ps, ps.b(d * 512 + q * 128, d * 512 + (q + 1) * 128), ap, identb[:], [t, identb])

                def tr_ev(p, ps, e):
                    pv = v3(ps.b(0, 1024))
                    ecopy(e, [ps], p["TF"], p["TF"][:], pv[:, :, 128:512])
                    ecopy(e, [ps], p["X"][0], p["X"][0][:, :, 0:128], pv[:, :, 0:128])
                stage(tr_pe, tr_ev)
                stage(lambda c, p, ps, d: K.mm(ps, ps.f(d * 128, d * 128 + 128), p["M2"][:, d, 0:128], p["TF"][:, d, 256:384],
                                               [p["M2"], p["TF"]]),
                      lambda p, ps, e: ecopy(e, [ps], p["X"][0], p["X"][0][:, :, 128:256], v3(ps.f(0, 256))), engs=(ACT, DVE))

                def sq_pe(m):
                    def f(c, p, ps, d):
                        if m == 0:
                            A_t, A_ap, AT_t, AT_ap = p["A0"], p["A0"][:, d, :], p["M1"], p["M1"][:, d, 0:128]
                        else:
                            q = p["SQ"][(m - 1) % 2]
                            A_t, A_ap, AT_t, AT_ap = q, q[:, d, 0:128], q, q[:, d, 128:256]
                        K.mm(ps, ps.f(d * 256, d * 256 + 128), AT_ap, A_ap, [A_t, AT_t])
                        K.mm(ps, ps.f(d * 256 + 128, d * 256 + 256), A_ap, AT_ap, [A_t, AT_t])
                    return f

                def neu_pe(m):
                    def f(c, p, ps, d):
                        if m == 0:
                            AT_t, AT_ap = p["M1"], p["M1"][:, d, 0:128]
                        else:
                            AT_t, AT_ap = p["SQ"][(m - 1) % 2], p["SQ"][(m - 1) % 2][:, d, 128:256]
                        o = ps.f(d * 256, d * 256 + 256)
                        K.mm(ps, o, AT_ap, p["X"][m % 2][:, d, :], [AT_t, p["X"][m % 2]], start=True, stop=False)
                        K.mm(ps, o, identb[:], p["X"][m % 2][:, d, :], [identb, p["X"][m % 2]], start=False, stop=True)
                    return f
                for m in range(5):
                    stage(sq_pe(m), lambda p, ps, e, m=m: ecopy(e, [ps], p["SQ"][m % 2], p["SQ"][m % 2][:], v3(ps.f(0, 512))))
                    stage(neu_pe(m), lambda p, ps, e, m=m: ecopy(e, [ps], p["X"][(m + 1) % 2], p["X"][(m + 1) % 2][:], v3(ps.f(0, 512))),
                          engs=(ACT, DVE))
                stage(neu_pe(5), lambda p, ps, e: ecopy(e, [ps], p["X"][0], p["X"][0][:], v3(ps.f(0, 512))))
                stage(lambda c, p, ps, d: K.tr(ps, ps.b(d * 256, d * 256 + 128), p["X"][0][:, d, 0:128], identb[:], [p["X"][0], identb]),
                      lambda p, ps, e: ecopy(e, [ps], p["WT"], p["WT"][:], v3(ps.b(0, 512))[:, :, 0:128]), engs=(ACT, DVE))
                stage(lambda c, p, ps, d: K.mm(ps, ps.f(d * 128, d * 128 + 128), p["WT"][:, d, :], p["Hbf"][:, d, :], [p["WT"], p["Hbf"]]),
                      lambda p, ps, e: K.X(DVE, "tensor_tensor", [ps, p["X"][0]], [p["U"]], out=p["U"][:], in0=v3(ps.f(0, 256)),
                                           in1=p["X"][0][:, :, 128:256], op=ALU.add), engs=(DVE,))

                def y_pe(c, p, ps, d):
                    o = ps.f(d * 128, d * 128 + 128)
                    K.mm(ps, o, c["ARB"][:, c["wi"], 128:256], p["Hbf"][:, d, :], [c["ARB"], p["Hbf"]], start=True, stop=False)
                    K.mm(ps, o, p["M1"][:, d, 128:256], p["U"][:, d, :], [p["M1"], p["U"]], start=False, stop=False)
                    K.mm(ps, o, p["M2"][:, d, 128:256], p["TF"][:, d, 256:384], [p["M2"], p["TF"]], start=False, stop=True)
                stage(y_pe, lambda p, ps, e: ecopy(e, [ps], p["Ybd"], p["Ybd"][:], v3(ps.f(0, 256))), engs=(ACT,))
                for c in chains:
                    p, d = c["pair"], c["d"]
                    K.X(POOL, "tensor_scalar", [p["Hf"], c["GL"]], [p["hg"]], out=p["hg"][:, d, :], in0=p["Hf"][:, d, :],
                        scalar1=c["GL"][:, c["n"]:c["n"] + 1], scalar2=None, op0=ALU.mult)

                def h_pe(c, p, ps, d):
                    o = ps.f(d * 128, d * 128 + 128)
                    K.mm(ps, o, p["TF"][:, d, 0:128], p["U"][:, d, :], [p["TF"], p["U"]], start=True, stop=False)
                    K.mm(ps, o, p["TF"][:, d, 128:256], p["TF"][:, d, 256:384], [p["TF"]], start=False, stop=True)

                def h_ev(p, ps, e):
                    for c in p["ch"]:
                        d = c["d"]
                        K.X(DVE, "scalar_tensor_tensor", [ps, c["GL"], p["hg"]], [p["Hf"]], out=p["Hf"][:, d, :],
                            in0=ps.f(d * 128, d * 128 + 128), scalar=c["GL"][:, c["n"]:c["n"] + 1], in1=p["hg"][:, d, :],
                            op0=ALU.mult, op1=ALU.add)
                    K.X(ACT, "copy", [p["Hf"]], [p["Hbf"]], out=p["Hbf"][:], in_=p["Hf"][:])
                stage(h_pe, h_ev, engs=(DVE,))

                def ys_ev(p, ps, e):
                    for c in p["ch"]:
                        d = c["d"]
                        ecopy(e, [ps], c["yst"][c["gk"]], c["yst"][c["gk"]][:, c["wi"] * 64:(c["wi"] + 1) * 64],
                              ps.f(d * 128, d * 128 + 64))
                stage(lambda c, p, ps, d: K.mm(ps, ps.f(d * 128, d * 128 + 64), p["Ybd"][:, d, :], C["sel"][:], [p["Ybd"], C["sel"]]),
                      ys_ev, engs=(ACT, DVE))
                if r % 4 == 3:
                    for c in chains:
                        ys = c["yst"][c["gk"]]
                        K.Dm(SP, [ys], [], out=S[f"y{c['d']}"][c["b"], c["rows"], c["grp"] * 256:(c["grp"] + 1) * 256], in_=ys[:])
            K.barrier()


def rwkv_out(K, i, ri, need_ctx):
    nc, C = K.nc, K.C
    S = K.rw
    with contextlib.ExitStack() as st:
        cols = rwkv_cols(K, st, i, ri)
        wo = K.sb(st, "rwo", [128, 8, 1024], BF16)
        K.Dm(POOL, [], [wo], out=wo[:], in_=K.w("rw_wo")[ri].rearrange("(kc p) n -> p kc n", p=128))
        g1bc = K.sb(st, "rg1bc", [128, 1024], F32)
        oT = K.sb(st, "roT", [128, 8, 256], BF16)
        ptmp = proj_tmp(K, st)

        def f32t(n):
            return [K.sb(st, f"{n}{k}", [128, 256], F32) for k in range(2)]
        Y0, Y1, YQ, MN, MQ, VAR, Z, BVt, Gt = [f32t(n) for n in ("oY0", "oY1", "oYQ", "oMN", "oMQ", "oVAR", "oZ", "oBV", "oG")]
        it = 0
        for b in range(NBC):
            seqs = [("x", TX, K.dram["xs"][b], b, TC)]
            if need_ctx:
                seqs = [("c", TC, K.dram["cs"][b], 2, 0)] + seqs
            for (kind, T, res, j, tl0) in seqs:
                bcast_row(K, g1bc, K.dram["modrow"][i, j:j + 1, 2 * 1024:3 * 1024])
                for tb in range(T // 256):
                    tl = tl0 + tb * 256
                    for dc in range(8):
                        k = it % 2
                        it += 1
                        dsl = (b, slice(dc * 128, (dc + 1) * 128), slice(tl, tl + 256))
                        y0, y1, yq, mn, mq, var, z, bv, g = Y0[k], Y1[k], YQ[k], MN[k], MQ[k], VAR[k], Z[k], BVt[k], Gt[k]
                        K.Dm(SP, [], [y0], out=y0[:], in_=S["y0"][dsl])
                        K.Dm(SP, [], [y1], out=y1[:], in_=S["y1"][dsl])
                        K.Dm(SP, [], [bv], out=bv[:], in_=S["bv"][dsl])
                        K.Dm(SP, [], [g], out=g[:], in_=S["g"][dsl])
                        K.X(POOL, "tensor_tensor", [y0, y1], [y0], out=y0[:], in0=y0[:], in1=y1[:], op=ALU.add)
                        K.X(POOL, "tensor_tensor", [y0], [yq], out=yq[:], in0=y0[:], in1=y0[:], op=ALU.mult)
                        ps = K.ps()
                        K.mm(ps, ps.f(0, 256), C["blockones_f"][:], y0[:], [C["blockones_f"], y0])
                        K.mm(ps, ps.f(256, 512), C["blockones_f"][:], yq[:], [C["blockones_f"], yq])
                        K.act([ps], [mn], out=mn[:], in_=ps.f(0, 256), func=AF.Copy, scale=1.0 / 64)
                        K.act([ps], [var], out=var[:], in_=ps.f(256, 512), func=AF.Copy, scale=1.0 / 64)
                        K.X(POOL, "tensor_tensor", [mn], [mq], out=mq[:], in0=mn[:], in1=mn[:], op=ALU.mult)
                        K.X(POOL, "tensor_tensor", [var, mq], [var], out=var[:], in0=var[:], in1=mq[:], op=ALU.subtract)
                        K.act([var, C["eps"]], [var], out=var[:], in_=var[:], func=AF.Sqrt, bias=C["eps"][:, 2:3], scale=1.0)
                        K.X(DVE, "reciprocal", [var], [var], out=var[:], in_=var[:])
                        K.X(POOL, "tensor_tensor", [y0, mn], [z], out=z[:], in0=y0[:], in1=mn[:], op=ALU.subtract)
                        K.X(POOL, "tensor_tensor", [z, var], [z], out=z[:], in0=z[:], in1=var[:], op=ALU.mult)
                        K.X(DVE, "tensor_scalar", [z, cols], [z], out=z[:], in0=z[:], scalar1=cols[:, dc, CLW:CLW + 1],
                            scalar2=cols[:, dc, CLB:CLB + 1], op0=ALU.mult, op1=ALU.add)
                        K.X(POOL, "tensor_tensor", [z, bv], [z], out=z[:], in0=z[:], in1=bv[:], op=ALU.add)
                        K.X(DVE, "tensor_tensor", [z, g], [oT], out=oT[:, dc, :], in0=z[:], in1=g[:], op=ALU.mult)
                    proj_residual(K, st, oT, 256, wo, res[tb * 256:(tb + 1) * 256, :], g1bc, None, ptmp)
        K.barrier()


def phase_rwkv(K, i, ri, need_ctx):
    K.set_psum("full")
    rwkv_prep(K, i, ri)
    if K.rw_stop >= 1:
        rwkv_scan(K)
    if K.debug and i == 1:
        for nm, dt in (("y0", F32), ("y1", F32), ("v", BF16), ("GL0", F32), ("GL1", F32), ("rt0", BF16), ("kt0", BF16), ("bv", F32), ("at1", BF16), ("bt1", BF16), ("at0", BF16), ("bt0", BF16), ("kt1", BF16), ("rt1", BF16), ("g", F32)):
            o = K.dout("dbg_rw_" + nm, list(K.rw[nm].shape), dt)
            for b in range(NBC):
                for dc in range(8):
                    K.Dm(SP, [], [], out=o[b, dc * 128:(dc + 1) * 128, :], in_=K.rw[nm][b, dc * 128:(dc + 1) * 128, :])
        K.barrier()
    if K.rw_stop >= 2:
        rwkv_out(K, i, ri, need_ctx)
```

```python
import contextlib
import numpy as np
import ml_dtypes
import concourse.bass as bass
import concourse.mybir as mybir
from concourse.bass_utils import run_bass_kernel_spmd

F32 = mybir.dt.float32
BF16 = mybir.dt.bfloat16
I32 = mybir.dt.int32
U32 = mybir.dt.uint32
AF = mybir.ActivationFunctionType
ALU = mybir.AluOpType
AX = mybir.AxisListType

PE, DVE, ACT, POOL, SP = 0, 1, 2, 3, 4
EPOCH = 30000
N_DSEM = 40


class Res:
    __slots__ = ("w", "r", "name", "excl")

    def __init__(self, name="", excl=False):
        self.w = None
        self.r = {}
        self.name = name
        self.excl = excl


class T:
    __slots__ = ("t", "res")

    def __init__(self, t, name=""):
        self.t = t
        self.res = Res(name)

    def __getitem__(self, k):
        return self.t[k]


class Prog:
    def __init__(self, nc, es):
        self.nc = nc
        self.es = es
        self.eng = [nc.tensor, nc.vector, nc.scalar, nc.gpsimd, nc.sync]
        self.cnt = [0] * 5
        self.epoch = [0] * 5
        self.esem = [[] for _ in range(5)]
        for e in range(4):
            self.esem[e].append(es.enter_context(nc.semaphore(f"e{e}_0")))
        self.known = [dict() for _ in range(5)]
        self.knownd = [dict() for _ in range(5)]
        self.dsem = [es.enter_context(nc.semaphore(f"d{i}")) for i in range(N_DSEM)]
        self.dval = [0] * N_DSEM
        self.dnext = 0
        self.n_inst = 0
        self.n_wait = 0
        self.psum_tiles = []
        self.psum_i = 0
        self.snap = {}

    def _learn(self, me, tok):
        sn = self.snap.get(tok)
        if sn is None:
            return
        km, kd = self.known[me], self.knownd[me]
        for e, v in sn[0].items():
            if km.get(e, (-1, 0)) < v:
                km[e] = v
        for s_, v in sn[1].items():
            if kd.get(s_, 0) < v:
                kd[s_] = v

    def wait_tok(self, me, tok):
        if tok is None:
            return
        if tok[0] == "e":
            _, e, ep, c = tok
            if e == me and me == PE:
                return
            k = self.known[me].get(e, (-1, 0))
            if k >= (ep, c):
                return
            self.eng[me].wait_ge(self.esem[e][ep], c)
            self.n_wait += 1
            self.known[me][e] = (ep, c)
            self._learn(me, tok)
        else:
            _, s, v = tok
            if self.knownd[me].get(s, 0) >= v:
                return
            self.eng[me].wait_ge(self.dsem[s], v)
            self.n_wait += 1
            self.knownd[me][s] = v
            self._learn(me, tok)

    def _deps(self, me, ins, outs):
        for r in ins:
            r = getattr(r, "res", r)
            self.wait_tok(me, r.w)
            if r.excl:
                for t in r.r.values():
                    self.wait_tok(me, t)
        for r in outs:
            r = getattr(r, "res", r)
            self.wait_tok(me, r.w)
            for t in r.r.values():
                self.wait_tok(me, t)

    def _mark(self, tok, key, ins, outs):
        for r in ins:
            r = getattr(r, "res", r)
            if r.excl:
                r.w = tok
                r.r = {}
            else:
                r.r[key] = tok
        for r in outs:
            r = getattr(r, "res", r)
            r.w = tok
            r.r = {}

    def op(self, me, fn, ins=(), outs=()):
        if getattr(self, "muted", False):
            return None
        self._deps(me, ins, outs)
        if self.cnt[me] >= EPOCH:
            self.epoch[me] += 1
            self.cnt[me] = 0
            self.esem[me].append(self.es.enter_context(self.nc.semaphore(f"e{me}_{self.epoch[me]}")))
        inst = fn()
        self.cnt[me] += 1
        inst.then_inc(self.esem[me][self.epoch[me]], 1)
        self.n_inst += 1
        tok = ("e", me, self.epoch[me], self.cnt[me])
        self.snap[tok] = (dict(self.known[me]), dict(self.knownd[me]))
        self._mark(tok, me, ins, outs)
        return tok

    def dma(self, me, fn, ins=(), outs=()):
        if getattr(self, "muted", False):
            return None
        self._deps(me, ins, outs)
        s = self.dnext
        self.dnext = (self.dnext + 1) % N_DSEM
        if self.dval[s] > 0:
            self.wait_tok(me, ("d", s, self.dval[s]))
        inst = fn()
        self.dval[s] += 16
        inst.then_inc(self.dsem[s], 16)
        self.n_inst += 1
        tok = ("d", s, self.dval[s])
        self.snap[tok] = (dict(self.known[me]), dict(self.knownd[me]))
        self._mark(tok, ("d", s), ins, outs)
        return tok

    def last_tok(self, e):
        if e == SP or (self.cnt[e] == 0 and self.epoch[e] == 0):
            return None
        return ("e", e, self.epoch[e], self.cnt[e])

    def barrier(self, engines=(PE, DVE, ACT, POOL, SP)):
        toks = [self.last_tok(e) for e in range(4)]
        for me in engines:
            for e in range(4):
                if e != me:
                    self.wait_tok(me, toks[e])
            for s in range(N_DSEM):
                if self.dval[s] > 0:
                    self.wait_tok(me, ("d", s, self.dval[s]))

    def sb(self, st, name, shape, dt):
        self.uid = getattr(self, "uid", 0) + 1
        return T(st.enter_context(self.nc.sbuf_tensor(f"s{self.uid}_{name}", list(shape), dt)), name)

    def init_psum(self, st, n=8):
        self.psum_tiles = [T(st.enter_context(self.nc.psum_tensor(f"psb{i}", [128, 512], F32)), f"psb{i}")
                           for i in range(n)]
        self.psum_i = 0

    def ps(self):
        t = self.psum_tiles[self.psum_i % len(self.psum_tiles)]
        self.psum_i += 1
        return t


D = 1024
NBC = 2
TX = 2048
TC = 256
TL = TC + TX
NE = 16
KAPPA = float(np.exp(-0.5))
DEPTH = 4

W_NAMES = ["ada_w", "ada_b", "norm_g", "fnet_wo", "fnet_bo", "rw_mix", "rw_wr", "rw_wk", "rw_wv", "rw_wo",
           "rw_w0", "rw_w1", "rw_w2", "rw_a0", "rw_a1", "rw_a2", "rw_v0", "rw_v1", "rw_v2", "rw_g1", "rw_g2",
           "rw_kk", "rw_ka", "rw_rk", "rw_lnx_w", "rw_lnx_b", "moe_router", "moe_wg", "moe_wu", "moe_wd", "final_g"]
W_SHAPES = {
    "ada_w": [4, 1024, 6144], "ada_b": [4, 6144], "norm_g": [4, 2, 1024], "fnet_wo": [2, 1024, 1024],
    "fnet_bo": [2, 1024], "rw_mix": [2, 6, 1024], "rw_wr": [2, 1024, 1024], "rw_wk": [2, 1024, 1024],
    "rw_wv": [2, 1024, 1024], "rw_wo": [2, 1024, 1024], "rw_w0": [2, 2, 1024], "rw_w1": [2, 2, 1024, 64],
    "rw_w2": [2, 2, 64, 1024], "rw_a0": [2, 2, 1024], "rw_a1": [2, 2, 1024, 64], "rw_a2": [2, 2, 64, 1024],
    "rw_v0": [1, 1024], "rw_v1": [1, 1024, 32], "rw_v2": [1, 32, 1024], "rw_g1": [2, 1024, 128],
    "rw_g2": [2, 128, 1024], "rw_kk": [2, 1024], "rw_ka": [2, 1024], "rw_rk": [2, 1024],
    "rw_lnx_w": [2, 1024], "rw_lnx_b": [2, 1024], "moe_router": [4, 1024, 16],
    "moe_wg": [4, 16, 1024, 1024], "moe_wu": [4, 16, 1024, 1024], "moe_wd": [4, 16, 1024, 1024], "final_g": [1, 1024],
}


def make_consts():
    bf = ml_dtypes.bfloat16
    c = {}
    c["ident_f"] = np.eye(128, dtype=np.float32)
    c["ident_b"] = np.eye(128, dtype=np.float32).astype(bf)
    cc = np.arange(256)
    ang = 2 * np.pi * np.outer(cc, cc) / 256.0
    cs = np.concatenate([np.cos(ang), np.sin(ang)], axis=1) / 16.0
    c["cs_tab"] = cs.reshape(2, 128, 512).transpose(1, 0, 2).astype(bf).copy()
    for T in (TX, TC):
        t = np.arange(T)
        a = 2 * np.pi * ((np.outer(t, t)) % T) / T
        tab = np.stack([np.cos(a), -np.sin(a)], axis=1) / np.sqrt(T)
        c[f"tok_tab{T}"] = tab.astype(bf)
    p = np.arange(128)
    blk = (p[:, None] // 64 == p[None, :] // 64)
    c["blockones_f"] = blk.astype(np.float32)
    c["blkmask"] = blk.astype(np.float32).astype(bf)
    row = p[:, None] % 64
    col = p[None, :] % 64
    mT = np.zeros((2, 128, 256), np.float32)
    mA = np.zeros((2, 128, 128), np.float32)
    mT[0, :, :128] = blk & (col > row)
    mT[0, :, 128:] = blk & (col >= row)
    mT[1, :, :128] = blk & (col < row)
    mT[1, :, 128:] = blk & (col <= row)
    mA[0] = blk & (col < row)
    mA[1] = blk & (col > row)
    c["mT"] = mT.transpose(1, 0, 2).copy()
    c["mA"] = mA.transpose(1, 0, 2).copy()
    seg = np.ones((128, 256), np.float32)
    seg[:, ::64] = 0
    c["segmask"] = seg
    sel = np.zeros((128, 64), np.float32)
    sel[p, p % 64] = 1.0
    c["sel"] = sel
    return c


PER_LAYER = ("ada_w", "moe_wg", "moe_wu", "moe_wd")
NP2DT = {np.dtype(np.float32): F32, np.dtype(ml_dtypes.bfloat16): BF16, np.dtype(np.int32): I32}


class PT:
    def __init__(self, hf, hb, off, n, res):
        self.hf, self.hb, self.off, self.n = hf, hb, off, n
        self.res = res

    def f(self, a=0, b=None, rows=128):
        b = self.n if b is None else b
        return self.hf[0:rows, self.off + a:self.off + b]

    def b(self, a=0, b=None, rows=128):
        b = 2 * self.n if b is None else b
        return self.hb[0:rows, 2 * self.off + a:2 * self.off + b]


class Multi:
    def __init__(self, aps):
        self.aps = aps

    def __getitem__(self, k):
        if isinstance(k, tuple):
            return self.aps[k[0]][k[1:]]
        return self.aps[k]


class Ctx:
    def __init__(self, nc, es, consts, debug):
        self.nc = nc
        self.es = es
        self.P = Prog(nc, es)
        self.debug = debug
        self.dram = {}
        self.wdecl = []
        self.consts_np = consts
        self.banks = []
        for i in range(8):
            h = es.enter_context(nc.psum_tensor(f"bank{i}", [128, 512], F32))
            self.banks.append((h, h.bitcast(BF16), Res(f"bank{i}", excl=True)))
        self.set_psum("full")
        self.uid = 0

    def set_psum(self, mode):
        self.ps_tiles = []
        for i, (hf, hb, res) in enumerate(self.banks):
            self.ps_tiles.append(PT(hf, hb, 0, 512, res))
        self.ps_i = 0

    def ps(self):
        t = self.ps_tiles[self.ps_i % len(self.ps_tiles)]
        self.ps_i += 1
        return t

    def din(self, name, shape, dt):
        self.dram[name] = self.nc.dram_tensor(name, list(shape), dt, kind="ExternalInput").ap()
        return self.dram[name]

    def w(self, name, layer=None):
        if layer is not None and name in PER_LAYER:
            key = f"{name}_{layer}"
            if key not in self.dram:
                self.din(key, W_SHAPES[name][1:], F32)
                self.wdecl.append((key, name, layer))
            return self.dram[key]
        if name not in self.dram:
            self.din(name, W_SHAPES[name], F32)
            self.wdecl.append((name, name, None))
        ap = self.dram[name]
        return ap if layer is None else ap[layer]

    def dout(self, name, shape, dt):
        self.dram[name] = self.nc.dram_tensor(name, list(shape), dt, kind="ExternalOutput").ap()
        return self.dram[name]

    def dscr(self, name, shape, dt):
        self.dram[name] = self.nc.dram_tensor(name, list(shape), dt).ap()
        return self.dram[name]

    def sb(self, st, name, shape, dt):
        return self.P.sb(st, name, shape, dt)

    def X(self, eng, name, ins, outs, **kw):
        e = self.P.eng[eng]
        return self.P.op(eng, lambda: getattr(e, name)(**kw), ins=ins, outs=outs)

    def mm(self, ps, out, lhsT, rhs, ins, start=True, stop=True):
        return self.P.op(PE, lambda: self.nc.tensor.matmul(out, lhsT=lhsT, rhs=rhs, start=start, stop=stop),
                         ins=ins, outs=[ps])

    def tr(self, ps, out, in_, ident, ins):
        return self.P.op(PE, lambda: self.nc.tensor.transpose(out, in_, ident), ins=ins, outs=[ps])

    def Dm(self, q, ins, outs, **kw):
        e = self.P.eng[q]
        return self.P.dma(q, lambda: e.dma_start(**kw), ins=ins, outs=outs)

    def DI(self, ins, outs, **kw):
        return self.P.dma(POOL, lambda: self.nc.gpsimd.indirect_dma_start(**kw), ins=ins, outs=outs)

    def act(self, ins, outs, **kw):
        return self.X(ACT, "activation", ins, outs, **kw)

    def barrier(self):
        self.P.barrier()


def load_consts(K, st):
    nc = K.nc
    C = {}
    for name in ["ident_f", "ident_b", "blockones_f", "blkmask", "mT", "mA", "segmask", "sel", "cs_tab"]:
        arr = K.consts_np[name]
        d = K.din("c_" + name, arr.shape, NP2DT[arr.dtype])
        t = K.sb(st, name, arr.shape, NP2DT[arr.dtype])
        K.Dm(SP, [], [t], out=t[:], in_=d)
        C[name] = t
    for T in (TX, TC):
        arr = K.consts_np[f"tok_tab{T}"]
        K.din(f"c_tok_tab{T}", arr.shape, BF16)
    eps = K.sb(st, "eps", [128, 4], F32)
    K.X(DVE, "memset", [], [eps], ap=eps[:, 0:1], constant=1e-6)
    K.X(DVE, "memset", [], [eps], ap=eps[:, 1:2], constant=1e-12)
    K.X(DVE, "memset", [], [eps], ap=eps[:, 2:3], constant=64e-5)
    K.X(DVE, "memset", [], [eps], ap=eps[:, 3:4], constant=0.0)
    C["eps"] = eps
    K.C = C


def phase_adaln(K, st_global, n_layers):
    nc, C = K.nc, K.C
    modA = K.sb(st_global, "modA", [128, DEPTH, 48, 3], F32)
    K.modA = modA
    modrow = K.dscr("modrow", [DEPTH, 3, 6144], F32)
    ada_b = K.w("ada_b")
    with contextlib.ExitStack() as st:
        crow = K.sb(st, "crow", [3, 1024], F32)
        K.Dm(SP, [], [crow], out=crow[0:2, :], in_=K.dram["c"])
        K.Dm(SP, [crow], [crow], out=crow[2:3, :], in_=K.dram["c_ctx"])
        srow = K.sb(st, "srow", [3, 1024], F32)
        K.act([crow], [srow], out=srow[:], in_=crow[:], func=AF.Silu)
        sT = K.sb(st, "sT", [128, 8, 3], BF16)
        for ch in range(8):
            ps = K.ps()
            K.tr(ps, ps.f(0, 3), srow[0:3, ch * 128:(ch + 1) * 128], C["ident_f"][0:3, 0:3], [srow, C["ident_f"]])
            K.X(DVE, "tensor_copy", [ps], [sT], out=sT[:, ch, :], in_=ps.f(0, 3))
        wt = [K.sb(st, f"adaw{i}", [128, 8, 512], BF16) for i in range(2)]
        brow = [K.sb(st, f"adab{i}", [3, 512], F32) for i in range(2)]
        rows = [K.sb(st, f"adar{i}", [3, 512], F32) for i in range(2)]
        it = 0
        for i in range(n_layers):
            for cb in range(12):
                w = wt[it % 2]
                bb = brow[it % 2]
                rr = rows[it % 2]
                it += 1
                K.Dm(POOL, [], [w], out=w[:], in_=K.w("ada_w", i)[:, cb * 512:(cb + 1) * 512].rearrange("(kc p) n -> p kc n", p=128))
                K.Dm(SP, [], [bb], out=bb[:], in_=ada_b[i:i + 1, cb * 512:(cb + 1) * 512].broadcast_to([3, 512]))
                ps = K.ps()
                for kc in range(8):
                    K.mm(ps, ps.f(0, 512, rows=3), sT[:, kc, :], w[:, kc, :], [sT, w], start=(kc == 0), stop=(kc == 7))
                K.X(DVE, "tensor_tensor", [ps, bb], [rr], out=rr[:], in0=ps.f(0, 512, rows=3), in1=bb[:], op=ALU.add)
                K.Dm(SP, [rr], [], out=modrow[i, :, cb * 512:(cb + 1) * 512], in_=rr[:])
                ps2 = K.ps()
                for oc in range(4):
                    K.tr(ps2, ps2.f(oc * 4, oc * 4 + 3), rr[0:3, oc * 128:(oc + 1) * 128], C["ident_f"][0:3, 0:3], [rr, C["ident_f"]])
                K.X(ACT, "copy", [ps2], [modA], out=modA[:, i, cb * 4:(cb + 1) * 4, :],
                    in_=ps2.f(0, 16).rearrange("p (a b) -> p a b", b=4)[:, :, 0:3])
    K.barrier()


def load_cols(K, st, name, row_aps):
    C = K.C
    n = len(row_aps)
    out = K.sb(st, name, [128, 8, n], F32)
    with contextlib.ExitStack() as s2:
        rows = K.sb(s2, name + "_rows", [n, 1024], F32)
        for j, ap in enumerate(row_aps):
            K.Dm(SP, [rows] if j else [], [rows], out=rows[j:j + 1, :], in_=ap)
        for ch in range(8):
            ps = K.ps()
            K.tr(ps, ps.f(0, n), rows[0:n, ch * 128:(ch + 1) * 128], C["ident_f"][0:n, 0:n], [rows, C["ident_f"]])
            K.X(DVE, "tensor_copy", [ps], [out], out=out[:, ch, :], in_=ps.f(0, n))
        K.barrier()
    return out


def mod_scalars(K, st, i, which_sc, which_sh, ng_cols, name, ng_idx=0):
    modA = K.modA
    scl = K.sb(st, name + "_scl", [128, 8, 3], F32)
    sh = K.sb(st, name + "_sh", [128, 8, 3], F32)
    K.X(DVE, "tensor_scalar", [modA], [scl], out=scl[:], in0=modA[:, i, which_sc * 8:(which_sc + 1) * 8, :],
        scalar1=1.0, scalar2=None, op0=ALU.add)
    K.X(DVE, "tensor_tensor", [scl, ng_cols], [scl], out=scl[:], in0=scl[:], in1=ng_cols[:, :, ng_idx:ng_idx + 1].broadcast_to([128, 8, 3]),
        op=ALU.mult)
    K.X(DVE, "tensor_copy", [modA], [sh], out=sh[:], in_=modA[:, i, which_sh * 8:(which_sh + 1) * 8, :])
    return scl, sh


def norm_hT(K, st, src, T, scl, sh, j, hT, tmp):
    nc, C = K.nc, K.C
    xin, xnb, stat = tmp
    nt = T // 128
    K.Dm(SP, [], [xin[0]], out=xin[0][:], in_=src[0:128, :])
    for tt in range(nt):
        xt = xin[tt % 2]
        if tt + 1 < nt:
            K.Dm(SP, [], [xin[(tt + 1) % 2]], out=xin[(tt + 1) % 2][:], in_=src[(tt + 1) * 128:(tt + 2) * 128, :])
        xb = xnb[tt % 2]
        s = stat[tt % 2]
        K.act([xt], [xb, s], out=xb[:], in_=xt[:], func=AF.Square, accum_out=s[:, 0:1])
        K.act([s, C["eps"]], [s], out=s[:, 1:2], in_=s[:, 0:1], func=AF.Sqrt, bias=C["eps"][:, 0:1], scale=1.0 / D)
        K.X(DVE, "reciprocal", [s], [s], out=s[:, 2:3], in_=s[:, 1:2])
        K.act([xt, s], [xb], out=xb[:], in_=xt[:], func=AF.Copy, scale=s[:, 2:3])
        for hf in range(2):
            ps = K.ps()
            for c4 in range(4):
                ch = hf * 4 + c4
                K.tr(ps, ps.b(c4 * 128, (c4 + 1) * 128), xb[:, ch * 128:(ch + 1) * 128], C["ident_b"][:], [xb, C["ident_b"]])
            for c4 in range(4):
                ch = hf * 4 + c4
                if hf == 0:
                    K.X(DVE, "tensor_scalar", [ps, scl, sh], [hT], out=hT[:, ch, tt * 128:(tt + 1) * 128],
                        in0=ps.b(c4 * 128, (c4 + 1) * 128), scalar1=scl[:, ch, j:j + 1], scalar2=sh[:, ch, j:j + 1],
                        op0=ALU.mult, op1=ALU.add)
                else:
                    K.act([ps, scl, sh], [hT], out=hT[:, ch, tt * 128:(tt + 1) * 128], in_=ps.b(c4 * 128, (c4 + 1) * 128),
                          func=AF.Identity, scale=scl[:, ch, j:j + 1], bias=sh[:, ch, j:j + 1])


def norm_tmp(K, st):
    xin = [K.sb(st, f"nx{i}", [128, 1024], F32) for i in range(2)]
    xnb = [K.sb(st, f"nb{i}", [128, 1024], BF16) for i in range(2)]
    stat = [K.sb(st, f"ns{i}", [128, 4], F32) for i in range(2)]
    return xin, xnb, stat


def seq_list(K, need_ctx):
    l = [("x", b, TX, K.dram["xs"][b], b) for b in range(NBC)]
    if need_ctx:
        l += [("c", b, TC, K.dram["cs"][b], 2) for b in range(NBC)]
    return l


def bcast_row(K, t, row_ap):
    n = row_ap.shape[-1]
    K.Dm(SP, [], [t], out=t[:], in_=row_ap.broadcast_to([128, n]))


def proj_residual(K, st, fT, T, wo, res_dram, g1bc, gb, tmp):
    xh, t1, t2 = tmp
    it = 0
    for tt in range(T // 128):
        for dh in range(2):
            k = it % 2
            it += 1
            sl = slice(dh * 512, (dh + 1) * 512)
            K.Dm(SP, [], [xh[k]], out=xh[k][:], in_=res_dram[tt * 128:(tt + 1) * 128, sl])
            ps = K.ps()
            for fc in range(8):
                K.mm(ps, ps.f(), fT[:, fc, tt * 128:(tt + 1) * 128], wo[:, fc, sl], [fT, wo], start=(fc == 0), stop=(fc == 7))
            K.X(DVE, "tensor_tensor", [ps, g1bc], [t1[k]], out=t1[k][:], in0=ps.f(), in1=g1bc[:, sl], op=ALU.mult)
            if gb is not None:
                K.X(POOL, "tensor_tensor", [xh[k], gb], [xh[k]], out=xh[k][:], in0=xh[k][:], in1=gb[:, sl], op=ALU.add)
            K.X(POOL, "tensor_tensor", [xh[k], t1[k]], [t2[k]], out=t2[k][:], in0=xh[k][:], in1=t1[k][:], op=ALU.add)
            K.Dm(ACT, [t2[k]], [], out=res_dram[tt * 128:(tt + 1) * 128, sl], in_=t2[k][:])


def proj_tmp(K, st):
    return ([K.sb(st, f"pxh{i}", [128, 512], F32) for i in range(2)],
            [K.sb(st, f"pt1{i}", [128, 512], F32) for i in range(2)],
            [K.sb(st, f"pt2{i}", [128, 512], F32) for i in range(2)])


def phase_fnet(K, i, fi, need_ctx):
    nc, C = K.nc, K.C
    with contextlib.ExitStack() as st:
        ng = load_cols(K, st, "ng", [K.w("norm_g")[i, 0:1, :]])
        scl, sh = mod_scalars(K, st, i, 1, 0, ng, "f")
        wo = K.sb(st, "fwo", [128, 8, 1024], BF16)
        K.Dm(POOL, [], [wo], out=wo[:], in_=K.w("fnet_wo")[fi].rearrange("(kc p) n -> p kc n", p=128))
        bobc = K.sb(st, "bobc", [128, 1024], F32)
        bcast_row(K, bobc, K.w("fnet_bo")[fi:fi + 1, :])
        hT = K.sb(st, "hT", [128, 8, TX], BF16)
        XCS = K.sb(st, "XCS", [128, 16, 2, 512], BF16)
        tabs = [K.sb(st, f"tab{k}", [128, 16, 2, 256], BF16) for k in range(2)]
        g1bc = K.sb(st, "g1bc", [128, 1024], F32)
        gb = K.sb(st, "gb", [128, 1024], F32)
        ntmp = norm_tmp(K, st)
        ptmp = proj_tmp(K, st)
        tab_it = 0
        for (kind, b, T, res, j) in seq_list(K, need_ctx):
            nt = T // 128
            tokt = K.dram[f"c_tok_tab{T}"]
            bcast_row(K, g1bc, K.dram["modrow"][i, j:j + 1, 2 * 1024:3 * 1024])
            K.X(POOL, "tensor_tensor", [g1bc, bobc], [gb], out=gb[:], in0=g1bc[:], in1=bobc[:], op=ALU.mult)
            norm_hT(K, st, res, T, scl, sh, j, hT, ntmp)
            fT = hT
            for half in range(2):
                for tt in range(nt):
                    for gg in range(2):
                        g = 2 * half + gg
                        ps = K.ps()
                        for ci in range(2):
                            K.mm(ps, ps.f(), hT[:, 2 * g + ci, tt * 128:(tt + 1) * 128], C["cs_tab"][:, ci, :],
                                 [hT, C["cs_tab"]], start=(ci == 0), stop=(ci == 1))
                        eng = DVE if (tt + gg) % 2 == 0 else ACT
                        if eng == DVE:
                            K.X(DVE, "tensor_copy", [ps], [XCS], out=XCS[:, tt, :, gg * 256:(gg + 1) * 256],
                                in_=ps.f().rearrange("p (a c) -> p a c", a=2))
                        else:
                            K.X(ACT, "copy", [ps], [XCS], out=XCS[:, tt, :, gg * 256:(gg + 1) * 256],
                                in_=ps.f().rearrange("p (a c) -> p a c", a=2))
                NBK = 256
                for tb in range(T // NBK):
                    tab = tabs[tab_it % 2]
                    tab_it += 1
                    for a in range(2):
                        K.Dm(SP, [tab] if a else [], [tab], out=tab[:, 0:nt, a, :],
                             in_=tokt[:, a, tb * NBK:(tb + 1) * NBK].rearrange("(tt p) n -> p tt n", p=128))
                    for fq in range(4):
                        fc = 4 * half + fq
                        ps = K.ps()
                        for tt in range(nt):
                            for a in range(2):
                                K.mm(ps, ps.f(0, NBK), XCS[:, tt, a, fq * 128:(fq + 1) * 128], tab[:, tt, a, :],
                                     [XCS, tab], start=(tt == 0 and a == 0), stop=(tt == nt - 1 and a == 1))
                        if fq % 2 == 0:
                            K.X(DVE, "tensor_copy", [ps], [fT], out=fT[:, fc, tb * NBK:(tb + 1) * NBK], in_=ps.f(0, NBK))
                        else:
                            K.X(ACT, "copy", [ps], [fT], out=fT[:, fc, tb * NBK:(tb + 1) * NBK], in_=ps.f(0, NBK))
            proj_residual(K, st, fT, T, wo, res, g1bc, gb, ptmp)
        K.barrier()


def phase_final(K):
    nc, C = K.nc, K.C
    with contextlib.ExitStack() as st:
        fg = K.sb(st, "fgbc", [128, 1024], F32)
        bcast_row(K, fg, K.w("final_g")[0:1, :])
        xin = [K.sb(st, f"fx{i}", [128, 1024], F32) for i in range(2)]
        xo = [K.sb(st, f"fo{i}", [128, 1024], F32) for i in range(2)]
        stat = [K.sb(st, f"fs{i}", [128, 4], F32) for i in range(2)]
        it = 0
        for b in range(NBC):
            for tt in range(TX // 128):
                k = it % 2
                it += 1
                K.Dm(SP, [], [xin[k]], out=xin[k][:], in_=K.dram["xs"][b, tt * 128:(tt + 1) * 128, :])
                s = stat[k]
                K.act([xin[k]], [xo[k], s], out=xo[k][:], in_=xin[k][:], func=AF.Square, accum_out=s[:, 0:1])
                K.act([s, C["eps"]], [s], out=s[:, 1:2], in_=s[:, 0:1], func=AF.Sqrt, bias=C["eps"][:, 0:1], scale=1.0 / D)
                K.X(DVE, "reciprocal", [s], [s], out=s[:, 2:3], in_=s[:, 1:2])
                K.X(DVE, "scalar_tensor_tensor", [xin[k], s, fg], [xo[k]], out=xo[k][:], in0=xin[k][:], scalar=s[:, 2:3],
                    in1=fg[:], op0=ALU.mult, op1=ALU.mult)
                K.Dm(ACT, [xo[k]], [], out=K.dram["out"][b, tt * 128:(tt + 1) * 128, :], in_=xo[k][:])
        K.barrier()


def build(n_layers=DEPTH, debug=False, phases=("fnet", "rwkv", "moe"), moe_route_only=False, moe_no_ctx=False, rw_stop=99, prep_stop=-1, prep_cnt=1):
    nc = bass.Bass("TRN2", target_bir_lowering=False)
    consts = make_consts()
    es = contextlib.ExitStack()
    dbg_names = []
    with es:
        K = Ctx(nc, es, consts, debug)
        K.moe_route_only = moe_route_only
        K.moe_no_ctx = moe_no_ctx
        K.rw_stop = rw_stop
        K.prep_stop = prep_stop
        K.prep_cnt = prep_cnt
        K.din("x", [NBC, TX, D], F32)
        K.din("c", [NBC, D], F32)
        K.din("ctx", [NBC, TC, D], F32)
        K.din("c_ctx", [1, D], F32)
        K.dout("out", [NBC, TX, D], F32)
        K.dram["xs"] = Multi([K.dscr(f"xs{b}", [TX, D], F32) for b in range(NBC)])
        K.dram["cs"] = Multi([K.dscr(f"cs{b}", [TC, D], F32) for b in range(NBC)])

        def dbg(name):
            if not debug:
                return
            ox = K.dout("dbg_x_" + name, [NBC, TX, D], F32)
            oc = K.dout("dbg_c_" + name, [NBC, TC, D], F32)
            for b in range(NBC):
                K.Dm(SP, [], [], out=ox[b], in_=K.dram["xs"][b])
                K.Dm(SP, [], [], out=oc[b], in_=K.dram["cs"][b])
            dbg_names.append(name)
            K.barrier()

        load_consts(K, es)
        phase_adaln(K, es, n_layers)
        if debug:
            om = K.dout("dbg_modrow", [DEPTH, 3, 6144], F32)
            K.Dm(SP, [], [], out=om, in_=K.dram["modrow"])
        for b in range(NBC):
            K.Dm(SP, [], [], out=K.dram["xs"][b], in_=K.dram["x"][b])
            K.Dm(SP, [], [], out=K.dram["cs"][b], in_=K.dram["ctx"][b])
        K.barrier()
        try:
            for i in range(n_layers):
                need_ctx = i < DEPTH - 1
                if i % 2 == 0:
                    if "fnet" in phases:
                        phase_fnet(K, i, i // 2, need_ctx)
                else:
                    if "rwkv" in phases:
                        phase_rwkv(K, i, i // 2, need_ctx)
                dbg(f"mix{i}")
                if "moe" in phases:
                    phase_moe(K, i, need_ctx)
                dbg(f"moe{i}")
        except _Stop:
            pass
        K.P.muted = False
        K.barrier()
        phase_final(K)
        print(f"[build] instructions={K.P.n_inst} waits={K.P.n_wait}", flush=True)
    return nc, consts, dbg_names, K.wdecl


def core_inputs(inputs, consts, core, wdecl):
    b0 = core * NBC
    m = {
        "x": np.ascontiguousarray(inputs["x"][b0:b0 + NBC], dtype=np.float32),
        "c": np.ascontiguousarray(inputs["c"][b0:b0 + NBC], dtype=np.float32),
        "ctx": np.ascontiguousarray(inputs["ctx"][b0:b0 + NBC], dtype=np.float32),
        "c_ctx": np.ascontiguousarray(np.asarray(inputs["c_ctx"], dtype=np.float32).reshape(1, D)),
    }
    for key, n, layer in wdecl:
        a = np.asarray(inputs[n], dtype=np.float32).reshape(W_SHAPES[n])
        m[key] = np.ascontiguousarray(a if layer is None else a[layer])
    for k, v in consts.items():
        m["c_" + k] = v
    return m


def kernel(**inputs):
    nc, consts, _, wdecl = build()
    n_cores = 8
    in_maps = [core_inputs(inputs, consts, c, wdecl) for c in range(n_cores)]
    res = run_bass_kernel_spmd(nc, in_maps, core_ids=list(range(n_cores)))
    return np.concatenate([np.asarray(r["out"]) for r in res.results], axis=0).astype(np.float32)


def IOA(ap):
    return bass.IndirectOffsetOnAxis(ap=ap, axis=0)


def topk_rows(K, aff, work, vals, idxs, rounds):
    cur = aff
    for r in range(rounds):
        sl = slice(r * 8, (r + 1) * 8)
        K.X(DVE, "max", [cur], [vals], out=vals[:, sl], in_=cur[:])
        K.X(DVE, "max_index", [vals, cur], [idxs], out=idxs[:, sl], in_max=vals[:, sl], in_values=cur[:])
        if r + 1 < rounds:
            K.X(DVE, "match_replace", [vals, cur], [work], out=work[:], in_to_replace=vals[:, sl], in_values=cur[:],
                imm_value=-1.0)
            cur = work


def phase_moe(K, i, need_ctx):
    nc, C = K.nc, K.C
    if getattr(K, "moe_no_ctx", False):
        need_ctx = False
    if "hsrc_x" not in K.dram:
        K.dram["hsrc_x"] = Multi([K.dscr(f"hsrc_x{b}", [TX, D], BF16) for b in range(NBC)])
        K.dram["hsrc_c"] = Multi([K.dscr(f"hsrc_c{b}", [TC, D], BF16) for b in range(NBC)])
    hsrc = {"x": K.dram["hsrc_x"], "c": K.dram["hsrc_c"]}
    resid = {"x": K.dram["xs"], "c": K.dram["cs"]}
    kinds = [("x", TX, 256)] + ([("c", TC, 32)] if need_ctx else [])
    modrow = K.dram["modrow"]
    with contextlib.ExitStack() as st0:
        idxT = K.sb(st0, "idxT", [128, 2, 48], I32)
        gateT = K.sb(st0, "gateT", [128, 2, 48], F32)
        idxTc = K.sb(st0, "idxTc", [32, 48], I32)
        gcT = K.sb(st0, "gcT", [32, 48], F32)
        g2bc = [K.sb(st0, f"g2bc{j}", [128, 1024], F32) for j in range(3)]
        for j in range(3):
            bcast_row(K, g2bc[j], modrow[i, j:j + 1, 5 * 1024:6 * 1024])
        with contextlib.ExitStack() as st:
            ngbc = K.sb(st, "ngbc", [128, 1024], F32)
            bcast_row(K, ngbc, K.w("norm_g")[i, 1:2, :])
            sclbc, shbc = [], []
            for j in range(3):
                a = K.sb(st, f"scl2bc{j}", [128, 1024], F32)
                b_ = K.sb(st, f"sh2bc{j}", [128, 1024], F32)
                bcast_row(K, a, modrow[i, j:j + 1, 4 * 1024:5 * 1024])
                bcast_row(K, b_, modrow[i, j:j + 1, 3 * 1024:4 * 1024])
                K.X(DVE, "scalar_tensor_tensor", [a, ngbc], [a], out=a[:], in0=a[:], scalar=1.0, in1=ngbc[:],
                    op0=ALU.add, op1=ALU.mult)
                sclbc.append(a)
                shbc.append(b_)
            router = K.sb(st, "router", [128, 8, 16], F32)
            K.Dm(SP, [], [router], out=router[:], in_=K.w("moe_router")[i].rearrange("(kc p) n -> p kc n", p=128))
            xin = [K.sb(st, f"mx{k}", [128, 1024], F32) for k in range(2)]
            hf = [K.sb(st, f"mh{k}", [128, 1024], F32) for k in range(2)]
            hb = [K.sb(st, f"mhb{k}", [128, 1024], BF16) for k in range(2)]
            hTt = [K.sb(st, f"mhT{k}", [128, 8, 128], F32) for k in range(2)]
            stat = [K.sb(st, f"ms{k}", [128, 8], F32) for k in range(2)]
            ex = [K.sb(st, f"mex{k}", [128, 16], F32) for k in range(2)]
            affp = [K.sb(st, f"affp{k}", [128, 48], F32) for k in range(2)]
            for k in range(2):
                K.X(DVE, "memset", [], [affp[k]], ap=affp[k][:], constant=0.0)
            it = 0
            for (kind, T, cap) in kinds:
                nt = T // 128
                affT = K.sb(st, f"affT{kind}", [48, T], F32)
                work = K.sb(st, f"work{kind}", [48, T], F32)
                vals = K.sb(st, f"vals{kind}", [48, max(cap, 64)], F32)
                idxs = K.sb(st, f"idxs{kind}", [48, max(cap, 64)], U32)
                idxf = K.sb(st, f"idxf{kind}", [48, max(cap, 64)], F32)
                for tt in range(nt):
                    ap_ = affp[tt % 2]
                    for si in range(NBC):
                        j = si if kind == "x" else 2
                        k = it % 2
                        it += 1
                        xt, h, s = xin[k], hf[k], stat[k]
                        K.Dm(SP, [], [xt], out=xt[:], in_=resid[kind][si, tt * 128:(tt + 1) * 128, :])
                        K.act([xt], [hb[k], s], out=hb[k][:], in_=xt[:], func=AF.Square, accum_out=s[:, 0:1])
                        K.act([s, C["eps"]], [s], out=s[:, 1:2], in_=s[:, 0:1], func=AF.Sqrt, bias=C["eps"][:, 0:1], scale=1.0 / D)
                        K.X(DVE, "reciprocal", [s], [s], out=s[:, 2:3], in_=s[:, 1:2])
                        K.X(DVE, "scalar_tensor_tensor", [xt, s, sclbc[j]], [h], out=h[:], in0=xt[:], scalar=s[:, 2:3],
                            in1=sclbc[j][:], op0=ALU.mult, op1=ALU.mult)
                        K.X(POOL, "tensor_tensor", [h, shbc[j]], [h], out=h[:], in0=h[:], in1=shbc[j][:], op=ALU.add)
                        K.X(ACT, "copy", [h], [hb[k]], out=hb[k][:], in_=h[:])
                        K.Dm(ACT, [hb[k]], [], out=hsrc[kind][si, tt * 128:(tt + 1) * 128, :], in_=hb[k][:])
                        for hh in range(2):
                            ps = K.ps()
                            for c4 in range(4):
                                ch = hh * 4 + c4
                                K.tr(ps, ps.f(c4 * 128, (c4 + 1) * 128), h[:, ch * 128:(ch + 1) * 128], C["ident_f"][:],
                                     [h, C["ident_f"]])
                            if hh == 0:
                                K.X(DVE, "tensor_copy", [ps], [hTt[k]], out=hTt[k][:, 0:4, :],
                                    in_=ps.f().rearrange("p (a c) -> p a c", a=4))
                            else:
                                K.X(ACT, "copy", [ps], [hTt[k]], out=hTt[k][:, 4:8, :],
                                    in_=ps.f().rearrange("p (a c) -> p a c", a=4))
                        ps = K.ps()
                        for kc in range(8):
                            K.mm(ps, ps.f(0, 16), hTt[k][:, kc, :], router[:, kc, :], [hTt[k], router],
                                 start=(kc == 0), stop=(kc == 7))
                        K.X(DVE, "tensor_reduce", [ps], [s], out=s[:, 3:4], in_=ps.f(0, 16), axis=AX.X, op=ALU.max)
                        K.X(DVE, "tensor_scalar", [s], [s], out=s[:, 4:5], in0=s[:, 3:4], scalar1=-1.0, scalar2=None, op0=ALU.mult)
                        K.act([ps, s], [ex[k], s], out=ex[k][:], in_=ps.f(0, 16), func=AF.Exp, bias=s[:, 4:5], scale=1.0,
                              accum_out=s[:, 5:6])
                        K.X(DVE, "reciprocal", [s], [s], out=s[:, 6:7], in_=s[:, 5:6])
                        K.X(DVE, "tensor_scalar", [ex[k], s], [ap_], out=ap_[:, si * 32:si * 32 + 16], in0=ex[k][:],
                            scalar1=s[:, 6:7], scalar2=None, op0=ALU.mult)
                    ps = K.ps()
                    K.tr(ps, ps.f(0, 128, rows=48), ap_[:, 0:48], C["ident_f"][:], [ap_, C["ident_f"]])
                    K.X(ACT, "copy", [ps], [affT], out=affT[:, tt * 128:(tt + 1) * 128], in_=ps.f(0, 128, rows=48))
                topk_rows(K, affT, work, vals, idxs, cap // 8)
                if kind == "x":
                    K.X(DVE, "tensor_copy", [idxs], [idxf], out=idxf[:, 0:256], in_=idxs[:, 0:256])
                    for ch in range(2):
                        ps = K.ps()
                        K.tr(ps, ps.f(0, 48), idxf[0:48, ch * 128:(ch + 1) * 128], C["ident_f"][0:48, 0:48], [idxf, C["ident_f"]])
                        K.tr(ps, ps.f(64, 112), vals[0:48, ch * 128:(ch + 1) * 128], C["ident_f"][0:48, 0:48], [vals, C["ident_f"]])
                        K.X(DVE, "tensor_copy", [ps], [idxT], out=idxT[:, ch, :], in_=ps.f(0, 48))
                        K.X(DVE, "tensor_copy", [ps], [gateT], out=gateT[:, ch, :], in_=ps.f(64, 112))
                else:
                    K.X(DVE, "tensor_copy", [idxs], [idxf], out=idxf[:, 0:32], in_=idxs[:, 0:32])
                    ps = K.ps()
                    K.tr(ps, ps.f(0, 48, rows=32), idxf[0:48, 0:32], C["ident_f"][0:48, 0:48], [idxf, C["ident_f"]])
                    K.tr(ps, ps.f(64, 112, rows=32), vals[0:48, 0:32], C["ident_f"][0:48, 0:48], [vals, C["ident_f"]])
                    K.X(DVE, "tensor_copy", [ps], [idxTc], out=idxTc[:], in_=ps.f(0, 48, rows=32))
                    K.X(DVE, "tensor_copy", [ps], [gcT], out=gcT[:], in_=ps.f(64, 112, rows=32))
            K.barrier()
            if K.debug and i == 0:
                for nm, t, shp, dt in [("idxT", idxT, [128, 2, 48], I32), ("gateT", gateT, [128, 2, 48], F32),
                                       ("idxTc", idxTc, [32, 48], I32), ("gcT", gcT, [32, 48], F32)]:
                    o = K.dout("dbg_" + nm, shp, dt)
                    K.Dm(SP, [t], [], out=o, in_=t[:])
                o = K.dout("dbg_hsrc_x", [NBC, TX, D], BF16)
                for b in range(NBC):
                    K.Dm(SP, [], [], out=o[b], in_=hsrc["x"][b])
                K.barrier()
        if getattr(K, "moe_route_only", False):
            return
        with contextlib.ExitStack() as st:
            wbuf = [[K.sb(st, f"mw{k}{m}", [128, 8, 1024], BF16) for m in range(3)] for k in range(2)]
            xg = [[K.sb(st, f"xg{k}{m}", [128, 1024], BF16) for m in range(4)] for k in range(2)]
            xgc = [[K.sb(st, f"xgc{k}{b}", [32, 1024], BF16) for b in range(NBC)] for k in range(2)]
            xsT = [K.sb(st, f"xsT{k}", [128, 8, 576], BF16) for k in range(2)]
            hidT = K.sb(st, "hidT", [128, 8, 576], BF16)
            sg = [K.sb(st, f"sg{k}", [128, 576], F32) for k in range(2)]
            yo = [K.sb(st, f"yo{k}", [128, 1024], F32) for k in range(3)]
            wnames = ["moe_wg", "moe_wu", "moe_wd"]
            res_x = [Res(f"xs{b}") for b in range(NBC)]
            res_c = [Res(f"cs{b}") for b in range(NBC)]
            NCX = 576 if need_ctx else 512
            yo_it = 0
            sg_it = 0

            def load_w(e):
                k = e % 2
                for m in range(3):
                    K.Dm(POOL, [], [wbuf[k][m]], out=wbuf[k][m][:],
                         in_=K.w(wnames[m], i)[e].rearrange("(kc p) n -> p kc n", p=128))

            def gather(e):
                k = e % 2
                for si in range(NBC):
                    for ch in range(2):
                        col = si * 32 + e
                        K.DI([idxT], [xg[k][si * 2 + ch]], out=xg[k][si * 2 + ch][:], out_offset=None,
                             in_=hsrc["x"][si], in_offset=IOA(idxT[:, ch, col:col + 1]))
                if need_ctx:
                    for b in range(NBC):
                        K.DI([idxTc], [xgc[k][b]], out=xgc[k][b][:], out_offset=None, in_=hsrc["c"][b],
                             in_offset=IOA(idxTc[:, b * 32 + e:b * 32 + e + 1]))

            load_w(0)
            gather(0)
            for e in range(NE):
                k = e % 2
                if e + 1 < NE:
                    load_w(e + 1)
                    gather(e + 1)
                wg, wu, wd = wbuf[k]
                for f in range(8):
                    ps = K.ps()
                    for t4 in range(4):
                        K.tr(ps, ps.b(t4 * 128, (t4 + 1) * 128), xg[k][t4][:, f * 128:(f + 1) * 128], C["ident_b"][:],
                             [xg[k][t4], C["ident_b"]])
                    if need_ctx:
                        for b in range(NBC):
                            K.tr(ps, ps.b(512 + b * 32, 544 + b * 32), xgc[k][b][:, f * 128:(f + 1) * 128], C["ident_b"][0:32, 0:32],
                                 [xgc[k][b], C["ident_b"]])
                    if f % 2 == 0:
                        K.X(DVE, "tensor_copy", [ps], [xsT[k]], out=xsT[k][:, f, 0:NCX], in_=ps.b(0, NCX))
                    else:
                        K.X(ACT, "copy", [ps], [xsT[k]], out=xsT[k][:, f, 0:NCX], in_=ps.b(0, NCX))
                for fc in range(8):
                    fs = slice(fc * 128, (fc + 1) * 128)
                    psG = K.ps()
                    for f in range(8):
                        K.mm(psG, psG.f(), wg[:, f, fs], xsT[k][:, f, 0:512], [wg, xsT[k]], start=(f == 0), stop=(f == 7))
                    psU = K.ps()
                    for f in range(8):
                        K.mm(psU, psU.f(), wu[:, f, fs], xsT[k][:, f, 0:512], [wu, xsT[k]], start=(f == 0), stop=(f == 7))
                    s_ = sg[sg_it % 2]
                    sg_it += 1
                    K.act([psG], [s_], out=s_[:, 0:512], in_=psG.f(), func=AF.Silu)
                    K.X(DVE, "tensor_tensor", [psU, s_], [hidT], out=hidT[:, fc, 0:512], in0=psU.f(), in1=s_[:, 0:512], op=ALU.mult)
                    if need_ctx:
                        psC = K.ps()
                        for f in range(8):
                            K.mm(psC, psC.f(0, 64), wg[:, f, fs], xsT[k][:, f, 512:576], [wg, xsT[k]], start=(f == 0), stop=(f == 7))
                        for f in range(8):
                            K.mm(psC, psC.f(64, 128), wu[:, f, fs], xsT[k][:, f, 512:576], [wu, xsT[k]], start=(f == 0), stop=(f == 7))
                        K.act([psC], [s_], out=s_[:, 512:576], in_=psC.f(0, 64), func=AF.Silu)
                        K.X(DVE, "tensor_tensor", [psC, s_], [hidT], out=hidT[:, fc, 512:576], in0=psC.f(64, 128), in1=s_[:, 512:576], op=ALU.mult)
                for t4 in range(4):
                    si, ch = t4 // 2, t4 % 2
                    col = si * 32 + e
                    y = yo[yo_it % 3]
                    yo_it += 1
                    for dh in range(2):
                        sl = slice(dh * 512, (dh + 1) * 512)
                        ps = K.ps()
                        for fc in range(8):
                            K.mm(ps, ps.f(), hidT[:, fc, t4 * 128:(t4 + 1) * 128], wd[:, fc, sl], [hidT, wd],
                                 start=(fc == 0), stop=(fc == 7))
                        K.X(DVE, "scalar_tensor_tensor", [ps, gateT, g2bc[si]], [y], out=y[:, sl], in0=ps.f(),
                            scalar=gateT[:, ch, col:col + 1], in1=g2bc[si][:, sl], op0=ALU.mult, op1=ALU.mult)
                    K.DI([y, idxT], [res_x[si]], out=resid["x"][si], out_offset=IOA(idxT[:, ch, col:col + 1]),
                         in_=y[:], in_offset=None, compute_op=ALU.add)
                if need_ctx:
                    for b in range(NBC):
                        y = yo[yo_it % 3]
                        yo_it += 1
                        col = b * 32 + e
                        for dh in range(2):
                            sl = slice(dh * 512, (dh + 1) * 512)
                            ps = K.ps()
                            for fc in range(8):
                                K.mm(ps, ps.f(0, 512, rows=32), hidT[:, fc, 512 + b * 32:544 + b * 32], wd[:, fc, sl], [hidT, wd],
                                     start=(fc == 0), stop=(fc == 7))
                            K.X(DVE, "scalar_tensor_tensor", [ps, gcT, g2bc[2]], [y], out=y[0:32, sl], in0=ps.f(0, 512, rows=32),
                                scalar=gcT[:, col:col + 1], in1=g2bc[2][0:32, sl], op0=ALU.mult, op1=ALU.mult)
                        K.DI([y, idxTc], [res_c[b]], out=resid["c"][b], out_offset=IOA(idxTc[:, col:col + 1]),
                             in_=y[0:32, :], in_offset=None, compute_op=ALU.add)
            K.barrier()


NCH = TL // 64
CMIX, CW0, CA0, CKK, CKA, CRK, CV0, CNG, CLW, CLB = 0, 6, 8, 10, 11, 12, 13, 14, 15, 16


def rw_scratch(K):
    if hasattr(K, "rw"):
        return K.rw
    S = {}
    for d in range(2):
        for n in ("at", "bt", "kt", "rt"):
            S[f"{n}{d}"] = K.dscr(f"rw_{n}{d}", [NBC, D, TL], BF16)
        S[f"GL{d}"] = K.dscr(f"rw_GL{d}", [NBC, D, NCH], F32)
        S[f"y{d}"] = K.dscr(f"rw_y{d}", [NBC, D, TL], F32)
    S["v"] = K.dscr("rw_v", [NBC, D, TL], BF16)
    S["bv"] = K.dscr("rw_bv", [NBC, D, TL], F32)
    S["g"] = K.dscr("rw_g", [NBC, D, TL], F32)
    S["vfirst"] = K.dscr("rw_vfirst", [NBC, D, TL], F32)
    K.rw = S
    return S


def rwkv_cols(K, st, i, ri):
    w = K.w
    rows = [w("rw_mix")[ri, jj:jj + 1, :] for jj in range(6)]
    rows += [w("rw_w0")[ri, 0:1, :], w("rw_w0")[ri, 1:2, :], w("rw_a0")[ri, 0:1, :], w("rw_a0")[ri, 1:2, :]]
    rows += [w("rw_kk")[ri:ri + 1, :], w("rw_ka")[ri:ri + 1, :], w("rw_rk")[ri:ri + 1, :]]
    rows += [w("rw_v0")[0:1, :]]
    rows += [w("norm_g")[i, 0:1, :], w("rw_lnx_w")[ri:ri + 1, :], w("rw_lnx_b")[ri:ri + 1, :]]
    return load_cols(K, st, "rwc", rows)


class _Stop(Exception):
    pass


def rwkv_prep(K, i, ri):
    nc, C = K.nc, K.C

    def chk(k):
        if getattr(K, "prep_stop", -1) == k:
            K.prep_seen = getattr(K, "prep_seen", 0) + 1
            if K.prep_seen >= K.prep_cnt:
                K.P.muted = True
    S = rw_scratch(K)
    with contextlib.ExitStack() as st:
        cols = rwkv_cols(K, st, i, ri)
        omka = K.sb(st, "omka", [128, 8, 1], F32)
        K.X(DVE, "tensor_scalar", [cols], [omka], out=omka[:], in0=cols[:, :, CKA:CKA + 1], scalar1=-1.0, scalar2=1.0,
            op0=ALU.mult, op1=ALU.add)
        scl, sh = mod_scalars(K, st, i, 1, 0, cols, "r", ng_idx=CNG)
        wr, wk, wv = [K.sb(st, n, [128, 8, 1024], BF16) for n in ("wr", "wk", "wv")]
        for t, n in ((wr, "rw_wr"), (wk, "rw_wk"), (wv, "rw_wv")):
            K.Dm(POOL, [], [t], out=t[:], in_=K.w(n)[ri].rearrange("(kc p) n -> p kc n", p=128))
        w1c = K.sb(st, "w1c", [128, 8, 128], BF16)
        a1c = K.sb(st, "a1c", [128, 8, 128], BF16)
        g1w = K.sb(st, "g1w", [128, 8, 128], BF16)
        for d in range(2):
            K.Dm(POOL, [w1c] if d else [], [w1c], out=w1c[:, :, d * 64:(d + 1) * 64],
                 in_=K.w("rw_w1")[ri, d].rearrange("(kc p) n -> p kc n", p=128))
            K.Dm(POOL, [a1c] if d else [], [a1c], out=a1c[:, :, d * 64:(d + 1) * 64],
                 in_=K.w("rw_a1")[ri, d].rearrange("(kc p) n -> p kc n", p=128))
        K.Dm(POOL, [], [g1w], out=g1w[:], in_=K.w("rw_g1")[ri].rearrange("(kc p) n -> p kc n", p=128))
        w2a = K.sb(st, "w2a", [128, 1024], BF16)
        a2a = K.sb(st, "a2a", [128, 1024], BF16)
        g2w = K.sb(st, "g2w", [128, 1024], BF16)
        for d in range(2):
            K.Dm(POOL, [w2a] if d else [], [w2a], out=w2a[d * 64:(d + 1) * 64, :], in_=K.w("rw_w2")[ri, d])
            K.Dm(POOL, [a2a] if d else [], [a2a], out=a2a[d * 64:(d + 1) * 64, :], in_=K.w("rw_a2")[ri, d])
        K.Dm(POOL, [], [g2w], out=g2w[:], in_=K.w("rw_g2")[ri])
        if ri == 1:
            v1w = K.sb(st, "v1w", [128, 8, 32], BF16)
            v2w = K.sb(st, "v2w", [32, 1024], BF16)
            K.Dm(POOL, [], [v1w], out=v1w[:], in_=K.w("rw_v1")[0].rearrange("(kc p) n -> p kc n", p=128))
            K.Dm(POOL, [], [v2w], out=v2w[:], in_=K.w("rw_v2")[0])
        hT = K.sb(st, "rhT", [128, 8, TX], BF16)
        xx = K.sb(st, "rxx", [128, 8, 256], BF16)
        xtmp = K.sb(st, "rxtmp", [128, 8, 256], BF16)
        xj = [K.sb(st, f"rxj{jj}", [128, 8, 256], BF16) for jj in range(6)]
        tw = K.sb(st, "rtw", [128, 256], BF16)
        ta = K.sb(st, "rta", [128, 256], BF16)
        tg = K.sb(st, "rtg", [128, 256], BF16)
        tgf = K.sb(st, "rtgf", [128, 256], F32)
        hcols = K.sb(st, "hcols", [128, 8, 17], F32)
        K.X(DVE, "tensor_scalar", [cols], [hcols], out=hcols[:], in0=cols[:], scalar1=0.5, scalar2=None, op0=ALU.mult)
        tv = K.sb(st, "rtv", [32, 256], BF16)
        ntmp = norm_tmp(K, st)

        def f32t(n, w_=256):
            return K.sb(st, n, [128, w_], F32)
        RK, VG = f32t("RK", 512), f32t("VG", 512)
        SGW, ASIG = K.sb(st, "SGW", [128, 2, 256], F32), K.sb(st, "ASIG", [128, 2, 256], F32)
        SV, KR, SQ_, RN, KKN, NKK, VF, DV = [f32t(n) for n in ("SV", "KR", "SQ", "RN", "KKN", "NKK", "VF", "DV")]
        TMP, BVEC, CUM, CUMR, T3, E1, E2, E3 = [f32t(n) for n in ("TMP", "BVEC", "CUM", "CUMR", "T3", "E1", "E2", "E3")]
        KD = [f32t("KD0"), f32t("KD1")]
        RKR, KS, PR, BVV = [f32t(n) for n in ("RKR", "KS", "PR", "BVV")]
        VB16 = K.sb(st, "VB16", [128, 256], BF16)
        stg = {(n, d): K.sb(st, f"stg_{n}{d}", [128, 256], BF16) for n in ("at", "bt", "kt", "rt") for d in range(2)}
        GLt = [K.sb(st, f"GLt{d}", [128, 4], F32) for d in range(2)]

        def pool_tt(out_t, out, a_t, a, b_t, b, op):
            K.X(POOL, "tensor_tensor", [a_t, b_t], [out_t], out=out, in0=a, in1=b, op=op)

        def dve_tt(out_t, out, a_t, a, b_t, b, op):
            K.X(DVE, "tensor_tensor", [a_t, b_t], [out_t], out=out, in0=a, in1=b, op=op)

        def shift_block(kind, T, t0):
            def sub(c0, c1, oa, ob, ia0, ia1, ib0, ib1):
                pool_tt(xx, xx[:, c0:c1, oa:ob], hT, hT[:, c0:c1, ia0:ia1], hT, hT[:, c0:c1, ib0:ib1], ALU.subtract)

            def neg(c0, c1, oa, ob, ia, ib):
                K.X(POOL, "tensor_scalar", [hT], [xx], out=xx[:, c0:c1, oa:ob], in0=hT[:, c0:c1, ia:ib], scalar1=-1.0,
                    scalar2=None, op0=ALU.mult)
            if kind == "c":
                sub(0, 4, 1, 256, 0, 255, 1, 256)
                neg(0, 4, 0, 1, 0, 1)
                sub(4, 8, 0, 255, 1, 256, 0, 255)
                neg(4, 8, 255, 256, 255, 256)
                return
            hv = lambda c0, c1: hT[:, c0:c1, t0:t0 + 256].rearrange("p c (r w) -> p c r w", w=64)
            xv = lambda c0, c1: xx[:, c0:c1, :].rearrange("p c (r w) -> p c r w", w=64)
            K.X(POOL, "tensor_tensor", [hT], [xx], out=xv(0, 2)[:, :, :, 1:64], in0=hv(0, 2)[:, :, :, 0:63],
                in1=hv(0, 2)[:, :, :, 1:64], op=ALU.subtract)
            K.X(POOL, "tensor_scalar", [hT], [xx], out=xv(0, 2)[:, :, :, 0:1], in0=hv(0, 2)[:, :, :, 0:1], scalar1=-1.0,
                scalar2=None, op0=ALU.mult)
            K.X(POOL, "tensor_tensor", [hT], [xx], out=xv(2, 4)[:, :, :, 0:63], in0=hv(2, 4)[:, :, :, 1:64],
                in1=hv(2, 4)[:, :, :, 0:63], op=ALU.subtract)
            K.X(POOL, "tensor_scalar", [hT], [xx], out=xv(2, 4)[:, :, :, 63:64], in0=hv(2, 4)[:, :, :, 63:64], scalar1=-1.0,
                scalar2=None, op0=ALU.mult)
            if t0 == 0:
                neg(4, 6, 0, 64, 0, 64)
                sub(4, 6, 64, 256, 0, 192, 64, 256)
            else:
                sub(4, 6, 0, 256, t0 - 64, t0 + 192, t0, t0 + 256)
            if t0 + 256 == T:
                sub(6, 8, 0, 192, t0 + 64, t0 + 256, t0, t0 + 192)
                neg(6, 8, 192, 256, t0 + 192, t0 + 256)
            else:
                sub(6, 8, 0, 256, t0 + 64, t0 + 320, t0, t0 + 256)

        for b in range(NBC):
            for (kind, T, src, j, tl0) in (("c", TC, K.dram["cs"][b], 2, 0), ("x", TX, K.dram["xs"][b], b, TC)):
                chk(0)
                norm_hT(K, st, src, T, scl, sh, j, hT, ntmp)
                chk(1)
                for tb in range(T // 256):
                    t0 = tb * 256
                    tl = tl0 + t0
                    shift_block(kind, T, t0)
                    chk(2)
                    for jj in range(6):
                        eng = DVE
                        K.X(eng, "tensor_tensor", [xx, cols], [xtmp], out=xtmp[:], in0=xx[:],
                            in1=cols[:, :, CMIX + jj:CMIX + jj + 1].broadcast_to([128, 8, 256]), op=ALU.mult)
                        K.X(eng, "tensor_tensor", [xtmp, hT], [xj[jj]], out=xj[jj][:], in0=xtmp[:], in1=hT[:, :, t0:t0 + 256],
                            op=ALU.add)
                    xr, xw, xk, xv_, xa, xg_ = xj
                    chk(3)
                    ps = K.ps()
                    for f in range(8):
                        K.mm(ps, ps.f(0, 256), w1c[:, f, :], xw[:, f, :], [w1c, xw], start=(f == 0), stop=(f == 7))
                    chk(31)
                    for f in range(8):
                        K.mm(ps, ps.f(256, 512), a1c[:, f, :], xa[:, f, :], [a1c, xa], start=(f == 0), stop=(f == 7))
                    chk(32)
                    K.act([ps], [tw], out=tw[:], in_=ps.f(0, 256), func=AF.Tanh)
                    chk(33)
                    K.X(ACT, "copy", [ps], [ta], out=ta[:], in_=ps.f(256, 512))
                    chk(34)
                    ps = K.ps()
                    for f in range(8):
                        K.mm(ps, ps.f(0, 256), g1w[:, f, :], xg_[:, f, :], [g1w, xg_], start=(f == 0), stop=(f == 7))
                    if ri == 1:
                        for f in range(8):
                            K.mm(ps, ps.f(256, 512, rows=32), v1w[:, f, :], xv_[:, f, :], [v1w, xv_], start=(f == 0), stop=(f == 7))
                    chk(35)
                    K.act([ps], [tgf], out=tgf[:], in_=ps.f(0, 256), func=AF.Tanh, scale=0.5)
                    K.X(DVE, "tensor_scalar", [tgf], [tg], out=tg[:], in0=tgf[:], scalar1=0.5, scalar2=0.5, op0=ALU.mult, op1=ALU.add)
                    if ri == 1:
                        K.X(ACT, "copy", [ps], [tv], out=tv[:], in_=ps.f(256, 512, rows=32))
                    chk(4)
                    for dc in range(8):
                        fs = slice(dc * 128, (dc + 1) * 128)
                        drow = slice(dc * 128, (dc + 1) * 128)
                        dsl = (b, drow, slice(tl, tl + 256))
                        ps1 = K.ps()
                        for f in range(8):
                            K.mm(ps1, ps1.f(0, 256), wr[:, f, fs], xr[:, f, :], [wr, xr], start=(f == 0), stop=(f == 7))
                        for f in range(8):
                            K.mm(ps1, ps1.f(256, 512), wk[:, f, fs], xk[:, f, :], [wk, xk], start=(f == 0), stop=(f == 7))
                        K.X(ACT, "copy", [ps1], [RK], out=RK[:], in_=ps1.f())
                        chk(41)
                        ps2 = K.ps()
                        for f in range(8):
                            K.mm(ps2, ps2.f(0, 256), wv[:, f, fs], xv_[:, f, :], [wv, xv_], start=(f == 0), stop=(f == 7))
                        K.mm(ps2, ps2.f(256, 512), g2w[:, fs], tg[:], [g2w, tg])
                        K.X(DVE, "tensor_copy", [ps2], [VG], out=VG[:], in_=ps2.f())
                        chk(42)
                        for (wt2, tin, dst, cidx) in ((w2a, tw, SGW, CW0), (a2a, ta, ASIG, CA0)):
                            pss = [K.ps(), K.ps()]
                            for d in range(2):
                                K.mm(pss[d], pss[d].f(0, 256), wt2[d * 64:(d + 1) * 64, fs], tin[d * 64:(d + 1) * 64, :], [wt2, tin])
                            for d in range(2):
                                K.act([pss[d], hcols], [dst], out=dst[:, d, :], in_=pss[d].f(0, 256), func=AF.Tanh,
                                      bias=hcols[:, dc, cidx + d:cidx + d + 1], scale=0.5)
                            K.X(DVE, "tensor_scalar", [dst], [dst], out=dst[:], in0=dst[:], scalar1=0.5, scalar2=0.5, op0=ALU.mult, op1=ALU.add)
                        chk(44)
                        R_, Kk, V_, G_ = RK[:, 0:256], RK[:, 256:512], VG[:, 0:256], VG[:, 256:512]
                        chk(5)
                        K.Dm(SP, [VG], [], out=S["g"][dsl], in_=G_)
                        if ri == 1:
                            ps5 = K.ps()
                            K.mm(ps5, ps5.f(0, 256), v2w[0:32, fs], tv[0:32, :], [v2w, tv])
                            K.act([ps5, hcols], [SV], out=SV[:], in_=ps5.f(0, 256), func=AF.Tanh, bias=hcols[:, dc, CV0:CV0 + 1], scale=0.5)
                            K.X(DVE, "tensor_scalar", [SV], [SV], out=SV[:], in0=SV[:], scalar1=0.5, scalar2=0.5, op0=ALU.mult, op1=ALU.add)
                            K.Dm(SP, [], [VF], out=VF[:], in_=S["vfirst"][dsl])
                            dve_tt(DV, DV[:], VF, VF[:], VG, V_, ALU.subtract)
                            dve_tt(DV, DV[:], DV, DV[:], SV, SV[:], ALU.mult)
                            dve_tt(VG, V_, VG, V_, DV, DV[:], ALU.add)
                        else:
                            K.Dm(SP, [VG], [], out=S["vfirst"][dsl], in_=V_)
                        K.X(ACT, "copy", [VG], [VB16], out=VB16[:], in_=V_)
                        K.Dm(SP, [VB16], [], out=S["v"][dsl], in_=VB16[:])
                        chk(6)
                        K.act([RK, cols], [KR], out=KR[:], in_=Kk, func=AF.Copy, scale=cols[:, dc, CKK:CKK + 1])
                        K.act([RK, cols], [SQ_], out=SQ_[:], in_=Kk, func=AF.Square, scale=cols[:, dc, CKK:CKK + 1])
                        ps6 = K.ps()
                        K.mm(ps6, ps6.f(0, 256), C["blockones_f"][:], SQ_[:], [C["blockones_f"], SQ_])
                        K.act([ps6, C["eps"]], [RN], out=RN[:], in_=ps6.f(0, 256), func=AF.Sqrt, bias=C["eps"][:, 1:2], scale=1.0)
                        K.X(DVE, "reciprocal", [RN], [RN], out=RN[:], in_=RN[:])
                        dve_tt(KKN, KKN[:], KR, KR[:], RN, RN[:], ALU.mult)
                        K.X(DVE, "tensor_scalar", [KKN], [NKK], out=NKK[:], in0=KKN[:], scalar1=-1.0, scalar2=None, op0=ALU.mult)
                        chk(7)
                        for d in range(2):
                            K.act([ASIG, cols, omka], [TMP], out=TMP[:], in_=ASIG[:, d, :], func=AF.Identity,
                                  scale=cols[:, dc, CKA:CKA + 1], bias=omka[:, dc, 0:1])
                            dve_tt(KD[d], KD[d][:], RK, Kk, TMP, TMP[:], ALU.mult)
                            dve_tt(BVEC, BVEC[:], KKN, KKN[:], ASIG, ASIG[:, d, :], ALU.mult)
                            K.X(DVE, "tensor_tensor_scan", [SGW, C["segmask"]], [CUM], out=CUM[:], data0=C["segmask"][:],
                                data1=SGW[:, d, :], initial=0.0, op0=ALU.mult, op1=ALU.add)
                            K.act([CUM], [GLt[d]], out=GLt[d][:], in_=CUM[:].rearrange("p (c j) -> p c j", j=64)[:, :, 63],
                                  func=AF.Exp, scale=-KAPPA)
                            K.Dm(SP, [GLt[d]], [], out=S[f"GL{d}"][b, drow, tl // 64:tl // 64 + 4], in_=GLt[d][:])
                            cum = CUM
                            if d == 1:
                                dve_tt(T3, T3[:], CUM, CUM[:], SGW, SGW[:, d, :], ALU.subtract)
                                K.X(DVE, "tensor_tensor", [CUM, T3], [CUMR], out=CUMR[:].rearrange("p (c j) -> p c j", j=64),
                                    in0=CUM[:].rearrange("p (c j) -> p c j", j=64)[:, :, 63:64].broadcast_to([128, 4, 64]),
                                    in1=T3[:].rearrange("p (c j) -> p c j", j=64), op=ALU.subtract)
                                cum = CUMR
                            K.act([cum], [E1], out=E1[:], in_=cum[:], func=AF.Exp, scale=-KAPPA)
                            pool_tt(stg[("rt", d)], stg[("rt", d)][:], RK, R_, E1, E1[:], ALU.mult)
                            dve_tt(T3, T3[:], cum, cum[:], SGW, SGW[:, d, :], ALU.subtract)
                            K.act([T3], [E2], out=E2[:], in_=T3[:], func=AF.Exp, scale=-KAPPA)
                            pool_tt(stg[("at", d)], stg[("at", d)][:], NKK, NKK[:], E2, E2[:], ALU.mult)
                            K.act([cum], [E3], out=E3[:], in_=cum[:], func=AF.Exp, scale=KAPPA)
                            K.X(DVE, "tensor_tensor", [BVEC, E3], [stg[("bt", d)]], out=stg[("bt", d)][:], in0=BVEC[:], in1=E3[:], op=ALU.mult)
                            K.X(DVE, "tensor_tensor", [KD[d], E3], [stg[("kt", d)]], out=stg[("kt", d)][:], in0=KD[d][:], in1=E3[:], op=ALU.mult)
                            for n in ("at", "bt", "kt", "rt"):
                                K.Dm(SP, [stg[(n, d)]], [], out=S[f"{n}{d}"][dsl], in_=stg[(n, d)][:])
                        chk(8)
                        K.act([RK, cols], [RKR], out=RKR[:], in_=R_, func=AF.Copy, scale=cols[:, dc, CRK:CRK + 1])
                        dve_tt(KS, KS[:], KD[0], KD[0][:], KD[1], KD[1][:], ALU.add)
                        pool_tt(PR, PR[:], RKR, RKR[:], KS, KS[:], ALU.mult)
                        ps7 = K.ps()
                        K.mm(ps7, ps7.f(0, 256), C["blockones_f"][:], PR[:], [C["blockones_f"], PR])
                        K.X(DVE, "tensor_tensor", [ps7, VG], [BVV], out=BVV[:], in0=ps7.f(0, 256), in1=V_, op=ALU.mult)
                        K.Dm(SP, [BVV], [], out=S["bv"][dsl], in_=BVV[:])
                        chk(9)
        K.barrier()


def rwkv_scan_v1(K):
    nc, C = K.nc, K.C
    S = K.rw
    order = {0: list(range(NCH)), 1: [3, 2, 1, 0] + list(range(NCH - 1, 3, -1))}
    identb = C["ident_b"]

    def ecopy(eng, ins, out_t, out, in_):
        if eng == ACT:
            K.X(ACT, "copy", ins, [out_t], out=out, in_=in_)
        else:
            K.X(DVE, "tensor_copy", ins, [out_t], out=out, in_=in_)

    for hpg in range(4):
        with contextlib.ExitStack() as st:
            chains = []
            for b in range(NBC):
                for hq in range(2):
                    for d in range(2):
                        c = dict(b=b, hp=2 * hpg + hq, d=d)
                        nm = f"{b}{hq}{d}"
                        c["cin"] = [{n: K.sb(st, f"ci{nm}{k}{n}", [128, 256], BF16) for n in ("at", "rt", "bt", "kt", "v")}
                                    for k in range(2)]
                        c["ARB"] = K.sb(st, "ARB" + nm, [128, 4, 256], BF16)
                        c["BTB"] = K.sb(st, "BTB" + nm, [128, 4, 128], BF16)
                        c["KTB"] = K.sb(st, "KTB" + nm, [128, 4, 128], BF16)
                        c["VB"] = K.sb(st, "VB" + nm, [128, 4, 128], BF16)
                        c["M1"] = K.sb(st, "M1" + nm, [128, 256], BF16)
                        c["M2"] = K.sb(st, "M2" + nm, [128, 256], BF16)
                        c["A0"] = K.sb(st, "A0" + nm, [128, 128], BF16)
                        c["TF"] = K.sb(st, "TF" + nm, [128, 384], BF16)
                        c["X"] = [K.sb(st, f"X{k}" + nm, [128, 256], BF16) for k in range(2)]
                        c["IP"] = [K.sb(st, f"IP{k}" + nm, [128, 128], BF16) for k in range(2)]
                        c["SQ"] = [K.sb(st, f"SQ{k}" + nm, [128, 256], BF16) for k in range(2)]
                        c["WT"] = K.sb(st, "WT" + nm, [128, 128], BF16)
                        c["U"] = K.sb(st, "U" + nm, [128, 128], BF16)
                        c["Ybd"] = K.sb(st, "Ybd" + nm, [128, 128], F32)
                        c["Hf"] = K.sb(st, "Hf" + nm, [128, 128], F32)
                        c["hg"] = K.sb(st, "hg" + nm, [128, 128], F32)
                        c["Hbf"] = K.sb(st, "Hbf" + nm, [128, 128], BF16)
                        c["yst"] = [K.sb(st, f"yst{k}" + nm, [128, 256], F32) for k in range(2)]
                        c["GL"] = K.sb(st, "GL" + nm, [128, NCH], F32)
                        rows = slice(c["hp"] * 128, (c["hp"] + 1) * 128)
                        c["rows"] = rows
                        K.Dm(SP, [], [c["GL"]], out=c["GL"][:], in_=S[f"GL{d}"][b, rows, :])
                        K.X(DVE, "memset", [], [c["Hf"]], ap=c["Hf"][:], constant=0.0)
                        K.X(DVE, "tensor_copy", [c["Hf"]], [c["Hbf"]], out=c["Hbf"][:], in_=c["Hf"][:])
                        chains.append(c)

            def load_group(c, grp, k):
                ci = c["cin"][k]
                b, d, rows = c["b"], c["d"], c["rows"]
                tsl = slice(grp * 256, (grp + 1) * 256)
                for n in ("at", "rt", "bt", "kt"):
                    K.Dm(SP, [], [ci[n]], out=ci[n][:], in_=S[f"{n}{d}"][b, rows, tsl])
                K.Dm(SP, [], [ci["v"]], out=ci["v"][:], in_=S["v"][b, rows, tsl])

            def expand_group(c, k):
                ci = c["cin"][k]
                msk = C["blkmask"][:].rearrange("p (h i) -> p h i", h=2).unsqueeze(1).broadcast_to([128, 4, 2, 64])

                def ex(src, dst_t, dst_ap):
                    K.X(POOL, "tensor_tensor", [src, C["blkmask"]], [dst_t],
                        out=dst_ap.rearrange("p c (h i) -> p c h i", h=2),
                        in0=src[:].rearrange("p (c i) -> p c i", i=64).unsqueeze(2).broadcast_to([128, 4, 2, 64]),
                        in1=msk, op=ALU.mult)
                ex(ci["at"], c["ARB"], c["ARB"][:, :, 0:128])
                ex(ci["rt"], c["ARB"], c["ARB"][:, :, 128:256])
                ex(ci["bt"], c["BTB"], c["BTB"][:, :, :])
                ex(ci["kt"], c["KTB"], c["KTB"][:, :, :])
                ex(ci["v"], c["VB"], c["VB"][:, :, :])

            def stage(per_bank, pe_fn, ev_fn, engs=(DVE, ACT)):
                for gi in range(0, len(chains), per_bank):
                    ps = K.ps()
                    grp = chains[gi:gi + per_bank]
                    for si, c in enumerate(grp):
                        pe_fn(c, ps, si)
                    eng = engs[(gi // per_bank) % len(engs)]
                    for si, c in enumerate(grp):
                        ev_fn(c, ps, si, eng)

            for c in chains:
                load_group(c, order[c["d"]][0] // 4, 0)
            for r in range(NCH):
                for c in chains:
                    n = order[c["d"]][r]
                    c["n"], c["wi"], c["grp"] = n, n % 4, n // 4
                    c["gk"] = (r // 4) % 2
                    if r % 4 == 0:
                        expand_group(c, c["gk"])
                        if r + 4 < NCH:
                            load_group(c, order[c["d"]][r + 4] // 4, 1 - c["gk"])
                stage(2, lambda c, ps, s: K.mm(ps, ps.f(s * 256, s * 256 + 256), c["BTB"][:, c["wi"], :], c["ARB"][:, c["wi"], :],
                                               [c["BTB"], c["ARB"]]),
                      lambda c, ps, s, e: K.X(DVE, "tensor_tensor", [ps, C["mT"]], [c["M1"]], out=c["M1"][:],
                                              in0=ps.f(s * 256, s * 256 + 256), in1=C["mT"][:, c["d"], :], op=ALU.mult), engs=(DVE,))
                stage(2, lambda c, ps, s: K.mm(ps, ps.f(s * 256, s * 256 + 256), c["KTB"][:, c["wi"], :], c["ARB"][:, c["wi"], :],
                                               [c["KTB"], c["ARB"]]),
                      lambda c, ps, s, e: K.X(DVE, "tensor_tensor", [ps, C["mT"]], [c["M2"]], out=c["M2"][:],
                                              in0=ps.f(s * 256, s * 256 + 256), in1=C["mT"][:, c["d"], :], op=ALU.mult), engs=(DVE,))
                stage(4, lambda c, ps, s: K.mm(ps, ps.f(s * 128, s * 128 + 128), c["ARB"][:, c["wi"], 0:128], c["BTB"][:, c["wi"], :],
                                               [c["ARB"], c["BTB"]]),
                      lambda c, ps, s, e: K.X(DVE, "tensor_tensor", [ps, C["mA"]], [c["A0"]], out=c["A0"][:],
                                              in0=ps.f(s * 128, s * 128 + 128), in1=C["mA"][:, c["d"], :], op=ALU.mult), engs=(DVE,))
                for c in chains:
                    K.X(POOL, "tensor_tensor", [c["M1"], identb], [c["IP"][0]], out=c["IP"][0][:], in0=c["M1"][:, 0:128],
                        in1=identb[:], op=ALU.add)

                def tr_pe(c, ps, s):
                    srcs = [(c["ARB"], c["ARB"][:, c["wi"], 0:128]), (c["BTB"], c["BTB"][:, c["wi"], :]),
                            (c["KTB"], c["KTB"][:, c["wi"], :]), (c["VB"], c["VB"][:, c["wi"], :])]
                    for q, (t, ap) in enumerate(srcs):
                        K.tr(ps, ps.b(s * 512 + q * 128, s * 512 + (q + 1) * 128), ap, identb[:], [t, identb])

                def tr_ev(c, ps, s, e):
                    ecopy(e, [ps], c["TF"], c["TF"][:], ps.b(s * 512 + 128, s * 512 + 512))
                    ecopy(e, [ps], c["X"][0], c["X"][0][:, 0:128], ps.b(s * 512, s * 512 + 128))
                stage(2, tr_pe, tr_ev)
                stage(4, lambda c, ps, s: K.mm(ps, ps.f(s * 128, s * 128 + 128), c["M2"][:, 0:128], c["TF"][:, 256:384], [c["M2"], c["TF"]]),
                      lambda c, ps, s, e: ecopy(e, [ps], c["X"][0], c["X"][0][:, 128:256], ps.f(s * 128, s * 128 + 128)))

                def sq_pe(m):
                    def f(c, ps, s):
                        if m == 0:
                            A_t, A_ap, AT_t, AT_ap = c["A0"], c["A0"][:], c["M1"], c["M1"][:, 0:128]
                        else:
                            q = c["SQ"][(m - 1) % 2]
                            A_t, A_ap, AT_t, AT_ap = q, q[:, 0:128], q, q[:, 128:256]
                        K.mm(ps, ps.f(s * 256, s * 256 + 128), AT_ap, A_ap, [A_t, AT_t])
                        K.mm(ps, ps.f(s * 256 + 128, s * 256 + 256), A_ap, AT_ap, [A_t, AT_t])
                    return f

                def sq_ev(m):
                    def f(c, ps, s, e):
                        ecopy(e, [ps], c["SQ"][m % 2], c["SQ"][m % 2][:], ps.f(s * 256, s * 256 + 256))
                    return f

                def neu_pe(m):
                    def f(c, ps, s):
                        K.mm(ps, ps.f(s * 256, s * 256 + 256), c["IP"][m % 2][:], c["X"][m % 2][:], [c["IP"][m % 2], c["X"][m % 2]])
                    return f

                def neu_ev(m):
                    def f(c, ps, s, e):
                        ecopy(e, [ps], c["X"][(m + 1) % 2], c["X"][(m + 1) % 2][:], ps.f(s * 256, s * 256 + 256))
                    return f
                for m in range(5):
                    stage(2, sq_pe(m), sq_ev(m))
                    stage(2, neu_pe(m), neu_ev(m), engs=(ACT, DVE))
                    for c in chains:
                        K.X(POOL, "tensor_tensor", [c["SQ"][m % 2], identb], [c["IP"][(m + 1) % 2]], out=c["IP"][(m + 1) % 2][:],
                            in0=c["SQ"][m % 2][:, 128:256], in1=identb[:], op=ALU.add)
                stage(2, neu_pe(5), neu_ev(5))
                stage(4, lambda c, ps, s: K.tr(ps, ps.b(s * 256, s * 256 + 128), c["X"][0][:, 0:128], identb[:], [c["X"][0], identb]),
                      lambda c, ps, s, e: ecopy(e, [ps], c["WT"], c["WT"][:], ps.b(s * 256, s * 256 + 128)))
                stage(4, lambda c, ps, s: K.mm(ps, ps.f(s * 128, s * 128 + 128), c["WT"][:], c["Hbf"][:], [c["WT"], c["Hbf"]]),
                      lambda c, ps, s, e: K.X(DVE, "tensor_tensor", [ps, c["X"][0]], [c["U"]], out=c["U"][:],
                                              in0=ps.f(s * 128, s * 128 + 128), in1=c["X"][0][:, 128:256], op=ALU.add), engs=(DVE,))

                def y_pe(c, ps, s):
                    o = ps.f(s * 128, s * 128 + 128)
                    K.mm(ps, o, c["ARB"][:, c["wi"], 128:256], c["Hbf"][:], [c["ARB"], c["Hbf"]], start=True, stop=False)
                    K.mm(ps, o, c["M1"][:, 128:256], c["U"][:], [c["M1"], c["U"]], start=False, stop=False)
                    K.mm(ps, o, c["M2"][:, 128:256], c["TF"][:, 256:384], [c["M2"], c["TF"]], start=False, stop=True)
                stage(4, y_pe, lambda c, ps, s, e: ecopy(e, [ps], c["Ybd"], c["Ybd"][:], ps.f(s * 128, s * 128 + 128)), engs=(ACT,))
                for c in chains:
                    K.X(POOL, "tensor_scalar", [c["Hf"], c["GL"]], [c["hg"]], out=c["hg"][:], in0=c["Hf"][:],
                        scalar1=c["GL"][:, c["n"]:c["n"] + 1], scalar2=None, op0=ALU.mult)

                def h_pe(c, ps, s):
                    o = ps.f(s * 128, s * 128 + 128)
                    K.mm(ps, o, c["TF"][:, 0:128], c["U"][:], [c["TF"], c["U"]], start=True, stop=False)
                    K.mm(ps, o, c["TF"][:, 128:256], c["TF"][:, 256:384], [c["TF"]], start=False, stop=True)

                def h_ev(c, ps, s, e):
                    K.X(DVE, "scalar_tensor_tensor", [ps, c["GL"], c["hg"]], [c["Hf"]], out=c["Hf"][:], in0=ps.f(s * 128, s * 128 + 128),
                        scalar=c["GL"][:, c["n"]:c["n"] + 1], in1=c["hg"][:], op0=ALU.mult, op1=ALU.add)
                stage(4, h_pe, h_ev, engs=(DVE,))
                for c in chains:
                    K.X(ACT, "copy", [c["Hf"]], [c["Hbf"]], out=c["Hbf"][:], in_=c["Hf"][:])
                stage(4, lambda c, ps, s: K.mm(ps, ps.f(s * 128, s * 128 + 64), c["Ybd"][:], C["sel"][:], [c["Ybd"], C["sel"]]),
                      lambda c, ps, s, e: ecopy(e, [ps], c["yst"][c["gk"]], c["yst"][c["gk"]][:, c["wi"] * 64:(c["wi"] + 1) * 64],
                                                ps.f(s * 128, s * 128 + 64)), engs=(ACT, DVE))
                if r % 4 == 3:
                    for c in chains:
                        ys = c["yst"][c["gk"]]
                        K.Dm(SP, [ys], [], out=S[f"y{c['d']}"][c["b"], c["rows"], c["grp"] * 256:(c["grp"] + 1) * 256], in_=ys[:])
            K.barrier()


def rwkv_scan(K):
    nc, C = K.nc, K.C
    S = K.rw
    order = {0: list(range(NCH)), 1: [3, 2, 1, 0] + list(range(NCH - 1, 3, -1))}
    identb = C["ident_b"]
    id2 = identb[:].unsqueeze(1).broadcast_to([128, 2, 128])

    def ecopy(eng, ins, out_t, out, in_):
        if eng == ACT:
            K.X(ACT, "copy", ins, [out_t], out=out, in_=in_)
        else:
            K.X(DVE, "tensor_copy", ins, [out_t], out=out, in_=in_)

    def v3(ap, a=2):
        return ap.rearrange("p (a b) -> p a b", a=a)

    for hpg in range(4):
        with contextlib.ExitStack() as st:
            pairs, chains = [], []
            for b in range(NBC):
                for hq in range(2):
                    nm = f"{b}{hq}"
                    p = dict(b=b, hp=2 * hpg + hq, idx=len(pairs))
                    p["M1"] = K.sb(st, "M1" + nm, [128, 2, 256], BF16)
                    p["M2"] = K.sb(st, "M2" + nm, [128, 2, 256], BF16)
                    p["A0"] = K.sb(st, "A0" + nm, [128, 2, 128], BF16)
                    p["TF"] = K.sb(st, "TF" + nm, [128, 2, 384], BF16)
                    p["X"] = [K.sb(st, f"X{k}" + nm, [128, 2, 256], BF16) for k in range(2)]
                    p["IP"] = [K.sb(st, f"IP{k}" + nm, [128, 2, 128], BF16) for k in range(2)]
                    p["SQ"] = [K.sb(st, f"SQ{k}" + nm, [128, 2, 256], BF16) for k in range(2)]
                    p["WT"] = K.sb(st, "WT" + nm, [128, 2, 128], BF16)
                    p["U"] = K.sb(st, "U" + nm, [128, 2, 128], BF16)
                    p["Ybd"] = K.sb(st, "Ybd" + nm, [128, 2, 128], F32)
                    p["Hf"] = K.sb(st, "Hf" + nm, [128, 2, 128], F32)
                    p["hg"] = K.sb(st, "hg" + nm, [128, 2, 128], F32)
                    p["Hbf"] = K.sb(st, "Hbf" + nm, [128, 2, 128], BF16)
                    K.X(DVE, "memset", [], [p["Hf"]], ap=p["Hf"][:], constant=0.0)
                    K.X(DVE, "tensor_copy", [p["Hf"]], [p["Hbf"]], out=p["Hbf"][:], in_=p["Hf"][:])
                    rows = slice(p["hp"] * 128, (p["hp"] + 1) * 128)
                    p["ch"] = []
                    for d in range(2):
                        cn = nm + str(d)
                        c = dict(b=b, hp=p["hp"], d=d, pair=p, rows=rows)
                        c["cin"] = [{n: K.sb(st, f"ci{cn}{k}{n}", [128, 256], BF16) for n in ("at", "rt", "bt", "kt", "v")}
                                    for k in range(2)]
                        c["ARB"] = K.sb(st, "ARB" + cn, [128, 4, 256], BF16)
                        c["BTB"] = K.sb(st, "BTB" + cn, [128, 4, 128], BF16)
                        c["KTB"] = K.sb(st, "KTB" + cn, [128, 4, 128], BF16)
                        c["VB"] = K.sb(st, "VB" + cn, [128, 4, 128], BF16)
                        c["yst"] = [K.sb(st, f"yst{k}" + cn, [128, 256], F32) for k in range(2)]
                        c["GL"] = K.sb(st, "GL" + cn, [128, NCH], F32)
                        K.Dm(SP, [], [c["GL"]], out=c["GL"][:], in_=S[f"GL{d}"][b, rows, :])
                        p["ch"].append(c)
                        chains.append(c)
                    pairs.append(p)

            def load_group(c, grp, k):
                ci = c["cin"][k]
                b, d, rows = c["b"], c["d"], c["rows"]
                tsl = slice(grp * 256, (grp + 1) * 256)
                for n in ("at", "rt", "bt", "kt"):
                    K.Dm(SP, [], [ci[n]], out=ci[n][:], in_=S[f"{n}{d}"][b, rows, tsl])
                K.Dm(SP, [], [ci["v"]], out=ci["v"][:], in_=S["v"][b, rows, tsl])

            def expand_group(c, k):
                ci = c["cin"][k]
                msk = C["blkmask"][:].rearrange("p (h i) -> p h i", h=2).unsqueeze(1).broadcast_to([128, 4, 2, 64])

                def ex(src, dst_t, dst_ap):
                    K.X(POOL, "tensor_tensor", [src, C["blkmask"]], [dst_t],
                        out=dst_ap.rearrange("p c (h i) -> p c h i", h=2),
                        in0=src[:].rearrange("p (c i) -> p c i", i=64).unsqueeze(2).broadcast_to([128, 4, 2, 64]),
                        in1=msk, op=ALU.mult)
                ex(ci["at"], c["ARB"], c["ARB"][:, :, 0:128])
                ex(ci["rt"], c["ARB"], c["ARB"][:, :, 128:256])
                ex(ci["bt"], c["BTB"], c["BTB"][:, :, :])
                ex(ci["kt"], c["KTB"], c["KTB"][:, :, :])
                ex(ci["v"], c["VB"], c["VB"][:, :, :])

            def stage(pe_fn, ev_fn, engs=(DVE, ACT)):
                for p in pairs:
                    ps = K.ps()
                    for c in p["ch"]:
                        pe_fn(c, p, ps, c["d"])
                    ev_fn(p, ps, engs[p["idx"] % len(engs)])

            for c in chains:
                load_group(c, order[c["d"]][0] // 4, 0)
            for r in range(NCH):
                for c in chains:
                    n = order[c["d"]][r]
                    c["n"], c["wi"], c["grp"] = n, n % 4, n // 4
                    c["gk"] = (r // 4) % 2
                    if r % 4 == 0:
                        expand_group(c, c["gk"])
                        if r + 4 < NCH:
                            load_group(c, order[c["d"]][r + 4] // 4, 1 - c["gk"])
                stage(lambda c, p, ps, d: K.mm(ps, ps.f(d * 256, d * 256 + 256), c["BTB"][:, c["wi"], :], c["ARB"][:, c["wi"], :],
                                               [c["BTB"], c["ARB"]]),
                      lambda p, ps, e: K.X(DVE, "tensor_tensor", [ps, C["mT"]], [p["M1"]], out=p["M1"][:], in0=v3(ps.f(0, 512)),
                                           in1=C["mT"][:], op=ALU.mult), engs=(DVE,))
                stage(lambda c, p, ps, d: K.mm(ps, ps.f(d * 256, d * 256 + 256), c["KTB"][:, c["wi"], :], c["ARB"][:, c["wi"], :],
                                               [c["KTB"], c["ARB"]]),
                      lambda p, ps, e: K.X(DVE, "tensor_tensor", [ps, C["mT"]], [p["M2"]], out=p["M2"][:], in0=v3(ps.f(0, 512)),
                                           in1=C["mT"][:], op=ALU.mult), engs=(DVE,))
                stage(lambda c, p, ps, d: K.mm(ps, ps.f(d * 128, d * 128 + 128), c["ARB"][:, c["wi"], 0:128], c["BTB"][:, c["wi"], :],
                                               [c["ARB"], c["BTB"]]),
                      lambda p, ps, e: K.X(DVE, "tensor_tensor", [ps, C["mA"]], [p["A0"]], out=p["A0"][:], in0=v3(ps.f(0, 256)),
                                           in1=C["mA"][:], op=ALU.mult), engs=(DVE,))

                def tr_pe(c, p, ps, d):
                    srcs = [(c["ARB"], c["ARB"][:, c["wi"], 0:128]), (c["BTB"], c["BTB"][:, c["wi"], :]),
                            (c["KTB"], c["KTB"][:, c["wi"], :]), (c["VB"], c["VB"][:, c["wi"], :])]
                    for q, (t, ap) in enumerate(srcs):
                        K.tr(ps, ps.b(d * 512 + q * 128, d * 512 + (q + 1) * 128), ap, identb[:], [t, identb])

                def tr_ev(p, ps, e):
                    pv = v3(ps.b(0, 1024))
                    ecopy(e, [ps], p["TF"], p["TF"][:], pv[:, :, 128:512])
                    ecopy(e, [ps], p["X"][0], p["X"][0][:, :, 0:128], pv[:, :, 0:128])
                stage(tr_pe, tr_ev)
                stage(lambda c, p, ps, d: K.mm(ps, ps.f(d * 128, d * 128 + 128), p["M2"][:, d, 0:128], p["TF"][:, d, 256:384],
                                               [p["M2"], p["TF"]]),
                      lambda p, ps, e: ecopy(e, [ps], p["X"][0], p["X"][0][:, :, 128:256], v3(ps.f(0, 256))), engs=(ACT, DVE))

                def sq_pe(m):
                    def f(c, p, ps, d):
                        if m == 0:
                            A_t, A_ap, AT_t, AT_ap = p["A0"], p["A0"][:, d, :], p["M1"], p["M1"][:, d, 0:128]
                        else:
                            q = p["SQ"][(m - 1) % 2]
                            A_t, A_ap, AT_t, AT_ap = q, q[:, d, 0:128], q, q[:, d, 128:256]
                        K.mm(ps, ps.f(d * 256, d * 256 + 128), AT_ap, A_ap, [A_t, AT_t])
                        K.mm(ps, ps.f(d * 256 + 128, d * 256 + 256), A_ap, AT_ap, [A_t, AT_t])
                    return f

                def neu_pe(m):
                    def f(c, p, ps, d):
                        if m == 0:
                            AT_t, AT_ap = p["M1"], p["M1"][:, d, 0:128]
                        else:
                            AT_t, AT_ap = p["SQ"][(m - 1) % 2], p["SQ"][(m - 1) % 2][:, d, 128:256]
                        o = ps.f(d * 256, d * 256 + 256)
                        K.mm(ps, o, AT_ap, p["X"][m % 2][:, d, :], [AT_t, p["X"][m % 2]], start=True, stop=False)
                        K.mm(ps, o, identb[:], p["X"][m % 2][:, d, :], [identb, p["X"][m % 2]], start=False, stop=True)
                    return f
                for m in range(5):
                    stage(sq_pe(m), lambda p, ps, e, m=m: ecopy(e, [ps], p["SQ"][m % 2], p["SQ"][m % 2][:], v3(ps.f(0, 512))))
                    stage(neu_pe(m), lambda p, ps, e, m=m: ecopy(e, [ps], p["X"][(m + 1) % 2], p["X"][(m + 1) % 2][:], v3(ps.f(0, 512))),
                          engs=(ACT, DVE))
                stage(neu_pe(5), lambda p, ps, e: ecopy(e, [ps], p["X"][0], p["X"][0][:], v3(ps.f(0, 512))))
                stage(lambda c, p, ps, d: K.tr(ps, ps.b(d * 256, d * 256 + 128), p["X"][0][:, d, 0:128], identb[:], [p["X"][0], identb]),
                      lambda p, ps, e: ecopy(e, [ps], p["WT"], p["WT"][:], v3(ps.b(0, 512))[:, :, 0:128]), engs=(ACT, DVE))
                stage(lambda c, p, ps, d: K.mm(ps, ps.f(d * 128, d * 128 + 128), p["WT"][:, d, :], p["Hbf"][:, d, :], [p["WT"], p["Hbf"]]),
                      lambda p, ps, e: K.X(DVE, "tensor_tensor", [ps, p["X"][0]], [p["U"]], out=p["U"][:], in0=v3(ps.f(0, 256)),
                                           in1=p["X"][0][:, :, 128:256], op=ALU.add), engs=(DVE,))

                def y_pe(c, p, ps, d):
                    o = ps.f(d * 128, d * 128 + 128)
                    K.mm(ps, o, c["ARB"][:, c["wi"], 128:256], p["Hbf"][:, d, :], [c["ARB"], p["Hbf"]], start=True, stop=False)
                    K.mm(ps, o, p["M1"][:, d, 128:256], p["U"][:, d, :], [p["M1"], p["U"]], start=False, stop=False)
                    K.mm(ps, o, p["M2"][:, d, 128:256], p["TF"][:, d, 256:384], [p["M2"], p["TF"]], start=False, stop=True)
                stage(y_pe, lambda p, ps, e: ecopy(e, [ps], p["Ybd"], p["Ybd"][:], v3(ps.f(0, 256))), engs=(ACT,))
                for c in chains:
                    p, d = c["pair"], c["d"]
                    K.X(POOL, "tensor_scalar", [p["Hf"], c["GL"]], [p["hg"]], out=p["hg"][:, d, :], in0=p["Hf"][:, d, :],
                        scalar1=c["GL"][:, c["n"]:c["n"] + 1], scalar2=None, op0=ALU.mult)

                def h_pe(c, p, ps, d):
                    o = ps.f(d * 128, d * 128 + 128)
                    K.mm(ps, o, p["TF"][:, d, 0:128], p["U"][:, d, :], [p["TF"], p["U"]], start=True, stop=False)
                    K.mm(ps, o, p["TF"][:, d, 128:256], p["TF"][:, d, 256:384], [p["TF"]], start=False, stop=True)

                def h_ev(p, ps, e):
                    for c in p["ch"]:
                        d = c["d"]
                        K.X(DVE, "scalar_tensor_tensor", [ps, c["GL"], p["hg"]], [p["Hf"]], out=p["Hf"][:, d, :],
                            in0=ps.f(d * 128, d * 128 + 128), scalar=c["GL"][:, c["n"]:c["n"] + 1], in1=p["hg"][:, d, :],
                            op0=ALU.mult, op1=ALU.add)
                    K.X(ACT, "copy", [p["Hf"]], [p["Hbf"]], out=p["Hbf"][:], in_=p["Hf"][:])
                stage(h_pe, h_ev, engs=(DVE,))

                def ys_ev(p, ps, e):
                    for c in p["ch"]:
                        d = c["d"]
                        ecopy(e, [ps], c["yst"][c["gk"]], c["yst"][c["gk"]][:, c["wi"] * 64:(c["wi"] + 1) * 64],
                              ps.f(d * 128, d * 128 + 64))
                stage(lambda c, p, ps, d: K.mm(ps, ps.f(d * 128, d * 128 + 64), p["Ybd"][:, d, :], C["sel"][:], [p["Ybd"], C["sel"]]),
                      ys_ev, engs=(ACT, DVE))
                if r % 4 == 3:
                    for c in chains:
                        ys = c["yst"][c["gk"]]
                        K.Dm(SP, [ys], [], out=S[f"y{c['d']}"][c["b"], c["rows"], c["grp"] * 256:(c["grp"] + 1) * 256], in_=ys[:])
            K.barrier()


def rwkv_out(K, i, ri, need_ctx):
    nc, C = K.nc, K.C
    S = K.rw
    with contextlib.ExitStack() as st:
        cols = rwkv_cols(K, st, i, ri)
        wo = K.sb(st, "rwo", [128, 8, 1024], BF16)
        K.Dm(POOL, [], [wo], out=wo[:], in_=K.w("rw_wo")[ri].rearrange("(kc p) n -> p kc n", p=128))
        g1bc = K.sb(st, "rg1bc", [128, 1024], F32)
        oT = K.sb(st, "roT", [128, 8, 256], BF16)
        ptmp = proj_tmp(K, st)

        def f32t(n):
            return [K.sb(st, f"{n}{k}", [128, 256], F32) for k in range(2)]
        Y0, Y1, YQ, MN, MQ, VAR, Z, BVt, Gt = [f32t(n) for n in ("oY0", "oY1", "oYQ", "oMN", "oMQ", "oVAR", "oZ", "oBV", "oG")]
        it = 0
        for b in range(NBC):
            seqs = [("x", TX, K.dram["xs"][b], b, TC)]
            if need_ctx:
                seqs = [("c", TC, K.dram["cs"][b], 2, 0)] + seqs
            for (kind, T, res, j, tl0) in seqs:
                bcast_row(K, g1bc, K.dram["modrow"][i, j:j + 1, 2 * 1024:3 * 1024])
                for tb in range(T // 256):
                    tl = tl0 + tb * 256
                    for dc in range(8):
                        k = it % 2
                        it += 1
                        dsl = (b, slice(dc * 128, (dc + 1) * 128), slice(tl, tl + 256))
                        y0, y1, yq, mn, mq, var, z, bv, g = Y0[k], Y1[k], YQ[k], MN[k], MQ[k], VAR[k], Z[k], BVt[k], Gt[k]
                        K.Dm(SP, [], [y0], out=y0[:], in_=S["y0"][dsl])
                        K.Dm(SP, [], [y1], out=y1[:], in_=S["y1"][dsl])
                        K.Dm(SP, [], [bv], out=bv[:], in_=S["bv"][dsl])
                        K.Dm(SP, [], [g], out=g[:], in_=S["g"][dsl])
                        K.X(POOL, "tensor_tensor", [y0, y1], [y0], out=y0[:], in0=y0[:], in1=y1[:], op=ALU.add)
                        K.X(POOL, "tensor_tensor", [y0], [yq], out=yq[:], in0=y0[:], in1=y0[:], op=ALU.mult)
                        ps = K.ps()
                        K.mm(ps, ps.f(0, 256), C["blockones_f"][:], y0[:], [C["blockones_f"], y0])
                        K.mm(ps, ps.f(256, 512), C["blockones_f"][:], yq[:], [C["blockones_f"], yq])
                        K.act([ps], [mn], out=mn[:], in_=ps.f(0, 256), func=AF.Copy, scale=1.0 / 64)
                        K.act([ps], [var], out=var[:], in_=ps.f(256, 512), func=AF.Copy, scale=1.0 / 64)
                        K.X(DVE, "tensor_tensor", [mn], [mq], out=mq[:], in0=mn[:], in1=mn[:], op=ALU.mult)
                        K.X(DVE, "tensor_tensor", [var, mq], [var], out=var[:], in0=var[:], in1=mq[:], op=ALU.subtract)
                        K.act([var, C["eps"]], [var], out=var[:], in_=var[:], func=AF.Sqrt, bias=C["eps"][:, 2:3], scale=1.0)
                        K.X(DVE, "reciprocal", [var], [var], out=var[:], in_=var[:])
                        K.X(DVE, "tensor_tensor", [y0, mn], [z], out=z[:], in0=y0[:], in1=mn[:], op=ALU.subtract)
                        K.X(DVE, "tensor_tensor", [z, var], [z], out=z[:], in0=z[:], in1=var[:], op=ALU.mult)
                        K.X(DVE, "tensor_scalar", [z, cols], [z], out=z[:], in0=z[:], scalar1=cols[:, dc, CLW:CLW + 1],
                            scalar2=cols[:, dc, CLB:CLB + 1], op0=ALU.mult, op1=ALU.add)
                        K.X(DVE, "tensor_tensor", [z, bv], [z], out=z[:], in0=z[:], in1=bv[:], op=ALU.add)
                        K.X(DVE, "tensor_tensor", [z, g], [oT], out=oT[:, dc, :], in0=z[:], in1=g[:], op=ALU.mult)
                    proj_residual(K, st, oT, 256, wo, res[tb * 256:(tb + 1) * 256, :], g1bc, None, ptmp)
        K.barrier()


def phase_rwkv(K, i, ri, need_ctx):
    K.set_psum("full")
    rwkv_prep(K, i, ri)
    if K.rw_stop >= 1:
        rwkv_scan(K)
    if K.debug and i == 1:
        for nm, dt in (("y0", F32), ("y1", F32), ("v", BF16), ("GL0", F32), ("GL1", F32), ("rt0", BF16), ("kt0", BF16), ("bv", F32), ("at1", BF16), ("bt1", BF16), ("at0", BF16), ("bt0", BF16), ("kt1", BF16), ("rt1", BF16), ("g", F32)):
            o = K.dout("dbg_rw_" + nm, list(K.rw[nm].shape), dt)
            for b in range(NBC):
                for dc in range(8):
                    K.Dm(SP, [], [], out=o[b, dc * 128:(dc + 1) * 128, :], in_=K.rw[nm][b, dc * 128:(dc + 1) * 128, :])
        K.barrier()
    if K.rw_stop >= 2:
        rwkv_out(K, i, ri, need_ctx)
```
